# Optimizing a Trainium2 kernel written in Bass

```python
import jax, jax.numpy as jnp
from jax import lax
import numpy as np

D_MODEL = 1024
BATCH = 16
SEQ = 2048
DEPTH = 2

HEAD_DIM = 64
N_HEADS = D_MODEL // HEAD_DIM
FOX_HEADS = N_HEADS // 2
DIL_HEADS = N_HEADS - FOX_HEADS
FOX_WIDTH = FOX_HEADS * HEAD_DIM
DIL_WIDTH = DIL_HEADS * HEAD_DIM
DIL_PATTERNS = ((128, 1), (512, 4), (2048, 16))
Q_BLOCK = 128
EVEN_IN = 3 * FOX_WIDTH + FOX_HEADS + 3 * DIL_WIDTH

NSA_HEADS = N_HEADS
NSA_GROUPS = 4
NSA_HPG = NSA_HEADS // NSA_GROUPS
NSA_KV = NSA_GROUPS * HEAD_DIM
NSA_CMP_LEN = 32
NSA_CMP_STRIDE = 16
NSA_CMP_HIDDEN = 4 * HEAD_DIM
NSA_SEL_LEN = 64
NSA_TOP_N = 8
NSA_WINDOW = 512
NSA_CHUNK = 32
ODD_IN = NSA_HEADS * HEAD_DIM + 6 * NSA_KV + 3 * NSA_HEADS

D_FF = 3584
N_EXPERTS = 8
TOP_K = 2
D_FF_EXPERT = 3584
PLE_DIM = 256
ROPE_THETA = 10000.0
RMS_EPS = 1e-6
NEG_INF = -1e30

kernel_name = 'hybrid_fox_dilated_nsa_moe_ple'


def rms_norm(x, g):
    xf = x.astype(jnp.float32)
    y = xf * lax.rsqrt(jnp.mean(xf * xf, axis=-1, keepdims=True) + RMS_EPS)
    return (y * g.astype(jnp.float32)).astype(x.dtype)


def to_heads(t, n):
    b, s, _ = t.shape
    return t.reshape(b, s, n, HEAD_DIM).transpose(0, 2, 1, 3)


def from_heads(t):
    b, n, s, d = t.shape
    return t.transpose(0, 2, 1, 3).reshape(b, s, n * d)


def rotary(t, positions):
    half = HEAD_DIM // 2
    inv_freq = ROPE_THETA ** (-jnp.arange(half, dtype=jnp.float32) / half)
    ang = positions.astype(jnp.float32)[:, None, :, None] * inv_freq
    cos, sin = jnp.cos(ang), jnp.sin(ang)
    tf = t.astype(jnp.float32)
    t1, t2 = tf[..., :half], tf[..., half:]
    return jnp.concatenate([t1 * cos - t2 * sin, t2 * cos + t1 * sin], axis=-1).astype(t.dtype)


def swiglu(x, w_gate, w_up, w_down):
    return (jax.nn.silu(x @ w_gate) * (x @ w_up)) @ w_down


def forgetting_attention(q, k, v, log_f):
    b, h, s, d = q.shape
    c = jnp.cumsum(log_f, axis=-1)
    nb = s // Q_BLOCK
    qb = q.reshape(b, h, nb, Q_BLOCK, d).transpose(2, 0, 1, 3, 4)
    cb = c.reshape(b, h, nb, Q_BLOCK).transpose(2, 0, 1, 3)
    kpos = jnp.arange(s)
    scale = d ** -0.5

    def block(args):
        qi, ci, bi = args
        sc = jnp.einsum('bhqd,bhkd->bhqk', qi, k).astype(jnp.float32) * scale
        sc = sc + ci[..., None] - c[:, :, None, :]
        qpos = bi * Q_BLOCK + jnp.arange(Q_BLOCK)
        sc = jnp.where(kpos[None, :] <= qpos[:, None], sc, NEG_INF)
        pr = jax.nn.softmax(sc, axis=-1).astype(v.dtype)
        return jnp.einsum('bhqk,bhkd->bhqd', pr, v)

    o = lax.map(block, (qb, cb, jnp.arange(nb)))
    return o.transpose(1, 2, 0, 3, 4).reshape(b, h, s, d)


def dilated_branch(q, k, v, window, dilation):
    b, h, s, d = q.shape
    span = window // dilation
    n = s // dilation
    nb = -(-n // Q_BLOCK)
    pad = nb * Q_BLOCK - n

    def residues(t):
        return t.reshape(b, h, n, dilation, d).transpose(0, 1, 3, 2, 4)

    def key_blocks(t):
        tp = jnp.pad(residues(t), ((0, 0), (0, 0), (0, 0), (Q_BLOCK, pad), (0, 0)))
        prev = tp[:, :, :, :nb * Q_BLOCK].reshape(b, h, dilation, nb, Q_BLOCK, d)
        cur = tp[:, :, :, Q_BLOCK:].reshape(b, h, dilation, nb, Q_BLOCK, d)
        return jnp.concatenate([prev, cur], axis=4)

    qb = jnp.pad(residues(q), ((0, 0), (0, 0), (0, 0), (0, pad), (0, 0))).reshape(b, h, dilation, nb, Q_BLOCK, d)
    kb, vb = key_blocks(k), key_blocks(v)
    qi = jnp.arange(Q_BLOCK)[:, None]
    kj = jnp.arange(2 * Q_BLOCK)[None, :]
    dist = qi + Q_BLOCK - kj
    band = (dist >= 0) & (dist <= span)
    after_start = (jnp.arange(nb)[:, None, None] > 0) | (kj >= Q_BLOCK)[None]
    mask = band[None] & after_start
    sc = jnp.einsum('bhrnqd,bhrnkd->bhrnqk', qb, kb).astype(jnp.float32) * (d ** -0.5)
    sc = jnp.where(mask, sc, NEG_INF)
    m = jnp.max(sc, axis=-1, keepdims=True)
    e = jnp.exp(sc - m)
    den = jnp.sum(e, axis=-1, keepdims=True)
    o = jnp.einsum('bhrnqk,bhrnkd->bhrnqd', (e / den).astype(v.dtype), vb)
    lse = (m + jnp.log(den))[..., 0]
    o = o.reshape(b, h, dilation, nb * Q_BLOCK, d)[:, :, :, :n].transpose(0, 1, 3, 2, 4).reshape(b, h, s, d)
    lse = lse.reshape(b, h, dilation, nb * Q_BLOCK)[..., :n].transpose(0, 1, 3, 2).reshape(b, h, s)
    return o, lse


def dilated_attention(q, k, v):
    outs, lses = [], []
    for window, dilation in DIL_PATTERNS:
        o, lse = dilated_branch(q, k, v, window, dilation)
        outs.append(o)
        lses.append(lse)
    w = jax.nn.softmax(jnp.stack(lses), axis=0)
    return jnp.einsum('pbhs,pbhsd->bhsd', w.astype(q.dtype), jnp.stack(outs))


def fox_dilated_mixer(hn, positions, w_in, forget_b, w_out):
    proj = hn @ w_in
    fw, dw = FOX_WIDTH, DIL_WIDTH
    cuts = [fw, 2 * fw, 3 * fw, 3 * fw + dw, 3 * fw + 2 * dw, 3 * fw + 3 * dw]
    fq, fk, fv, dq, dk, dv, f_logit = jnp.split(proj, cuts, axis=-1)
    log_f = jax.nn.log_sigmoid(f_logit.astype(jnp.float32) + forget_b.astype(jnp.float32)).transpose(0, 2, 1)
    o_fox = forgetting_attention(to_heads(fq, FOX_HEADS), to_heads(fk, FOX_HEADS), to_heads(fv, FOX_HEADS), log_f)
    o_dil = dilated_attention(rotary(to_heads(dq, DIL_HEADS), positions),
                              rotary(to_heads(dk, DIL_HEADS), positions),
                              to_heads(dv, DIL_HEADS))
    return jnp.concatenate([from_heads(o_fox), from_heads(o_dil)], axis=-1) @ w_out


def compress_blocks(t, pos, w1, b1, w2):
    b, g, s, d = t.shape
    nc = (s - NSA_CMP_LEN) // NSA_CMP_STRIDE + 1
    idx = jnp.arange(nc)[:, None] * NSA_CMP_STRIDE + jnp.arange(NSA_CMP_LEN)[None, :]
    blk = (t[:, :, idx, :] + pos).reshape(b, g, nc, NSA_CMP_LEN * d)
    return jax.nn.gelu(blk @ w1 + b1) @ w2


def native_sparse_attention(q, kc, vc, ks, vs, kw, vw, gates):
    b, h, s, d = q.shape
    g = NSA_GROUPS
    nc = kc.shape[2]
    ns = s // NSA_SEL_LEN
    n_sel = min(NSA_TOP_N, ns)
    n_ch = s // NSA_CHUNK
    scale = d ** -0.5
    cmp_start = jnp.arange(nc) * NSA_CMP_STRIDE
    cmp_end = cmp_start + NSA_CMP_LEN - 1
    sel_ids = jnp.arange(ns)
    overlap = ((cmp_start[:, None] < (sel_ids[None, :] + 1) * NSA_SEL_LEN)
               & (cmp_start[:, None] + NSA_CMP_LEN > sel_ids[None, :] * NSA_SEL_LEN)).astype(jnp.float32)
    ks_blk = ks.reshape(b, g, ns, NSA_SEL_LEN, d)
    vs_blk = vs.reshape(b, g, ns, NSA_SEL_LEN, d)
    kw_pad = jnp.pad(kw, ((0, 0), (0, 0), (NSA_WINDOW, 0), (0, 0)))
    vw_pad = jnp.pad(vw, ((0, 0), (0, 0), (NSA_WINDOW, 0), (0, 0)))
    qc = q.reshape(b, g, NSA_HPG, n_ch, NSA_CHUNK, d).transpose(3, 0, 1, 2, 4, 5)
    bi = jnp.arange(b)[:, None, None, None]
    gi = jnp.arange(g)[None, :, None, None]
    n_key_sel = n_sel * NSA_SEL_LEN

    def chunk(args):
        qi, ci = args
        tq = ci * NSA_CHUNK + jnp.arange(NSA_CHUNK)
        sc = jnp.einsum('bghqd,bgnd->bghqn', qi, kc).astype(jnp.float32) * scale
        valid_c = cmp_end[None, :] <= tq[:, None]
        pc = jax.nn.softmax(jnp.where(valid_c, sc, NEG_INF), axis=-1) * jnp.any(valid_c, axis=-1)[:, None]
        o_c = jnp.einsum('bghqn,bgnd->bghqd', pc.astype(vc.dtype), vc)
        imp = jnp.einsum('bghqn,nj->bgqj', pc, overlap)
        cur = tq // NSA_SEL_LEN
        forced = (sel_ids[None, :] == 0) | (sel_ids[None, :] == cur[:, None]) | (sel_ids[None, :] == cur[:, None] - 1)
        imp = jnp.where(sel_ids[None, :] > cur[:, None], -1.0, jnp.where(forced, 1e6, imp))
        _, top = lax.top_k(imp, n_sel)
        kg = ks_blk[bi, gi, top].reshape(b, g, NSA_CHUNK, n_key_sel, d)
        vg = vs_blk[bi, gi, top].reshape(b, g, NSA_CHUNK, n_key_sel, d)
        kpos = top[..., None] * NSA_SEL_LEN + jnp.arange(NSA_SEL_LEN)
        valid_s = (kpos <= tq[:, None, None]).reshape(b, g, 1, NSA_CHUNK, n_key_sel)
        ss = jnp.einsum('bghqd,bgqkd->bghqk', qi, kg).astype(jnp.float32) * scale
        ps = jax.nn.softmax(jnp.where(valid_s, ss, NEG_INF), axis=-1)
        o_s = jnp.einsum('bghqk,bgqkd->bghqd', ps.astype(vs.dtype), vg)
        start = ci * NSA_CHUNK
        kwin = lax.dynamic_slice_in_dim(kw_pad, start, NSA_WINDOW + NSA_CHUNK, axis=2)
        vwin = lax.dynamic_slice_in_dim(vw_pad, start, NSA_WINDOW + NSA_CHUNK, axis=2)
        wpos = start - NSA_WINDOW + jnp.arange(NSA_WINDOW + NSA_CHUNK)
        delta = tq[:, None] - wpos[None, :]
        valid_w = (delta >= 0) & (delta < NSA_WINDOW) & (wpos[None, :] >= 0)
        sw = jnp.einsum('bghqd,bgkd->bghqk', qi, kwin).astype(jnp.float32) * scale
        pw = jax.nn.softmax(jnp.where(valid_w, sw, NEG_INF), axis=-1)
        o_w = jnp.einsum('bghqk,bgkd->bghqd', pw.astype(vw.dtype), vwin)
        return jnp.stack([o_c, o_s, o_w], axis=-1)

    o = lax.map(chunk, (qc, jnp.arange(n_ch)))
    o = o.transpose(1, 0, 4, 2, 3, 5, 6).reshape(b, s, h, d, 3)
    return jnp.einsum('bshdc,bshc->bshd', o, gates).reshape(b, s, h * d)


def nsa_mixer(hn, positions, w_in, pos_k, w1_k, b1_k, w2_k, pos_v, w1_v, b1_v, w2_v, w_out):
    b, s, _ = hn.shape
    proj = hn @ w_in
    qw = NSA_HEADS * HEAD_DIM
    cuts = [qw + i * NSA_KV for i in range(7)]
    q, kc, vc, ks, vs, kw, vw, g = jnp.split(proj, cuts, axis=-1)
    q = rotary(to_heads(q, NSA_HEADS), positions)
    kc = rotary(to_heads(kc, NSA_GROUPS), positions)
    ks = rotary(to_heads(ks, NSA_GROUPS), positions)
    kw = rotary(to_heads(kw, NSA_GROUPS), positions)
    kc_cmp = compress_blocks(kc, pos_k, w1_k, b1_k, w2_k)
    vc_cmp = compress_blocks(to_heads(vc, NSA_GROUPS), pos_v, w1_v, b1_v, w2_v)
    gates = jax.nn.sigmoid(g.reshape(b, s, NSA_HEADS, 3))
    o = native_sparse_attention(q, kc_cmp, vc_cmp, ks, to_heads(vs, NSA_GROUPS), kw, to_heads(vw, NSA_GROUPS), gates)
    return o @ w_out


def moe_swiglu(x, w_router, b_router, w_gate, w_up, w_down):
    b, s, dm = x.shape
    xt = x.reshape(b * s, dm)
    logits = (xt @ w_router).astype(jnp.float32) + b_router.astype(jnp.float32)
    top_val, top_idx = lax.top_k(logits, TOP_K)
    top_w = jax.nn.softmax(top_val, axis=-1)
    combine = jnp.sum(jax.nn.one_hot(top_idx, N_EXPERTS, dtype=jnp.float32) * top_w[..., None], axis=1)
    y = jnp.zeros_like(xt)
    for e in range(N_EXPERTS):
        y = y + combine[:, e:e + 1].astype(x.dtype) * swiglu(xt, w_gate[e], w_up[e], w_down[e])
    return y.reshape(b, s, dm)


def setup_inputs(seed: int = 0) -> dict:
    key = jax.random.key(seed)
    keys = jax.random.split(key, 40)
    counter = [0]
    n_even = (DEPTH + 1) // 2
    n_odd = DEPTH // 2

    def next_key():
        k = keys[counter[0]]
        counter[0] += 1
        return k

    def normal(shape, scale):
        return jax.random.normal(next_key(), shape, jnp.float32) * scale

    def gain(shape):
        return 1.0 + normal(shape, 0.05)

    cmp_in = NSA_CMP_LEN * HEAD_DIM
    x = normal((BATCH, SEQ, D_MODEL), 1.0)
    p = normal((DEPTH, BATCH, SEQ, PLE_DIM), 1.0)
    offset = jax.random.randint(next_key(), (BATCH,), 0, 4096, dtype=jnp.int32)
    positions = offset[:, None] + jnp.arange(SEQ, dtype=jnp.int32)[None, :]
    return {
        'x': x,
        'p': p,
        'positions': positions,
        'mix_norm': gain((DEPTH, D_MODEL)),
        'ffn_norm': gain((DEPTH, D_MODEL)),
        'ple_norm': gain((DEPTH, D_MODEL)),
        'ple_gate_w': normal((DEPTH, D_MODEL, D_MODEL), D_MODEL ** -0.5),
        'ple_proj_w': normal((DEPTH, PLE_DIM, D_MODEL), 0.5 * PLE_DIM ** -0.5),
        'fd_w_in': normal((n_even, D_MODEL, EVEN_IN), D_MODEL ** -0.5),
        'fd_forget_b': 3.0 + normal((n_even, FOX_HEADS), 0.5),
        'fd_w_out': normal((n_even, D_MODEL, D_MODEL), D_MODEL ** -0.5),
        'dense_w_gate': normal((n_even, D_MODEL, D_FF), D_MODEL ** -0.5),
        'dense_w_up': normal((n_even, D_MODEL, D_FF), D_MODEL ** -0.5),
        'dense_w_down': normal((n_even, D_FF, D_MODEL), D_FF ** -0.5),
        'nsa_w_in': normal((n_odd, D_MODEL, ODD_IN), D_MODEL ** -0.5),
        'nsa_pos_k': normal((n_odd, NSA_CMP_LEN, HEAD_DIM), 0.02),
        'nsa_w1_k': normal((n_odd, cmp_in, NSA_CMP_HIDDEN), cmp_in ** -0.5),
        'nsa_b1_k': normal((n_odd, NSA_CMP_HIDDEN), 0.01),
        'nsa_w2_k': normal((n_odd, NSA_CMP_HIDDEN, HEAD_DIM), NSA_CMP_HIDDEN ** -0.5),
        'nsa_pos_v': normal((n_odd, NSA_CMP_LEN, HEAD_DIM), 0.02),
        'nsa_w1_v': normal((n_odd, cmp_in, NSA_CMP_HIDDEN), cmp_in ** -0.5),
        'nsa_b1_v': normal((n_odd, NSA_CMP_HIDDEN), 0.01),
        'nsa_w2_v': normal((n_odd, NSA_CMP_HIDDEN, HEAD_DIM), NSA_CMP_HIDDEN ** -0.5),
        'nsa_w_out': normal((n_odd, D_MODEL, D_MODEL), D_MODEL ** -0.5),
        'moe_w_router': normal((n_odd, D_MODEL, N_EXPERTS), D_MODEL ** -0.5),
        'moe_b_router': normal((n_odd, N_EXPERTS), 0.01),
        'moe_w_gate': normal((n_odd, N_EXPERTS, D_MODEL, D_FF_EXPERT), D_MODEL ** -0.5),
        'moe_w_up': normal((n_odd, N_EXPERTS, D_MODEL, D_FF_EXPERT), D_MODEL ** -0.5),
        'moe_w_down': normal((n_odd, N_EXPERTS, D_FF_EXPERT, D_MODEL), D_FF_EXPERT ** -0.5),
        'final_norm': gain((D_MODEL,)),
    }


def reference(x, p, positions, mix_norm, ffn_norm, ple_norm, ple_gate_w, ple_proj_w,
              fd_w_in, fd_forget_b, fd_w_out, dense_w_gate, dense_w_up, dense_w_down,
              nsa_w_in, nsa_pos_k, nsa_w1_k, nsa_b1_k, nsa_w2_k,
              nsa_pos_v, nsa_w1_v, nsa_b1_v, nsa_w2_v, nsa_w_out,
              moe_w_router, moe_b_router, moe_w_gate, moe_w_up, moe_w_down, final_norm):
    h = x
    for i in range(DEPTH):
        j = i // 2
        hn = rms_norm(h, mix_norm[i])
        if i % 2 == 0:
            h = h + fox_dilated_mixer(hn, positions, fd_w_in[j], fd_forget_b[j], fd_w_out[j])
            h = h + swiglu(rms_norm(h, ffn_norm[i]), dense_w_gate[j], dense_w_up[j], dense_w_down[j])
        else:
            h = h + nsa_mixer(hn, positions, nsa_w_in[j], nsa_pos_k[j], nsa_w1_k[j], nsa_b1_k[j], nsa_w2_k[j],
                              nsa_pos_v[j], nsa_w1_v[j], nsa_b1_v[j], nsa_w2_v[j], nsa_w_out[j])
            h = h + moe_swiglu(rms_norm(h, ffn_norm[i]), moe_w_router[j], moe_b_router[j],
                               moe_w_gate[j], moe_w_up[j], moe_w_down[j])
        gate = jax.nn.sigmoid(rms_norm(h, ple_norm[i]) @ ple_gate_w[i])
        h = h + gate * (p[i] @ ple_proj_w[i])
    return rms_norm(h, final_norm)
```

```python
import numpy as np
import ml_dtypes
from contextlib import ExitStack
import concourse.bass as bass
import concourse.mybir as mybir
from concourse.bass_utils import run_bass_kernel_spmd

dt = mybir.dt
F32, BF16, I32 = dt.float32, dt.bfloat16, dt.int32
AF = mybir.ActivationFunctionType
ALU = mybir.AluOpType
AX = mybir.AxisListType

S = 2048
D = 1024
NB = 16
NCH = 4
DFF = 3584
PI = float(np.pi)
TWO_PI = float(2 * np.pi)

import os as _os
NSA_STOP = int(_os.environ.get('NSA_STOP', '0'))
PD = 4
NPT = PD + 4
ENG_NAMES = ['pe', 'act', 'dve', 'pool', 'sp']


def ssl(start, n, step):
    return slice(start, start + (n - 1) * step + 1, step)

SAME_ENG_SYNC = True


class DSem:
    def __init__(self, sem):
        self.sem = sem
        self.total = 0
        self.open = None


class Op:
    __slots__ = ('eng', 'fn', 'deps', 'dsem', 'sig', 'sem', 'val')


class Prog:
    def __init__(self, nc, stack):
        self.nc = nc
        self.stack = stack
        self.ops = []
        self.by_eng = {e: [] for e in ENG_NAMES}
        self.lastw = {}
        self.readers = {}
        self.nsem = 0
        self.bar_deps = {}
        self.bar_done = set()
        self.since_bar_dma = {}
        self.final = []
        self.uid = 0

    def new_sem(self, name):
        self.nsem += 1
        return self.stack.enter_context(self.nc.semaphore(f"{name}_{self.nsem}"))

    def dsem(self, name):
        return DSem(self.new_sem(name))

    def group(self, ds):
        ds.open = []

    def endgroup(self, ds):
        for o in ds.open:
            o.val = ds.total * 16
        ds.open = None

    def barrier(self):
        deps = dict(self.bar_deps)
        for e in ENG_NAMES:
            for o in reversed(self.by_eng[e]):
                if o.dsem is None:
                    deps[e] = o
                    o.sig = True
                    break
        for k, o in self.since_bar_dma.items():
            deps[k] = o
        self.bar_deps = deps
        self.bar_done = set()
        self.since_bar_dma = {}
        self.lastw = {}
        self.readers = {}

    def add(self, eng, fn, r=(), w=(), dsem=None):
        o = Op()
        o.eng = eng
        o.fn = fn
        o.dsem = dsem
        o.sig = False
        o.sem = None
        o.val = 0
        deps = {}

        def need(p):
            if p is None:
                return
            if p.dsem is None and dsem is None and p.eng == eng:
                if eng == 'pe' or not SAME_ENG_SYNC:
                    return
            deps[id(p)] = p

        for k in r:
            need(self.lastw.get(k))
        for k in w:
            need(self.lastw.get(k))
            rd = self.readers.get(k)
            if rd:
                for q in rd.values():
                    need(q)
        if eng not in self.bar_done:
            self.bar_done.add(eng)
            for p in self.bar_deps.values():
                need(p)
        for k in w:
            self.lastw[k] = o
            self.readers[k] = {}
        for k in r:
            d = self.readers.setdefault(k, {})
            if dsem is not None:
                self.uid += 1
                d[('dma', self.uid)] = o
            else:
                d[eng] = o
        o.deps = list(deps.values())
        for p in o.deps:
            p.sig = True
        if dsem is not None:
            dsem.total += 1
            o.sem = dsem.sem
            o.val = dsem.total * 16
            if dsem.open is not None:
                dsem.open.append(o)
            self.since_bar_dma[id(dsem)] = o
        self.ops.append(o)
        self.by_eng[eng].append(o)
        return o

    def emit(self):
        nc = self.nc
        LIMIT = 30000
        for e in ENG_NAMES:
            cur = None
            cnt = 0
            for o in self.by_eng[e]:
                if o.dsem is None and o.sig:
                    if cur is None or cnt >= LIMIT:
                        cur = self.new_sem(f"e_{e}")
                        cnt = 0
                    cnt += 1
                    o.sem = cur
                    o.val = cnt
        final = self.final

        def run(e, eng):
            known = {}
            for o in self.by_eng[e]:
                need = {}
                for p in o.deps:
                    s = p.sem
                    if s is None:
                        continue
                    cur = need.get(id(s))
                    if cur is None or cur[1] < p.val:
                        need[id(s)] = (s, p.val)
                for sid, (s, v) in need.items():
                    if known.get(sid, 0) < v:
                        eng.wait_ge(s, v)
                        known[sid] = v
                inst = o.fn(eng)
                if o.dsem is not None:
                    inst.then_inc(o.sem, 16)
                elif o.sig:
                    inst.then_inc(o.sem, 1)
            if e == 'sp':
                for o in final:
                    eng.wait_ge(o.sem, o.val)

        with nc.Block() as block:
            @block.tensor
            def _(eng):
                run('pe', eng)

            @block.scalar
            def _(eng):
                run('act', eng)

            @block.vector
            def _(eng):
                run('dve', eng)

            @block.gpsimd
            def _(eng):
                run('pool', eng)

            @block.sync
            def _(eng):
                run('sp', eng)


class Region:
    def __init__(self, ap, nelem):
        self.ap = ap
        self.n = nelem
        self.off = 0
        self.peak = 0

    def reset(self):
        self.off = 0

    def alloc(self, free_shape, dtype, parts=128):
        n = int(np.prod(free_shape))
        mult = 2 if dtype == F32 or dtype == I32 else 1
        nb = n * mult
        nb = (nb + 15) // 16 * 16
        assert self.off + nb <= self.n, f"region overflow {self.off}+{nb}>{self.n}"
        v = self.ap[0:parts, self.off:self.off + n * mult]
        self.off += nb
        self.peak = max(self.peak, self.off)
        if mult == 2:
            v = v.bitcast(dtype)
        if len(free_shape) == 2:
            v = v.rearrange('p (a b) -> p a b', b=free_shape[1])
        elif len(free_shape) == 3:
            v = v.rearrange('p (a b c) -> p a b c', b=free_shape[1], c=free_shape[2])
        return v


def _bf(a):
    return np.ascontiguousarray(a.astype(np.float32)).astype(ml_dtypes.bfloat16)


def make_consts():
    c = {}
    k = np.arange(128)[:, None]
    c['c_ident'] = np.eye(128, dtype=np.float32)
    c['c_utri'] = (k <= np.arange(128)[None, :]).astype(np.float32)
    c['c_ones'] = np.ones((128, 128), np.float32)
    os_ = np.zeros((128, 64), np.float32)
    os_[64, :] = 1.0
    c['c_os'] = _bf(os_)
    y = np.arange(-384, 1024)[None, :]
    c['c_wm'] = _bf(((y - k) >= 0) & ((y - k) < 512))
    q = np.arange(128)[None, :]
    cur = (k <= q)
    prev = (k >= q)
    c['c_dm'] = _bf(np.concatenate([cur, prev, cur, prev], axis=1))
    n = np.arange(128)[:, None]
    qq = np.arange(2048)[None, :]
    c['c_cmpT'] = _bf((16 * n + 31 <= qq) & (n <= 126))
    xq = np.arange(128)[:, None]
    xx = np.arange(-120, 128)[None, :]
    c['c_m0'] = (16 * xx + 31 <= xq).astype(np.float32)
    xs = np.arange(-30, 32)[None, :]
    curp = (xq // 64)
    fut = xs > curp
    forced = (xs == curp) | (xs == curp - 1)
    keep = (~fut) & (~forced)
    addv = np.where(fut, -1.0, np.where(forced, 1e6, 0.0))
    c['c_keep'] = keep.astype(np.float32)
    c['c_addv'] = addv.astype(np.float32)
    j = np.arange(32)[:, None, None]
    kb = np.arange(16)[None, :, None]
    kk = np.arange(128)[None, None, :]
    E = (j == 2 * kb + kk // 64)
    Ef = np.zeros((128, 16 * 128), np.float32)
    Ef[:32] = E.reshape(32, 16 * 128)
    c['c_E'] = _bf(Ef)
    Z = np.zeros((128, 48 * 64), np.float32)
    for p_ in range(48):
        Z[p_, 64 * p_:64 * p_ + 64] = 1.0
    c['c_Z'] = _bf(Z)
    half = 32
    inv_freq = (10000.0 ** (-np.arange(half, dtype=np.float32) / half)).astype(np.float32)
    pp = np.arange(128)
    rot = np.zeros((128, 4), np.float32)
    rot[:, 0] = inv_freq[(pp % 64) % 32]
    rot[:, 1] = PI / 2
    rot[:, 2] = np.where((pp % 64) < 32, PI, 0.0)
    c['c_rot'] = rot
    return c


CONST_SHAPES = None


class Slots:
    def __init__(self, P, name, aps, dsems):
        self.items = [(ap, f"{name}{i}", dsems[i]) for i, ap in enumerate(aps)]
        self.i = 0

    def get(self):
        it = self.items[self.i % len(self.items)]
        self.i += 1
        return it


class Builder:
    def __init__(self, n_seq=2, stages=('l0', 'l1'), dbg=None):
        self.n_seq = n_seq
        self.stages = stages
        self.dbg = dbg
        nc = bass.Bass("TRN2", target_bir_lowering=False)
        self.nc = nc
        self.stack = ExitStack()
        self.P = Prog(nc, self.stack)
        self.rr = {}
        self.fifo = []
        self.tick = 0
        self._inputs()
        self._alloc()

    def din(self, name, shape, dtype=F32):
        return self.nc.dram_tensor(name, list(shape), dtype, kind="ExternalInput").ap()

    def sb(self, name, shape, dtype):
        return self.stack.enter_context(self.nc.sbuf_tensor(name, list(shape), dtype))

    def _inputs(self):
        n = self.n_seq
        self.x = self.din('x', [n, S, D])
        self.p = self.din('p', [2, n, S, 256])
        self.pos = self.din('positions', [n, S], I32)
        self.mix_norm = self.din('mix_norm', [2, D])
        self.ffn_norm = self.din('ffn_norm', [2, D])
        self.ple_norm = self.din('ple_norm', [2, D])
        self.final_norm = self.din('final_norm', [1, D])
        self.ple_gate_w = self.din('ple_gate_w', [2, D, D])
        self.ple_proj_w = self.din('ple_proj_w', [2, 256, D])
        self.fd_w_in = self.din('fd_w_in', [D, 3080])
        self.fd_w_sw = self.din('fd_w_sw', [D, 1024])
        self.fd_forget_b = self.din('fd_forget_b', [1, 8])
        self.fd_w_out = self.din('fd_w_out', [D, D])
        self.dense_w_gate = self.din('dense_w_gate', [D, DFF])
        self.dense_w_up = self.din('dense_w_up', [D, DFF])
        self.dense_w_down = self.din('dense_w_down', [DFF, D])
        self.nsa_w_in = self.din('nsa_w_in', [D, 2608])
        self.nsa_w_sw = self.din('nsa_w_sw', [D, 1792])
        self.nsa_pos_k = self.din('nsa_pos_k', [32, 64])
        self.nsa_w1_k = self.din('nsa_w1_k', [2048, 256])
        self.nsa_b1_k = self.din('nsa_b1_k', [256, 1])
        self.nsa_w2_k = self.din('nsa_w2_k', [256, 64])
        self.nsa_pos_v = self.din('nsa_pos_v', [32, 64])
        self.nsa_w1_v = self.din('nsa_w1_v', [2048, 256])
        self.nsa_b1_v = self.din('nsa_b1_v', [256, 1])
        self.nsa_w2_v = self.din('nsa_w2_v', [256, 64])
        self.nsa_w_out = self.din('nsa_w_out', [D, D])
        self.moe_w_router = self.din('moe_w_router', [D, 8])
        self.moe_b_router = self.din('moe_b_router', [1, 8])
        self.moe_w_gate = self.din('moe_w_gate', [8, D, DFF])
        self.moe_w_up = self.din('moe_w_up', [8, D, DFF])
        self.moe_w_down = self.din('moe_w_down', [8, DFF, D])
        self.cst = {}
        for k, v in make_consts().items():
            d = BF16 if v.dtype == ml_dtypes.bfloat16 else F32
            self.cst[k] = self.din(k, v.shape, d)
        self.y = self.nc.dram_tensor('y', [n, S, D], F32, kind="ExternalOutput").ap()

    def _alloc(self):
        P = self.P
        self.h = self.sb('h', [128, NB, D], F32)
        self.hnT = self.sb('hnT', [128, 8, S], BF16)
        self.cosT = self.sb('cosT', [128, S], BF16)
        self.sinT = self.sb('sinT', [128, S], BF16)
        self.c = {}
        for k, ap in self.cst.items():
            if k == 'c_E':
                continue
            self.c[k] = self.sb('s_' + k, list(ap.shape), ap.dtype)
        self.ps = [self.stack.enter_context(self.nc.psum_tensor(f"ps{i}", [128, 512], F32)) for i in range(8)]
        RN = 43520
        self.Rt = self.sb('R', [128, RN], BF16)
        self.R = Region(self.Rt, RN)
        self.ds_c = P.dsem('consts')
        self.ds_h = P.dsem('hload')
        self.ds_g = P.dsem('gtile')
        self.ds_y = P.dsem('yout')
        self.ds_misc = [P.dsem(f'misc{i}') for i in range(4)]
        self.ds_w = [P.dsem(f'w{i}') for i in range(6)]
        self.ds_wo = [P.dsem(f'wo{i}') for i in range(2)]
        self.ds_ffn = [P.dsem(f'ffn{i}') for i in range(6)]

    def defer(self, fn, delay):
        self.fifo.append((self.tick + delay, fn))

    def step(self):
        self.tick += 1
        while self.fifo and self.fifo[0][0] <= self.tick:
            self.fifo.pop(0)[1]()

    def flush(self):
        while self.fifo:
            self.fifo.pop(0)[1]()

    def rot(self, name, n):
        i = self.rr.get(name, 0)
        self.rr[name] = i + 1
        return i % n

    def load_consts(self):
        P = self.P
        P.group(self.ds_c)
        for k, ap in self.cst.items():
            if k == 'c_E':
                continue
            P.add('sp', lambda e, k=k, ap=ap: e.dma_start(out=self.c[k][:], in_=ap), w=[k], dsem=self.ds_c)
        P.endgroup(self.ds_c)

    def load_w(self, dst, dkey, dsem, src, cast=True):
        srcv = src.rearrange('(k p) c -> p k c', p=128)
        eng = 'pool' if cast else 'sp'
        return self.P.add(eng, lambda e: e.dma_start(out=dst, in_=srcv), w=[dkey], dsem=dsem)

    def norm(self, gain_row, final_out=None):
        P, R, h = self.P, self.R, self.h
        mark = R.off
        self.gtile = R.alloc([D], F32)
        ss = R.alloc([NB], F32)
        junk = R.alloc([D], BF16)
        hs = [R.alloc([D], F32) for _ in range(2)]
        P.add('sp', lambda e: e.dma_start(out=self.gtile, in_=gain_row[0, :].partition_broadcast(128)),
              w=['gtile'], dsem=self.ds_g)
        for b in range(NB):
            P.add('act', lambda e, b=b: e.activation(out=junk, in_=h[:, b, :], func=AF.Square,
                                                     accum_out=ss[:, b:b + 1]),
                  r=self.hk(b), w=['junk', ('ss', b)])
        allss = [('ss', b) for b in range(NB)]
        P.add('dve', lambda e: e.tensor_scalar(out=ss, in0=ss, scalar1=1.0 / D, scalar2=1e-6,
                                               op0=ALU.mult, op1=ALU.add), r=allss, w=allss)
        P.add('act', lambda e: e.activation(out=ss, in_=ss, func=AF.Sqrt), r=allss, w=allss)
        P.add('dve', lambda e: e.reciprocal(out=ss, in_=ss), r=allss, w=allss)
        ident = self.c['c_ident']
        for b in range(NB):
            i = b % 2
            P.add('dve', lambda e, b=b, i=i: e.scalar_tensor_tensor(
                out=hs[i], in0=h[:, b, :], scalar=ss[:, b:b + 1], in1=self.gtile,
                op0=ALU.mult, op1=ALU.mult), r=self.hk(b) + [('ss', b), 'gtile'], w=[('hs', i)])
            if final_out is not None:
                s = final_out
                P.add('sp', lambda e, b=b, i=i, s=s: e.dma_start(out=self.y[s, b * 128:(b + 1) * 128, :], in_=hs[i]),
                      r=[('hs', i)], w=[('y', s, b)], dsem=self.ds_y)
                continue
            for half in range(2):
                pi = 6 + half
                ps = self.ps[pi]
                for j in range(4):
                    cidx = half * 4 + j
                    P.add('pe', lambda e, ps=ps, j=j, cidx=cidx, i=i: e.transpose(
                        out=ps[:, j * 128:(j + 1) * 128], in_=hs[i][:, cidx * 128:(cidx + 1) * 128],
                        identity=ident[:]), r=[('hs', i), 'c_ident'], w=[('ps', pi)])
                P.add('act', lambda e, ps=ps, half=half, b=b: e.activation(
                    out=self.hnT[:, half * 4:half * 4 + 4, b * 128:(b + 1) * 128],
                    in_=ps[:].rearrange('p (a t) -> p a t', t=128), func=AF.Copy),
                    r=[('ps', pi)], w=[('hnT', b)])
        R.off = mark

    def hk(self, b):
        return [('h', b, 0), ('h', b, 1)]

    def hn_keys(self, tc=None):
        if tc is None:
            return [('hnT', b) for b in range(NB)]
        return [('hnT', 4 * tc + j) for j in range(4)]

    def load_seq(self, s):
        P = self.P
        P.group(self.ds_h)
        for b in range(NB):
            P.add('sp', lambda e, b=b: e.dma_start(out=self.h[:, b, :], in_=self.x[s, b * 128:(b + 1) * 128, :]),
                  w=self.hk(b), dsem=self.ds_h)
        P.endgroup(self.ds_h)

    def rotary_tables(self, s):
        P, R = self.P, self.R
        mark = R.off
        posi = R.alloc([S], I32)
        posf = R.alloc([S], F32)
        t = R.alloc([S], F32)
        kf = R.alloc([S], F32)
        ki = R.alloc([S], I32)
        rot = self.c['c_rot']
        P.add('sp', lambda e: e.dma_start(out=posi, in_=self.pos[s, :].partition_broadcast(128)),
              w=['posi'], dsem=self.ds_misc[0])
        P.add('dve', lambda e: e.tensor_copy(out=posf, in_=posi), r=['posi'], w=['posf'])
        for (dst, dkey, ph) in ((self.cosT, 'cosT', 1), (self.sinT, 'sinT', 2)):
            P.add('dve', lambda e, ph=ph: e.tensor_scalar(out=t, in0=posf, scalar1=rot[:, 0:1], scalar2=rot[:, ph:ph + 1],
                                                          op0=ALU.mult, op1=ALU.add), r=['posf', 'c_rot'], w=['rt'])
            P.add('dve', lambda e: e.tensor_scalar(out=kf, in0=t, scalar1=1.0 / TWO_PI, scalar2=None, op0=ALU.mult),
                  r=['rt'], w=['rk'])
            P.add('dve', lambda e: e.tensor_copy(out=ki, in_=kf), r=['rk'], w=['rki'])
            P.add('dve', lambda e: e.tensor_copy(out=kf, in_=ki), r=['rki'], w=['rk'])
            P.add('dve', lambda e: e.scalar_tensor_tensor(out=t, in0=kf, scalar=-TWO_PI, in1=t,
                                                          op0=ALU.mult, op1=ALU.add), r=['rk', 'rt'], w=['rt'])
            P.add('dve', lambda e: e.tensor_scalar(out=kf, in0=t, scalar1=PI, scalar2=-TWO_PI,
                                                   op0=ALU.is_gt, op1=ALU.mult), r=['rt'], w=['rk'])
            P.add('dve', lambda e: e.tensor_tensor(out=t, in0=t, in1=kf, op=ALU.add), r=['rt', 'rk'], w=['rt'])
            P.add('dve', lambda e: e.tensor_scalar(out=t, in0=t, scalar1=-PI, scalar2=PI,
                                                   op0=ALU.max, op1=ALU.min), r=['rt'], w=['rt'])
            P.add('act', lambda e, dst=dst: e.activation(out=dst[:], in_=t, func=AF.Sin), r=['rt'], w=[dkey])
        R.off = mark

    def phase(self):
        self.flush()
        self.P.barrier()
        self.R.reset()

    def attn_common(self):
        R = self.R
        self.Pt = [R.alloc([512], BF16) for _ in range(NPT)]
        self.U = [R.alloc([512], F32, parts=65) for _ in range(2)]
        self.rd16 = [R.alloc([512], BF16) for _ in range(2)]
        for i_ in range(2):
            self.P.add('pool', lambda e, i_=i_: e.memset(self.rd16[i_][:, :], 0.0), w=[('rd16', i_)])
        self.OT = [R.alloc([S], BF16, parts=64) for _ in range(2)]
        self.Wo = [R.alloc([D], BF16, parts=64) for _ in range(2)]
        self.wsl = Slots(self.P, 'wsl', [R.alloc([8, 128], BF16) for _ in range(4)], self.ds_w)

    def proj_fm(self, wt, wkey, ncols, evac, wcols=None):
        P = self.P
        for tc in range(NCH):
            pi = 6 + self.rot('PJ', 2)
            ps = self.ps[pi]
            for kc in range(8):
                lhsT = wt[:, kc, 0:ncols] if wcols is None else wt[:, kc, wcols[0]:wcols[1]]
                P.add('pe', lambda e, ps=ps, kc=kc, tc=tc, lhsT=lhsT: e.matmul(
                    ps[0:ncols, :], lhsT=lhsT, rhs=self.hnT[:, kc, tc * 512:(tc + 1) * 512],
                    start=(kc == 0), stop=(kc == 7)), r=[wkey] + self.hn_keys(tc), w=[('ps', pi)])
            evac(tc, ps, ('ps', pi))

    def proj_qk_plain(self, src_cols, dst, dkey, split=None):
        P = self.P
        wt, wk, wds = self.wsl.get()
        self.load_w(wt, wk, wds, src_cols)

        def evac(tc, ps, pk):
            sl = slice(tc * 512, (tc + 1) * 512)
            if split is None:
                P.add('act', lambda e: e.activation(out=dst[:, sl], in_=ps[:, :], func=AF.Copy), r=[pk], w=[(dkey, tc)])
            else:
                for hh in range(2):
                    rows = slice(64 * hh, 64 * hh + 64)
                    P.add('act', lambda e, hh=hh, rows=rows: e.activation(out=split[hh][rows, sl], in_=ps[rows, :], func=AF.Copy),
                          r=[pk], w=[(('QZ', hh), tc)])
        self.proj_fm(wt, wk, 128, evac)

    def proj_qk_rot(self, src_cols, src_sw_cols, dst, dkey, dup=False, split=None, ncols=128):
        P, R = self.P, self.R
        wt, wk, wds = self.wsl.get()
        wt2, wk2, wds2 = self.wsl.get()
        if dup:
            P.group(wds)
            self.load_w(wt[:, :, 0:64], wk, wds, src_cols)
            self.load_w(wt[:, :, 64:128], wk + 'b', wds, src_cols)
            P.endgroup(wds)
            P.group(wds2)
            self.load_w(wt2[:, :, 0:64], wk2, wds2, src_sw_cols)
            self.load_w(wt2[:, :, 64:128], wk2 + 'b', wds2, src_sw_cols)
            P.endgroup(wds2)
            wkeys, wkeys2 = [wk, wk + 'b'], [wk2, wk2 + 'b']
        else:
            self.load_w(wt[:, :, 0:ncols], wk, wds, src_cols)
            self.load_w(wt2[:, :, 0:ncols], wk2, wds2, src_sw_cols)
            wkeys, wkeys2 = [wk], [wk2]
        for tc in range(NCH):
            sl = slice(tc * 512, (tc + 1) * 512)
            pa, pb = 6, 7
            for (pi, w_, wkk) in ((pa, wt, wkeys), (pb, wt2, wkeys2)):
                ps = self.ps[pi]
                for kc in range(8):
                    P.add('pe', lambda e, ps=ps, kc=kc, w_=w_, sl=sl: e.matmul(
                        ps[0:ncols, :], lhsT=w_[:, kc, 0:ncols], rhs=self.hnT[:, kc, sl], start=(kc == 0), stop=(kc == 7)),
                        r=wkk + self.hn_keys(tc), w=[('ps', pi)])
            ti = self.rot('rt', 2)
            t1 = self.rtmp[ti]
            P.add('dve', lambda e, t1=t1, sl=sl: e.tensor_tensor(out=t1[0:ncols, :], in0=self.ps[pa][0:ncols, :], in1=self.cosT[0:ncols, sl],
                                                                 op=ALU.mult), r=[('ps', pa), 'cosT'], w=[('rtmp', ti)])
            if split is None:
                P.add('dve', lambda e, sl=sl: e.tensor_tensor(out=dst[0:ncols, sl], in0=self.ps[pb][0:ncols, :], in1=self.sinT[0:ncols, sl],
                                                              op=ALU.mult), r=[('ps', pb), 'sinT'], w=[(dkey, tc)])
                P.add('pool', lambda e, t1=t1, sl=sl: e.tensor_tensor(out=dst[0:ncols, sl], in0=dst[0:ncols, sl], in1=t1[0:ncols, :], op=ALU.add),
                      r=[('rtmp', ti), (dkey, tc)], w=[(dkey, tc)])
            else:
                t2 = self.rtmp2[ti]
                P.add('dve', lambda e, t2=t2, sl=sl: e.tensor_tensor(out=t2, in0=self.ps[pb][:, :], in1=self.sinT[:, sl],
                                                                     op=ALU.mult), r=[('ps', pb), 'sinT'], w=[('rtmp2', ti)])
                for hh in range(2):
                    rows = slice(64 * hh, 64 * hh + 64)
                    P.add('pool', lambda e, t1=t1, t2=t2, sl=sl, hh=hh, rows=rows: e.tensor_tensor(
                        out=split[hh][rows, sl], in0=t1[rows, :], in1=t2[rows, :], op=ALU.add),
                        r=[('rtmp', ti), ('rtmp2', ti)], w=[(('QZ', hh), tc)])

    def proj_v(self, src_cols, ncols, dst_fn, dkey, tok_fn=None, nblk=NB):
        P = self.P
        wt, wk, wds = self.wsl.get()
        self.load_w(wt[:, :, 0:ncols], wk, wds, src_cols)
        for g in range(nblk // 4):
            pi = 6 + self.rot('PJ', 2)
            ps = self.ps[pi]
            for bb in range(4):
                blk = 4 * g + bb
                tsl = slice(blk * 128, (blk + 1) * 128) if tok_fn is None else tok_fn(blk)
                for kc in range(8):
                    P.add('pe', lambda e, ps=ps, bb=bb, kc=kc, tsl=tsl: e.matmul(
                        ps[:, bb * ncols:(bb + 1) * ncols], lhsT=self.hnT[:, kc, tsl], rhs=wt[:, kc, 0:ncols],
                        start=(kc == 0), stop=(kc == 7)), r=[wk] + self.hn_keys(), w=[('ps', pi)])
            P.add('act', lambda e, ps=ps, g=g: e.activation(
                out=dst_fn(g), in_=ps[:, 0:4 * ncols].rearrange('p (b h d) -> p b h d', b=4, d=64), func=AF.Copy),
                r=[('ps', pi)], w=[(dkey, g)])

    def finalize(self, acc, acck, dst, dkey, gate=None, first=True, last=True, extra=None):
        P = self.P
        ui = self.rot('U', 2)
        U = self.U[ui]
        bc = self.ps[5]
        ones = self.c['c_ones']

        rd = self.rd16[ui]
        os16 = self.c['c_os']

        def stage_a():
            if extra is None:
                P.add('act', lambda e: e.activation(out=U[0:64, :], in_=acc[0:64, :], func=AF.Copy), r=[acck], w=[('U', ui)])
                P.add('act', lambda e: e.activation(out=rd[64:65, :], in_=acc[64:65, :], func=AF.Copy, bias=1e-30),
                      r=[acck], w=[('rd16', ui)])
            else:
                extra(U, ('U', ui))
                P.add('dve', lambda e: e.tensor_scalar(out=rd[64:65, :], in0=U[64:65, :], scalar1=1e-30, scalar2=None,
                                                       op0=ALU.add), r=[('U', ui)], w=[('rd16', ui)])

        def stage_b():
            P.add('pe', lambda e: e.matmul(bc[0:64, :], lhsT=os16[:, 0:64], rhs=rd[:, :], start=True, stop=True),
                  r=[('rd16', ui), 'c_os'], w=[('ps', 5)])
            P.add('dve', lambda e: e.reciprocal(out=bc[0:64, :], in_=bc[0:64, :]), r=[('ps', 5)], w=[('ps', 5)])
            if gate is None:
                P.add('dve', lambda e: e.tensor_tensor(out=dst, in0=U[0:64, :], in1=bc[0:64, :], op=ALU.mult),
                      r=[('U', ui), ('ps', 5)], w=[dkey])
            else:
                P.add('dve', lambda e: e.tensor_tensor(out=U[0:64, :], in0=U[0:64, :], in1=bc[0:64, :], op=ALU.mult),
                      r=[('U', ui), ('ps', 5)], w=[('U', ui)])

        def stage_c():
            hc, sl = gate
            Z = self.c['c_Z']
            P.add('pe', lambda e: e.matmul(bc[0:64, :], lhsT=Z[:, hc * 64:hc * 64 + 64], rhs=self.sigT[:, sl],
                                           start=True, stop=True), r=['sigT', 'c_Z', ('U', ui)], w=[('ps', 5)])
            if first:
                P.add('dve', lambda e: e.tensor_tensor(out=self.gacc[0:64, :], in0=U[0:64, :], in1=bc[0:64, :], op=ALU.mult),
                      r=[('U', ui), ('ps', 5)], w=['gacc'])
            else:
                P.add('dve', lambda e: e.tensor_tensor(out=U[0:64, :], in0=U[0:64, :], in1=bc[0:64, :], op=ALU.mult),
                      r=[('U', ui), ('ps', 5)], w=[('U', ui)])
                if last:
                    P.add('dve', lambda e: e.tensor_tensor(out=dst, in0=U[0:64, :], in1=self.gacc[0:64, :], op=ALU.add),
                          r=[('U', ui), 'gacc'], w=[dkey])
                else:
                    P.add('dve', lambda e: e.tensor_tensor(out=self.gacc[0:64, :], in0=U[0:64, :], in1=self.gacc[0:64, :],
                                                           op=ALU.add), r=[('U', ui), 'gacc'], w=['gacc'])
        self.defer(stage_a, PD)
        self.defer(stage_b, PD + 1)
        if gate is not None:
            self.defer(stage_c, PD + 2)

    def outproj_pair(self, w_out, pair, ot_keys):
        P = self.P
        self.flush()
        wkeys = []
        for hh in range(2):
            r0 = (2 * pair + hh) * 64
            src = w_out[r0:r0 + 64, :]
            P.add('pool', lambda e, hh=hh, src=src: e.dma_start(out=self.Wo[hh][:, :], in_=src), w=[('Wo', hh)],
                  dsem=self.ds_wo[hh])
            wkeys.append(('Wo', hh))
        for b in range(NB):
            for half in range(2):
                pi = 6 + self.rot('PJ', 2)
                ps = self.ps[pi]
                for hh in range(2):
                    P.add('pe', lambda e, ps=ps, hh=hh, b=b, half=half: e.matmul(
                        ps[:, :], lhsT=self.OT[hh][0:64, b * 128:(b + 1) * 128],
                        rhs=self.Wo[hh][0:64, half * 512:(half + 1) * 512], start=(hh == 0), stop=(hh == 1)),
                        r=[('Wo', hh)] + ot_keys[hh], w=[('ps', pi)])
                hsl = self.h[:, b, half * 512:(half + 1) * 512]
                P.add('dve', lambda e, ps=ps, hsl=hsl: e.tensor_tensor(out=hsl, in0=ps[:, :], in1=hsl, op=ALU.add),
                      r=[('ps', pi), ('h', b, half)], w=[('h', b, half)])

    def l0_mixer(self, s):
        P, R = self.P, self.R
        self.phase()
        self.attn_common()
        self.rtmp = [R.alloc([512], F32) for _ in range(2)]
        self.rtmp2 = [R.alloc([512], F32) for _ in range(2)]
        mark0 = R.off
        Qz = [R.alloc([S], BF16) for _ in range(2)]
        KT = R.alloc([S], BF16)
        Vt = R.alloc([NB, 2, 65], BF16)
        P.add('pool', lambda e: e.memset(Qz[0][64:128, :], 0.0), w=['qz0'])
        P.add('pool', lambda e: e.memset(Qz[1][0:64, :], 0.0), w=['qz1'])
        w_in = self.fd_w_in
        fb = R.alloc([1, 8], F32)
        lg = R.alloc([NB, 8], F32)
        cl = R.alloc([NB, 8], F32)
        offs = R.alloc([NB, 8], F32)
        tot = R.alloc([NB, 8], F32)
        Bt = R.alloc([8, NB, NB], F32)
        P.add('sp', lambda e: e.dma_start(out=fb[:, 0, :], in_=self.fd_forget_b[0, :].partition_broadcast(128)),
              w=['fb'], dsem=self.ds_misc[1])
        wf, wfk, wfds = self.wsl.get()
        self.load_w(wf[:, :, 0:8], wfk, wfds, w_in[:, 3072:3080])
        psf = self.ps[4]
        for b in range(NB):
            for kc in range(8):
                P.add('pe', lambda e, b=b, kc=kc: e.matmul(psf[:, b * 8:(b + 1) * 8],
                                                           lhsT=self.hnT[:, kc, b * 128:(b + 1) * 128], rhs=wf[:, kc, 0:8],
                                                           start=(kc == 0), stop=(kc == 7)),
                      r=[wfk] + self.hn_keys(), w=[('ps', 4)])
        P.add('dve', lambda e: e.tensor_tensor(out=lg, in0=psf[:, 0:128].rearrange('p (b h) -> p b h', h=8),
                                               in1=fb[:, 0:1, :].broadcast_to([128, NB, 8]), op=ALU.add),
              r=[('ps', 4), 'fb'], w=['lg'])
        P.add('act', lambda e: e.activation(out=lg, in_=lg, func=AF.Exp, scale=-1.0), r=['lg'], w=['lg'])
        P.add('dve', lambda e: e.tensor_scalar(out=lg, in0=lg, scalar1=1.0, scalar2=None, op0=ALU.add), r=['lg'], w=['lg'])
        P.add('act', lambda e: e.activation(out=lg, in_=lg, func=AF.Ln), r=['lg'], w=['lg'])
        lg2 = lg.rearrange('p b h -> p (b h)')
        P.add('pe', lambda e: e.matmul(self.ps[6][:, 0:128], lhsT=self.c['c_utri'][:], rhs=lg2, start=True, stop=True),
              r=['lg', 'c_utri'], w=[('ps', 6)])
        P.add('pe', lambda e: e.matmul(self.ps[7][:, 0:128], lhsT=self.c['c_ones'][:], rhs=lg2, start=True, stop=True),
              r=['lg', 'c_ones'], w=[('ps', 7)])
        P.add('act', lambda e: e.activation(out=tot, in_=self.ps[7][:, 0:128].rearrange('p (b h) -> p b h', h=8),
                                            func=AF.Copy), r=[('ps', 7)], w=['tot'])
        P.add('pool', lambda e: e.memset(offs[:, 0, :], 0.0), w=[('offs', 0)])
        for j in range(1, NB):
            P.add('dve', lambda e, j=j: e.tensor_tensor(out=offs[:, j, :], in0=offs[:, j - 1, :], in1=tot[:, j - 1, :],
                                                        op=ALU.add), r=[('offs', j - 1), 'tot'], w=[('offs', j)])
        allo = [('offs', j) for j in range(NB)]
        P.add('dve', lambda e: e.tensor_tensor(out=cl, in0=self.ps[6][:, 0:128].rearrange('p (b h) -> p b h', h=8),
                                               in1=offs, op=ALU.add), r=[('ps', 6)] + allo, w=['cl'])
        for hh in range(8):
            for qb in range(NB):
                P.add('dve', lambda e, hh=hh, qb=qb: e.tensor_scalar(
                    out=Bt[:, hh, qb, :], in0=cl[:, :, hh], scalar1=offs[:, qb, hh:hh + 1], scalar2=None,
                    op0=ALU.subtract), r=['cl'] + allo, w=[('Bt', hh)])
        wm = self.c['c_wm']
        for hp in range(4):
            c0 = hp * 128
            self.proj_qk_plain(w_in[:, c0:c0 + 128], None, None, split=Qz)
            self.proj_qk_plain(w_in[:, 512 + c0:512 + c0 + 128], KT, 'KT')
            P.add('pool', lambda e: e.memset(Vt[:, :, :, 64:65], 1.0), w=['Vones'])
            self.proj_v(w_in[:, 1024 + c0:1024 + c0 + 128], 128, lambda g: Vt[:, 4 * g:4 * g + 4, :, 0:64], 'Vt')
            qk = [('QT', t) for t in range(NCH)]
            kk = [('KT', t) for t in range(NCH)]
            ot_keys = []
            for hh in range(2):
                head = 2 * hp + hh
                rows = slice(64 * hh, 64 * hh + 64)
                for i in range(NCH):
                    ai = 3 + self.rot('ACC', 2)
                    acc = self.ps[ai]
                    nkb = 4 * i + 4
                    for kb in range(nkb):
                        self.step()
                        si = self.rot('S', 3)
                        ps = self.ps[si]
                        P.add('pe', lambda e, ps=ps, kb=kb, i=i, hh=hh: e.matmul(
                            ps[:, :], lhsT=KT[:, kb * 128:(kb + 1) * 128], rhs=Qz[hh][:, i * 512:(i + 1) * 512],
                            start=True, stop=True), r=[('KT', kb // 4), (('QZ', hh), i), 'qz0', 'qz1'], w=[('ps', si)])
                        pi = self.rot('Pt', NPT)
                        pt = self.Pt[pi]
                        r0 = max(0, kb - 4 * i)
                        pkeys = []
                        for rr in range(r0, 4):
                            qb = 4 * i + rr
                            P.add('act', lambda e, ps=ps, pt=pt, rr=rr, qb=qb, kb=kb, head=head: e.activation(
                                out=pt[:, rr * 128:(rr + 1) * 128], in_=ps[:, rr * 128:(rr + 1) * 128], func=AF.Exp,
                                scale=0.125, bias=Bt[:, head, qb, kb:kb + 1]),
                                r=[('ps', si), ('Bt', head)], w=[('Pt', pi, rr)])
                            pkeys.append(('Pt', pi, rr))
                        c_lo = r0 * 128
                        if kb >= 4 * i:
                            r = kb - 4 * i
                            P.add('dve', lambda e, pt=pt, r=r, c_lo=c_lo: e.tensor_tensor(
                                out=pt[:, c_lo:512], in0=pt[:, c_lo:512], in1=wm[:, 384 - 128 * r + c_lo:384 - 128 * r + 512],
                                op=ALU.mult), r=pkeys + ['c_wm'], w=pkeys)
                        def back(acc=acc, pt=pt, kb=kb, hh=hh, c_lo=c_lo, st=(kb == 0), sp=(kb == nkb - 1), pkeys=pkeys, ai=ai):
                            P.add('pe', lambda e: e.matmul(
                                acc[0:65, c_lo:512], lhsT=Vt[:, kb, hh, :], rhs=pt[:, c_lo:512], start=st, stop=sp),
                                r=pkeys + [('Vt', kb // 4), 'Vones'], w=[('ps', ai)])
                        self.defer(back, PD)
                    self.finalize(acc, ('ps', ai), self.OT[hh][0:64, i * 512:(i + 1) * 512], ('OT', hh, i))
                ot_keys.append([('OT', hh, i) for i in range(NCH)])
            self.outproj_pair(self.fd_w_out, hp, ot_keys)
        self.flush()
        P.barrier()
        R.off = mark0
        Qz = [R.alloc([S], BF16) for _ in range(2)]
        KT = R.alloc([S], BF16)
        P.add('pool', lambda e: e.memset(Qz[0][64:128, :], 0.0), w=['qz0'])
        P.add('pool', lambda e: e.memset(Qz[1][0:64, :], 0.0), w=['qz1'])
        Vd = [R.alloc([NB, 2, 65], BF16) for _ in range(3)]
        self.Vd = Vd
        dm3 = self.c['c_dm'][:, :].rearrange('p (a b) -> p a b', b=128)
        for dp in range(4):
            c0 = dp * 128
            self.proj_qk_rot(w_in[:, 1536 + c0:1536 + c0 + 128], self.fd_w_sw[:, c0:c0 + 128], None, None, split=Qz)
            self.proj_qk_rot(w_in[:, 2048 + c0:2048 + c0 + 128], self.fd_w_sw[:, 512 + c0:512 + c0 + 128], KT, 'KT')
            for pat in range(3):
                P.add('pool', lambda e, pat=pat: e.memset(Vd[pat][:, :, :, 64:65], 1.0), w=[('Vones', pat)])
            vsrc = w_in[:, 2560 + c0:2560 + c0 + 128]
            self.proj_v(vsrc, 128, lambda g: Vd[0][:, 4 * g:4 * g + 4, :, 0:64], ('Vd', 0))
            self.proj_v(vsrc, 128, lambda g: Vd[1][:, 4 * g:4 * g + 4, :, 0:64], ('Vd', 1),
                        tok_fn=lambda blk: ssl((blk // 4) + 512 * (blk % 4), 128, 4))
            self.proj_v(vsrc, 128, lambda g: Vd[2][:, 4 * g:4 * g + 4, :, 0:64], ('Vd', 2),
                        tok_fn=lambda blk: ssl(blk, 128, 16))
            ot_keys = []
            for hh in range(2):
                rows = slice(64 * hh, 64 * hh + 64)
                for i in range(NCH):
                    acc1, acc4, acc16 = self.ps[3], self.ps[4], self.ps[6]
                    for g in range(2):
                        slots = []
                        for qb in (4 * i + 2 * g, 4 * i + 2 * g + 1):
                            hasp = qb >= 1
                            kb = max(qb - 1, 0)
                            qsl = slice(qb * 128, qb * 128 + 128)
                            slots.append((qsl, qsl, (acc1, ('ps', 3), (qb - 4 * i) * 128, 0, qb, True, not hasp)))
                            slots.append((slice(kb * 128, kb * 128 + 128), qsl,
                                          (acc1, ('ps', 3), (qb - 4 * i) * 128, 0, kb, False, True) if hasp else None))
                        self.dil_group(KT, Qz[hh], rows, hh, slots, dm3, ['c_dm'], 128)
                    for g in range(2):
                        slots = []
                        for r_ in (2 * g, 2 * g + 1):
                            ip = max(i - 1, 0)
                            qsl = ssl(r_ + 512 * i, 128, 4)
                            ksl = ssl(r_ + 512 * ip, 128, 4)
                            slots.append((qsl, qsl, (acc4, ('ps', 4), r_ * 128, 1, r_ * 4 + i, True, i == 0)))
                            slots.append((ksl, qsl, (acc4, ('ps', 4), r_ * 128, 1, r_ * 4 + ip, False, True) if i >= 1 else None))
                        self.dil_group(KT, Qz[hh], rows, hh, slots, dm3, ['c_dm'], 128)
                    slots = []
                    for r_ in range(16):
                        slots.append((ssl(r_, 128, 16), ssl(r_ + 512 * i, 32, 16),
                                      (acc16, ('ps', 6), r_ * 32, 2, r_, True, True)))
                    m16 = self.c['c_wm'][:, 384 + 32 * i:416 + 32 * i].unsqueeze(1).broadcast_to([128, 16, 32])
                    self.dil_group(KT, Qz[hh], rows, hh, slots, m16, ['c_wm'], 32)

                    def extra(U, uk):
                        P.add('act', lambda e: e.activation(out=U[0:65, :], in_=self.ps[3][0:65, :], func=AF.Copy),
                              r=[('ps', 3)], w=[uk])
                        P.add('dve', lambda e: e.tensor_tensor(
                            out=U[0:65, :].rearrange('p (q r) -> p r q', r=4),
                            in0=self.ps[4][0:65, :].rearrange('p (r q) -> p r q', r=4),
                            in1=U[0:65, :].rearrange('p (q r) -> p r q', r=4), op=ALU.add), r=[('ps', 4), uk], w=[uk])
                        P.add('dve', lambda e: e.tensor_tensor(
                            out=U[0:65, :].rearrange('p (q r) -> p r q', r=16),
                            in0=self.ps[6][0:65, :].rearrange('p (r q) -> p r q', r=16),
                            in1=U[0:65, :].rearrange('p (q r) -> p r q', r=16), op=ALU.add), r=[('ps', 6), uk], w=[uk])
                    self.finalize(None, None, self.OT[hh][0:64, i * 512:(i + 1) * 512], ('OT', hh, i), extra=extra)
                ot_keys.append([('OT', hh, i) for i in range(NCH)])
            self.outproj_pair(self.fd_w_out, 4 + dp, ot_keys)

    def dil_group(self, KT, QT, rows, hh, slots, mask_ap, mkeys, width):
        P = self.P
        Vd = self.Vd
        allq = [(('QZ', hh), t) for t in range(NCH)] + ['qz0', 'qz1']
        allk = [('KT', t) for t in range(NCH)]
        self.step()
        si = self.rot('Sd', 3)
        ps = self.ps[si]
        for j, (ksl, qsl, pv) in enumerate(slots):
            P.add('pe', lambda e, j=j, ksl=ksl, qsl=qsl: e.matmul(
                ps[:, j * width:(j + 1) * width], lhsT=KT[:, ksl], rhs=QT[:, qsl],
                start=True, stop=True), r=allk + allq, w=[('ps', si)])
        pi = self.rot('Pt', NPT)
        pt = self.Pt[pi]
        P.add('act', lambda e: e.activation(out=pt[:, :], in_=ps[:, :], func=AF.Exp, scale=0.125),
              r=[('ps', si)], w=[('Pt', pi, 0)])
        ptv = pt[:, :].rearrange('p (a b) -> p a b', b=width)
        P.add('dve', lambda e: e.tensor_tensor(out=ptv, in0=ptv, in1=mask_ap, op=ALU.mult),
              r=[('Pt', pi, 0)] + mkeys, w=[('Pt', pi, 0)])
        def back():
            for j, (ksl, qsl, pv) in enumerate(slots):
                if pv is None:
                    continue
                acc, ak, col0, vpat, vblk, st, sp = pv
                vkeys = [(('Vd', vpat), g) for g in range(4)] + [('Vones', vpat)]
                P.add('pe', lambda e, j=j, acc=acc, col0=col0, vpat=vpat, vblk=vblk, st=st, sp=sp: e.matmul(
                    acc[0:65, col0:col0 + width], lhsT=Vd[vpat][:, vblk, hh, :], rhs=pt[:, j * width:(j + 1) * width],
                    start=st, stop=sp), r=[('Pt', pi, 0)] + vkeys, w=[ak])
        self.defer(back, PD)

    def dump_h(self, s):
        P = self.P
        for b in range(NB):
            o = P.add('sp', lambda e, b=b: e.dma_start(out=self.y[s, b * 128:(b + 1) * 128, :], in_=self.h[:, b, :]),
                      r=self.hk(b), w=[('y', s, b)], dsem=self.ds_y)
        self.P.final.append(o)

    def build(self):
        P = self.P
        st = self.stages
        self.load_consts()
        for s in range(self.n_seq):
            self.phase()
            self.load_seq(s)
            self.rotary_tables(s)
            if 'l0' in st:
                self.phase()
                self.norm(self.mix_norm[0:1, :])
                self.l0_mixer(s)
                if 'l0ffn' in st:
                    self.phase()
                    self.norm(self.ffn_norm[0:1, :])
                    self.phase()
                    self.ffn_setup()
                    if 'noffn' not in st:
                        self.ffn(self.dense_w_gate, self.dense_w_up, self.dense_w_down, None)
                    if 'nople' not in st:
                        self.ple(s, 0)
            if 'l1' in st:
                self.phase()
                self.norm(self.mix_norm[1:2, :])
                self.nsa_mixer(s)
                if 'l1ffn' in st:
                    self.moe()
                    self.ple(s, 1)
            if 'final' in st:
                self.phase()
                self.norm(self.final_norm[0:1, :], final_out=s)
            else:
                self.dump_h(s)
        if 'final' in st:
            last = [o for o in P.by_eng['sp'] if o.dsem is self.ds_y][-1]
            P.final.append(last)
        P.emit()
        return self.nc


SWAP64 = np.concatenate([np.arange(32, 64), np.arange(0, 32)])


def _swap_cols(w):
    d, n = w.shape
    idx = (np.arange(n) // 64) * 64 + SWAP64[np.arange(n) % 64]
    return np.ascontiguousarray(w[:, idx])


def host_inputs(inputs, n_cores, n_seq, consts):
    f = lambda a: np.ascontiguousarray(np.asarray(a, dtype=np.float32))
    shared = {}
    for k in ('mix_norm', 'ffn_norm', 'ple_norm', 'ple_gate_w', 'ple_proj_w'):
        shared[k] = f(inputs[k])
    shared['final_norm'] = f(inputs['final_norm']).reshape(1, D)
    for k in ('fd_w_in', 'fd_forget_b', 'fd_w_out', 'dense_w_gate', 'dense_w_up', 'dense_w_down', 'nsa_w_in',
              'nsa_pos_k', 'nsa_w1_k', 'nsa_w2_k', 'nsa_pos_v', 'nsa_w1_v', 'nsa_w2_v', 'nsa_w_out',
              'moe_w_router', 'moe_b_router', 'moe_w_gate', 'moe_w_up', 'moe_w_down'):
        shared[k] = f(inputs[k])[0]
    shared['nsa_b1_k'] = f(inputs['nsa_b1_k'])[0].reshape(256, 1)
    shared['nsa_b1_v'] = f(inputs['nsa_b1_v'])[0].reshape(256, 1)
    shared['fd_w_sw'] = _swap_cols(shared['fd_w_in'][:, 1536:2560])
    nw = shared['nsa_w_in']
    shared['nsa_w_sw'] = _swap_cols(np.concatenate([nw[:, 0:1280], nw[:, 1536:1792], nw[:, 2048:2304]], axis=1))
    shared.update(consts)
    x = f(inputs['x'])
    p = f(inputs['p'])
    pos = np.ascontiguousarray(np.asarray(inputs['positions'], dtype=np.int32))
    maps = []
    for c in range(n_cores):
        m = dict(shared)
        m['x'] = np.ascontiguousarray(x[c * n_seq:(c + 1) * n_seq])
        m['p'] = np.ascontiguousarray(p[:, c * n_seq:(c + 1) * n_seq])
        m['positions'] = np.ascontiguousarray(pos[c * n_seq:(c + 1) * n_seq])
        maps.append(m)
    return maps


_CACHE = {}


def kernel(**inputs):
    n_cores, n_seq = 8, 2
    if 'nc' not in _CACHE:
        b = Builder(n_seq=n_seq, stages=('l0', 'l0ffn', 'l1', 'l1ffn', 'final'))
        _CACHE['nc'] = b.build()
        _CACHE['b'] = b
    nc = _CACHE['nc']
    maps = host_inputs(inputs, n_cores, n_seq, make_consts())
    res = run_bass_kernel_spmd(nc, maps, core_ids=list(range(n_cores)))
    out = np.concatenate([np.asarray(r['y'], dtype=np.float32) for r in res.results], axis=0)
    return out


def _ffn_setup(self):
    R = self.R
    self.Wg = [R.alloc([8, 512], BF16) for _ in range(2)]
    self.Wu = [R.alloc([8, 512], BF16) for _ in range(2)]
    self.Wd = [R.alloc([4, D], BF16) for _ in range(2)]
    self.At = [R.alloc([4, 512], BF16) for _ in range(2)]
    self.Sg = [R.alloc([512], F32) for _ in range(2)]


def _ffn(self, wg, wu, wd, comb=None):
    P, R = self.P, self.R
    for fg in range(7):
        wi = self.rot('ffnw', 2)
        Wg, Wu, Wd = self.Wg[wi], self.Wu[wi], self.Wd[wi]
        fsl = slice(fg * 512, (fg + 1) * 512)
        self.load_w(Wg, ('Wg', wi), self.ds_ffn[wi], wg[:, fsl])
        self.load_w(Wu, ('Wu', wi), self.ds_ffn[2 + wi], wu[:, fsl])
        self.load_w(Wd, ('Wd', wi), self.ds_ffn[4 + wi], wd[fsl, :])
        for tc in range(NCH):
            ai = self.rot('At', 2)
            At = self.At[ai]
            for fc in range(4):
                gi = self.rot('Gp', 2)
                ui = 2 + self.rot('Up', 2)
                for (pi, W_, wk) in ((gi, Wg, ('Wg', wi)), (ui, Wu, ('Wu', wi))):
                    ps = self.ps[pi]
                    for kc in range(8):
                        P.add('pe', lambda e, ps=ps, W_=W_, kc=kc, fc=fc, tc=tc: e.matmul(
                            ps[:, :], lhsT=W_[:, kc, fc * 128:(fc + 1) * 128], rhs=self.hnT[:, kc, tc * 512:(tc + 1) * 512],
                            start=(kc == 0), stop=(kc == 7)), r=[wk] + self.hn_keys(tc), w=[('ps', pi)])
                si = self.rot('Sg', 2)
                Sg = self.Sg[si]
                P.add('act', lambda e, Sg=Sg, gi=gi: e.activation(out=Sg, in_=self.ps[gi][:, :], func=AF.Silu),
                      r=[('ps', gi)], w=[('Sg', si)])
                P.add('dve', lambda e, Sg=Sg, ui=ui, At=At, fc=fc: e.tensor_tensor(
                    out=At[:, fc, :], in0=self.ps[ui][:, :], in1=Sg, op=ALU.mult),
                    r=[('ps', ui), ('Sg', si)], w=[('At', ai, fc)])
            akeys = [('At', ai, fc) for fc in range(4)]
            for bb in range(4):
                b = 4 * tc + bb
                for half in range(2):
                    yi = 4 + self.rot('Yp', 3)
                    ps = self.ps[yi]
                    for fc in range(4):
                        P.add('pe', lambda e, ps=ps, At=At, Wd=Wd, fc=fc, bb=bb, half=half: e.matmul(
                            ps[:, :], lhsT=At[:, fc, bb * 128:(bb + 1) * 128], rhs=Wd[:, fc, half * 512:(half + 1) * 512],
                            start=(fc == 0), stop=(fc == 3)), r=akeys + [('Wd', wi)], w=[('ps', yi)])
                    hsl = self.h[:, b, half * 512:(half + 1) * 512]
                    if comb is None:
                        P.add('dve', lambda e, ps=ps, hsl=hsl: e.tensor_tensor(out=hsl, in0=ps[:, :], in1=hsl, op=ALU.add),
                              r=[('ps', yi), ('h', b, half)], w=[('h', b, half)])
                    else:
                        P.add('dve', lambda e, ps=ps, hsl=hsl, b=b: e.scalar_tensor_tensor(
                            out=hsl, in0=ps[:, :], scalar=comb[:, b:b + 1], in1=hsl, op0=ALU.mult, op1=ALU.add),
                            r=[('ps', yi), ('h', b, half), 'comb'], w=[('h', b, half)])


def _ple(self, s, layer):
    P, R = self.P, self.R
    self.phase()
    self.norm(self.ple_norm[layer:layer + 1, :])
    self.phase()
    Wpg = R.alloc([8, D], BF16)
    Wpp = R.alloc([2, D], BF16)
    p32 = R.alloc([NB, 256], F32)
    pT = R.alloc([2, S], BF16)
    sg = [R.alloc([512], F32) for _ in range(2)]
    self.load_w(Wpg, 'Wpg', self.ds_ffn[0], self.ple_gate_w[layer])
    self.load_w(Wpp, 'Wpp', self.ds_ffn[1], self.ple_proj_w[layer])
    P.add('sp', lambda e: e.dma_start(out=p32, in_=self.p[layer, s].rearrange('(b p) c -> p b c', p=128)),
          w=['p32'], dsem=self.ds_misc[2])
    ident = self.c['c_ident']
    for g in range(NB // 2):
        pi = 6 + self.rot('PJ', 2)
        ps = self.ps[pi]
        for bb in range(2):
            b = 2 * g + bb
            for c2 in range(2):
                j = bb * 2 + c2
                P.add('pe', lambda e, ps=ps, j=j, b=b, c2=c2: e.transpose(
                    out=ps[:, j * 128:(j + 1) * 128], in_=p32[:, b, c2 * 128:(c2 + 1) * 128], identity=ident[:]),
                    r=['p32', 'c_ident'], w=[('ps', pi)])
        P.add('act', lambda e, ps=ps, g=g: e.activation(
            out=pT[:, :, g * 256:(g + 1) * 256].rearrange('p c (b t) -> p b c t', b=2),
            in_=ps[:, :].rearrange('p (b c t) -> p b c t', b=2, c=2), func=AF.Copy), r=[('ps', pi)], w=[('pT', g)])
    for b in range(NB):
        for half in range(2):
            gi = self.rot('Gp', 2)
            ui = 2 + self.rot('Up', 2)
            csl = slice(half * 512, (half + 1) * 512)
            for kc in range(8):
                P.add('pe', lambda e, gi=gi, kc=kc, b=b, csl=csl: e.matmul(
                    self.ps[gi][:, :], lhsT=self.hnT[:, kc, b * 128:(b + 1) * 128], rhs=Wpg[:, kc, csl],
                    start=(kc == 0), stop=(kc == 7)), r=['Wpg', ('hnT', b)], w=[('ps', gi)])
            for c2 in range(2):
                P.add('pe', lambda e, ui=ui, c2=c2, b=b, csl=csl: e.matmul(
                    self.ps[ui][:, :], lhsT=pT[:, c2, b * 128:(b + 1) * 128], rhs=Wpp[:, c2, csl],
                    start=(c2 == 0), stop=(c2 == 1)), r=['Wpp', ('pT', b // 2)], w=[('ps', ui)])
            si = self.rot('Sg', 2)
            P.add('act', lambda e, si=si, gi=gi: e.activation(out=sg[si], in_=self.ps[gi][:, :], func=AF.Sigmoid),
                  r=[('ps', gi)], w=[('sg', si)])
            P.add('dve', lambda e, si=si, ui=ui: e.tensor_tensor(out=sg[si], in0=self.ps[ui][:, :], in1=sg[si], op=ALU.mult),
                  r=[('ps', ui), ('sg', si)], w=[('sg', si)])
            hsl = self.h[:, b, csl]
            P.add('pool', lambda e, si=si, hsl=hsl: e.tensor_tensor(out=hsl, in0=hsl, in1=sg[si], op=ALU.add),
                  r=[('sg', si), ('h', b, half)], w=[('h', b, half)])


Builder.ffn = _ffn
Builder.ffn_setup = _ffn_setup
Builder.ple = _ple


def _unit(self, lhsT, rhs, rkeys, mask_ap, mkeys, v_ap, vkeys, acc, ai, st, sp, aug=None):
    P = self.P
    self.step()
    si = self.rot('S', 3)
    ps = self.ps[si]
    P.add('pe', lambda e: e.matmul(ps[:, :], lhsT=lhsT, rhs=rhs, start=True, stop=(aug is None)), r=rkeys, w=[('ps', si)])
    if aug is not None:
        l2, r2, k2 = aug
        P.add('pe', lambda e: e.matmul(ps[:, :], lhsT=l2, rhs=r2, start=False, stop=True), r=k2, w=[('ps', si)])
    pi = self.rot('Pt', NPT)
    pt = self.Pt[pi]
    P.add('act', lambda e: e.activation(out=pt[:, :], in_=ps[:, :], func=AF.Exp, scale=0.125), r=[('ps', si)], w=[('Pt', pi, 0)])
    if mask_ap is not None:
        P.add('dve', lambda e: e.tensor_tensor(out=pt[:, :], in0=pt[:, :], in1=mask_ap, op=ALU.mult),
              r=[('Pt', pi, 0)] + mkeys, w=[('Pt', pi, 0)])
    self.defer(lambda: P.add('pe', lambda e: e.matmul(acc[0:65, :], lhsT=v_ap, rhs=pt[:, :], start=st, stop=sp),
                             r=[('Pt', pi, 0)] + vkeys, w=[('ps', ai)]), PD)


def _nsa_mixer(self, s):
    P, R = self.P, self.R
    self.phase()
    self.attn_common()
    self.rtmp = [R.alloc([512], F32) for _ in range(2)]
    self.sigT = R.alloc([S], BF16)
    P.add('pool', lambda e: e.memset(self.sigT[:, :], 0.0), w=['sigT'])
    self.gacc = R.alloc([512], F32, parts=64)
    Qz = [R.alloc([S], BF16) for _ in range(4)]
    KA = R.alloc([S], BF16)
    KB = R.alloc([S], BF16)
    Vs = R.alloc([NB, 1, 65], BF16)
    Vw = R.alloc([NB, 1, 65], BF16)
    W1c = R.alloc([8, 256], BF16, parts=64)
    W2k = R.alloc([2, 64], BF16)
    W2v = R.alloc([2, 64], BF16)
    gel = [R.alloc([128], BF16) for _ in range(2)]
    xs = R.alloc([128], F32)
    x2 = R.alloc([128], F32)
    hb = R.alloc([2], F32)
    b1t = R.alloc([2], F32)
    pos32 = R.alloc([64], F32, parts=32)
    posT = R.alloc([32], BF16, parts=64)
    kcmpT = R.alloc([128], BF16)
    vcmp = R.alloc([65], BF16)
    E4 = R.alloc([4, 128], F32)
    pcs = R.alloc([128], F32)
    den4 = R.alloc([4], F32)
    imp = R.alloc([32], F32)
    impm = R.alloc([32], F32)
    top8 = R.alloc([8], F32)
    sel = R.alloc([96], F32)
    w_in, w_sw = self.nsa_w_in, self.nsa_w_sw
    ident = self.c['c_ident']
    wm = self.c['c_wm']
    wt, wk, wds = self.wsl.get()
    self.load_w(wt[:, :, 0:48], wk, wds, w_in[:, 2560:2608])

    def evac_g(tc, ps, pk):
        P.add('act', lambda e: e.activation(out=self.sigT[0:48, tc * 512:(tc + 1) * 512], in_=ps[0:48, :], func=AF.Sigmoid),
              r=[pk], w=['sigT'])
    self.proj_fm(wt, wk, 48, evac_g)
    if NSA_STOP == 1:
        return
    P.add('pool', lambda e: e.memset(kcmpT[:, :], 0.0), w=['kcmpT'])
    P.add('pool', lambda e: e.memset(sel[:, :], 0.0), w=['sel'])
    for hh in range(4):
        P.add('pool', lambda e, hh=hh: e.memset(Qz[hh][64:128, :], 0.0), w=[(('NT', hh), t) for t in range(NCH)])
    P.add('pool', lambda e: e.memset(KA[64:128, :], 0.0), w=['KAz'])
    P.add('pool', lambda e: e.memset(KB[64:128, :], 0.0), w=['KBz'])
    P.add('sp', lambda e: e.dma_start(out=KA[64:96, :], in_=self.cst['c_E'][0:32, :]), r=['KAz'], w=['KAe'], dsem=self.ds_misc[0])
    P.add('pool', lambda e: e.memset(vcmp[:, :], 0.0), w=['vcmp'])
    P.add('pool', lambda e: e.memset(Vs[:, :, :, 64:65], 1.0), w=['Vs1'])
    P.add('pool', lambda e: e.memset(Vw[:, :, :, 64:65], 1.0), w=['Vw1'])
    P.add('pool', lambda e: e.memset(gel[0][:, :], 0.0), w=[('gel', 0)])
    P.add('pool', lambda e: e.memset(gel[1][:, :], 0.0), w=[('gel', 1)])
    allq = [[(('QZ', hh), t) for t in range(NCH)] for hh in range(4)]
    KAk = [('KA', t) for t in range(NCH)]
    KBk = [('KB', t) for t in range(NCH)]
    for g in range(4):
        for hh in range(4):
            c0 = (4 * g + hh) * 64
            self.proj_qk_rot(w_in[:, c0:c0 + 64], w_sw[:, c0:c0 + 64], Qz[hh], ('QZ', hh), ncols=64)
        self.proj_qk_rot(w_in[:, 1024 + 64 * g:1024 + 64 * g + 64], w_sw[:, 1024 + 64 * g:1024 + 64 * g + 64], KA, 'KA', ncols=64)
        wt, wk, wds = self.wsl.get()
        self.load_w(wt[:, :, 0:64], wk, wds, w_in[:, 1280 + 64 * g:1280 + 64 * g + 64])

        def evac_vc(tc, ps, pk):
            P.add('act', lambda e: e.activation(out=KB[0:64, tc * 512:(tc + 1) * 512], in_=ps[0:64, :], func=AF.Copy),
                  r=[pk], w=[('KB', tc)])
        self.proj_fm(wt, wk, 64, evac_vc)
        for which in range(2):
            src = KA if which == 0 else KB
            skeys = KAk if which == 0 else KBk
            w1 = self.nsa_w1_k if which == 0 else self.nsa_w1_v
            w2 = self.nsa_w2_k if which == 0 else self.nsa_w2_v
            b1 = self.nsa_b1_k if which == 0 else self.nsa_b1_v
            posd = self.nsa_pos_k if which == 0 else self.nsa_pos_v
            P.add('sp', lambda e, posd=posd: e.dma_start(out=pos32[0:32, :], in_=posd), w=['pos32'], dsem=self.ds_misc[0])
            P.add('sp', lambda e, b1=b1: e.dma_start(out=b1t[:, 0:1], in_=b1[0:128, :]), w=[('b1t', 0)], dsem=self.ds_misc[1])
            P.add('sp', lambda e, b1=b1: e.dma_start(out=b1t[:, 1:2], in_=b1[128:256, :]), w=[('b1t', 1)], dsem=self.ds_misc[2])
            if which == 0:
                self.load_w(W2k[:, :, :], 'W2a', self.ds_misc[3], w2)
                w2keys = ['W2a']
            else:
                self.load_w(W2v[:, :, :], 'W2v', self.ds_misc[3], w2)
                w2keys = ['W2v']
            P.add('pe', lambda e: e.transpose(out=self.ps[5][0:64, 0:32], in_=pos32[0:32, 0:64], identity=ident[0:32, 0:32]),
                  r=['pos32', 'c_ident'], w=[('ps', 5)])
            P.add('act', lambda e: e.activation(out=posT[0:64, :], in_=self.ps[5][0:64, 0:32], func=AF.Copy),
                  r=[('ps', 5)], w=['posT'])
            first = True
            for ic in range(4):
                srcw = w1[ic * 512:(ic + 1) * 512, :].rearrange('(i d) c -> d i c', d=64)
                P.add('pool', lambda e, srcw=srcw: e.dma_start(out=W1c[0:64, :, :], in_=srcw), w=['W1c'], dsem=self.ds_w[4])
                for ii in range(8):
                    i = ic * 8 + ii
                    for hc in range(2):
                        P.add('pe', lambda e, ii=ii, i=i, hc=hc, src=src: e.matmul(
                            self.ps[6 + hc][:, 0:127], lhsT=W1c[0:64, ii, hc * 128:(hc + 1) * 128],
                            rhs=src[0:64, ssl(i, 127, 16)], start=(i == 0), stop=(i == 31)),
                            r=['W1c'] + skeys, w=[('ps', 6 + hc)])
                        P.add('pe', lambda e, ii=ii, i=i, hc=hc, st=first: e.matmul(
                            self.ps[5][:, hc:hc + 1], lhsT=W1c[0:64, ii, hc * 128:(hc + 1) * 128],
                            rhs=posT[0:64, i:i + 1], start=st, stop=(i == 31)),
                            r=['W1c', 'posT'], w=[('ps', 5)])
                        first = False
            P.add('dve', lambda e: e.tensor_tensor(out=hb[:, 0:2], in0=self.ps[5][:, 0:2], in1=b1t[:, 0:2], op=ALU.add),
                  r=[('ps', 5), ('b1t', 0), ('b1t', 1)], w=['hb'])
            for hc in range(2):
                pk = ('ps', 6 + hc)
                psh = self.ps[6 + hc]
                P.add('act', lambda e, psh=psh, hc=hc: e.activation(out=xs[:, 0:127], in_=psh[:, 0:127], func=AF.Identity,
                                                                    bias=hb[:, hc:hc + 1]), r=[pk, 'hb'], w=['xs'])
                P.add('dve', lambda e: e.tensor_tensor(out=x2[:, 0:127], in0=xs[:, 0:127], in1=xs[:, 0:127], op=ALU.mult),
                      r=['xs'], w=['x2'])
                P.add('dve', lambda e: e.tensor_scalar(out=x2[:, 0:127], in0=x2[:, 0:127], scalar1=0.044715, scalar2=1.0,
                                                       op0=ALU.mult, op1=ALU.add), r=['x2'], w=['x2'])
                P.add('dve', lambda e: e.tensor_tensor(out=x2[:, 0:127], in0=x2[:, 0:127], in1=xs[:, 0:127], op=ALU.mult),
                      r=['x2', 'xs'], w=['x2'])
                P.add('act', lambda e: e.activation(out=x2[:, 0:127], in_=x2[:, 0:127], func=AF.Sigmoid,
                                                    scale=float(2.0 * np.sqrt(2.0 / np.pi))), r=['x2'], w=['x2'])
                P.add('dve', lambda e, hc=hc: e.tensor_tensor(out=gel[hc][:, 0:127], in0=xs[:, 0:127], in1=x2[:, 0:127],
                                                              op=ALU.mult), r=['x2', 'xs'], w=[('gel', hc)])
            if which == 0:
                for hc in range(2):
                    P.add('pe', lambda e, hc=hc: e.matmul(self.ps[6][0:64, 0:127], lhsT=W2k[:, hc, :], rhs=gel[hc][:, 0:127],
                                                          start=(hc == 0), stop=(hc == 1)),
                          r=[('gel', hc)] + w2keys, w=[('ps', 6)])
                P.add('act', lambda e: e.activation(out=kcmpT[0:64, 0:127], in_=self.ps[6][0:64, 0:127], func=AF.Copy),
                      r=[('ps', 6)], w=['kcmpT'])
            else:
                for hc in range(2):
                    P.add('pe', lambda e, hc=hc: e.matmul(self.ps[6][0:127, 0:64], lhsT=gel[hc][:, 0:127], rhs=W2v[:, hc, :],
                                                          start=(hc == 0), stop=(hc == 1)),
                          r=[('gel', hc)] + w2keys, w=[('ps', 6)])
                P.add('act', lambda e: e.activation(out=vcmp[0:127, 0:64], in_=self.ps[6][0:127, 0:64], func=AF.Copy),
                      r=[('ps', 6)], w=['vcmp'])
                P.add('pool', lambda e: e.memset(vcmp[:, 64:65], 1.0), r=[], w=['vcmp1'])
        if NSA_STOP == 2:
            continue
        self.proj_qk_rot(w_in[:, 1536 + 64 * g:1536 + 64 * g + 64], w_sw[:, 1280 + 64 * g:1280 + 64 * g + 64], KA, 'KA', ncols=64)
        self.proj_qk_rot(w_in[:, 2048 + 64 * g:2048 + 64 * g + 64], w_sw[:, 1536 + 64 * g:1536 + 64 * g + 64], KB, 'KB', ncols=64)
        self.proj_v(w_in[:, 1792 + 64 * g:1792 + 64 * g + 64], 64, lambda gg: Vs[:, 4 * gg:4 * gg + 4, :, 0:64], 'Vs')
        self.proj_v(w_in[:, 2304 + 64 * g:2304 + 64 * g + 64], 64, lambda gg: Vw[:, 4 * gg:4 * gg + 4, :, 0:64], 'Vw')
        for qb in range(NB):
            si = self.rot('S', 3)
            ps = self.ps[si]
            for hh in range(4):
                P.add('pe', lambda e, ps=ps, hh=hh, qb=qb: e.matmul(
                    ps[:, hh * 128:(hh + 1) * 128], lhsT=Qz[hh][:, qb * 128:(qb + 1) * 128], rhs=kcmpT[:, :],
                    start=True, stop=True), r=['kcmpT'] + allq[hh] + [(('NT', hh), t) for t in range(NCH)], w=[('ps', si)])
            P.add('act', lambda e, ps=ps: e.activation(out=E4, in_=ps[:, :].rearrange('p (h n) -> p h n', h=4), func=AF.Exp,
                                                       scale=0.125), r=[('ps', si)], w=['E4'])
            m0s = self.c['c_m0'][:, 120 - 8 * qb:248 - 8 * qb].unsqueeze(1).broadcast_to([128, 4, 128])
            P.add('dve', lambda e, m0s=m0s: e.tensor_tensor(out=E4, in0=E4, in1=m0s, op=ALU.mult), r=['E4', 'c_m0'], w=['E4'])
            P.add('dve', lambda e: e.tensor_reduce(out=den4, in_=E4, axis=AX.X, op=ALU.add), r=['E4'], w=['den4'])
            P.add('dve', lambda e: e.tensor_scalar(out=den4, in0=den4, scalar1=1e-30, scalar2=None, op0=ALU.max),
                  r=['den4'], w=['den4'])
            P.add('dve', lambda e: e.reciprocal(out=den4, in_=den4), r=['den4'], w=['den4'])
            P.add('dve', lambda e: e.tensor_scalar(out=pcs, in0=E4[:, 0, :], scalar1=den4[:, 0:1], scalar2=None, op0=ALU.mult),
                  r=['E4', 'den4'], w=['pcs'])
            for hh in range(1, 4):
                P.add('dve', lambda e, hh=hh: e.scalar_tensor_tensor(out=pcs, in0=E4[:, hh, :], scalar=den4[:, hh:hh + 1],
                                                                     in1=pcs, op0=ALU.mult, op1=ALU.add),
                      r=['E4', 'den4', 'pcs'], w=['pcs'])
            P.add('dve', lambda e: e.tensor_reduce(out=imp, in_=pcs[:, :].rearrange('p (j f) -> p j f', f=4), axis=AX.X,
                                                   op=ALU.add), r=['pcs'], w=['imp'])
            P.add('dve', lambda e: e.tensor_tensor(out=imp[:, 1:32], in0=imp[:, 1:32], in1=pcs[:, ssl(3, 31, 4)], op=ALU.add),
                  r=['pcs', 'imp'], w=['imp'])
            P.add('dve', lambda e, qb=qb: e.tensor_tensor(out=impm, in0=imp, in1=self.c['c_keep'][:, 30 - 2 * qb:62 - 2 * qb],
                                                          op=ALU.mult), r=['imp', 'c_keep'], w=['impm'])
            P.add('dve', lambda e, qb=qb: e.tensor_tensor(out=impm, in0=impm, in1=self.c['c_addv'][:, 30 - 2 * qb:62 - 2 * qb],
                                                          op=ALU.add), r=['impm', 'c_addv'], w=['impm'])
            P.add('dve', lambda e: e.memset(impm[:, 0:1], 1e6), r=['impm'], w=['impm'])
            P.add('dve', lambda e: e.max(out=top8, in_=impm), r=['impm'], w=['top8'])
            P.add('dve', lambda e: e.tensor_scalar(out=sel[:, 64:96], in0=impm, scalar1=top8[:, 7:8], scalar2=None, op0=ALU.is_ge),
                  r=['impm', 'top8'], w=['sel'])
            P.add('pe', lambda e: e.transpose(out=self.ps[5][0:96, 0:128], in_=sel[:, 0:96], identity=ident[:]),
                  r=['sel', 'c_ident'], w=[('ps', 5)])
            for hh in range(4):
                P.add('dve', lambda e, qb=qb, hh=hh: e.tensor_scalar(
                    out=Qz[hh][64:96, qb * 128:(qb + 1) * 128], in0=self.ps[5][64:96, 0:128],
                    scalar1=30000.0, scalar2=-30000.0, op0=ALU.mult, op1=ALU.add),
                    r=[('ps', 5)], w=[(('NT', hh), qb // 4)])
        if NSA_STOP in (3, 5):
            continue
        for jp in range(2):
            ot_keys = []
            for h2 in range(2):
                hh = 2 * jp + h2
                head = 4 * g + hh
                for i in range(NCH):
                    csl = slice(i * 512, (i + 1) * 512)
                    q_ap = Qz[hh][:, csl]
                    qk = [(('QZ', hh), i), (('NT', hh), i)]
                    ai = 3 + self.rot('ACC', 2)
                    self.unit(kcmpT[:, :], q_ap, ['kcmpT'] + qk, self.c['c_cmpT'][:, csl], ['c_cmpT'],
                              vcmp[:, 0:65], ['vcmp', 'vcmp1'], self.ps[ai], ai, True, True)
                    self.finalize(self.ps[ai], ('ps', ai), None, None, gate=(3 * head + 0, csl), first=True, last=False)
                    ai = 3 + self.rot('ACC', 2)
                    nkb = 4 * i + 4
                    for kb in range(nkb):
                        mask = wm[:, 384 - 128 * (kb - 4 * i):384 - 128 * (kb - 4 * i) + 512] if kb >= 4 * i else None
                        self.unit(KA[:, kb * 128:(kb + 1) * 128], q_ap, [('KA', kb // 4), 'KAz', 'KAe'] + qk, mask, ['c_wm'],
                                  Vs[:, kb, 0, :], [('Vs', kb // 4), 'Vs1'], self.ps[ai], ai, kb == 0, kb == nkb - 1)
                    self.finalize(self.ps[ai], ('ps', ai), None, None, gate=(3 * head + 1, csl), first=False, last=False)
                    ai = 3 + self.rot('ACC', 2)
                    kb0 = max(0, 4 * i - 4)
                    for kb in range(kb0, nkb):
                        r_ = kb - 4 * i
                        mask = wm[:, 384 - 128 * r_:384 - 128 * r_ + 512]
                        self.unit(KB[:, kb * 128:(kb + 1) * 128], q_ap, [('KB', kb // 4), 'KBz'] + qk, mask, ['c_wm'],
                                  Vw[:, kb, 0, :], [('Vw', kb // 4), 'Vw1'], self.ps[ai], ai, kb == kb0, kb == nkb - 1)
                    self.finalize(self.ps[ai], ('ps', ai), self.OT[h2][0:64, csl], ('OT', h2, i),
                                  gate=(3 * head + 2, csl), first=False, last=True)
                ot_keys.append([('OT', h2, i) for i in range(NCH)])
            self.outproj_pair(self.nsa_w_out, 2 * g + jp, ot_keys)


Builder.unit = _unit
Builder.nsa_mixer = _nsa_mixer


def _moe(self):
    P, R = self.P, self.R
    self.phase()
    self.norm(self.ffn_norm[1:2, :])
    self.phase()
    comb = R.alloc([8, NB], F32)
    lg = R.alloc([NB, 8], F32)
    rb = R.alloc([1, 8], F32)
    t8 = R.alloc([NB, 8], F32)
    w1 = R.alloc([NB], F32)
    w2 = R.alloc([NB], F32)
    ta = R.alloc([NB], F32)
    tb = R.alloc([NB], F32)
    wr = R.alloc([8, 8], BF16)
    self.load_w(wr, 'wr', self.ds_misc[0], self.moe_w_router)
    P.add('sp', lambda e: e.dma_start(out=rb[:, 0, :], in_=self.moe_b_router[0, :].partition_broadcast(128)),
          w=['rb'], dsem=self.ds_misc[1])
    ps = self.ps[7]
    for b in range(NB):
        for kc in range(8):
            P.add('pe', lambda e, b=b, kc=kc: e.matmul(ps[:, b * 8:(b + 1) * 8], lhsT=self.hnT[:, kc, b * 128:(b + 1) * 128],
                                                       rhs=wr[:, kc, :], start=(kc == 0), stop=(kc == 7)),
                  r=['wr', ('hnT', b)], w=[('ps', 7)])
    P.add('dve', lambda e: e.tensor_tensor(out=lg, in0=ps[:, 0:128].rearrange('p (b h) -> p b h', h=8),
                                           in1=rb[:, 0:1, :].broadcast_to([128, NB, 8]), op=ALU.add),
          r=[('ps', 7), 'rb'], w=['lg'])
    for b in range(NB):
        P.add('dve', lambda e, b=b: e.max(out=t8[:, b, :], in_=lg[:, b, :]), r=['lg'], w=[('t8', b)])
    t8k = [('t8', b) for b in range(NB)]
    P.add('dve', lambda e: e.tensor_tensor(out=w1, in0=t8[:, :, 1], in1=t8[:, :, 0], op=ALU.subtract), r=t8k, w=['w1'])
    P.add('act', lambda e: e.activation(out=w1, in_=w1, func=AF.Exp), r=['w1'], w=['w1'])
    P.add('dve', lambda e: e.tensor_scalar(out=w1, in0=w1, scalar1=1.0, scalar2=None, op0=ALU.add), r=['w1'], w=['w1'])
    P.add('dve', lambda e: e.reciprocal(out=w1, in_=w1), r=['w1'], w=['w1'])
    P.add('dve', lambda e: e.tensor_scalar(out=w2, in0=w1, scalar1=-1.0, scalar2=1.0, op0=ALU.mult, op1=ALU.add),
          r=['w1'], w=['w2'])
    for ex in range(8):
        P.add('dve', lambda e, ex=ex: e.tensor_tensor(out=ta, in0=lg[:, :, ex], in1=t8[:, :, 0], op=ALU.is_equal),
              r=['lg'] + t8k, w=['ta'])
        P.add('dve', lambda e: e.tensor_tensor(out=ta, in0=ta, in1=w1, op=ALU.mult), r=['ta', 'w1'], w=['ta'])
        P.add('dve', lambda e, ex=ex: e.tensor_tensor(out=tb, in0=lg[:, :, ex], in1=t8[:, :, 1], op=ALU.is_equal),
              r=['lg'] + t8k, w=['tb'])
        P.add('dve', lambda e: e.tensor_tensor(out=tb, in0=tb, in1=w2, op=ALU.mult), r=['tb', 'w2'], w=['tb'])
        P.add('dve', lambda e, ex=ex: e.tensor_tensor(out=comb[:, ex, :], in0=ta, in1=tb, op=ALU.add),
              r=['ta', 'tb'], w=['comb'])
    self.ffn_setup()
    for ex in range(8):
        self.ffn(self.moe_w_gate[ex], self.moe_w_up[ex], self.moe_w_down[ex], comb=comb[:, ex, :])


Builder.moe = _moe
```

```python
import numpy as np
import ml_dtypes
from contextlib import ExitStack
import concourse.bass as bass
import concourse.mybir as mybir
from concourse.bass_utils import run_bass_kernel_spmd

dt = mybir.dt
F32, BF16, I32 = dt.float32, dt.bfloat16, dt.int32
AF = mybir.ActivationFunctionType
ALU = mybir.AluOpType
AX = mybir.AxisListType

S = 2048
D = 1024
NB = 16
NCH = 4
DFF = 3584
PI = float(np.pi)
TWO_PI = float(2 * np.pi)

import os as _os
NSA_STOP = int(_os.environ.get('NSA_STOP', '0'))
PD = 4
NPT = PD + 4
ENG_NAMES = ['pe', 'act', 'dve', 'pool', 'sp']


def ssl(start, n, step):
    return slice(start, start + (n - 1) * step + 1, step)

SAME_ENG_SYNC = True


class DSem:
    def __init__(self, sem):
        self.sem = sem
        self.total = 0
        self.open = None


class Op:
    __slots__ = ('eng', 'fn', 'deps', 'dsem', 'sig', 'sem', 'val')


class Prog:
    def __init__(self, nc, stack):
        self.nc = nc
        self.stack = stack
        self.ops = []
        self.by_eng = {e: [] for e in ENG_NAMES}
        self.lastw = {}
        self.readers = {}
        self.nsem = 0
        self.bar_deps = {}
        self.bar_done = set()
        self.since_bar_dma = {}
        self.final = []
        self.uid = 0

    def new_sem(self, name):
        self.nsem += 1
        return self.stack.enter_context(self.nc.semaphore(f"{name}_{self.nsem}"))

    def dsem(self, name):
        return DSem(self.new_sem(name))

    def group(self, ds):
        ds.open = []

    def endgroup(self, ds):
        for o in ds.open:
            o.val = ds.total * 16
        ds.open = None

    def barrier(self):
        deps = dict(self.bar_deps)
        for e in ENG_NAMES:
            for o in reversed(self.by_eng[e]):
                if o.dsem is None:
                    deps[e] = o
                    o.sig = True
                    break
        for k, o in self.since_bar_dma.items():
            deps[k] = o
        self.bar_deps = deps
        self.bar_done = set()
        self.since_bar_dma = {}
        self.lastw = {}
        self.readers = {}

    def add(self, eng, fn, r=(), w=(), dsem=None):
        o = Op()
        o.eng = eng
        o.fn = fn
        o.dsem = dsem
        o.sig = False
        o.sem = None
        o.val = 0
        deps = {}

        def need(p):
            if p is None:
                return
            if p.dsem is None and dsem is None and p.eng == eng:
                if eng == 'pe' or not SAME_ENG_SYNC:
                    return
            deps[id(p)] = p

        for k in r:
            need(self.lastw.get(k))
        for k in w:
            need(self.lastw.get(k))
            rd = self.readers.get(k)
            if rd:
                for q in rd.values():
                    need(q)
        if eng not in self.bar_done:
            self.bar_done.add(eng)
            for p in self.bar_deps.values():
                need(p)
        for k in w:
            self.lastw[k] = o
            self.readers[k] = {}
        for k in r:
            d = self.readers.setdefault(k, {})
            if dsem is not None:
                self.uid += 1
                d[('dma', self.uid)] = o
            else:
                d[eng] = o
        o.deps = list(deps.values())
        for p in o.deps:
            p.sig = True
        if dsem is not None:
            dsem.total += 1
            o.sem = dsem.sem
            o.val = dsem.total * 16
            if dsem.open is not None:
                dsem.open.append(o)
            self.since_bar_dma[id(dsem)] = o
        self.ops.append(o)
        self.by_eng[eng].append(o)
        return o

    def emit(self):
        nc = self.nc
        LIMIT = 30000
        for e in ENG_NAMES:
            cur = None
            cnt = 0
            for o in self.by_eng[e]:
                if o.dsem is None and o.sig:
                    if cur is None or cnt >= LIMIT:
                        cur = self.new_sem(f"e_{e}")
                        cnt = 0
                    cnt += 1
                    o.sem = cur
                    o.val = cnt
        final = self.final

        def run(e, eng):
            known = {}
            for o in self.by_eng[e]:
                need = {}
                for p in o.deps:
                    s = p.sem
                    if s is None:
                        continue
                    cur = need.get(id(s))
                    if cur is None or cur[1] < p.val:
                        need[id(s)] = (s, p.val)
                for sid, (s, v) in need.items():
                    if known.get(sid, 0) < v:
                        eng.wait_ge(s, v)
                        known[sid] = v
                inst = o.fn(eng)
                if o.dsem is not None:
                    inst.then_inc(o.sem, 16)
                elif o.sig:
                    inst.then_inc(o.sem, 1)
            if e == 'sp':
                for o in final:
                    eng.wait_ge(o.sem, o.val)

        with nc.Block() as block:
            @block.tensor
            def _(eng):
                run('pe', eng)

            @block.scalar
            def _(eng):
                run('act', eng)

            @block.vector
            def _(eng):
                run('dve', eng)

            @block.gpsimd
            def _(eng):
                run('pool', eng)

            @block.sync
            def _(eng):
                run('sp', eng)


class Region:
    def __init__(self, ap, nelem):
        self.ap = ap
        self.n = nelem
        self.off = 0
        self.peak = 0

    def reset(self):
        self.off = 0

    def alloc(self, free_shape, dtype, parts=128):
        n = int(np.prod(free_shape))
        mult = 2 if dtype == F32 or dtype == I32 else 1
        nb = n * mult
        nb = (nb + 15) // 16 * 16
        assert self.off + nb <= self.n, f"region overflow {self.off}+{nb}>{self.n}"
        v = self.ap[0:parts, self.off:self.off + n * mult]
        self.off += nb
        self.peak = max(self.peak, self.off)
        if mult == 2:
            v = v.bitcast(dtype)
        if len(free_shape) == 2:
            v = v.rearrange('p (a b) -> p a b', b=free_shape[1])
        elif len(free_shape) == 3:
            v = v.rearrange('p (a b c) -> p a b c', b=free_shape[1], c=free_shape[2])
        return v


def _bf(a):
    return np.ascontiguousarray(a.astype(np.float32)).astype(ml_dtypes.bfloat16)


def make_consts():
    c = {}
    k = np.arange(128)[:, None]
    c['c_ident'] = np.eye(128, dtype=np.float32)
    c['c_utri'] = (k <= np.arange(128)[None, :]).astype(np.float32)
    c['c_ones'] = np.ones((128, 128), np.float32)
    os_ = np.zeros((128, 64), np.float32)
    os_[64, :] = 1.0
    c['c_os'] = _bf(os_)
    y = np.arange(-384, 1024)[None, :]
    c['c_wm'] = _bf(((y - k) >= 0) & ((y - k) < 512))
    q = np.arange(128)[None, :]
    cur = (k <= q)
    prev = (k >= q)
    c['c_dm'] = _bf(np.concatenate([cur, prev, cur, prev], axis=1))
    n = np.arange(128)[:, None]
    qq = np.arange(2048)[None, :]
    c['c_cmpT'] = _bf((16 * n + 31 <= qq) & (n <= 126))
    xq = np.arange(128)[:, None]
    xx = np.arange(-120, 128)[None, :]
    c['c_m0'] = (16 * xx + 31 <= xq).astype(np.float32)
    xs = np.arange(-30, 32)[None, :]
    curp = (xq // 64)
    fut = xs > curp
    forced = (xs == curp) | (xs == curp - 1)
    keep = (~fut) & (~forced)
    addv = np.where(fut, -1.0, np.where(forced, 1e6, 0.0))
    c['c_keep'] = keep.astype(np.float32)
    c['c_addv'] = addv.astype(np.float32)
    j = np.arange(32)[:, None, None]
    kb = np.arange(16)[None, :, None]
    kk = np.arange(128)[None, None, :]
    E = (j == 2 * kb + kk // 64)
    Ef = np.zeros((128, 16 * 128), np.float32)
    Ef[:32] = E.reshape(32, 16 * 128)
    c['c_E'] = _bf(Ef)
    Z = np.zeros((128, 48 * 64), np.float32)
    for p_ in range(48):
        Z[p_, 64 * p_:64 * p_ + 64] = 1.0
    c['c_Z'] = _bf(Z)
    half = 32
    inv_freq = (10000.0 ** (-np.arange(half, dtype=np.float32) / half)).astype(np.float32)
    pp = np.arange(128)
    rot = np.zeros((128, 4), np.float32)
    rot[:, 0] = inv_freq[(pp % 64) % 32]
    rot[:, 1] = PI / 2
    rot[:, 2] = np.where((pp % 64) < 32, PI, 0.0)
    c['c_rot'] = rot
    return c


CONST_SHAPES = None


class Slots:
    def __init__(self, P, name, aps, dsems):
        self.items = [(ap, f"{name}{i}", dsems[i]) for i, ap in enumerate(aps)]
        self.i = 0

    def get(self):
        it = self.items[self.i % len(self.items)]
        self.i += 1
        return it


class Builder:
    def __init__(self, n_seq=2, stages=('l0', 'l1'), dbg=None):
        self.n_seq = n_seq
        self.stages = stages
        self.dbg = dbg
        nc = bass.Bass("TRN2", target_bir_lowering=False)
        self.nc = nc
        self.stack = ExitStack()
        self.P = Prog(nc, self.stack)
        self.rr = {}
        self.fifo = []
        self.tick = 0
        self._inputs()
        self._alloc()

    def din(self, name, shape, dtype=F32):
        return self.nc.dram_tensor(name, list(shape), dtype, kind="ExternalInput").ap()

    def sb(self, name, shape, dtype):
        return self.stack.enter_context(self.nc.sbuf_tensor(name, list(shape), dtype))

    def _inputs(self):
        n = self.n_seq
        self.x = self.din('x', [n, S, D])
        self.p = self.din('p', [2, n, S, 256])
        self.pos = self.din('positions', [n, S], I32)
        self.mix_norm = self.din('mix_norm', [2, D])
        self.ffn_norm = self.din('ffn_norm', [2, D])
        self.ple_norm = self.din('ple_norm', [2, D])
        self.final_norm = self.din('final_norm', [1, D])
        self.ple_gate_w = self.din('ple_gate_w', [2, D, D])
        self.ple_proj_w = self.din('ple_proj_w', [2, 256, D])
        self.fd_w_in = self.din('fd_w_in', [D, 3080])
        self.fd_w_sw = self.din('fd_w_sw', [D, 1024])
        self.fd_forget_b = self.din('fd_forget_b', [1, 8])
        self.fd_w_out = self.din('fd_w_out', [D, D])
        self.dense_w_gate = self.din('dense_w_gate', [D, DFF])
        self.dense_w_up = self.din('dense_w_up', [D, DFF])
        self.dense_w_down = self.din('dense_w_down', [DFF, D])
        self.nsa_w_in = self.din('nsa_w_in', [D, 2608])
        self.nsa_w_sw = self.din('nsa_w_sw', [D, 1792])
        self.nsa_pos_k = self.din('nsa_pos_k', [32, 64])
        self.nsa_w1_k = self.din('nsa_w1_k', [2048, 256])
        self.nsa_b1_k = self.din('nsa_b1_k', [256, 1])
        self.nsa_w2_k = self.din('nsa_w2_k', [256, 64])
        self.nsa_pos_v = self.din('nsa_pos_v', [32, 64])
        self.nsa_w1_v = self.din('nsa_w1_v', [2048, 256])
        self.nsa_b1_v = self.din('nsa_b1_v', [256, 1])
        self.nsa_w2_v = self.din('nsa_w2_v', [256, 64])
        self.nsa_w_out = self.din('nsa_w_out', [D, D])
        self.moe_w_router = self.din('moe_w_router', [D, 8])
        self.moe_b_router = self.din('moe_b_router', [1, 8])
        self.moe_w_gate = self.din('moe_w_gate', [8, D, DFF])
        self.moe_w_up = self.din('moe_w_up', [8, D, DFF])
        self.moe_w_down = self.din('moe_w_down', [8, DFF, D])
        self.cst = {}
        for k, v in make_consts().items():
            d = BF16 if v.dtype == ml_dtypes.bfloat16 else F32
            self.cst[k] = self.din(k, v.shape, d)
        self.y = self.nc.dram_tensor('y', [n, S, D], F32, kind="ExternalOutput").ap()

    def _alloc(self):
        P = self.P
        self.h = self.sb('h', [128, NB, D], F32)
        self.hnT = self.sb('hnT', [128, 8, S], BF16)
        self.cosT = self.sb('cosT', [128, S], BF16)
        self.sinT = self.sb('sinT', [128, S], BF16)
        self.c = {}
        for k, ap in self.cst.items():
            if k == 'c_E':
                continue
            self.c[k] = self.sb('s_' + k, list(ap.shape), ap.dtype)
        self.ps = [self.stack.enter_context(self.nc.psum_tensor(f"ps{i}", [128, 512], F32)) for i in range(8)]
        RN = 43520
        self.Rt = self.sb('R', [128, RN], BF16)
        self.R = Region(self.Rt, RN)
        self.ds_c = P.dsem('consts')
        self.ds_h = P.dsem('hload')
        self.ds_g = P.dsem('gtile')
        self.ds_y = P.dsem('yout')
        self.ds_misc = [P.dsem(f'misc{i}') for i in range(4)]
        self.ds_w = [P.dsem(f'w{i}') for i in range(6)]
        self.ds_wo = [P.dsem(f'wo{i}') for i in range(2)]
        self.ds_ffn = [P.dsem(f'ffn{i}') for i in range(6)]

    def defer(self, fn, delay):
        self.fifo.append((self.tick + delay, fn))

    def step(self):
        self.tick += 1
        while self.fifo and self.fifo[0][0] <= self.tick:
            self.fifo.pop(0)[1]()

    def flush(self):
        while self.fifo:
            self.fifo.pop(0)[1]()

    def rot(self, name, n):
        i = self.rr.get(name, 0)
        self.rr[name] = i + 1
        return i % n

    def load_consts(self):
        P = self.P
        P.group(self.ds_c)
        for k, ap in self.cst.items():
            if k == 'c_E':
                continue
            P.add('sp', lambda e, k=k, ap=ap: e.dma_start(out=self.c[k][:], in_=ap), w=[k], dsem=self.ds_c)
        P.endgroup(self.ds_c)

    def load_w(self, dst, dkey, dsem, src, cast=True):
        srcv = src.rearrange('(k p) c -> p k c', p=128)
        eng = 'pool' if cast else 'sp'
        return self.P.add(eng, lambda e: e.dma_start(out=dst, in_=srcv), w=[dkey], dsem=dsem)

    def norm(self, gain_row, final_out=None):
        P, R, h = self.P, self.R, self.h
        mark = R.off
        self.gtile = R.alloc([D], F32)
        ss = R.alloc([NB], F32)
        junk = R.alloc([D], BF16)
        hs = [R.alloc([D], F32) for _ in range(2)]
        P.add('sp', lambda e: e.dma_start(out=self.gtile, in_=gain_row[0, :].partition_broadcast(128)),
              w=['gtile'], dsem=self.ds_g)
        for b in range(NB):
            P.add('act', lambda e, b=b: e.activation(out=junk, in_=h[:, b, :], func=AF.Square,
                                                     accum_out=ss[:, b:b + 1]),
                  r=self.hk(b), w=['junk', ('ss', b)])
        allss = [('ss', b) for b in range(NB)]
        P.add('dve', lambda e: e.tensor_scalar(out=ss, in0=ss, scalar1=1.0 / D, scalar2=1e-6,
                                               op0=ALU.mult, op1=ALU.add), r=allss, w=allss)
        P.add('act', lambda e: e.activation(out=ss, in_=ss, func=AF.Sqrt), r=allss, w=allss)
        P.add('dve', lambda e: e.reciprocal(out=ss, in_=ss), r=allss, w=allss)
        ident = self.c['c_ident']
        for b in range(NB):
            i = b % 2
            P.add('dve', lambda e, b=b, i=i: e.scalar_tensor_tensor(
                out=hs[i], in0=h[:, b, :], scalar=ss[:, b:b + 1], in1=self.gtile,
                op0=ALU.mult, op1=ALU.mult), r=self.hk(b) + [('ss', b), 'gtile'], w=[('hs', i)])
            if final_out is not None:
                s = final_out
                P.add('sp', lambda e, b=b, i=i, s=s: e.dma_start(out=self.y[s, b * 128:(b + 1) * 128, :], in_=hs[i]),
                      r=[('hs', i)], w=[('y', s, b)], dsem=self.ds_y)
                continue
            for half in range(2):
                pi = 6 + half
                ps = self.ps[pi]
                for j in range(4):
                    cidx = half * 4 + j
                    P.add('pe', lambda e, ps=ps, j=j, cidx=cidx, i=i: e.transpose(
                        out=ps[:, j * 128:(j + 1) * 128], in_=hs[i][:, cidx * 128:(cidx + 1) * 128],
                        identity=ident[:]), r=[('hs', i), 'c_ident'], w=[('ps', pi)])
                P.add('act', lambda e, ps=ps, half=half, b=b: e.activation(
                    out=self.hnT[:, half * 4:half * 4 + 4, b * 128:(b + 1) * 128],
                    in_=ps[:].rearrange('p (a t) -> p a t', t=128), func=AF.Copy),
                    r=[('ps', pi)], w=[('hnT', b)])
        R.off = mark

    def hk(self, b):
        return [('h', b, 0), ('h', b, 1)]

    def hn_keys(self, tc=None):
        if tc is None:
            return [('hnT', b) for b in range(NB)]
        return [('hnT', 4 * tc + j) for j in range(4)]

    def load_seq(self, s):
        P = self.P
        P.group(self.ds_h)
        for b in range(NB):
            P.add('sp', lambda e, b=b: e.dma_start(out=self.h[:, b, :], in_=self.x[s, b * 128:(b + 1) * 128, :]),
                  w=self.hk(b), dsem=self.ds_h)
        P.endgroup(self.ds_h)

    def rotary_tables(self, s):
        P, R = self.P, self.R
        mark = R.off
        posi = R.alloc([S], I32)
        posf = R.alloc([S], F32)
        t = R.alloc([S], F32)
        kf = R.alloc([S], F32)
        ki = R.alloc([S], I32)
        rot = self.c['c_rot']
        P.add('sp', lambda e: e.dma_start(out=posi, in_=self.pos[s, :].partition_broadcast(128)),
              w=['posi'], dsem=self.ds_misc[0])
        P.add('dve', lambda e: e.tensor_copy(out=posf, in_=posi), r=['posi'], w=['posf'])
        for (dst, dkey, ph) in ((self.cosT, 'cosT', 1), (self.sinT, 'sinT', 2)):
            P.add('dve', lambda e, ph=ph: e.tensor_scalar(out=t, in0=posf, scalar1=rot[:, 0:1], scalar2=rot[:, ph:ph + 1],
                                                          op0=ALU.mult, op1=ALU.add), r=['posf', 'c_rot'], w=['rt'])
            P.add('dve', lambda e: e.tensor_scalar(out=kf, in0=t, scalar1=1.0 / TWO_PI, scalar2=None, op0=ALU.mult),
                  r=['rt'], w=['rk'])
            P.add('dve', lambda e: e.tensor_copy(out=ki, in_=kf), r=['rk'], w=['rki'])
            P.add('dve', lambda e: e.tensor_copy(out=kf, in_=ki), r=['rki'], w=['rk'])
            P.add('dve', lambda e: e.scalar_tensor_tensor(out=t, in0=kf, scalar=-TWO_PI, in1=t,
                                                          op0=ALU.mult, op1=ALU.add), r=['rk', 'rt'], w=['rt'])
            P.add('dve', lambda e: e.tensor_scalar(out=kf, in0=t, scalar1=PI, scalar2=-TWO_PI,
                                                   op0=ALU.is_gt, op1=ALU.mult), r=['rt'], w=['rk'])
            P.add('dve', lambda e: e.tensor_tensor(out=t, in0=t, in1=kf, op=ALU.add), r=['rt', 'rk'], w=['rt'])
            P.add('dve', lambda e: e.tensor_scalar(out=t, in0=t, scalar1=-PI, scalar2=PI,
                                                   op0=ALU.max, op1=ALU.min), r=['rt'], w=['rt'])
            P.add('act', lambda e, dst=dst: e.activation(out=dst[:], in_=t, func=AF.Sin), r=['rt'], w=[dkey])
        R.off = mark

    def phase(self):
        self.flush()
        self.P.barrier()
        self.R.reset()

    def attn_common(self):
        R = self.R
        self.Pt = [R.alloc([512], BF16) for _ in range(NPT)]
        self.U = [R.alloc([512], F32, parts=65) for _ in range(2)]
        self.rd16 = [R.alloc([512], BF16) for _ in range(2)]
        for i_ in range(2):
            self.P.add('pool', lambda e, i_=i_: e.memset(self.rd16[i_][:, :], 0.0), w=[('rd16', i_)])
        self.OT = [R.alloc([S], BF16, parts=64) for _ in range(2)]
        self.Wo = [R.alloc([D], BF16, parts=64) for _ in range(2)]
        self.wsl = Slots(self.P, 'wsl', [R.alloc([8, 128], BF16) for _ in range(4)], self.ds_w)

    def proj_fm(self, wt, wkey, ncols, evac, wcols=None):
        P = self.P
        for tc in range(NCH):
            pi = 6 + self.rot('PJ', 2)
            ps = self.ps[pi]
            for kc in range(8):
                lhsT = wt[:, kc, 0:ncols] if wcols is None else wt[:, kc, wcols[0]:wcols[1]]
                P.add('pe', lambda e, ps=ps, kc=kc, tc=tc, lhsT=lhsT: e.matmul(
                    ps[0:ncols, :], lhsT=lhsT, rhs=self.hnT[:, kc, tc * 512:(tc + 1) * 512],
                    start=(kc == 0), stop=(kc == 7)), r=[wkey] + self.hn_keys(tc), w=[('ps', pi)])
            evac(tc, ps, ('ps', pi))

    def proj_qk_plain(self, src_cols, dst, dkey, split=None):
        P = self.P
        wt, wk, wds = self.wsl.get()
        self.load_w(wt, wk, wds, src_cols)

        def evac(tc, ps, pk):
            sl = slice(tc * 512, (tc + 1) * 512)
            if split is None:
                P.add('act', lambda e: e.activation(out=dst[:, sl], in_=ps[:, :], func=AF.Copy), r=[pk], w=[(dkey, tc)])
            else:
                for hh in range(2):
                    rows = slice(64 * hh, 64 * hh + 64)
                    P.add('act', lambda e, hh=hh, rows=rows: e.activation(out=split[hh][rows, sl], in_=ps[rows, :], func=AF.Copy),
                          r=[pk], w=[(('QZ', hh), tc)])
        self.proj_fm(wt, wk, 128, evac)

    def proj_qk_rot(self, src_cols, src_sw_cols, dst, dkey, dup=False, split=None, ncols=128):
        P, R = self.P, self.R
        wt, wk, wds = self.wsl.get()
        wt2, wk2, wds2 = self.wsl.get()
        if dup:
            P.group(wds)
            self.load_w(wt[:, :, 0:64], wk, wds, src_cols)
            self.load_w(wt[:, :, 64:128], wk + 'b', wds, src_cols)
            P.endgroup(wds)
            P.group(wds2)
            self.load_w(wt2[:, :, 0:64], wk2, wds2, src_sw_cols)
            self.load_w(wt2[:, :, 64:128], wk2 + 'b', wds2, src_sw_cols)
            P.endgroup(wds2)
            wkeys, wkeys2 = [wk, wk + 'b'], [wk2, wk2 + 'b']
        else:
            self.load_w(wt[:, :, 0:ncols], wk, wds, src_cols)
            self.load_w(wt2[:, :, 0:ncols], wk2, wds2, src_sw_cols)
            wkeys, wkeys2 = [wk], [wk2]
        for tc in range(NCH):
            sl = slice(tc * 512, (tc + 1) * 512)
            pa, pb = 6, 7
            for (pi, w_, wkk) in ((pa, wt, wkeys), (pb, wt2, wkeys2)):
                ps = self.ps[pi]
                for kc in range(8):
                    P.add('pe', lambda e, ps=ps, kc=kc, w_=w_, sl=sl: e.matmul(
                        ps[0:ncols, :], lhsT=w_[:, kc, 0:ncols], rhs=self.hnT[:, kc, sl], start=(kc == 0), stop=(kc == 7)),
                        r=wkk + self.hn_keys(tc), w=[('ps', pi)])
            ti = self.rot('rt', 2)
            t1 = self.rtmp[ti]
            P.add('dve', lambda e, t1=t1, sl=sl: e.tensor_tensor(out=t1[0:ncols, :], in0=self.ps[pa][0:ncols, :], in1=self.cosT[0:ncols, sl],
                                                                 op=ALU.mult), r=[('ps', pa), 'cosT'], w=[('rtmp', ti)])
            if split is None:
                P.add('dve', lambda e, sl=sl: e.tensor_tensor(out=dst[0:ncols, sl], in0=self.ps[pb][0:ncols, :], in1=self.sinT[0:ncols, sl],
                                                              op=ALU.mult), r=[('ps', pb), 'sinT'], w=[(dkey, tc)])
                P.add('pool', lambda e, t1=t1, sl=sl: e.tensor_tensor(out=dst[0:ncols, sl], in0=dst[0:ncols, sl], in1=t1[0:ncols, :], op=ALU.add),
                      r=[('rtmp', ti), (dkey, tc)], w=[(dkey, tc)])
            else:
                t2 = self.rtmp2[ti]
                P.add('dve', lambda e, t2=t2, sl=sl: e.tensor_tensor(out=t2, in0=self.ps[pb][:, :], in1=self.sinT[:, sl],
                                                                     op=ALU.mult), r=[('ps', pb), 'sinT'], w=[('rtmp2', ti)])
                for hh in range(2):
                    rows = slice(64 * hh, 64 * hh + 64)
                    P.add('pool', lambda e, t1=t1, t2=t2, sl=sl, hh=hh, rows=rows: e.tensor_tensor(
                        out=split[hh][rows, sl], in0=t1[rows, :], in1=t2[rows, :], op=ALU.add),
                        r=[('rtmp', ti), ('rtmp2', ti)], w=[(('QZ', hh), tc)])

    def proj_v(self, src_cols, ncols, dst_fn, dkey, tok_fn=None, nblk=NB):
        P = self.P
        wt, wk, wds = self.wsl.get()
        self.load_w(wt[:, :, 0:ncols], wk, wds, src_cols)
        for g in range(nblk // 4):
            pi = 6 + self.rot('PJ', 2)
            ps = self.ps[pi]
            for bb in range(4):
                blk = 4 * g + bb
                tsl = slice(blk * 128, (blk + 1) * 128) if tok_fn is None else tok_fn(blk)
                for kc in range(8):
                    P.add('pe', lambda e, ps=ps, bb=bb, kc=kc, tsl=tsl: e.matmul(
                        ps[:, bb * ncols:(bb + 1) * ncols], lhsT=self.hnT[:, kc, tsl], rhs=wt[:, kc, 0:ncols],
                        start=(kc == 0), stop=(kc == 7)), r=[wk] + self.hn_keys(), w=[('ps', pi)])
            P.add('act', lambda e, ps=ps, g=g: e.activation(
                out=dst_fn(g), in_=ps[:, 0:4 * ncols].rearrange('p (b h d) -> p b h d', b=4, d=64), func=AF.Copy),
                r=[('ps', pi)], w=[(dkey, g)])

    def finalize(self, acc, acck, dst, dkey, gate=None, first=True, last=True, extra=None):
        P = self.P
        ui = self.rot('U', 2)
        U = self.U[ui]
        bc = self.ps[5]
        ones = self.c['c_ones']

        rd = self.rd16[ui]
        os16 = self.c['c_os']

        def stage_a():
            if extra is None:
                P.add('act', lambda e: e.activation(out=U[0:64, :], in_=acc[0:64, :], func=AF.Copy), r=[acck], w=[('U', ui)])
                P.add('act', lambda e: e.activation(out=rd[64:65, :], in_=acc[64:65, :], func=AF.Copy, bias=1e-30),
                      r=[acck], w=[('rd16', ui)])
            else:
                extra(U, ('U', ui))
                P.add('dve', lambda e: e.tensor_scalar(out=rd[64:65, :], in0=U[64:65, :], scalar1=1e-30, scalar2=None,
                                                       op0=ALU.add), r=[('U', ui)], w=[('rd16', ui)])

        def stage_b():
            P.add('pe', lambda e: e.matmul(bc[0:64, :], lhsT=os16[:, 0:64], rhs=rd[:, :], start=True, stop=True),
                  r=[('rd16', ui), 'c_os'], w=[('ps', 5)])
            P.add('act', lambda e: e.activation(out=bc[0:64, :], in_=bc[0:64, :], func=AF.Ln), r=[('ps', 5)], w=[('ps', 5)])
            P.add('act', lambda e: e.activation(out=bc[0:64, :], in_=bc[0:64, :], func=AF.Exp, scale=-1.0),
                  r=[('ps', 5)], w=[('ps', 5)])
            if gate is None:
                P.add('dve', lambda e: e.tensor_tensor(out=dst, in0=U[0:64, :], in1=bc[0:64, :], op=ALU.mult),
                      r=[('U', ui), ('ps', 5)], w=[dkey])
            else:
                P.add('dve', lambda e: e.tensor_tensor(out=U[0:64, :], in0=U[0:64, :], in1=bc[0:64, :], op=ALU.mult),
                      r=[('U', ui), ('ps', 5)], w=[('U', ui)])

        def stage_c():
            hc, sl = gate
            Z = self.c['c_Z']
            P.add('pe', lambda e: e.matmul(bc[0:64, :], lhsT=Z[:, hc * 64:hc * 64 + 64], rhs=self.sigT[:, sl],
                                           start=True, stop=True), r=['sigT', 'c_Z', ('U', ui)], w=[('ps', 5)])
            if first:
                P.add('dve', lambda e: e.tensor_tensor(out=self.gacc[0:64, :], in0=U[0:64, :], in1=bc[0:64, :], op=ALU.mult),
                      r=[('U', ui), ('ps', 5)], w=['gacc'])
            else:
                P.add('dve', lambda e: e.tensor_tensor(out=U[0:64, :], in0=U[0:64, :], in1=bc[0:64, :], op=ALU.mult),
                      r=[('U', ui), ('ps', 5)], w=[('U', ui)])
                if last:
                    P.add('dve', lambda e: e.tensor_tensor(out=dst, in0=U[0:64, :], in1=self.gacc[0:64, :], op=ALU.add),
                          r=[('U', ui), 'gacc'], w=[dkey])
                else:
                    P.add('dve', lambda e: e.tensor_tensor(out=self.gacc[0:64, :], in0=U[0:64, :], in1=self.gacc[0:64, :],
                                                           op=ALU.add), r=[('U', ui), 'gacc'], w=['gacc'])
        self.defer(stage_a, PD)
        self.defer(stage_b, PD + 1)
        if gate is not None:
            self.defer(stage_c, PD + 2)

    def outproj_pair(self, w_out, pair, ot_keys):
        P = self.P
        self.flush()
        wkeys = []
        for hh in range(2):
            r0 = (2 * pair + hh) * 64
            src = w_out[r0:r0 + 64, :]
            P.add('pool', lambda e, hh=hh, src=src: e.dma_start(out=self.Wo[hh][:, :], in_=src), w=[('Wo', hh)],
                  dsem=self.ds_wo[hh])
            wkeys.append(('Wo', hh))
        for b in range(NB):
            for half in range(2):
                pi = 6 + self.rot('PJ', 2)
                ps = self.ps[pi]
                for hh in range(2):
                    P.add('pe', lambda e, ps=ps, hh=hh, b=b, half=half: e.matmul(
                        ps[:, :], lhsT=self.OT[hh][0:64, b * 128:(b + 1) * 128],
                        rhs=self.Wo[hh][0:64, half * 512:(half + 1) * 512], start=(hh == 0), stop=(hh == 1)),
                        r=[('Wo', hh)] + ot_keys[hh], w=[('ps', pi)])
                hsl = self.h[:, b, half * 512:(half + 1) * 512]
                P.add('dve', lambda e, ps=ps, hsl=hsl: e.tensor_tensor(out=hsl, in0=ps[:, :], in1=hsl, op=ALU.add),
                      r=[('ps', pi), ('h', b, half)], w=[('h', b, half)])

    def l0_mixer(self, s):
        P, R = self.P, self.R
        self.phase()
        self.attn_common()
        self.rtmp = [R.alloc([512], F32) for _ in range(2)]
        self.rtmp2 = [R.alloc([512], F32) for _ in range(2)]
        mark0 = R.off
        Qz = [R.alloc([S], BF16) for _ in range(2)]
        KT = R.alloc([S], BF16)
        Vt = R.alloc([NB, 2, 65], BF16)
        P.add('pool', lambda e: e.memset(Qz[0][64:128, :], 0.0), w=['qz0'])
        P.add('pool', lambda e: e.memset(Qz[1][0:64, :], 0.0), w=['qz1'])
        w_in = self.fd_w_in
        fb = R.alloc([1, 8], F32)
        lg = R.alloc([NB, 8], F32)
        cl = R.alloc([NB, 8], F32)
        offs = R.alloc([NB, 8], F32)
        tot = R.alloc([NB, 8], F32)
        Bt = R.alloc([8, NB, NB], F32)
        P.add('sp', lambda e: e.dma_start(out=fb[:, 0, :], in_=self.fd_forget_b[0, :].partition_broadcast(128)),
              w=['fb'], dsem=self.ds_misc[1])
        wf, wfk, wfds = self.wsl.get()
        self.load_w(wf[:, :, 0:8], wfk, wfds, w_in[:, 3072:3080])
        psf = self.ps[4]
        for b in range(NB):
            for kc in range(8):
                P.add('pe', lambda e, b=b, kc=kc: e.matmul(psf[:, b * 8:(b + 1) * 8],
                                                           lhsT=self.hnT[:, kc, b * 128:(b + 1) * 128], rhs=wf[:, kc, 0:8],
                                                           start=(kc == 0), stop=(kc == 7)),
                      r=[wfk] + self.hn_keys(), w=[('ps', 4)])
        P.add('dve', lambda e: e.tensor_tensor(out=lg, in0=psf[:, 0:128].rearrange('p (b h) -> p b h', h=8),
                                               in1=fb[:, 0:1, :].broadcast_to([128, NB, 8]), op=ALU.add),
              r=[('ps', 4), 'fb'], w=['lg'])
        P.add('act', lambda e: e.activation(out=lg, in_=lg, func=AF.Exp, scale=-1.0), r=['lg'], w=['lg'])
        P.add('dve', lambda e: e.tensor_scalar(out=lg, in0=lg, scalar1=1.0, scalar2=None, op0=ALU.add), r=['lg'], w=['lg'])
        P.add('act', lambda e: e.activation(out=lg, in_=lg, func=AF.Ln), r=['lg'], w=['lg'])
        lg2 = lg.rearrange('p b h -> p (b h)')
        P.add('pe', lambda e: e.matmul(self.ps[6][:, 0:128], lhsT=self.c['c_utri'][:], rhs=lg2, start=True, stop=True),
              r=['lg', 'c_utri'], w=[('ps', 6)])
        P.add('pe', lambda e: e.matmul(self.ps[7][:, 0:128], lhsT=self.c['c_ones'][:], rhs=lg2, start=True, stop=True),
              r=['lg', 'c_ones'], w=[('ps', 7)])
        P.add('act', lambda e: e.activation(out=tot, in_=self.ps[7][:, 0:128].rearrange('p (b h) -> p b h', h=8),
                                            func=AF.Copy), r=[('ps', 7)], w=['tot'])
        P.add('pool', lambda e: e.memset(offs[:, 0, :], 0.0), w=[('offs', 0)])
        for j in range(1, NB):
            P.add('dve', lambda e, j=j: e.tensor_tensor(out=offs[:, j, :], in0=offs[:, j - 1, :], in1=tot[:, j - 1, :],
                                                        op=ALU.add), r=[('offs', j - 1), 'tot'], w=[('offs', j)])
        allo = [('offs', j) for j in range(NB)]
        P.add('dve', lambda e: e.tensor_tensor(out=cl, in0=self.ps[6][:, 0:128].rearrange('p (b h) -> p b h', h=8),
                                               in1=offs, op=ALU.add), r=[('ps', 6)] + allo, w=['cl'])
        for hh in range(8):
            for qb in range(NB):
                P.add('dve', lambda e, hh=hh, qb=qb: e.tensor_scalar(
                    out=Bt[:, hh, qb, :], in0=cl[:, :, hh], scalar1=offs[:, qb, hh:hh + 1], scalar2=None,
                    op0=ALU.subtract), r=['cl'] + allo, w=[('Bt', hh)])
        wm = self.c['c_wm']
        for hp in range(4):
            c0 = hp * 128
            self.proj_qk_plain(w_in[:, c0:c0 + 128], None, None, split=Qz)
            self.proj_qk_plain(w_in[:, 512 + c0:512 + c0 + 128], KT, 'KT')
            P.add('pool', lambda e: e.memset(Vt[:, :, :, 64:65], 1.0), w=['Vones'])
            self.proj_v(w_in[:, 1024 + c0:1024 + c0 + 128], 128, lambda g: Vt[:, 4 * g:4 * g + 4, :, 0:64], 'Vt')
            qk = [('QT', t) for t in range(NCH)]
            kk = [('KT', t) for t in range(NCH)]
            ot_keys = []
            for hh in range(2):
                head = 2 * hp + hh
                rows = slice(64 * hh, 64 * hh + 64)
                for i in range(NCH):
                    ai = 3 + self.rot('ACC', 2)
                    acc = self.ps[ai]
                    nkb = 4 * i + 4
                    for kb in range(nkb):
                        self.step()
                        si = self.rot('S', 3)
                        ps = self.ps[si]
                        P.add('pe', lambda e, ps=ps, kb=kb, i=i, hh=hh: e.matmul(
                            ps[:, :], lhsT=KT[:, kb * 128:(kb + 1) * 128], rhs=Qz[hh][:, i * 512:(i + 1) * 512],
                            start=True, stop=True), r=[('KT', kb // 4), (('QZ', hh), i), 'qz0', 'qz1'], w=[('ps', si)])
                        pi = self.rot('Pt', NPT)
                        pt = self.Pt[pi]
                        r0 = max(0, kb - 4 * i)
                        pkeys = []
                        for rr in range(r0, 4):
                            qb = 4 * i + rr
                            P.add('act', lambda e, ps=ps, pt=pt, rr=rr, qb=qb, kb=kb, head=head: e.activation(
                                out=pt[:, rr * 128:(rr + 1) * 128], in_=ps[:, rr * 128:(rr + 1) * 128], func=AF.Exp,
                                scale=0.125, bias=Bt[:, head, qb, kb:kb + 1]),
                                r=[('ps', si), ('Bt', head)], w=[('Pt', pi, rr)])
                            pkeys.append(('Pt', pi, rr))
                        c_lo = r0 * 128
                        if kb >= 4 * i:
                            r = kb - 4 * i
                            P.add('dve', lambda e, pt=pt, r=r, c_lo=c_lo: e.tensor_tensor(
                                out=pt[:, c_lo:512], in0=pt[:, c_lo:512], in1=wm[:, 384 - 128 * r + c_lo:384 - 128 * r + 512],
                                op=ALU.mult), r=pkeys + ['c_wm'], w=pkeys)
                        def back(acc=acc, pt=pt, kb=kb, hh=hh, c_lo=c_lo, st=(kb == 0), sp=(kb == nkb - 1), pkeys=pkeys, ai=ai):
                            P.add('pe', lambda e: e.matmul(
                                acc[0:65, c_lo:512], lhsT=Vt[:, kb, hh, :], rhs=pt[:, c_lo:512], start=st, stop=sp),
                                r=pkeys + [('Vt', kb // 4), 'Vones'], w=[('ps', ai)])
                        self.defer(back, PD)
                    self.finalize(acc, ('ps', ai), self.OT[hh][0:64, i * 512:(i + 1) * 512], ('OT', hh, i))
                ot_keys.append([('OT', hh, i) for i in range(NCH)])
            self.outproj_pair(self.fd_w_out, hp, ot_keys)
        self.flush()
        P.barrier()
        R.off = mark0
        Qz = [R.alloc([S], BF16) for _ in range(2)]
        KT = R.alloc([S], BF16)
        P.add('pool', lambda e: e.memset(Qz[0][64:128, :], 0.0), w=['qz0'])
        P.add('pool', lambda e: e.memset(Qz[1][0:64, :], 0.0), w=['qz1'])
        Vd = [R.alloc([NB, 2, 65], BF16) for _ in range(3)]
        self.Vd = Vd
        dm3 = self.c['c_dm'][:, :].rearrange('p (a b) -> p a b', b=128)
        for dp in range(4):
            c0 = dp * 128
            self.proj_qk_rot(w_in[:, 1536 + c0:1536 + c0 + 128], self.fd_w_sw[:, c0:c0 + 128], None, None, split=Qz)
            self.proj_qk_rot(w_in[:, 2048 + c0:2048 + c0 + 128], self.fd_w_sw[:, 512 + c0:512 + c0 + 128], KT, 'KT')
            for pat in range(3):
                P.add('pool', lambda e, pat=pat: e.memset(Vd[pat][:, :, :, 64:65], 1.0), w=[('Vones', pat)])
            vsrc = w_in[:, 2560 + c0:2560 + c0 + 128]
            self.proj_v(vsrc, 128, lambda g: Vd[0][:, 4 * g:4 * g + 4, :, 0:64], ('Vd', 0))
            self.proj_v(vsrc, 128, lambda g: Vd[1][:, 4 * g:4 * g + 4, :, 0:64], ('Vd', 1),
                        tok_fn=lambda blk: ssl((blk // 4) + 512 * (blk % 4), 128, 4))
            self.proj_v(vsrc, 128, lambda g: Vd[2][:, 4 * g:4 * g + 4, :, 0:64], ('Vd', 2),
                        tok_fn=lambda blk: ssl(blk, 128, 16))
            ot_keys = []
            for hh in range(2):
                rows = slice(64 * hh, 64 * hh + 64)
                for i in range(NCH):
                    acc1, acc4, acc16 = self.ps[3], self.ps[4], self.ps[6]
                    for g in range(2):
                        slots = []
                        for qb in (4 * i + 2 * g, 4 * i + 2 * g + 1):
                            hasp = qb >= 1
                            kb = max(qb - 1, 0)
                            qsl = slice(qb * 128, qb * 128 + 128)
                            slots.append((qsl, qsl, (acc1, ('ps', 3), (qb - 4 * i) * 128, 0, qb, True, not hasp)))
                            slots.append((slice(kb * 128, kb * 128 + 128), qsl,
                                          (acc1, ('ps', 3), (qb - 4 * i) * 128, 0, kb, False, True) if hasp else None))
                        self.dil_group(KT, Qz[hh], rows, hh, slots, dm3, ['c_dm'], 128)
                    for g in range(2):
                        slots = []
                        for r_ in (2 * g, 2 * g + 1):
                            ip = max(i - 1, 0)
                            qsl = ssl(r_ + 512 * i, 128, 4)
                            ksl = ssl(r_ + 512 * ip, 128, 4)
                            slots.append((qsl, qsl, (acc4, ('ps', 4), r_ * 128, 1, r_ * 4 + i, True, i == 0)))
                            slots.append((ksl, qsl, (acc4, ('ps', 4), r_ * 128, 1, r_ * 4 + ip, False, True) if i >= 1 else None))
                        self.dil_group(KT, Qz[hh], rows, hh, slots, dm3, ['c_dm'], 128)
                    slots = []
                    for r_ in range(16):
                        slots.append((ssl(r_, 128, 16), ssl(r_ + 512 * i, 32, 16),
                                      (acc16, ('ps', 6), r_ * 32, 2, r_, True, True)))
                    m16 = self.c['c_wm'][:, 384 + 32 * i:416 + 32 * i].unsqueeze(1).broadcast_to([128, 16, 32])
                    self.dil_group(KT, Qz[hh], rows, hh, slots, m16, ['c_wm'], 32)

                    def extra(U, uk):
                        P.add('act', lambda e: e.activation(out=U[0:65, :], in_=self.ps[3][0:65, :], func=AF.Copy),
                              r=[('ps', 3)], w=[uk])
                        P.add('dve', lambda e: e.tensor_tensor(
                            out=U[0:65, :].rearrange('p (q r) -> p r q', r=4),
                            in0=self.ps[4][0:65, :].rearrange('p (r q) -> p r q', r=4),
                            in1=U[0:65, :].rearrange('p (q r) -> p r q', r=4), op=ALU.add), r=[('ps', 4), uk], w=[uk])
                        P.add('dve', lambda e: e.tensor_tensor(
                            out=U[0:65, :].rearrange('p (q r) -> p r q', r=16),
                            in0=self.ps[6][0:65, :].rearrange('p (r q) -> p r q', r=16),
                            in1=U[0:65, :].rearrange('p (q r) -> p r q', r=16), op=ALU.add), r=[('ps', 6), uk], w=[uk])
                    self.finalize(None, None, self.OT[hh][0:64, i * 512:(i + 1) * 512], ('OT', hh, i), extra=extra)
                ot_keys.append([('OT', hh, i) for i in range(NCH)])
            self.outproj_pair(self.fd_w_out, 4 + dp, ot_keys)

    def dil_group(self, KT, QT, rows, hh, slots, mask_ap, mkeys, width):
        P = self.P
        Vd = self.Vd
        allq = [(('QZ', hh), t) for t in range(NCH)] + ['qz0', 'qz1']
        allk = [('KT', t) for t in range(NCH)]
        self.step()
        si = self.rot('Sd', 3)
        ps = self.ps[si]
        for j, (ksl, qsl, pv) in enumerate(slots):
            P.add('pe', lambda e, j=j, ksl=ksl, qsl=qsl: e.matmul(
                ps[:, j * width:(j + 1) * width], lhsT=KT[:, ksl], rhs=QT[:, qsl],
                start=True, stop=True), r=allk + allq, w=[('ps', si)])
        pi = self.rot('Pt', NPT)
        pt = self.Pt[pi]
        P.add('act', lambda e: e.activation(out=pt[:, :], in_=ps[:, :], func=AF.Exp, scale=0.125),
              r=[('ps', si)], w=[('Pt', pi, 0)])
        ptv = pt[:, :].rearrange('p (a b) -> p a b', b=width)
        P.add('dve', lambda e: e.tensor_tensor(out=ptv, in0=ptv, in1=mask_ap, op=ALU.mult),
              r=[('Pt', pi, 0)] + mkeys, w=[('Pt', pi, 0)])
        def back():
            for j, (ksl, qsl, pv) in enumerate(slots):
                if pv is None:
                    continue
                acc, ak, col0, vpat, vblk, st, sp = pv
                vkeys = [(('Vd', vpat), g) for g in range(4)] + [('Vones', vpat)]
                P.add('pe', lambda e, j=j, acc=acc, col0=col0, vpat=vpat, vblk=vblk, st=st, sp=sp: e.matmul(
                    acc[0:65, col0:col0 + width], lhsT=Vd[vpat][:, vblk, hh, :], rhs=pt[:, j * width:(j + 1) * width],
                    start=st, stop=sp), r=[('Pt', pi, 0)] + vkeys, w=[ak])
        self.defer(back, PD)

    def dump_h(self, s):
        P = self.P
        for b in range(NB):
            o = P.add('sp', lambda e, b=b: e.dma_start(out=self.y[s, b * 128:(b + 1) * 128, :], in_=self.h[:, b, :]),
                      r=self.hk(b), w=[('y', s, b)], dsem=self.ds_y)
        self.P.final.append(o)

    def build(self):
        P = self.P
        st = self.stages
        self.load_consts()
        for s in range(self.n_seq):
            self.phase()
            self.load_seq(s)
            self.rotary_tables(s)
            if 'l0' in st:
                self.phase()
                self.norm(self.mix_norm[0:1, :])
                self.l0_mixer(s)
                if 'l0ffn' in st:
                    self.phase()
                    self.norm(self.ffn_norm[0:1, :])
                    self.phase()
                    self.ffn_setup()
                    if 'noffn' not in st:
                        self.ffn(self.dense_w_gate, self.dense_w_up, self.dense_w_down, None)
                    if 'nople' not in st:
                        self.ple(s, 0)
            if 'l1' in st:
                self.phase()
                self.norm(self.mix_norm[1:2, :])
                self.nsa_mixer(s)
                if 'l1ffn' in st:
                    self.moe()
                    self.ple(s, 1)
            if 'final' in st:
                self.phase()
                self.norm(self.final_norm[0:1, :], final_out=s)
            else:
                self.dump_h(s)
        if 'final' in st:
            last = [o for o in P.by_eng['sp'] if o.dsem is self.ds_y][-1]
            P.final.append(last)
        P.emit()
        return self.nc


SWAP64 = np.concatenate([np.arange(32, 64), np.arange(0, 32)])


def _swap_cols(w):
    d, n = w.shape
    idx = (np.arange(n) // 64) * 64 + SWAP64[np.arange(n) % 64]
    return np.ascontiguousarray(w[:, idx])


def host_inputs(inputs, n_cores, n_seq, consts):
    f = lambda a: np.ascontiguousarray(np.asarray(a, dtype=np.float32))
    shared = {}
    for k in ('mix_norm', 'ffn_norm', 'ple_norm', 'ple_gate_w', 'ple_proj_w'):
        shared[k] = f(inputs[k])
    shared['final_norm'] = f(inputs['final_norm']).reshape(1, D)
    for k in ('fd_w_in', 'fd_forget_b', 'fd_w_out', 'dense_w_gate', 'dense_w_up', 'dense_w_down', 'nsa_w_in',
              'nsa_pos_k', 'nsa_w1_k', 'nsa_w2_k', 'nsa_pos_v', 'nsa_w1_v', 'nsa_w2_v', 'nsa_w_out',
              'moe_w_router', 'moe_b_router', 'moe_w_gate', 'moe_w_up', 'moe_w_down'):
        shared[k] = f(inputs[k])[0]
    shared['nsa_b1_k'] = f(inputs['nsa_b1_k'])[0].reshape(256, 1)
    shared['nsa_b1_v'] = f(inputs['nsa_b1_v'])[0].reshape(256, 1)
    shared['fd_w_sw'] = _swap_cols(shared['fd_w_in'][:, 1536:2560])
    nw = shared['nsa_w_in']
    shared['nsa_w_sw'] = _swap_cols(np.concatenate([nw[:, 0:1280], nw[:, 1536:1792], nw[:, 2048:2304]], axis=1))
    shared.update(consts)
    x = f(inputs['x'])
    p = f(inputs['p'])
    pos = np.ascontiguousarray(np.asarray(inputs['positions'], dtype=np.int32))
    maps = []
    for c in range(n_cores):
        m = dict(shared)
        m['x'] = np.ascontiguousarray(x[c * n_seq:(c + 1) * n_seq])
        m['p'] = np.ascontiguousarray(p[:, c * n_seq:(c + 1) * n_seq])
        m['positions'] = np.ascontiguousarray(pos[c * n_seq:(c + 1) * n_seq])
        maps.append(m)
    return maps


_CACHE = {}


def kernel(**inputs):
    n_cores, n_seq = 8, 2
    if 'nc' not in _CACHE:
        b = Builder(n_seq=n_seq, stages=('l0', 'l0ffn', 'l1', 'l1ffn', 'final'))
        _CACHE['nc'] = b.build()
        _CACHE['b'] = b
    nc = _CACHE['nc']
    maps = host_inputs(inputs, n_cores, n_seq, make_consts())
    res = run_bass_kernel_spmd(nc, maps, core_ids=list(range(n_cores)))
    out = np.concatenate([np.asarray(r['y'], dtype=np.float32) for r in res.results], axis=0)
    return out


def _ffn_setup(self):
    R = self.R
    self.Wg = [R.alloc([8, 512], BF16) for _ in range(2)]
    self.Wu = [R.alloc([8, 512], BF16) for _ in range(2)]
    self.Wd = [R.alloc([4, D], BF16) for _ in range(2)]
    self.At = [R.alloc([4, 512], BF16) for _ in range(2)]
    self.Sg = [R.alloc([512], F32) for _ in range(2)]


def _ffn(self, wg, wu, wd, comb=None):
    P, R = self.P, self.R
    for fg in range(7):
        wi = self.rot('ffnw', 2)
        Wg, Wu, Wd = self.Wg[wi], self.Wu[wi], self.Wd[wi]
        fsl = slice(fg * 512, (fg + 1) * 512)
        self.load_w(Wg, ('Wg', wi), self.ds_ffn[wi], wg[:, fsl])
        self.load_w(Wu, ('Wu', wi), self.ds_ffn[2 + wi], wu[:, fsl])
        self.load_w(Wd, ('Wd', wi), self.ds_ffn[4 + wi], wd[fsl, :])
        for tc in range(NCH):
            ai = self.rot('At', 2)
            At = self.At[ai]
            for fc in range(4):
                gi = self.rot('Gp', 2)
                ui = 2 + self.rot('Up', 2)
                for (pi, W_, wk) in ((gi, Wg, ('Wg', wi)), (ui, Wu, ('Wu', wi))):
                    ps = self.ps[pi]
                    for kc in range(8):
                        P.add('pe', lambda e, ps=ps, W_=W_, kc=kc, fc=fc, tc=tc: e.matmul(
                            ps[:, :], lhsT=W_[:, kc, fc * 128:(fc + 1) * 128], rhs=self.hnT[:, kc, tc * 512:(tc + 1) * 512],
                            start=(kc == 0), stop=(kc == 7)), r=[wk] + self.hn_keys(tc), w=[('ps', pi)])
                si = self.rot('Sg', 2)
                Sg = self.Sg[si]
                P.add('act', lambda e, Sg=Sg, gi=gi: e.activation(out=Sg, in_=self.ps[gi][:, :], func=AF.Silu),
                      r=[('ps', gi)], w=[('Sg', si)])
                P.add('dve', lambda e, Sg=Sg, ui=ui, At=At, fc=fc: e.tensor_tensor(
                    out=At[:, fc, :], in0=self.ps[ui][:, :], in1=Sg, op=ALU.mult),
                    r=[('ps', ui), ('Sg', si)], w=[('At', ai, fc)])
            akeys = [('At', ai, fc) for fc in range(4)]
            for bb in range(4):
                b = 4 * tc + bb
                for half in range(2):
                    yi = 4 + self.rot('Yp', 3)
                    ps = self.ps[yi]
                    for fc in range(4):
                        P.add('pe', lambda e, ps=ps, At=At, Wd=Wd, fc=fc, bb=bb, half=half: e.matmul(
                            ps[:, :], lhsT=At[:, fc, bb * 128:(bb + 1) * 128], rhs=Wd[:, fc, half * 512:(half + 1) * 512],
                            start=(fc == 0), stop=(fc == 3)), r=akeys + [('Wd', wi)], w=[('ps', yi)])
                    hsl = self.h[:, b, half * 512:(half + 1) * 512]
                    if comb is None:
                        P.add('dve', lambda e, ps=ps, hsl=hsl: e.tensor_tensor(out=hsl, in0=ps[:, :], in1=hsl, op=ALU.add),
                              r=[('ps', yi), ('h', b, half)], w=[('h', b, half)])
                    else:
                        P.add('dve', lambda e, ps=ps, hsl=hsl, b=b: e.scalar_tensor_tensor(
                            out=hsl, in0=ps[:, :], scalar=comb[:, b:b + 1], in1=hsl, op0=ALU.mult, op1=ALU.add),
                            r=[('ps', yi), ('h', b, half), 'comb'], w=[('h', b, half)])


def _ple(self, s, layer):
    P, R = self.P, self.R
    self.phase()
    self.norm(self.ple_norm[layer:layer + 1, :])
    self.phase()
    Wpg = R.alloc([8, D], BF16)
    Wpp = R.alloc([2, D], BF16)
    p32 = R.alloc([NB, 256], F32)
    pT = R.alloc([2, S], BF16)
    sg = [R.alloc([512], F32) for _ in range(2)]
    self.load_w(Wpg, 'Wpg', self.ds_ffn[0], self.ple_gate_w[layer])
    self.load_w(Wpp, 'Wpp', self.ds_ffn[1], self.ple_proj_w[layer])
    P.add('sp', lambda e: e.dma_start(out=p32, in_=self.p[layer, s].rearrange('(b p) c -> p b c', p=128)),
          w=['p32'], dsem=self.ds_misc[2])
    ident = self.c['c_ident']
    for g in range(NB // 2):
        pi = 6 + self.rot('PJ', 2)
        ps = self.ps[pi]
        for bb in range(2):
            b = 2 * g + bb
            for c2 in range(2):
                j = bb * 2 + c2
                P.add('pe', lambda e, ps=ps, j=j, b=b, c2=c2: e.transpose(
                    out=ps[:, j * 128:(j + 1) * 128], in_=p32[:, b, c2 * 128:(c2 + 1) * 128], identity=ident[:]),
                    r=['p32', 'c_ident'], w=[('ps', pi)])
        P.add('act', lambda e, ps=ps, g=g: e.activation(
            out=pT[:, :, g * 256:(g + 1) * 256].rearrange('p c (b t) -> p b c t', b=2),
            in_=ps[:, :].rearrange('p (b c t) -> p b c t', b=2, c=2), func=AF.Copy), r=[('ps', pi)], w=[('pT', g)])
    for b in range(NB):
        for half in range(2):
            gi = self.rot('Gp', 2)
            ui = 2 + self.rot('Up', 2)
            csl = slice(half * 512, (half + 1) * 512)
            for kc in range(8):
                P.add('pe', lambda e, gi=gi, kc=kc, b=b, csl=csl: e.matmul(
                    self.ps[gi][:, :], lhsT=self.hnT[:, kc, b * 128:(b + 1) * 128], rhs=Wpg[:, kc, csl],
                    start=(kc == 0), stop=(kc == 7)), r=['Wpg', ('hnT', b)], w=[('ps', gi)])
            for c2 in range(2):
                P.add('pe', lambda e, ui=ui, c2=c2, b=b, csl=csl: e.matmul(
                    self.ps[ui][:, :], lhsT=pT[:, c2, b * 128:(b + 1) * 128], rhs=Wpp[:, c2, csl],
                    start=(c2 == 0), stop=(c2 == 1)), r=['Wpp', ('pT', b // 2)], w=[('ps', ui)])
            si = self.rot('Sg', 2)
            P.add('act', lambda e, si=si, gi=gi: e.activation(out=sg[si], in_=self.ps[gi][:, :], func=AF.Sigmoid),
                  r=[('ps', gi)], w=[('sg', si)])
            P.add('dve', lambda e, si=si, ui=ui: e.tensor_tensor(out=sg[si], in0=self.ps[ui][:, :], in1=sg[si], op=ALU.mult),
                  r=[('ps', ui), ('sg', si)], w=[('sg', si)])
            hsl = self.h[:, b, csl]
            P.add('pool', lambda e, si=si, hsl=hsl: e.tensor_tensor(out=hsl, in0=hsl, in1=sg[si], op=ALU.add),
                  r=[('sg', si), ('h', b, half)], w=[('h', b, half)])


Builder.ffn = _ffn
Builder.ffn_setup = _ffn_setup
Builder.ple = _ple


def _unit(self, lhsT, rhs, rkeys, mask_ap, mkeys, v_ap, vkeys, acc, ai, st, sp, aug=None):
    P = self.P
    self.step()
    si = self.rot('S', 3)
    ps = self.ps[si]
    P.add('pe', lambda e: e.matmul(ps[:, :], lhsT=lhsT, rhs=rhs, start=True, stop=(aug is None)), r=rkeys, w=[('ps', si)])
    if aug is not None:
        l2, r2, k2 = aug
        P.add('pe', lambda e: e.matmul(ps[:, :], lhsT=l2, rhs=r2, start=False, stop=True), r=k2, w=[('ps', si)])
    pi = self.rot('Pt', NPT)
    pt = self.Pt[pi]
    P.add('act', lambda e: e.activation(out=pt[:, :], in_=ps[:, :], func=AF.Exp, scale=0.125), r=[('ps', si)], w=[('Pt', pi, 0)])
    if mask_ap is not None:
        meng = 'pool' if self.rot('meng', 3) == 2 else 'dve'
        P.add(meng, lambda e: e.tensor_tensor(out=pt[:, :], in0=pt[:, :], in1=mask_ap, op=ALU.mult),
              r=[('Pt', pi, 0)] + mkeys, w=[('Pt', pi, 0)])
    self.defer(lambda: P.add('pe', lambda e: e.matmul(acc[0:65, :], lhsT=v_ap, rhs=pt[:, :], start=st, stop=sp),
                             r=[('Pt', pi, 0)] + vkeys, w=[('ps', ai)]), PD)


def _nsa_mixer(self, s):
    P, R = self.P, self.R
    self.phase()
    self.attn_common()
    self.rtmp = [R.alloc([512], F32) for _ in range(2)]
    self.sigT = R.alloc([S], BF16)
    P.add('pool', lambda e: e.memset(self.sigT[:, :], 0.0), w=['sigT'])
    self.gacc = R.alloc([512], F32, parts=64)
    Qz = [R.alloc([S], BF16) for _ in range(4)]
    KA = R.alloc([S], BF16)
    KB = R.alloc([S], BF16)
    Vs = R.alloc([NB, 1, 65], BF16)
    Vw = R.alloc([NB, 1, 65], BF16)
    W1c = R.alloc([8, 256], BF16, parts=64)
    W2k = R.alloc([2, 64], BF16)
    W2v = R.alloc([2, 64], BF16)
    gel = [R.alloc([128], BF16) for _ in range(2)]
    xs = R.alloc([128], F32)
    x2 = R.alloc([128], F32)
    hb = R.alloc([2], F32)
    b1t = R.alloc([2], F32)
    pos32 = R.alloc([64], F32, parts=32)
    posT = R.alloc([32], BF16, parts=64)
    kcmpT = R.alloc([128], BF16)
    vcmp = R.alloc([65], BF16)
    E4 = R.alloc([4, 128], F32)
    pcs = R.alloc([128], F32)
    den4 = R.alloc([4], F32)
    imp = R.alloc([32], F32)
    impm = R.alloc([32], F32)
    top8 = R.alloc([8], F32)
    sel = R.alloc([96], F32)
    w_in, w_sw = self.nsa_w_in, self.nsa_w_sw
    ident = self.c['c_ident']
    wm = self.c['c_wm']
    wt, wk, wds = self.wsl.get()
    self.load_w(wt[:, :, 0:48], wk, wds, w_in[:, 2560:2608])

    def evac_g(tc, ps, pk):
        P.add('act', lambda e: e.activation(out=self.sigT[0:48, tc * 512:(tc + 1) * 512], in_=ps[0:48, :], func=AF.Sigmoid),
              r=[pk], w=['sigT'])
    self.proj_fm(wt, wk, 48, evac_g)
    if NSA_STOP == 1:
        return
    P.add('pool', lambda e: e.memset(kcmpT[:, :], 0.0), w=['kcmpT'])
    P.add('pool', lambda e: e.memset(sel[:, :], 0.0), w=['sel'])
    for hh in range(4):
        P.add('pool', lambda e, hh=hh: e.memset(Qz[hh][64:128, :], 0.0), w=[(('NT', hh), t) for t in range(NCH)])
    P.add('pool', lambda e: e.memset(KA[64:128, :], 0.0), w=['KAz'])
    P.add('pool', lambda e: e.memset(KB[64:128, :], 0.0), w=['KBz'])
    P.add('sp', lambda e: e.dma_start(out=KA[64:96, :], in_=self.cst['c_E'][0:32, :]), r=['KAz'], w=['KAe'], dsem=self.ds_misc[0])
    P.add('pool', lambda e: e.memset(vcmp[:, :], 0.0), w=['vcmp'])
    P.add('pool', lambda e: e.memset(Vs[:, :, :, 64:65], 1.0), w=['Vs1'])
    P.add('pool', lambda e: e.memset(Vw[:, :, :, 64:65], 1.0), w=['Vw1'])
    P.add('pool', lambda e: e.memset(gel[0][:, :], 0.0), w=[('gel', 0)])
    P.add('pool', lambda e: e.memset(gel[1][:, :], 0.0), w=[('gel', 1)])
    allq = [[(('QZ', hh), t) for t in range(NCH)] for hh in range(4)]
    KAk = [('KA', t) for t in range(NCH)]
    KBk = [('KB', t) for t in range(NCH)]
    for g in range(4):
        for hh in range(4):
            c0 = (4 * g + hh) * 64
            self.proj_qk_rot(w_in[:, c0:c0 + 64], w_sw[:, c0:c0 + 64], Qz[hh], ('QZ', hh), ncols=64)
        self.proj_qk_rot(w_in[:, 1024 + 64 * g:1024 + 64 * g + 64], w_sw[:, 1024 + 64 * g:1024 + 64 * g + 64], KA, 'KA', ncols=64)
        wt, wk, wds = self.wsl.get()
        self.load_w(wt[:, :, 0:64], wk, wds, w_in[:, 1280 + 64 * g:1280 + 64 * g + 64])

        def evac_vc(tc, ps, pk):
            P.add('act', lambda e: e.activation(out=KB[0:64, tc * 512:(tc + 1) * 512], in_=ps[0:64, :], func=AF.Copy),
                  r=[pk], w=[('KB', tc)])
        self.proj_fm(wt, wk, 64, evac_vc)
        for which in range(2):
            src = KA if which == 0 else KB
            skeys = KAk if which == 0 else KBk
            w1 = self.nsa_w1_k if which == 0 else self.nsa_w1_v
            w2 = self.nsa_w2_k if which == 0 else self.nsa_w2_v
            b1 = self.nsa_b1_k if which == 0 else self.nsa_b1_v
            posd = self.nsa_pos_k if which == 0 else self.nsa_pos_v
            P.add('sp', lambda e, posd=posd: e.dma_start(out=pos32[0:32, :], in_=posd), w=['pos32'], dsem=self.ds_misc[0])
            P.add('sp', lambda e, b1=b1: e.dma_start(out=b1t[:, 0:1], in_=b1[0:128, :]), w=[('b1t', 0)], dsem=self.ds_misc[1])
            P.add('sp', lambda e, b1=b1: e.dma_start(out=b1t[:, 1:2], in_=b1[128:256, :]), w=[('b1t', 1)], dsem=self.ds_misc[2])
            if which == 0:
                self.load_w(W2k[:, :, :], 'W2a', self.ds_misc[3], w2)
                w2keys = ['W2a']
            else:
                self.load_w(W2v[:, :, :], 'W2v', self.ds_misc[3], w2)
                w2keys = ['W2v']
            P.add('pe', lambda e: e.transpose(out=self.ps[5][0:64, 0:32], in_=pos32[0:32, 0:64], identity=ident[0:32, 0:32]),
                  r=['pos32', 'c_ident'], w=[('ps', 5)])
            P.add('act', lambda e: e.activation(out=posT[0:64, :], in_=self.ps[5][0:64, 0:32], func=AF.Copy),
                  r=[('ps', 5)], w=['posT'])
            first = True
            for ic in range(4):
                srcw = w1[ic * 512:(ic + 1) * 512, :].rearrange('(i d) c -> d i c', d=64)
                P.add('pool', lambda e, srcw=srcw: e.dma_start(out=W1c[0:64, :, :], in_=srcw), w=['W1c'], dsem=self.ds_w[4])
                for ii in range(8):
                    i = ic * 8 + ii
                    for hc in range(2):
                        P.add('pe', lambda e, ii=ii, i=i, hc=hc, src=src: e.matmul(
                            self.ps[6 + hc][:, 0:127], lhsT=W1c[0:64, ii, hc * 128:(hc + 1) * 128],
                            rhs=src[0:64, ssl(i, 127, 16)], start=(i == 0), stop=(i == 31)),
                            r=['W1c'] + skeys, w=[('ps', 6 + hc)])
                        P.add('pe', lambda e, ii=ii, i=i, hc=hc, st=first: e.matmul(
                            self.ps[5][:, hc:hc + 1], lhsT=W1c[0:64, ii, hc * 128:(hc + 1) * 128],
                            rhs=posT[0:64, i:i + 1], start=st, stop=(i == 31)),
                            r=['W1c', 'posT'], w=[('ps', 5)])
                        first = False
            P.add('dve', lambda e: e.tensor_tensor(out=hb[:, 0:2], in0=self.ps[5][:, 0:2], in1=b1t[:, 0:2], op=ALU.add),
                  r=[('ps', 5), ('b1t', 0), ('b1t', 1)], w=['hb'])
            for hc in range(2):
                pk = ('ps', 6 + hc)
                psh = self.ps[6 + hc]
                P.add('act', lambda e, psh=psh, hc=hc: e.activation(out=xs[:, 0:127], in_=psh[:, 0:127], func=AF.Identity,
                                                                    bias=hb[:, hc:hc + 1]), r=[pk, 'hb'], w=['xs'])
                P.add('dve', lambda e: e.tensor_tensor(out=x2[:, 0:127], in0=xs[:, 0:127], in1=xs[:, 0:127], op=ALU.mult),
                      r=['xs'], w=['x2'])
                P.add('dve', lambda e: e.tensor_scalar(out=x2[:, 0:127], in0=x2[:, 0:127], scalar1=0.044715, scalar2=1.0,
                                                       op0=ALU.mult, op1=ALU.add), r=['x2'], w=['x2'])
                P.add('dve', lambda e: e.tensor_tensor(out=x2[:, 0:127], in0=x2[:, 0:127], in1=xs[:, 0:127], op=ALU.mult),
                      r=['x2', 'xs'], w=['x2'])
                P.add('act', lambda e: e.activation(out=x2[:, 0:127], in_=x2[:, 0:127], func=AF.Sigmoid,
                                                    scale=float(2.0 * np.sqrt(2.0 / np.pi))), r=['x2'], w=['x2'])
                P.add('dve', lambda e, hc=hc: e.tensor_tensor(out=gel[hc][:, 0:127], in0=xs[:, 0:127], in1=x2[:, 0:127],
                                                              op=ALU.mult), r=['x2', 'xs'], w=[('gel', hc)])
            if which == 0:
                for hc in range(2):
                    P.add('pe', lambda e, hc=hc: e.matmul(self.ps[6][0:64, 0:127], lhsT=W2k[:, hc, :], rhs=gel[hc][:, 0:127],
                                                          start=(hc == 0), stop=(hc == 1)),
                          r=[('gel', hc)] + w2keys, w=[('ps', 6)])
                P.add('act', lambda e: e.activation(out=kcmpT[0:64, 0:127], in_=self.ps[6][0:64, 0:127], func=AF.Copy),
                      r=[('ps', 6)], w=['kcmpT'])
            else:
                for hc in range(2):
                    P.add('pe', lambda e, hc=hc: e.matmul(self.ps[6][0:127, 0:64], lhsT=gel[hc][:, 0:127], rhs=W2v[:, hc, :],
                                                          start=(hc == 0), stop=(hc == 1)),
                          r=[('gel', hc)] + w2keys, w=[('ps', 6)])
                P.add('act', lambda e: e.activation(out=vcmp[0:127, 0:64], in_=self.ps[6][0:127, 0:64], func=AF.Copy),
                      r=[('ps', 6)], w=['vcmp'])
                P.add('pool', lambda e: e.memset(vcmp[:, 64:65], 1.0), r=[], w=['vcmp1'])
        if NSA_STOP == 2:
            continue
        self.proj_qk_rot(w_in[:, 1536 + 64 * g:1536 + 64 * g + 64], w_sw[:, 1280 + 64 * g:1280 + 64 * g + 64], KA, 'KA', ncols=64)
        self.proj_qk_rot(w_in[:, 2048 + 64 * g:2048 + 64 * g + 64], w_sw[:, 1536 + 64 * g:1536 + 64 * g + 64], KB, 'KB', ncols=64)
        self.proj_v(w_in[:, 1792 + 64 * g:1792 + 64 * g + 64], 64, lambda gg: Vs[:, 4 * gg:4 * gg + 4, :, 0:64], 'Vs')
        self.proj_v(w_in[:, 2304 + 64 * g:2304 + 64 * g + 64], 64, lambda gg: Vw[:, 4 * gg:4 * gg + 4, :, 0:64], 'Vw')
        def imp_stage(qb):
            si = self.rot('S', 3)
            ps = self.ps[si]
            for hh in range(4):
                P.add('pe', lambda e, ps=ps, hh=hh, qb=qb: e.matmul(
                    ps[:, hh * 128:(hh + 1) * 128], lhsT=Qz[hh][:, qb * 128:(qb + 1) * 128], rhs=kcmpT[:, :],
                    start=True, stop=True), r=['kcmpT'] + allq[hh] + [(('NT', hh), t) for t in range(NCH)], w=[('ps', si)])
            P.add('act', lambda e, ps=ps: e.activation(out=E4, in_=ps[:, :].rearrange('p (h n) -> p h n', h=4), func=AF.Exp,
                                                       scale=0.125), r=[('ps', si)], w=['E4'])
            m0s = self.c['c_m0'][:, 120 - 8 * qb:248 - 8 * qb].unsqueeze(1).broadcast_to([128, 4, 128])
            P.add('dve', lambda e, m0s=m0s: e.tensor_tensor(out=E4, in0=E4, in1=m0s, op=ALU.mult), r=['E4', 'c_m0'], w=['E4'])
            P.add('dve', lambda e: e.tensor_reduce(out=den4, in_=E4, axis=AX.X, op=ALU.add), r=['E4'], w=['den4'])
            P.add('dve', lambda e: e.tensor_scalar(out=den4, in0=den4, scalar1=1e-30, scalar2=None, op0=ALU.max),
                  r=['den4'], w=['den4'])
            P.add('dve', lambda e: e.reciprocal(out=den4, in_=den4), r=['den4'], w=['den4'])
            P.add('dve', lambda e: e.tensor_scalar(out=pcs, in0=E4[:, 0, :], scalar1=den4[:, 0:1], scalar2=None, op0=ALU.mult),
                  r=['E4', 'den4'], w=['pcs'])
            for hh in range(1, 4):
                P.add('dve', lambda e, hh=hh: e.scalar_tensor_tensor(out=pcs, in0=E4[:, hh, :], scalar=den4[:, hh:hh + 1],
                                                                     in1=pcs, op0=ALU.mult, op1=ALU.add),
                      r=['E4', 'den4', 'pcs'], w=['pcs'])
            P.add('dve', lambda e: e.tensor_reduce(out=imp, in_=pcs[:, :].rearrange('p (j f) -> p j f', f=4), axis=AX.X,
                                                   op=ALU.add), r=['pcs'], w=['imp'])
            P.add('dve', lambda e: e.tensor_tensor(out=imp[:, 1:32], in0=imp[:, 1:32], in1=pcs[:, ssl(3, 31, 4)], op=ALU.add),
                  r=['pcs', 'imp'], w=['imp'])
            P.add('dve', lambda e, qb=qb: e.tensor_tensor(out=impm, in0=imp, in1=self.c['c_keep'][:, 30 - 2 * qb:62 - 2 * qb],
                                                          op=ALU.mult), r=['imp', 'c_keep'], w=['impm'])
            P.add('dve', lambda e, qb=qb: e.tensor_tensor(out=impm, in0=impm, in1=self.c['c_addv'][:, 30 - 2 * qb:62 - 2 * qb],
                                                          op=ALU.add), r=['impm', 'c_addv'], w=['impm'])
            P.add('dve', lambda e: e.memset(impm[:, 0:1], 1e6), r=['impm'], w=['impm'])
            P.add('dve', lambda e: e.max(out=top8, in_=impm), r=['impm'], w=['top8'])
            P.add('dve', lambda e: e.tensor_scalar(out=sel[:, 64:96], in0=impm, scalar1=top8[:, 7:8], scalar2=None, op0=ALU.is_ge),
                  r=['impm', 'top8'], w=['sel'])
            P.add('pe', lambda e: e.transpose(out=self.ps[5][0:96, 0:128], in_=sel[:, 0:96], identity=ident[:]),
                  r=['sel', 'c_ident'], w=[('ps', 5)])
            for hh in range(4):
                P.add('dve', lambda e, qb=qb, hh=hh: e.tensor_scalar(
                    out=Qz[hh][64:96, qb * 128:(qb + 1) * 128], in0=self.ps[5][64:96, 0:128],
                    scalar1=30000.0, scalar2=-30000.0, op0=ALU.mult, op1=ALU.add),
                    r=[('ps', 5)], w=[(('NT', hh), qb // 4)])
        for qb in range(4):
            imp_stage(qb)
        if NSA_STOP in (3, 5):
            continue
        for jp in range(2):
            ot_keys = []
            for h2 in range(2):
                hh = 2 * jp + h2
                head = 4 * g + hh
                for i in range(NCH):
                    if hh == 0 and i + 1 < NCH:
                        for qb in range(4 * (i + 1), 4 * (i + 2)):
                            imp_stage(qb)
                    csl = slice(i * 512, (i + 1) * 512)
                    q_ap = Qz[hh][:, csl]
                    qk = [(('QZ', hh), i), (('NT', hh), i)]
                    ai = 3 + self.rot('ACC', 2)
                    self.unit(kcmpT[:, :], q_ap, ['kcmpT'] + qk, self.c['c_cmpT'][:, csl], ['c_cmpT'],
                              vcmp[:, 0:65], ['vcmp', 'vcmp1'], self.ps[ai], ai, True, True)
                    self.finalize(self.ps[ai], ('ps', ai), None, None, gate=(3 * head + 0, csl), first=True, last=False)
                    ai = 3 + self.rot('ACC', 2)
                    nkb = 4 * i + 4
                    for kb in range(nkb):
                        mask = wm[:, 384 - 128 * (kb - 4 * i):384 - 128 * (kb - 4 * i) + 512] if kb >= 4 * i else None
                        self.unit(KA[:, kb * 128:(kb + 1) * 128], q_ap, [('KA', kb // 4), 'KAz', 'KAe'] + qk, mask, ['c_wm'],
                                  Vs[:, kb, 0, :], [('Vs', kb // 4), 'Vs1'], self.ps[ai], ai, kb == 0, kb == nkb - 1)
                    self.finalize(self.ps[ai], ('ps', ai), None, None, gate=(3 * head + 1, csl), first=False, last=False)
                    ai = 3 + self.rot('ACC', 2)
                    kb0 = max(0, 4 * i - 4)
                    for kb in range(kb0, nkb):
                        r_ = kb - 4 * i
                        mask = wm[:, 384 - 128 * r_:384 - 128 * r_ + 512]
                        self.unit(KB[:, kb * 128:(kb + 1) * 128], q_ap, [('KB', kb // 4), 'KBz'] + qk, mask, ['c_wm'],
                                  Vw[:, kb, 0, :], [('Vw', kb // 4), 'Vw1'], self.ps[ai], ai, kb == kb0, kb == nkb - 1)
                    self.finalize(self.ps[ai], ('ps', ai), self.OT[h2][0:64, csl], ('OT', h2, i),
                                  gate=(3 * head + 2, csl), first=False, last=True)
                ot_keys.append([('OT', h2, i) for i in range(NCH)])
            self.outproj_pair(self.nsa_w_out, 2 * g + jp, ot_keys)


Builder.unit = _unit
Builder.nsa_mixer = _nsa_mixer


def _moe(self):
    P, R = self.P, self.R
    self.phase()
    self.norm(self.ffn_norm[1:2, :])
    self.phase()
    comb = R.alloc([8, NB], F32)
    lg = R.alloc([NB, 8], F32)
    rb = R.alloc([1, 8], F32)
    t8 = R.alloc([NB, 8], F32)
    w1 = R.alloc([NB], F32)
    w2 = R.alloc([NB], F32)
    ta = R.alloc([NB], F32)
    tb = R.alloc([NB], F32)
    wr = R.alloc([8, 8], BF16)
    self.load_w(wr, 'wr', self.ds_misc[0], self.moe_w_router)
    P.add('sp', lambda e: e.dma_start(out=rb[:, 0, :], in_=self.moe_b_router[0, :].partition_broadcast(128)),
          w=['rb'], dsem=self.ds_misc[1])
    ps = self.ps[7]
    for b in range(NB):
        for kc in range(8):
            P.add('pe', lambda e, b=b, kc=kc: e.matmul(ps[:, b * 8:(b + 1) * 8], lhsT=self.hnT[:, kc, b * 128:(b + 1) * 128],
                                                       rhs=wr[:, kc, :], start=(kc == 0), stop=(kc == 7)),
                  r=['wr', ('hnT', b)], w=[('ps', 7)])
    P.add('dve', lambda e: e.tensor_tensor(out=lg, in0=ps[:, 0:128].rearrange('p (b h) -> p b h', h=8),
                                           in1=rb[:, 0:1, :].broadcast_to([128, NB, 8]), op=ALU.add),
          r=[('ps', 7), 'rb'], w=['lg'])
    for b in range(NB):
        P.add('dve', lambda e, b=b: e.max(out=t8[:, b, :], in_=lg[:, b, :]), r=['lg'], w=[('t8', b)])
    t8k = [('t8', b) for b in range(NB)]
    P.add('dve', lambda e: e.tensor_tensor(out=w1, in0=t8[:, :, 1], in1=t8[:, :, 0], op=ALU.subtract), r=t8k, w=['w1'])
    P.add('act', lambda e: e.activation(out=w1, in_=w1, func=AF.Exp), r=['w1'], w=['w1'])
    P.add('dve', lambda e: e.tensor_scalar(out=w1, in0=w1, scalar1=1.0, scalar2=None, op0=ALU.add), r=['w1'], w=['w1'])
    P.add('dve', lambda e: e.reciprocal(out=w1, in_=w1), r=['w1'], w=['w1'])
    P.add('dve', lambda e: e.tensor_scalar(out=w2, in0=w1, scalar1=-1.0, scalar2=1.0, op0=ALU.mult, op1=ALU.add),
          r=['w1'], w=['w2'])
    for ex in range(8):
        P.add('dve', lambda e, ex=ex: e.tensor_tensor(out=ta, in0=lg[:, :, ex], in1=t8[:, :, 0], op=ALU.is_equal),
              r=['lg'] + t8k, w=['ta'])
        P.add('dve', lambda e: e.tensor_tensor(out=ta, in0=ta, in1=w1, op=ALU.mult), r=['ta', 'w1'], w=['ta'])
        P.add('dve', lambda e, ex=ex: e.tensor_tensor(out=tb, in0=lg[:, :, ex], in1=t8[:, :, 1], op=ALU.is_equal),
              r=['lg'] + t8k, w=['tb'])
        P.add('dve', lambda e: e.tensor_tensor(out=tb, in0=tb, in1=w2, op=ALU.mult), r=['tb', 'w2'], w=['tb'])
        P.add('dve', lambda e, ex=ex: e.tensor_tensor(out=comb[:, ex, :], in0=ta, in1=tb, op=ALU.add),
              r=['ta', 'tb'], w=['comb'])
    self.ffn_setup()
    for ex in range(8):
        self.ffn(self.moe_w_gate[ex], self.moe_w_up[ex], self.moe_w_down[ex], comb=comb[:, ex, :])


Builder.moe = _moe
```

```python
import numpy as np
import ml_dtypes
from contextlib import ExitStack
import concourse.bass as bass
import concourse.mybir as mybir
from concourse.bass_utils import run_bass_kernel_spmd

dt = mybir.dt
F32, BF16, I32 = dt.float32, dt.bfloat16, dt.int32
AF = mybir.ActivationFunctionType
ALU = mybir.AluOpType
AX = mybir.AxisListType

S = 2048
D = 1024
NB = 16
NCH = 4
DFF = 3584
PI = float(np.pi)
TWO_PI = float(2 * np.pi)

import os as _os
NSA_STOP = int(_os.environ.get('NSA_STOP', '0'))
PD = 4
NPT = PD + 2
ENG_NAMES = ['pe', 'act', 'dve', 'pool', 'sp']


def ssl(start, n, step):
    return slice(start, start + (n - 1) * step + 1, step)

SAME_ENG_SYNC = True


class DSem:
    def __init__(self, sem):
        self.sem = sem
        self.total = 0
        self.open = None


class Op:
    __slots__ = ('eng', 'fn', 'deps', 'dsem', 'sig', 'sem', 'val')


class Prog:
    def __init__(self, nc, stack):
        self.nc = nc
        self.stack = stack
        self.ops = []
        self.by_eng = {e: [] for e in ENG_NAMES}
        self.lastw = {}
        self.readers = {}
        self.nsem = 0
        self.bar_deps = {}
        self.bar_done = set()
        self.since_bar_dma = {}
        self.final = []
        self.uid = 0

    def new_sem(self, name):
        self.nsem += 1
        return self.stack.enter_context(self.nc.semaphore(f"{name}_{self.nsem}"))

    def dsem(self, name):
        return DSem(self.new_sem(name))

    def group(self, ds):
        ds.open = []

    def endgroup(self, ds):
        for o in ds.open:
            o.val = ds.total * 16
        ds.open = None

    def barrier(self):
        deps = dict(self.bar_deps)
        for e in ENG_NAMES:
            for o in reversed(self.by_eng[e]):
                if o.dsem is None:
                    deps[e] = o
                    o.sig = True
                    break
        for k, o in self.since_bar_dma.items():
            deps[k] = o
        self.bar_deps = deps
        self.bar_done = set()
        self.since_bar_dma = {}
        self.lastw = {}
        self.readers = {}

    def add(self, eng, fn, r=(), w=(), dsem=None):
        o = Op()
        o.eng = eng
        o.fn = fn
        o.dsem = dsem
        o.sig = False
        o.sem = None
        o.val = 0
        deps = {}

        def need(p):
            if p is None:
                return
            if p.dsem is None and dsem is None and p.eng == eng:
                if eng == 'pe' or not SAME_ENG_SYNC:
                    return
            deps[id(p)] = p

        for k in r:
            need(self.lastw.get(k))
        for k in w:
            need(self.lastw.get(k))
            rd = self.readers.get(k)
            if rd:
                for q in rd.values():
                    need(q)
        if eng not in self.bar_done:
            self.bar_done.add(eng)
            for p in self.bar_deps.values():
                need(p)
        for k in w:
            self.lastw[k] = o
            self.readers[k] = {}
        for k in r:
            d = self.readers.setdefault(k, {})
            if dsem is not None:
                self.uid += 1
                d[('dma', self.uid)] = o
            else:
                d[eng] = o
        o.deps = list(deps.values())
        for p in o.deps:
            p.sig = True
        if dsem is not None:
            dsem.total += 1
            o.sem = dsem.sem
            o.val = dsem.total * 16
            if dsem.open is not None:
                dsem.open.append(o)
            self.since_bar_dma[id(dsem)] = o
        self.ops.append(o)
        self.by_eng[eng].append(o)
        return o

    def emit(self):
        nc = self.nc
        LIMIT = 30000
        for e in ENG_NAMES:
            cur = None
            cnt = 0
            for o in self.by_eng[e]:
                if o.dsem is None and o.sig:
                    if cur is None or cnt >= LIMIT:
                        cur = self.new_sem(f"e_{e}")
                        cnt = 0
                    cnt += 1
                    o.sem = cur
                    o.val = cnt
        final = self.final

        def run(e, eng):
            known = {}
            for o in self.by_eng[e]:
                need = {}
                for p in o.deps:
                    s = p.sem
                    if s is None:
                        continue
                    cur = need.get(id(s))
                    if cur is None or cur[1] < p.val:
                        need[id(s)] = (s, p.val)
                for sid, (s, v) in need.items():
                    if known.get(sid, 0) < v:
                        eng.wait_ge(s, v)
                        known[sid] = v
                inst = o.fn(eng)
                if o.dsem is not None:
                    inst.then_inc(o.sem, 16)
                elif o.sig:
                    inst.then_inc(o.sem, 1)
            if e == 'sp':
                for o in final:
                    eng.wait_ge(o.sem, o.val)

        with nc.Block() as block:
            @block.tensor
            def _(eng):
                run('pe', eng)

            @block.scalar
            def _(eng):
                run('act', eng)

            @block.vector
            def _(eng):
                run('dve', eng)

            @block.gpsimd
            def _(eng):
                run('pool', eng)

            @block.sync
            def _(eng):
                run('sp', eng)


class Region:
    def __init__(self, ap, nelem):
        self.ap = ap
        self.n = nelem
        self.off = 0
        self.peak = 0

    def reset(self):
        self.off = 0

    def alloc(self, free_shape, dtype, parts=128):
        n = int(np.prod(free_shape))
        mult = 2 if dtype == F32 or dtype == I32 else 1
        nb = n * mult
        nb = (nb + 15) // 16 * 16
        assert self.off + nb <= self.n, f"region overflow {self.off}+{nb}>{self.n}"
        v = self.ap[0:parts, self.off:self.off + n * mult]
        self.off += nb
        self.peak = max(self.peak, self.off)
        if mult == 2:
            v = v.bitcast(dtype)
        if len(free_shape) == 2:
            v = v.rearrange('p (a b) -> p a b', b=free_shape[1])
        elif len(free_shape) == 3:
            v = v.rearrange('p (a b c) -> p a b c', b=free_shape[1], c=free_shape[2])
        return v


def _bf(a):
    return np.ascontiguousarray(a.astype(np.float32)).astype(ml_dtypes.bfloat16)


def make_consts():
    c = {}
    k = np.arange(128)[:, None]
    c['c_ident'] = np.eye(128, dtype=np.float32)
    c['c_utri'] = (k <= np.arange(128)[None, :]).astype(np.float32)
    c['c_ones'] = np.ones((128, 128), np.float32)
    os_ = np.zeros((128, 64), np.float32)
    os_[64, :] = 1.0
    c['c_os'] = _bf(os_)
    y = np.arange(-384, 1024)[None, :]
    c['c_wm'] = _bf(((y - k) >= 0) & ((y - k) < 512))
    q = np.arange(128)[None, :]
    cur = (k <= q)
    prev = (k >= q)
    c['c_dm'] = _bf(np.concatenate([cur, prev, cur, prev], axis=1))
    n = np.arange(128)[:, None]
    qq = np.arange(2048)[None, :]
    c['c_cmpT'] = _bf((16 * n + 31 <= qq) & (n <= 126))
    xq = np.arange(128)[:, None]
    xx = np.arange(-120, 128)[None, :]
    c['c_m0'] = (16 * xx + 31 <= xq).astype(np.float32)
    xs = np.arange(-30, 32)[None, :]
    curp = (xq // 64)
    fut = xs > curp
    forced = (xs == curp) | (xs == curp - 1)
    keep = (~fut) & (~forced)
    addv = np.where(fut, -1.0, np.where(forced, 1e6, 0.0))
    c['c_keep'] = keep.astype(np.float32)
    c['c_addv'] = addv.astype(np.float32)
    j = np.arange(32)[:, None, None]
    kb = np.arange(16)[None, :, None]
    kk = np.arange(128)[None, None, :]
    E = (j == 2 * kb + kk // 64)
    Ef = np.zeros((128, 16 * 128), np.float32)
    Ef[:32] = E.reshape(32, 16 * 128)
    c['c_E'] = _bf(Ef)
    Z = np.zeros((128, 48 * 64), np.float32)
    for p_ in range(48):
        Z[p_, 64 * p_:64 * p_ + 64] = 1.0
    c['c_Z'] = _bf(Z)
    half = 32
    inv_freq = (10000.0 ** (-np.arange(half, dtype=np.float32) / half)).astype(np.float32)
    pp = np.arange(128)
    rot = np.zeros((128, 4), np.float32)
    rot[:, 0] = inv_freq[(pp % 64) % 32]
    rot[:, 1] = PI / 2
    rot[:, 2] = np.where((pp % 64) < 32, PI, 0.0)
    c['c_rot'] = rot
    return c


CONST_SHAPES = None


class Slots:
    def __init__(self, P, name, aps, dsems):
        self.items = [(ap, f"{name}{i}", dsems[i]) for i, ap in enumerate(aps)]
        self.i = 0

    def get(self):
        it = self.items[self.i % len(self.items)]
        self.i += 1
        return it


class Builder:
    def __init__(self, n_seq=2, stages=('l0', 'l1'), dbg=None):
        self.n_seq = n_seq
        self.stages = stages
        self.dbg = dbg
        nc = bass.Bass("TRN2", target_bir_lowering=False)
        self.nc = nc
        self.stack = ExitStack()
        self.P = Prog(nc, self.stack)
        self.rr = {}
        self.fifo = []
        self.fins = []
        self.fseq = 0
        self.gate_free_at = 0
        self.u_busy = [False, False]
        self.acc_pending = {}
        self.acc_gen = {}
        self.tick = 0
        self._inputs()
        self._alloc()

    def din(self, name, shape, dtype=F32):
        return self.nc.dram_tensor(name, list(shape), dtype, kind="ExternalInput").ap()

    def sb(self, name, shape, dtype):
        return self.stack.enter_context(self.nc.sbuf_tensor(name, list(shape), dtype))

    def _inputs(self):
        n = self.n_seq
        self.x = self.din('x', [n, S, D])
        self.p = self.din('p', [2, n, S, 256])
        self.pos = self.din('positions', [n, S], I32)
        self.mix_norm = self.din('mix_norm', [2, D])
        self.ffn_norm = self.din('ffn_norm', [2, D])
        self.ple_norm = self.din('ple_norm', [2, D])
        self.final_norm = self.din('final_norm', [1, D])
        self.ple_gate_w = self.din('ple_gate_w', [2, D, D])
        self.ple_proj_w = self.din('ple_proj_w', [2, 256, D])
        self.fd_w_in = self.din('fd_w_in', [D, 3080])
        self.fd_w_sw = self.din('fd_w_sw', [D, 1024])
        self.fd_forget_b = self.din('fd_forget_b', [1, 8])
        self.fd_w_out = self.din('fd_w_out', [D, D])
        self.dense_w_gate = self.din('dense_w_gate', [D, DFF])
        self.dense_w_up = self.din('dense_w_up', [D, DFF])
        self.dense_w_down = self.din('dense_w_down', [DFF, D])
        self.nsa_w_in = self.din('nsa_w_in', [D, 2608])
        self.nsa_w_sw = self.din('nsa_w_sw', [D, 1792])
        self.nsa_pos_k = self.din('nsa_pos_k', [32, 64])
        self.nsa_w1_k = self.din('nsa_w1_k', [2048, 256])
        self.nsa_b1_k = self.din('nsa_b1_k', [256, 1])
        self.nsa_w2_k = self.din('nsa_w2_k', [256, 64])
        self.nsa_pos_v = self.din('nsa_pos_v', [32, 64])
        self.nsa_w1_v = self.din('nsa_w1_v', [2048, 256])
        self.nsa_b1_v = self.din('nsa_b1_v', [256, 1])
        self.nsa_w2_v = self.din('nsa_w2_v', [256, 64])
        self.nsa_w_out = self.din('nsa_w_out', [D, D])
        self.moe_w_router = self.din('moe_w_router', [D, 8])
        self.moe_b_router = self.din('moe_b_router', [1, 8])
        self.moe_w_gate = self.din('moe_w_gate', [8, D, DFF])
        self.moe_w_up = self.din('moe_w_up', [8, D, DFF])
        self.moe_w_down = self.din('moe_w_down', [8, DFF, D])
        self.cst = {}
        for k, v in make_consts().items():
            d = BF16 if v.dtype == ml_dtypes.bfloat16 else F32
            self.cst[k] = self.din(k, v.shape, d)
        self.y = self.nc.dram_tensor('y', [n, S, D], F32, kind="ExternalOutput").ap()

    def _alloc(self):
        P = self.P
        self.h = self.sb('h', [128, NB, D], F32)
        self.hnT = self.sb('hnT', [128, 8, S], BF16)
        self.cosT = self.sb('cosT', [128, S], BF16)
        self.sinT = self.sb('sinT', [128, S], BF16)
        self.c = {}
        for k, ap in self.cst.items():
            if k == 'c_E':
                continue
            self.c[k] = self.sb('s_' + k, list(ap.shape), ap.dtype)
        self.ps = [self.stack.enter_context(self.nc.psum_tensor(f"ps{i}", [128, 512], F32)) for i in range(8)]
        RN = 43520
        self.Rt = self.sb('R', [128, RN], BF16)
        self.R = Region(self.Rt, RN)
        self.ds_c = P.dsem('consts')
        self.ds_h = P.dsem('hload')
        self.ds_g = P.dsem('gtile')
        self.ds_y = P.dsem('yout')
        self.ds_misc = [P.dsem(f'misc{i}') for i in range(4)]
        self.ds_w = [P.dsem(f'w{i}') for i in range(6)]
        self.ds_wo = [P.dsem(f'wo{i}') for i in range(2)]
        self.ds_ffn = [P.dsem(f'ffn{i}') for i in range(6)]

    def defer(self, fn, delay, fin=False):
        if fin:
            self.fseq += 1
            self.fins.append((self.tick + delay, self.fseq, fn))
            self.fins.sort(key=lambda x: (x[0], x[1]))
        else:
            self.fifo.append((self.tick + delay, fn))

    def _drain(self):
        while self.fins and self.fins[0][0] <= self.tick:
            self.fins.pop(0)[2]()
        while self.fifo and self.fifo[0][0] <= self.tick:
            self.fifo.pop(0)[1]()

    def step(self):
        self.tick += 1
        self._drain()

    def flush(self):
        while self.fifo or self.fins:
            self.tick += 1
            self._drain()

    def acc_gen_of(self, key):
        return self.acc_gen.get(key, 0)

    def acc_start(self, key, gen):
        assert not (key in self.acc_pending and gen > self.acc_pending[key]), \
            f"accumulator {key} reused before its finalize was emitted"


    def step(self):
        self.tick += 1
        self._drain()

    def flush(self):
        while self.fifo or self.fins:
            self.tick += 1
            self._drain()

    def rot(self, name, n):
        i = self.rr.get(name, 0)
        self.rr[name] = i + 1
        return i % n

    def load_consts(self):
        P = self.P
        P.group(self.ds_c)
        for k, ap in self.cst.items():
            if k == 'c_E':
                continue
            P.add('sp', lambda e, k=k, ap=ap: e.dma_start(out=self.c[k][:], in_=ap), w=[k], dsem=self.ds_c)
        P.endgroup(self.ds_c)

    def load_w(self, dst, dkey, dsem, src, cast=True):
        srcv = src.rearrange('(k p) c -> p k c', p=128)
        eng = 'pool' if cast else 'sp'
        return self.P.add(eng, lambda e: e.dma_start(out=dst, in_=srcv), w=[dkey], dsem=dsem)

    def norm(self, gain_row, final_out=None):
        P, R, h = self.P, self.R, self.h
        mark = R.off
        self.gtile = R.alloc([D], F32)
        ss = R.alloc([NB], F32)
        junk = R.alloc([D], BF16)
        hs = [R.alloc([D], F32) for _ in range(2)]
        P.add('sp', lambda e: e.dma_start(out=self.gtile, in_=gain_row[0, :].partition_broadcast(128)),
              w=['gtile'], dsem=self.ds_g)
        for b in range(NB):
            P.add('act', lambda e, b=b: e.activation(out=junk, in_=h[:, b, :], func=AF.Square,
                                                     accum_out=ss[:, b:b + 1]),
                  r=self.hk(b), w=['junk', ('ss', b)])
        allss = [('ss', b) for b in range(NB)]
        P.add('dve', lambda e: e.tensor_scalar(out=ss, in0=ss, scalar1=1.0 / D, scalar2=1e-6,
                                               op0=ALU.mult, op1=ALU.add), r=allss, w=allss)
        P.add('act', lambda e: e.activation(out=ss, in_=ss, func=AF.Sqrt), r=allss, w=allss)
        P.add('dve', lambda e: e.reciprocal(out=ss, in_=ss), r=allss, w=allss)
        ident = self.c['c_ident']
        for b in range(NB):
            i = b % 2
            P.add('dve', lambda e, b=b, i=i: e.scalar_tensor_tensor(
                out=hs[i], in0=h[:, b, :], scalar=ss[:, b:b + 1], in1=self.gtile,
                op0=ALU.mult, op1=ALU.mult), r=self.hk(b) + [('ss', b), 'gtile'], w=[('hs', i)])
            if final_out is not None:
                s = final_out
                P.add('sp', lambda e, b=b, i=i, s=s: e.dma_start(out=self.y[s, b * 128:(b + 1) * 128, :], in_=hs[i]),
                      r=[('hs', i)], w=[('y', s, b)], dsem=self.ds_y)
                continue
            for half in range(2):
                pi = 6 + half
                ps = self.ps[pi]
                for j in range(4):
                    cidx = half * 4 + j
                    P.add('pe', lambda e, ps=ps, j=j, cidx=cidx, i=i: e.transpose(
                        out=ps[:, j * 128:(j + 1) * 128], in_=hs[i][:, cidx * 128:(cidx + 1) * 128],
                        identity=ident[:]), r=[('hs', i), 'c_ident'], w=[('ps', pi)])
                P.add('act', lambda e, ps=ps, half=half, b=b: e.activation(
                    out=self.hnT[:, half * 4:half * 4 + 4, b * 128:(b + 1) * 128],
                    in_=ps[:].rearrange('p (a t) -> p a t', t=128), func=AF.Copy),
                    r=[('ps', pi)], w=[('hnT', b)])
        R.off = mark

    def hk(self, b):
        return [('h', b, 0), ('h', b, 1)]

    def hn_keys(self, tc=None):
        if tc is None:
            return [('hnT', b) for b in range(NB)]
        return [('hnT', 4 * tc + j) for j in range(4)]

    def load_seq(self, s):
        P = self.P
        P.group(self.ds_h)
        for b in range(NB):
            P.add('sp', lambda e, b=b: e.dma_start(out=self.h[:, b, :], in_=self.x[s, b * 128:(b + 1) * 128, :]),
                  w=self.hk(b), dsem=self.ds_h)
        P.endgroup(self.ds_h)

    def rotary_tables(self, s):
        P, R = self.P, self.R
        mark = R.off
        posi = R.alloc([S], I32)
        posf = R.alloc([S], F32)
        t = R.alloc([S], F32)
        kf = R.alloc([S], F32)
        ki = R.alloc([S], I32)
        rot = self.c['c_rot']
        P.add('sp', lambda e: e.dma_start(out=posi, in_=self.pos[s, :].partition_broadcast(128)),
              w=['posi'], dsem=self.ds_misc[0])
        P.add('dve', lambda e: e.tensor_copy(out=posf, in_=posi), r=['posi'], w=['posf'])
        for (dst, dkey, ph) in ((self.cosT, 'cosT', 1), (self.sinT, 'sinT', 2)):
            P.add('dve', lambda e, ph=ph: e.tensor_scalar(out=t, in0=posf, scalar1=rot[:, 0:1], scalar2=rot[:, ph:ph + 1],
                                                          op0=ALU.mult, op1=ALU.add), r=['posf', 'c_rot'], w=['rt'])
            P.add('dve', lambda e: e.tensor_scalar(out=kf, in0=t, scalar1=1.0 / TWO_PI, scalar2=None, op0=ALU.mult),
                  r=['rt'], w=['rk'])
            P.add('dve', lambda e: e.tensor_copy(out=ki, in_=kf), r=['rk'], w=['rki'])
            P.add('dve', lambda e: e.tensor_copy(out=kf, in_=ki), r=['rki'], w=['rk'])
            P.add('dve', lambda e: e.scalar_tensor_tensor(out=t, in0=kf, scalar=-TWO_PI, in1=t,
                                                          op0=ALU.mult, op1=ALU.add), r=['rk', 'rt'], w=['rt'])
            P.add('dve', lambda e: e.tensor_scalar(out=kf, in0=t, scalar1=PI, scalar2=-TWO_PI,
                                                   op0=ALU.is_gt, op1=ALU.mult), r=['rt'], w=['rk'])
            P.add('dve', lambda e: e.tensor_tensor(out=t, in0=t, in1=kf, op=ALU.add), r=['rt', 'rk'], w=['rt'])
            P.add('dve', lambda e: e.tensor_scalar(out=t, in0=t, scalar1=-PI, scalar2=PI,
                                                   op0=ALU.max, op1=ALU.min), r=['rt'], w=['rt'])
            P.add('act', lambda e, dst=dst: e.activation(out=dst[:], in_=t, func=AF.Sin), r=['rt'], w=[dkey])
        R.off = mark

    def phase(self):
        self.flush()
        self.P.barrier()
        self.R.reset()

    def attn_common(self):
        R = self.R
        self.Pt = [R.alloc([512], BF16) for _ in range(NPT)]
        self.U = [R.alloc([512], F32, parts=65) for _ in range(2)]
        self.rd16 = [R.alloc([512], BF16) for _ in range(2)]
        self.Rt = [R.alloc([512], F32, parts=64) for _ in range(2)]
        for i_ in range(2):
            self.P.add('pool', lambda e, i_=i_: e.memset(self.rd16[i_][:, :], 0.0), w=[('rd16', i_)])
        self.OT = [R.alloc([S], BF16, parts=64) for _ in range(2)]
        self.Wo = [R.alloc([D], BF16, parts=64) for _ in range(2)]
        self.wsl = Slots(self.P, 'wsl', [R.alloc([8, 128], BF16) for _ in range(4)], self.ds_w)

    def proj_fm(self, wt, wkey, ncols, evac, wcols=None):
        P = self.P
        for tc in range(NCH):
            pi = 6 + self.rot('PJ', 2)
            ps = self.ps[pi]
            for kc in range(8):
                lhsT = wt[:, kc, 0:ncols] if wcols is None else wt[:, kc, wcols[0]:wcols[1]]
                P.add('pe', lambda e, ps=ps, kc=kc, tc=tc, lhsT=lhsT: e.matmul(
                    ps[0:ncols, :], lhsT=lhsT, rhs=self.hnT[:, kc, tc * 512:(tc + 1) * 512],
                    start=(kc == 0), stop=(kc == 7)), r=[wkey] + self.hn_keys(tc), w=[('ps', pi)])
            evac(tc, ps, ('ps', pi))

    def proj_qk_plain(self, src_cols, dst, dkey, split=None):
        P = self.P
        wt, wk, wds = self.wsl.get()
        self.load_w(wt, wk, wds, src_cols)

        def evac(tc, ps, pk):
            sl = slice(tc * 512, (tc + 1) * 512)
            if split is None:
                P.add('act', lambda e: e.activation(out=dst[:, sl], in_=ps[:, :], func=AF.Copy), r=[pk], w=[(dkey, tc)])
            else:
                for hh in range(2):
                    rows = slice(64 * hh, 64 * hh + 64)
                    P.add('act', lambda e, hh=hh, rows=rows: e.activation(out=split[hh][rows, sl], in_=ps[rows, :], func=AF.Copy),
                          r=[pk], w=[(('QZ', hh), tc)])
        self.proj_fm(wt, wk, 128, evac)

    def proj_qk_rot(self, src_cols, src_sw_cols, dst, dkey, dup=False, split=None, ncols=128):
        P, R = self.P, self.R
        wt, wk, wds = self.wsl.get()
        wt2, wk2, wds2 = self.wsl.get()
        if dup:
            P.group(wds)
            self.load_w(wt[:, :, 0:64], wk, wds, src_cols)
            self.load_w(wt[:, :, 64:128], wk + 'b', wds, src_cols)
            P.endgroup(wds)
            P.group(wds2)
            self.load_w(wt2[:, :, 0:64], wk2, wds2, src_sw_cols)
            self.load_w(wt2[:, :, 64:128], wk2 + 'b', wds2, src_sw_cols)
            P.endgroup(wds2)
            wkeys, wkeys2 = [wk, wk + 'b'], [wk2, wk2 + 'b']
        else:
            self.load_w(wt[:, :, 0:ncols], wk, wds, src_cols)
            self.load_w(wt2[:, :, 0:ncols], wk2, wds2, src_sw_cols)
            wkeys, wkeys2 = [wk], [wk2]
        for tc in range(NCH):
            sl = slice(tc * 512, (tc + 1) * 512)
            pa, pb = ((6, 7), (0, 1), (2, 3))[self.rot('rotp', 3)]
            for (pi, w_, wkk) in ((pa, wt, wkeys), (pb, wt2, wkeys2)):
                ps = self.ps[pi]
                for kc in range(8):
                    P.add('pe', lambda e, ps=ps, kc=kc, w_=w_, sl=sl: e.matmul(
                        ps[0:ncols, :], lhsT=w_[:, kc, 0:ncols], rhs=self.hnT[:, kc, sl], start=(kc == 0), stop=(kc == 7)),
                        r=wkk + self.hn_keys(tc), w=[('ps', pi)])
            ti = self.rot('rt', 2)
            t1 = self.rtmp[ti]
            P.add('dve', lambda e, t1=t1, sl=sl, pa=pa: e.tensor_tensor(out=t1[0:ncols, :], in0=self.ps[pa][0:ncols, :], in1=self.cosT[0:ncols, sl],
                                                                 op=ALU.mult), r=[('ps', pa), 'cosT'], w=[('rtmp', ti)])
            if split is None:
                P.add('dve', lambda e, sl=sl, pb=pb: e.tensor_tensor(out=dst[0:ncols, sl], in0=self.ps[pb][0:ncols, :], in1=self.sinT[0:ncols, sl],
                                                              op=ALU.mult), r=[('ps', pb), 'sinT'], w=[(dkey, tc)])
                P.add('pool', lambda e, t1=t1, sl=sl: e.tensor_tensor(out=dst[0:ncols, sl], in0=dst[0:ncols, sl], in1=t1[0:ncols, :], op=ALU.add),
                      r=[('rtmp', ti), (dkey, tc)], w=[(dkey, tc)])
            else:
                t2 = self.rtmp2[ti]
                P.add('dve', lambda e, t2=t2, sl=sl, pb=pb: e.tensor_tensor(out=t2, in0=self.ps[pb][:, :], in1=self.sinT[:, sl],
                                                                     op=ALU.mult), r=[('ps', pb), 'sinT'], w=[('rtmp2', ti)])
                for hh in range(2):
                    rows = slice(64 * hh, 64 * hh + 64)
                    P.add('pool', lambda e, t1=t1, t2=t2, sl=sl, hh=hh, rows=rows: e.tensor_tensor(
                        out=split[hh][rows, sl], in0=t1[rows, :], in1=t2[rows, :], op=ALU.add),
                        r=[('rtmp', ti), ('rtmp2', ti)], w=[(('QZ', hh), tc)])

    def proj_v(self, src_cols, ncols, dst_fn, dkey, tok_fn=None, nblk=NB):
        P = self.P
        wt, wk, wds = self.wsl.get()
        self.load_w(wt[:, :, 0:ncols], wk, wds, src_cols)
        for g in range(nblk // 4):
            pi = 6 + self.rot('PJ', 2)
            ps = self.ps[pi]
            for bb in range(4):
                blk = 4 * g + bb
                tsl = slice(blk * 128, (blk + 1) * 128) if tok_fn is None else tok_fn(blk)
                for kc in range(8):
                    P.add('pe', lambda e, ps=ps, bb=bb, kc=kc, tsl=tsl: e.matmul(
                        ps[:, bb * ncols:(bb + 1) * ncols], lhsT=self.hnT[:, kc, tsl], rhs=wt[:, kc, 0:ncols],
                        start=(kc == 0), stop=(kc == 7)), r=[wk] + self.hn_keys(), w=[('ps', pi)])
            P.add('act', lambda e, ps=ps, g=g: e.activation(
                out=dst_fn(g), in_=ps[:, 0:4 * ncols].rearrange('p (b h d) -> p b h d', b=4, d=64), func=AF.Copy),
                r=[('ps', pi)], w=[(dkey, g)])

    def finalize(self, acc, acck, dst, dkey, gate=None, first=True, last=True, extra=None):
        P = self.P
        ui = self.rot('U', 2)
        U = self.U[ui]
        bc = self.ps[5]
        ones = self.c['c_ones']

        rd = self.rd16[ui]
        os16 = self.c['c_os']
        pend = [acck] if extra is None else [('ps', 3), ('ps', 4), ('ps', 6)]
        for k_ in pend:
            self.acc_pending[k_] = self.acc_gen_of(k_)
            self.acc_gen[k_] = self.acc_gen_of(k_) + 1

        Rt = self.Rt[ui]
        src_ = U[0:64, :] if extra is not None else acc[0:64, :]
        srck = ('U', ui) if extra is not None else acck

        def stage_a():
            if extra is None:
                P.add('act', lambda e: e.activation(out=rd[64:65, :], in_=acc[64:65, :], func=AF.Copy, bias=1e-30),
                      r=[acck], w=[('rd16', ui)])
            else:
                for k_ in pend:
                    self.acc_pending.pop(k_, None)
                extra(U, ('U', ui))
                P.add('dve', lambda e: e.tensor_scalar(out=rd[64:65, :], in0=U[64:65, :], scalar1=1e-30, scalar2=None,
                                                       op0=ALU.add), r=[('U', ui)], w=[('rd16', ui)])

        def stage_b():
            if extra is None:
                for k_ in pend:
                    self.acc_pending.pop(k_, None)
            P.add('pe', lambda e: e.matmul(bc[0:64, :], lhsT=os16[:, 0:64], rhs=rd[:, :], start=True, stop=True),
                  r=[('rd16', ui), 'c_os'], w=[('ps', 5)])
            P.add('act', lambda e: e.activation(out=bc[0:64, :], in_=bc[0:64, :], func=AF.Ln), r=[('ps', 5)], w=[('ps', 5)])
            P.add('act', lambda e: e.activation(out=Rt[0:64, :], in_=bc[0:64, :], func=AF.Exp, scale=-1.0),
                  r=[('ps', 5)], w=[('Rt', ui)])
            if gate is None:
                P.add('dve', lambda e: e.tensor_tensor(out=dst, in0=src_, in1=Rt[0:64, :], op=ALU.mult),
                      r=[srck, ('Rt', ui)], w=[dkey])
            else:
                P.add('dve', lambda e: e.tensor_tensor(out=U[0:64, :], in0=src_, in1=Rt[0:64, :], op=ALU.mult),
                      r=[srck, ('Rt', ui)], w=[('U', ui)])

        bg = self.ps[7]

        def stage_c():
            if first:
                P.add('dve', lambda e: e.tensor_tensor(out=self.gacc[0:64, :], in0=U[0:64, :], in1=bg[0:64, :], op=ALU.mult),
                      r=[('U', ui), ('ps', 7)], w=['gacc'])
            else:
                P.add('dve', lambda e: e.tensor_tensor(out=U[0:64, :], in0=U[0:64, :], in1=bg[0:64, :], op=ALU.mult),
                      r=[('U', ui), ('ps', 7)], w=[('U', ui)])
                if last:
                    P.add('dve', lambda e: e.tensor_tensor(out=dst, in0=U[0:64, :], in1=self.gacc[0:64, :], op=ALU.add),
                          r=[('U', ui), 'gacc'], w=[dkey])
                else:
                    P.add('dve', lambda e: e.tensor_tensor(out=self.gacc[0:64, :], in0=U[0:64, :], in1=self.gacc[0:64, :],
                                                           op=ALU.add), r=[('U', ui), 'gacc'], w=['gacc'])

        def stage_g():
            hc, sl = gate
            Z = self.c['c_Z']
            P.add('pe', lambda e: e.matmul(bg[0:64, :], lhsT=Z[:, hc * 64:hc * 64 + 64], rhs=self.sigT[:, sl],
                                           start=True, stop=True), r=['sigT', 'c_Z'], w=[('ps', 7)])
        def first_stage():
            assert not self.u_busy[ui], "finalize scratch slot reused too early"
            self.u_busy[ui] = True
            stage_a()

        def last_stage():
            (stage_c if gate is not None else stage_b)()
            self.u_busy[ui] = False
        self.defer(first_stage, PD + 1, fin=True)
        if gate is not None:
            g_delay = max(PD + 2, self.gate_free_at - self.tick)
            c_delay = max(PD + 5, g_delay + 2)
            self.gate_free_at = self.tick + c_delay
            self.defer(stage_g, g_delay, fin=True)
            self.defer(stage_b, PD + 3, fin=True)
            self.defer(last_stage, c_delay, fin=True)
        else:
            self.defer(last_stage, PD + 3, fin=True)

    def outproj_pair(self, w_out, pair, ot_keys):
        P = self.P
        self.flush()
        wkeys = []
        for hh in range(2):
            r0 = (2 * pair + hh) * 64
            src = w_out[r0:r0 + 64, :]
            P.add('pool', lambda e, hh=hh, src=src: e.dma_start(out=self.Wo[hh][:, :], in_=src), w=[('Wo', hh)],
                  dsem=self.ds_wo[hh])
            wkeys.append(('Wo', hh))
        for b in range(NB):
            for half in range(2):
                pi = 6 + self.rot('PJ', 2)
                ps = self.ps[pi]
                for hh in range(2):
                    P.add('pe', lambda e, ps=ps, hh=hh, b=b, half=half: e.matmul(
                        ps[:, :], lhsT=self.OT[hh][0:64, b * 128:(b + 1) * 128],
                        rhs=self.Wo[hh][0:64, half * 512:(half + 1) * 512], start=(hh == 0), stop=(hh == 1)),
                        r=[('Wo', hh)] + ot_keys[hh], w=[('ps', pi)])
                hsl = self.h[:, b, half * 512:(half + 1) * 512]
                P.add('dve', lambda e, ps=ps, hsl=hsl: e.tensor_tensor(out=hsl, in0=ps[:, :], in1=hsl, op=ALU.add),
                      r=[('ps', pi), ('h', b, half)], w=[('h', b, half)])

    def l0_mixer(self, s):
        P, R = self.P, self.R
        self.phase()
        self.attn_common()
        self.rtmp = [R.alloc([512], F32) for _ in range(2)]
        self.rtmp2 = [R.alloc([512], F32) for _ in range(2)]
        mark0 = R.off
        Qz = [R.alloc([S], BF16) for _ in range(2)]
        KT = R.alloc([S], BF16)
        Vt = R.alloc([NB, 2, 65], BF16)
        P.add('pool', lambda e: e.memset(Qz[0][64:128, :], 0.0), w=['qz0'])
        P.add('pool', lambda e: e.memset(Qz[1][0:64, :], 0.0), w=['qz1'])
        w_in = self.fd_w_in
        fb = R.alloc([1, 8], F32)
        lg = R.alloc([NB, 8], F32)
        cl = R.alloc([NB, 8], F32)
        offs = R.alloc([NB, 8], F32)
        tot = R.alloc([NB, 8], F32)
        Bt = R.alloc([8, NB, NB], F32)
        P.add('sp', lambda e: e.dma_start(out=fb[:, 0, :], in_=self.fd_forget_b[0, :].partition_broadcast(128)),
              w=['fb'], dsem=self.ds_misc[1])
        wf, wfk, wfds = self.wsl.get()
        self.load_w(wf[:, :, 0:8], wfk, wfds, w_in[:, 3072:3080])
        psf = self.ps[4]
        for b in range(NB):
            for kc in range(8):
                P.add('pe', lambda e, b=b, kc=kc: e.matmul(psf[:, b * 8:(b + 1) * 8],
                                                           lhsT=self.hnT[:, kc, b * 128:(b + 1) * 128], rhs=wf[:, kc, 0:8],
                                                           start=(kc == 0), stop=(kc == 7)),
                      r=[wfk] + self.hn_keys(), w=[('ps', 4)])
        P.add('dve', lambda e: e.tensor_tensor(out=lg, in0=psf[:, 0:128].rearrange('p (b h) -> p b h', h=8),
                                               in1=fb[:, 0:1, :].broadcast_to([128, NB, 8]), op=ALU.add),
              r=[('ps', 4), 'fb'], w=['lg'])
        P.add('act', lambda e: e.activation(out=lg, in_=lg, func=AF.Exp, scale=-1.0), r=['lg'], w=['lg'])
        P.add('dve', lambda e: e.tensor_scalar(out=lg, in0=lg, scalar1=1.0, scalar2=None, op0=ALU.add), r=['lg'], w=['lg'])
        P.add('act', lambda e: e.activation(out=lg, in_=lg, func=AF.Ln), r=['lg'], w=['lg'])
        lg2 = lg.rearrange('p b h -> p (b h)')
        P.add('pe', lambda e: e.matmul(self.ps[6][:, 0:128], lhsT=self.c['c_utri'][:], rhs=lg2, start=True, stop=True),
              r=['lg', 'c_utri'], w=[('ps', 6)])
        P.add('pe', lambda e: e.matmul(self.ps[7][:, 0:128], lhsT=self.c['c_ones'][:], rhs=lg2, start=True, stop=True),
              r=['lg', 'c_ones'], w=[('ps', 7)])
        P.add('act', lambda e: e.activation(out=tot, in_=self.ps[7][:, 0:128].rearrange('p (b h) -> p b h', h=8),
                                            func=AF.Copy), r=[('ps', 7)], w=['tot'])
        P.add('pool', lambda e: e.memset(offs[:, 0, :], 0.0), w=[('offs', 0)])
        for j in range(1, NB):
            P.add('dve', lambda e, j=j: e.tensor_tensor(out=offs[:, j, :], in0=offs[:, j - 1, :], in1=tot[:, j - 1, :],
                                                        op=ALU.add), r=[('offs', j - 1), 'tot'], w=[('offs', j)])
        allo = [('offs', j) for j in range(NB)]
        P.add('dve', lambda e: e.tensor_tensor(out=cl, in0=self.ps[6][:, 0:128].rearrange('p (b h) -> p b h', h=8),
                                               in1=offs, op=ALU.add), r=[('ps', 6)] + allo, w=['cl'])
        for hh in range(8):
            for qb in range(NB):
                P.add('dve', lambda e, hh=hh, qb=qb: e.tensor_scalar(
                    out=Bt[:, hh, qb, :], in0=cl[:, :, hh], scalar1=offs[:, qb, hh:hh + 1], scalar2=None,
                    op0=ALU.subtract), r=['cl'] + allo, w=[('Bt', hh)])
        wm = self.c['c_wm']
        for hp in range(4):
            c0 = hp * 128
            self.proj_qk_plain(w_in[:, c0:c0 + 128], None, None, split=Qz)
            self.proj_qk_plain(w_in[:, 512 + c0:512 + c0 + 128], KT, 'KT')
            P.add('pool', lambda e: e.memset(Vt[:, :, :, 64:65], 1.0), w=['Vones'])
            self.proj_v(w_in[:, 1024 + c0:1024 + c0 + 128], 128, lambda g: Vt[:, 4 * g:4 * g + 4, :, 0:64], 'Vt')
            qk = [('QT', t) for t in range(NCH)]
            kk = [('KT', t) for t in range(NCH)]
            ot_keys = []
            for hh in range(2):
                head = 2 * hp + hh
                rows = slice(64 * hh, 64 * hh + 64)
                for i in range(NCH):
                    ai = (3, 4, 6)[self.rot('ACC', 3)]
                    acc = self.ps[ai]
                    nkb = 4 * i + 4
                    for kb in range(nkb):
                        self.step()
                        si = self.rot('S', 3)
                        ps = self.ps[si]
                        P.add('pe', lambda e, ps=ps, kb=kb, i=i, hh=hh: e.matmul(
                            ps[:, :], lhsT=KT[:, kb * 128:(kb + 1) * 128], rhs=Qz[hh][:, i * 512:(i + 1) * 512],
                            start=True, stop=True), r=[('KT', kb // 4), (('QZ', hh), i), 'qz0', 'qz1'], w=[('ps', si)])
                        pi = self.rot('Pt', NPT)
                        pt = self.Pt[pi]
                        r0 = max(0, kb - 4 * i)
                        pkeys = []
                        for rr in range(r0, 4):
                            qb = 4 * i + rr
                            P.add('act', lambda e, ps=ps, pt=pt, rr=rr, qb=qb, kb=kb, head=head: e.activation(
                                out=pt[:, rr * 128:(rr + 1) * 128], in_=ps[:, rr * 128:(rr + 1) * 128], func=AF.Exp,
                                scale=0.125, bias=Bt[:, head, qb, kb:kb + 1]),
                                r=[('ps', si), ('Bt', head)], w=[('Pt', pi, rr)])
                            pkeys.append(('Pt', pi, rr))
                        c_lo = r0 * 128
                        if kb >= 4 * i:
                            r = kb - 4 * i
                            P.add('dve', lambda e, pt=pt, r=r, c_lo=c_lo: e.tensor_tensor(
                                out=pt[:, c_lo:512], in0=pt[:, c_lo:512], in1=wm[:, 384 - 128 * r + c_lo:384 - 128 * r + 512],
                                op=ALU.mult), r=pkeys + ['c_wm'], w=pkeys)
                        def back(acc=acc, pt=pt, kb=kb, hh=hh, c_lo=c_lo, st=(kb == 0), sp=(kb == nkb - 1), pkeys=pkeys, ai=ai,
                                 gen=self.acc_gen_of(('ps', ai))):
                            self.acc_start(('ps', ai), gen)
                            P.add('pe', lambda e: e.matmul(
                                acc[0:65, c_lo:512], lhsT=Vt[:, kb, hh, :], rhs=pt[:, c_lo:512], start=st, stop=sp),
                                r=pkeys + [('Vt', kb // 4), 'Vones'], w=[('ps', ai)])
                        self.defer(back, PD)
                    self.finalize(acc, ('ps', ai), self.OT[hh][0:64, i * 512:(i + 1) * 512], ('OT', hh, i))
                ot_keys.append([('OT', hh, i) for i in range(NCH)])
            self.outproj_pair(self.fd_w_out, hp, ot_keys)
        self.flush()
        P.barrier()
        R.off = mark0
        Qz = [R.alloc([S], BF16) for _ in range(2)]
        KT = R.alloc([S], BF16)
        P.add('pool', lambda e: e.memset(Qz[0][64:128, :], 0.0), w=['qz0'])
        P.add('pool', lambda e: e.memset(Qz[1][0:64, :], 0.0), w=['qz1'])
        Vd = [R.alloc([NB, 2, 65], BF16) for _ in range(3)]
        self.Vd = Vd
        dm3 = self.c['c_dm'][:, :].rearrange('p (a b) -> p a b', b=128)
        for dp in range(4):
            c0 = dp * 128
            self.proj_qk_rot(w_in[:, 1536 + c0:1536 + c0 + 128], self.fd_w_sw[:, c0:c0 + 128], None, None, split=Qz)
            self.proj_qk_rot(w_in[:, 2048 + c0:2048 + c0 + 128], self.fd_w_sw[:, 512 + c0:512 + c0 + 128], KT, 'KT')
            for pat in range(3):
                P.add('pool', lambda e, pat=pat: e.memset(Vd[pat][:, :, :, 64:65], 1.0), w=[('Vones', pat)])
            vsrc = w_in[:, 2560 + c0:2560 + c0 + 128]
            self.proj_v(vsrc, 128, lambda g: Vd[0][:, 4 * g:4 * g + 4, :, 0:64], ('Vd', 0))
            self.proj_v(vsrc, 128, lambda g: Vd[1][:, 4 * g:4 * g + 4, :, 0:64], ('Vd', 1),
                        tok_fn=lambda blk: ssl((blk // 4) + 512 * (blk % 4), 128, 4))
            self.proj_v(vsrc, 128, lambda g: Vd[2][:, 4 * g:4 * g + 4, :, 0:64], ('Vd', 2),
                        tok_fn=lambda blk: ssl(blk, 128, 16))
            ot_keys = []
            for hh in range(2):
                rows = slice(64 * hh, 64 * hh + 64)
                for i in range(NCH):
                    acc1, acc4, acc16 = self.ps[3], self.ps[4], self.ps[6]
                    for g in range(2):
                        slots = []
                        for qb in (4 * i + 2 * g, 4 * i + 2 * g + 1):
                            hasp = qb >= 1
                            kb = max(qb - 1, 0)
                            qsl = slice(qb * 128, qb * 128 + 128)
                            slots.append((qsl, qsl, (acc1, ('ps', 3), (qb - 4 * i) * 128, 0, qb, True, not hasp)))
                            slots.append((slice(kb * 128, kb * 128 + 128), qsl,
                                          (acc1, ('ps', 3), (qb - 4 * i) * 128, 0, kb, False, True) if hasp else None))
                        self.dil_group(KT, Qz[hh], rows, hh, slots, dm3, ['c_dm'], 128)
                    for g in range(2):
                        slots = []
                        for r_ in (2 * g, 2 * g + 1):
                            ip = max(i - 1, 0)
                            qsl = ssl(r_ + 512 * i, 128, 4)
                            ksl = ssl(r_ + 512 * ip, 128, 4)
                            slots.append((qsl, qsl, (acc4, ('ps', 4), r_ * 128, 1, r_ * 4 + i, True, i == 0)))
                            slots.append((ksl, qsl, (acc4, ('ps', 4), r_ * 128, 1, r_ * 4 + ip, False, True) if i >= 1 else None))
                        self.dil_group(KT, Qz[hh], rows, hh, slots, dm3, ['c_dm'], 128)
                    slots = []
                    for r_ in range(16):
                        slots.append((ssl(r_, 128, 16), ssl(r_ + 512 * i, 32, 16),
                                      (acc16, ('ps', 6), r_ * 32, 2, r_, True, True)))
                    m16 = self.c['c_wm'][:, 384 + 32 * i:416 + 32 * i].unsqueeze(1).broadcast_to([128, 16, 32])
                    self.dil_group(KT, Qz[hh], rows, hh, slots, m16, ['c_wm'], 32)

                    def extra(U, uk):
                        P.add('act', lambda e: e.activation(out=U[0:65, :], in_=self.ps[3][0:65, :], func=AF.Copy),
                              r=[('ps', 3)], w=[uk])
                        P.add('dve', lambda e: e.tensor_tensor(
                            out=U[0:65, :].rearrange('p (q r) -> p r q', r=4),
                            in0=self.ps[4][0:65, :].rearrange('p (r q) -> p r q', r=4),
                            in1=U[0:65, :].rearrange('p (q r) -> p r q', r=4), op=ALU.add), r=[('ps', 4), uk], w=[uk])
                        P.add('dve', lambda e: e.tensor_tensor(
                            out=U[0:65, :].rearrange('p (q r) -> p r q', r=16),
                            in0=self.ps[6][0:65, :].rearrange('p (r q) -> p r q', r=16),
                            in1=U[0:65, :].rearrange('p (q r) -> p r q', r=16), op=ALU.add), r=[('ps', 6), uk], w=[uk])
                    self.finalize(None, None, self.OT[hh][0:64, i * 512:(i + 1) * 512], ('OT', hh, i), extra=extra)
                ot_keys.append([('OT', hh, i) for i in range(NCH)])
            self.outproj_pair(self.fd_w_out, 4 + dp, ot_keys)

    def dil_group(self, KT, QT, rows, hh, slots, mask_ap, mkeys, width):
        P = self.P
        Vd = self.Vd
        allq = [(('QZ', hh), t) for t in range(NCH)] + ['qz0', 'qz1']
        allk = [('KT', t) for t in range(NCH)]
        self.step()
        si = self.rot('Sd', 3)
        ps = self.ps[si]
        for j, (ksl, qsl, pv) in enumerate(slots):
            P.add('pe', lambda e, j=j, ksl=ksl, qsl=qsl: e.matmul(
                ps[:, j * width:(j + 1) * width], lhsT=KT[:, ksl], rhs=QT[:, qsl],
                start=True, stop=True), r=allk + allq, w=[('ps', si)])
        pi = self.rot('Pt', NPT)
        pt = self.Pt[pi]
        P.add('act', lambda e: e.activation(out=pt[:, :], in_=ps[:, :], func=AF.Exp, scale=0.125),
              r=[('ps', si)], w=[('Pt', pi, 0)])
        ptv = pt[:, :].rearrange('p (a b) -> p a b', b=width)
        P.add('dve', lambda e: e.tensor_tensor(out=ptv, in0=ptv, in1=mask_ap, op=ALU.mult),
              r=[('Pt', pi, 0)] + mkeys, w=[('Pt', pi, 0)])
        gens = {k_: self.acc_gen_of(k_) for k_ in (('ps', 3), ('ps', 4), ('ps', 6))}

        def back():
            for j, (ksl, qsl, pv) in enumerate(slots):
                if pv is None:
                    continue
                acc, ak, col0, vpat, vblk, st, sp = pv
                self.acc_start(ak, gens[ak])
                vkeys = [(('Vd', vpat), g) for g in range(4)] + [('Vones', vpat)]
                P.add('pe', lambda e, j=j, acc=acc, col0=col0, vpat=vpat, vblk=vblk, st=st, sp=sp: e.matmul(
                    acc[0:65, col0:col0 + width], lhsT=Vd[vpat][:, vblk, hh, :], rhs=pt[:, j * width:(j + 1) * width],
                    start=st, stop=sp), r=[('Pt', pi, 0)] + vkeys, w=[ak])
        self.defer(back, PD)

    def dump_h(self, s):
        P = self.P
        for b in range(NB):
            o = P.add('sp', lambda e, b=b: e.dma_start(out=self.y[s, b * 128:(b + 1) * 128, :], in_=self.h[:, b, :]),
                      r=self.hk(b), w=[('y', s, b)], dsem=self.ds_y)
        self.P.final.append(o)

    def build(self):
        P = self.P
        st = self.stages
        self.load_consts()
        for s in range(self.n_seq):
            self.phase()
            self.load_seq(s)
            self.rotary_tables(s)
            if 'l0' in st:
                self.phase()
                self.norm(self.mix_norm[0:1, :])
                self.l0_mixer(s)
                if 'l0ffn' in st:
                    self.phase()
                    self.norm(self.ffn_norm[0:1, :])
                    self.phase()
                    self.ffn_setup()
                    if 'noffn' not in st:
                        self.ffn(self.dense_w_gate, self.dense_w_up, self.dense_w_down, None)
                    if 'nople' not in st:
                        self.ple(s, 0)
            if 'l1' in st:
                self.phase()
                self.norm(self.mix_norm[1:2, :])
                self.nsa_mixer(s)
                if 'l1ffn' in st:
                    self.moe()
                    self.ple(s, 1)
            if 'final' in st:
                self.phase()
                self.norm(self.final_norm[0:1, :], final_out=s)
            else:
                self.dump_h(s)
        if 'final' in st:
            last = [o for o in P.by_eng['sp'] if o.dsem is self.ds_y][-1]
            P.final.append(last)
        P.emit()
        return self.nc


SWAP64 = np.concatenate([np.arange(32, 64), np.arange(0, 32)])


def _swap_cols(w):
    d, n = w.shape
    idx = (np.arange(n) // 64) * 64 + SWAP64[np.arange(n) % 64]
    return np.ascontiguousarray(w[:, idx])


def host_inputs(inputs, n_cores, n_seq, consts):
    f = lambda a: np.ascontiguousarray(np.asarray(a, dtype=np.float32))
    shared = {}
    for k in ('mix_norm', 'ffn_norm', 'ple_norm', 'ple_gate_w', 'ple_proj_w'):
        shared[k] = f(inputs[k])
    shared['final_norm'] = f(inputs['final_norm']).reshape(1, D)
    for k in ('fd_w_in', 'fd_forget_b', 'fd_w_out', 'dense_w_gate', 'dense_w_up', 'dense_w_down', 'nsa_w_in',
              'nsa_pos_k', 'nsa_w1_k', 'nsa_w2_k', 'nsa_pos_v', 'nsa_w1_v', 'nsa_w2_v', 'nsa_w_out',
              'moe_w_router', 'moe_b_router', 'moe_w_gate', 'moe_w_up', 'moe_w_down'):
        shared[k] = f(inputs[k])[0]
    shared['nsa_b1_k'] = f(inputs['nsa_b1_k'])[0].reshape(256, 1)
    shared['nsa_b1_v'] = f(inputs['nsa_b1_v'])[0].reshape(256, 1)
    shared['fd_w_sw'] = _swap_cols(shared['fd_w_in'][:, 1536:2560])
    nw = shared['nsa_w_in']
    shared['nsa_w_sw'] = _swap_cols(np.concatenate([nw[:, 0:1280], nw[:, 1536:1792], nw[:, 2048:2304]], axis=1))
    shared.update(consts)
    x = f(inputs['x'])
    p = f(inputs['p'])
    pos = np.ascontiguousarray(np.asarray(inputs['positions'], dtype=np.int32))
    maps = []
    for c in range(n_cores):
        m = dict(shared)
        m['x'] = np.ascontiguousarray(x[c * n_seq:(c + 1) * n_seq])
        m['p'] = np.ascontiguousarray(p[:, c * n_seq:(c + 1) * n_seq])
        m['positions'] = np.ascontiguousarray(pos[c * n_seq:(c + 1) * n_seq])
        maps.append(m)
    return maps


_CACHE = {}


def kernel(**inputs):
    n_cores, n_seq = 8, 2
    if 'nc' not in _CACHE:
        b = Builder(n_seq=n_seq, stages=('l0', 'l0ffn', 'l1', 'l1ffn', 'final'))
        _CACHE['nc'] = b.build()
        _CACHE['b'] = b
    nc = _CACHE['nc']
    maps = host_inputs(inputs, n_cores, n_seq, make_consts())
    res = run_bass_kernel_spmd(nc, maps, core_ids=list(range(n_cores)))
    out = np.concatenate([np.asarray(r['y'], dtype=np.float32) for r in res.results], axis=0)
    return out


def _ffn_setup(self):
    R = self.R
    self.Wg = [R.alloc([8, 512], BF16) for _ in range(2)]
    self.Wu = [R.alloc([8, 512], BF16) for _ in range(2)]
    self.Wd = [R.alloc([4, D], BF16) for _ in range(2)]
    self.At = [R.alloc([4, 512], BF16) for _ in range(2)]
    self.Sg = [R.alloc([512], F32) for _ in range(2)]


def _ffn(self, wg, wu, wd, comb=None):
    P, R = self.P, self.R
    for fg in range(7):
        wi = self.rot('ffnw', 2)
        Wg, Wu, Wd = self.Wg[wi], self.Wu[wi], self.Wd[wi]
        fsl = slice(fg * 512, (fg + 1) * 512)
        self.load_w(Wg, ('Wg', wi), self.ds_ffn[wi], wg[:, fsl])
        self.load_w(Wu, ('Wu', wi), self.ds_ffn[2 + wi], wu[:, fsl])
        self.load_w(Wd, ('Wd', wi), self.ds_ffn[4 + wi], wd[fsl, :])
        for tc in range(NCH):
            ai = self.rot('At', 2)
            At = self.At[ai]
            for fc in range(4):
                gi = self.rot('Gp', 2)
                ui = 2 + self.rot('Up', 2)
                for (pi, W_, wk) in ((gi, Wg, ('Wg', wi)), (ui, Wu, ('Wu', wi))):
                    ps = self.ps[pi]
                    for kc in range(8):
                        P.add('pe', lambda e, ps=ps, W_=W_, kc=kc, fc=fc, tc=tc: e.matmul(
                            ps[:, :], lhsT=W_[:, kc, fc * 128:(fc + 1) * 128], rhs=self.hnT[:, kc, tc * 512:(tc + 1) * 512],
                            start=(kc == 0), stop=(kc == 7)), r=[wk] + self.hn_keys(tc), w=[('ps', pi)])
                si = self.rot('Sg', 2)
                Sg = self.Sg[si]
                P.add('act', lambda e, Sg=Sg, gi=gi: e.activation(out=Sg, in_=self.ps[gi][:, :], func=AF.Silu),
                      r=[('ps', gi)], w=[('Sg', si)])
                P.add('dve', lambda e, Sg=Sg, ui=ui, At=At, fc=fc: e.tensor_tensor(
                    out=At[:, fc, :], in0=self.ps[ui][:, :], in1=Sg, op=ALU.mult),
                    r=[('ps', ui), ('Sg', si)], w=[('At', ai, fc)])
            akeys = [('At', ai, fc) for fc in range(4)]
            for bb in range(4):
                b = 4 * tc + bb
                for half in range(2):
                    yi = 4 + self.rot('Yp', 3)
                    ps = self.ps[yi]
                    for fc in range(4):
                        P.add('pe', lambda e, ps=ps, At=At, Wd=Wd, fc=fc, bb=bb, half=half: e.matmul(
                            ps[:, :], lhsT=At[:, fc, bb * 128:(bb + 1) * 128], rhs=Wd[:, fc, half * 512:(half + 1) * 512],
                            start=(fc == 0), stop=(fc == 3)), r=akeys + [('Wd', wi)], w=[('ps', yi)])
                    hsl = self.h[:, b, half * 512:(half + 1) * 512]
                    if comb is None:
                        P.add('dve', lambda e, ps=ps, hsl=hsl: e.tensor_tensor(out=hsl, in0=ps[:, :], in1=hsl, op=ALU.add),
                              r=[('ps', yi), ('h', b, half)], w=[('h', b, half)])
                    else:
                        P.add('dve', lambda e, ps=ps, hsl=hsl, b=b: e.scalar_tensor_tensor(
                            out=hsl, in0=ps[:, :], scalar=comb[:, b:b + 1], in1=hsl, op0=ALU.mult, op1=ALU.add),
                            r=[('ps', yi), ('h', b, half), 'comb'], w=[('h', b, half)])


def _ple(self, s, layer):
    P, R = self.P, self.R
    self.phase()
    self.norm(self.ple_norm[layer:layer + 1, :])
    self.phase()
    Wpg = R.alloc([8, D], BF16)
    Wpp = R.alloc([2, D], BF16)
    p32 = R.alloc([NB, 256], F32)
    pT = R.alloc([2, S], BF16)
    sg = [R.alloc([512], F32) for _ in range(2)]
    self.load_w(Wpg, 'Wpg', self.ds_ffn[0], self.ple_gate_w[layer])
    self.load_w(Wpp, 'Wpp', self.ds_ffn[1], self.ple_proj_w[layer])
    P.add('sp', lambda e: e.dma_start(out=p32, in_=self.p[layer, s].rearrange('(b p) c -> p b c', p=128)),
          w=['p32'], dsem=self.ds_misc[2])
    ident = self.c['c_ident']
    for g in range(NB // 2):
        pi = 6 + self.rot('PJ', 2)
        ps = self.ps[pi]
        for bb in range(2):
            b = 2 * g + bb
            for c2 in range(2):
                j = bb * 2 + c2
                P.add('pe', lambda e, ps=ps, j=j, b=b, c2=c2: e.transpose(
                    out=ps[:, j * 128:(j + 1) * 128], in_=p32[:, b, c2 * 128:(c2 + 1) * 128], identity=ident[:]),
                    r=['p32', 'c_ident'], w=[('ps', pi)])
        P.add('act', lambda e, ps=ps, g=g: e.activation(
            out=pT[:, :, g * 256:(g + 1) * 256].rearrange('p c (b t) -> p b c t', b=2),
            in_=ps[:, :].rearrange('p (b c t) -> p b c t', b=2, c=2), func=AF.Copy), r=[('ps', pi)], w=[('pT', g)])
    for b in range(NB):
        for half in range(2):
            gi = self.rot('Gp', 2)
            ui = 2 + self.rot('Up', 2)
            csl = slice(half * 512, (half + 1) * 512)
            for kc in range(8):
                P.add('pe', lambda e, gi=gi, kc=kc, b=b, csl=csl: e.matmul(
                    self.ps[gi][:, :], lhsT=self.hnT[:, kc, b * 128:(b + 1) * 128], rhs=Wpg[:, kc, csl],
                    start=(kc == 0), stop=(kc == 7)), r=['Wpg', ('hnT', b)], w=[('ps', gi)])
            for c2 in range(2):
                P.add('pe', lambda e, ui=ui, c2=c2, b=b, csl=csl: e.matmul(
                    self.ps[ui][:, :], lhsT=pT[:, c2, b * 128:(b + 1) * 128], rhs=Wpp[:, c2, csl],
                    start=(c2 == 0), stop=(c2 == 1)), r=['Wpp', ('pT', b // 2)], w=[('ps', ui)])
            si = self.rot('Sg', 2)
            P.add('act', lambda e, si=si, gi=gi: e.activation(out=sg[si], in_=self.ps[gi][:, :], func=AF.Sigmoid),
                  r=[('ps', gi)], w=[('sg', si)])
            P.add('dve', lambda e, si=si, ui=ui: e.tensor_tensor(out=sg[si], in0=self.ps[ui][:, :], in1=sg[si], op=ALU.mult),
                  r=[('ps', ui), ('sg', si)], w=[('sg', si)])
            hsl = self.h[:, b, csl]
            P.add('pool', lambda e, si=si, hsl=hsl: e.tensor_tensor(out=hsl, in0=hsl, in1=sg[si], op=ALU.add),
                  r=[('sg', si), ('h', b, half)], w=[('h', b, half)])


Builder.ffn = _ffn
Builder.ffn_setup = _ffn_setup
Builder.ple = _ple


def _unit(self, lhsT, rhs, rkeys, mask_ap, mkeys, v_ap, vkeys, acc, ai, st, sp, aug=None):
    P = self.P
    self.step()
    si = self.rot('S', 3)
    ps = self.ps[si]
    P.add('pe', lambda e: e.matmul(ps[:, :], lhsT=lhsT, rhs=rhs, start=True, stop=(aug is None)), r=rkeys, w=[('ps', si)])
    if aug is not None:
        l2, r2, k2 = aug
        P.add('pe', lambda e: e.matmul(ps[:, :], lhsT=l2, rhs=r2, start=False, stop=True), r=k2, w=[('ps', si)])
    pi = self.rot('Pt', NPT)
    pt = self.Pt[pi]
    P.add('act', lambda e: e.activation(out=pt[:, :], in_=ps[:, :], func=AF.Exp, scale=0.125), r=[('ps', si)], w=[('Pt', pi, 0)])
    if mask_ap is not None:
        meng = 'pool' if self.rot('meng', 3) == 2 else 'dve'
        P.add(meng, lambda e: e.tensor_tensor(out=pt[:, :], in0=pt[:, :], in1=mask_ap, op=ALU.mult),
              r=[('Pt', pi, 0)] + mkeys, w=[('Pt', pi, 0)])
    gen = self.acc_gen_of(('ps', ai))

    def back():
        self.acc_start(('ps', ai), gen)
        P.add('pe', lambda e: e.matmul(acc[0:65, :], lhsT=v_ap, rhs=pt[:, :], start=st, stop=sp),
              r=[('Pt', pi, 0)] + vkeys, w=[('ps', ai)])
    self.defer(back, PD)


def _nsa_mixer(self, s):
    P, R = self.P, self.R
    self.phase()
    self.attn_common()
    self.rtmp = [R.alloc([512], F32) for _ in range(2)]
    self.sigT = R.alloc([S], BF16)
    P.add('pool', lambda e: e.memset(self.sigT[:, :], 0.0), w=['sigT'])
    self.gacc = R.alloc([512], F32, parts=64)
    Qz = [R.alloc([S], BF16) for _ in range(4)]
    KA = R.alloc([S], BF16)
    KB = R.alloc([S], BF16)
    Vs = R.alloc([NB, 1, 65], BF16)
    Vw = R.alloc([NB, 1, 65], BF16)
    W1c = R.alloc([8, 256], BF16, parts=64)
    W2k = R.alloc([2, 64], BF16)
    W2v = R.alloc([2, 64], BF16)
    gel = [R.alloc([128], BF16) for _ in range(2)]
    xs = R.alloc([128], F32)
    x2 = R.alloc([128], F32)
    hb = R.alloc([2], F32)
    b1t = R.alloc([2], F32)
    pos32 = R.alloc([64], F32, parts=32)
    posT = R.alloc([32], BF16, parts=64)
    kcmpT = R.alloc([128], BF16)
    vcmp = R.alloc([65], BF16)
    E4 = R.alloc([4, 128], F32)
    pcs = R.alloc([128], F32)
    den4 = R.alloc([4], F32)
    imp = R.alloc([32], F32)
    impm = R.alloc([32], F32)
    top8 = R.alloc([8], F32)
    sel = R.alloc([96], F32)
    w_in, w_sw = self.nsa_w_in, self.nsa_w_sw
    ident = self.c['c_ident']
    wm = self.c['c_wm']
    wt, wk, wds = self.wsl.get()
    self.load_w(wt[:, :, 0:48], wk, wds, w_in[:, 2560:2608])

    def evac_g(tc, ps, pk):
        P.add('act', lambda e: e.activation(out=self.sigT[0:48, tc * 512:(tc + 1) * 512], in_=ps[0:48, :], func=AF.Sigmoid),
              r=[pk], w=['sigT'])
    self.proj_fm(wt, wk, 48, evac_g)
    if NSA_STOP == 1:
        return
    P.add('pool', lambda e: e.memset(kcmpT[:, :], 0.0), w=['kcmpT'])
    P.add('pool', lambda e: e.memset(sel[:, :], 0.0), w=['sel'])
    for hh in range(4):
        P.add('pool', lambda e, hh=hh: e.memset(Qz[hh][64:128, :], 0.0), w=[(('NT', hh), t) for t in range(NCH)])
    P.add('pool', lambda e: e.memset(KA[64:128, :], 0.0), w=['KAz'])
    P.add('pool', lambda e: e.memset(KB[64:128, :], 0.0), w=['KBz'])
    P.add('sp', lambda e: e.dma_start(out=KA[64:96, :], in_=self.cst['c_E'][0:32, :]), r=['KAz'], w=['KAe'], dsem=self.ds_misc[0])
    P.add('pool', lambda e: e.memset(vcmp[:, :], 0.0), w=['vcmp'])
    P.add('pool', lambda e: e.memset(Vs[:, :, :, 64:65], 1.0), w=['Vs1'])
    P.add('pool', lambda e: e.memset(Vw[:, :, :, 64:65], 1.0), w=['Vw1'])
    P.add('pool', lambda e: e.memset(gel[0][:, :], 0.0), w=[('gel', 0)])
    P.add('pool', lambda e: e.memset(gel[1][:, :], 0.0), w=[('gel', 1)])
    allq = [[(('QZ', hh), t) for t in range(NCH)] for hh in range(4)]
    KAk = [('KA', t) for t in range(NCH)]
    KBk = [('KB', t) for t in range(NCH)]
    for g in range(4):
        for hh in range(4):
            c0 = (4 * g + hh) * 64
            self.proj_qk_rot(w_in[:, c0:c0 + 64], w_sw[:, c0:c0 + 64], Qz[hh], ('QZ', hh), ncols=64)
        self.proj_qk_rot(w_in[:, 1024 + 64 * g:1024 + 64 * g + 64], w_sw[:, 1024 + 64 * g:1024 + 64 * g + 64], KA, 'KA', ncols=64)
        wt, wk, wds = self.wsl.get()
        self.load_w(wt[:, :, 0:64], wk, wds, w_in[:, 1280 + 64 * g:1280 + 64 * g + 64])

        def evac_vc(tc, ps, pk):
            P.add('act', lambda e: e.activation(out=KB[0:64, tc * 512:(tc + 1) * 512], in_=ps[0:64, :], func=AF.Copy),
                  r=[pk], w=[('KB', tc)])
        self.proj_fm(wt, wk, 64, evac_vc)
        for which in range(2):
            src = KA if which == 0 else KB
            skeys = KAk if which == 0 else KBk
            w1 = self.nsa_w1_k if which == 0 else self.nsa_w1_v
            w2 = self.nsa_w2_k if which == 0 else self.nsa_w2_v
            b1 = self.nsa_b1_k if which == 0 else self.nsa_b1_v
            posd = self.nsa_pos_k if which == 0 else self.nsa_pos_v
            P.add('sp', lambda e, posd=posd: e.dma_start(out=pos32[0:32, :], in_=posd), w=['pos32'], dsem=self.ds_misc[0])
            P.add('sp', lambda e, b1=b1: e.dma_start(out=b1t[:, 0:1], in_=b1[0:128, :]), w=[('b1t', 0)], dsem=self.ds_misc[1])
            P.add('sp', lambda e, b1=b1: e.dma_start(out=b1t[:, 1:2], in_=b1[128:256, :]), w=[('b1t', 1)], dsem=self.ds_misc[2])
            if which == 0:
                self.load_w(W2k[:, :, :], 'W2a', self.ds_misc[3], w2)
                w2keys = ['W2a']
            else:
                self.load_w(W2v[:, :, :], 'W2v', self.ds_misc[3], w2)
                w2keys = ['W2v']
            P.add('pe', lambda e: e.transpose(out=self.ps[5][0:64, 0:32], in_=pos32[0:32, 0:64], identity=ident[0:32, 0:32]),
                  r=['pos32', 'c_ident'], w=[('ps', 5)])
            P.add('act', lambda e: e.activation(out=posT[0:64, :], in_=self.ps[5][0:64, 0:32], func=AF.Copy),
                  r=[('ps', 5)], w=['posT'])
            first = True
            for ic in range(4):
                srcw = w1[ic * 512:(ic + 1) * 512, :].rearrange('(i d) c -> d i c', d=64)
                P.add('pool', lambda e, srcw=srcw: e.dma_start(out=W1c[0:64, :, :], in_=srcw), w=['W1c'], dsem=self.ds_w[4])
                for ii in range(8):
                    i = ic * 8 + ii
                    for hc in range(2):
                        P.add('pe', lambda e, ii=ii, i=i, hc=hc, src=src: e.matmul(
                            self.ps[6 + hc][:, 0:127], lhsT=W1c[0:64, ii, hc * 128:(hc + 1) * 128],
                            rhs=src[0:64, ssl(i, 127, 16)], start=(i == 0), stop=(i == 31)),
                            r=['W1c'] + skeys, w=[('ps', 6 + hc)])
                        P.add('pe', lambda e, ii=ii, i=i, hc=hc, st=first: e.matmul(
                            self.ps[5][:, hc:hc + 1], lhsT=W1c[0:64, ii, hc * 128:(hc + 1) * 128],
                            rhs=posT[0:64, i:i + 1], start=st, stop=(i == 31), skip_group_check=True),
                            r=['W1c', 'posT'], w=[('ps', 5)])
                        first = False
            P.add('dve', lambda e: e.tensor_tensor(out=hb[:, 0:2], in0=self.ps[5][:, 0:2], in1=b1t[:, 0:2], op=ALU.add),
                  r=[('ps', 5), ('b1t', 0), ('b1t', 1)], w=['hb'])
            for hc in range(2):
                pk = ('ps', 6 + hc)
                psh = self.ps[6 + hc]
                P.add('act', lambda e, psh=psh, hc=hc: e.activation(out=xs[:, 0:127], in_=psh[:, 0:127], func=AF.Identity,
                                                                    bias=hb[:, hc:hc + 1]), r=[pk, 'hb'], w=['xs'])
                P.add('dve', lambda e: e.tensor_tensor(out=x2[:, 0:127], in0=xs[:, 0:127], in1=xs[:, 0:127], op=ALU.mult),
                      r=['xs'], w=['x2'])
                P.add('dve', lambda e: e.tensor_scalar(out=x2[:, 0:127], in0=x2[:, 0:127], scalar1=0.044715, scalar2=1.0,
                                                       op0=ALU.mult, op1=ALU.add), r=['x2'], w=['x2'])
                P.add('dve', lambda e: e.tensor_tensor(out=x2[:, 0:127], in0=x2[:, 0:127], in1=xs[:, 0:127], op=ALU.mult),
                      r=['x2', 'xs'], w=['x2'])
                P.add('act', lambda e: e.activation(out=x2[:, 0:127], in_=x2[:, 0:127], func=AF.Sigmoid,
                                                    scale=float(2.0 * np.sqrt(2.0 / np.pi))), r=['x2'], w=['x2'])
                P.add('dve', lambda e, hc=hc: e.tensor_tensor(out=gel[hc][:, 0:127], in0=xs[:, 0:127], in1=x2[:, 0:127],
                                                              op=ALU.mult), r=['x2', 'xs'], w=[('gel', hc)])
            if which == 0:
                for hc in range(2):
                    P.add('pe', lambda e, hc=hc: e.matmul(self.ps[6][0:64, 0:127], lhsT=W2k[:, hc, :], rhs=gel[hc][:, 0:127],
                                                          start=(hc == 0), stop=(hc == 1)),
                          r=[('gel', hc)] + w2keys, w=[('ps', 6)])
                P.add('act', lambda e: e.activation(out=kcmpT[0:64, 0:127], in_=self.ps[6][0:64, 0:127], func=AF.Copy),
                      r=[('ps', 6)], w=['kcmpT'])
            else:
                for hc in range(2):
                    P.add('pe', lambda e, hc=hc: e.matmul(self.ps[6][0:127, 0:64], lhsT=gel[hc][:, 0:127], rhs=W2v[:, hc, :],
                                                          start=(hc == 0), stop=(hc == 1)),
                          r=[('gel', hc)] + w2keys, w=[('ps', 6)])
                P.add('act', lambda e: e.activation(out=vcmp[0:127, 0:64], in_=self.ps[6][0:127, 0:64], func=AF.Copy),
                      r=[('ps', 6)], w=['vcmp'])
                P.add('pool', lambda e: e.memset(vcmp[:, 64:65], 1.0), r=[], w=['vcmp1'])
        if NSA_STOP == 2:
            continue
        def imp_stage(qb):
            si = self.rot('S', 3)
            ps = self.ps[si]
            for hh in range(4):
                P.add('pe', lambda e, ps=ps, hh=hh, qb=qb: e.matmul(
                    ps[:, hh * 128:(hh + 1) * 128], lhsT=Qz[hh][:, qb * 128:(qb + 1) * 128], rhs=kcmpT[:, :],
                    start=True, stop=True), r=['kcmpT'] + allq[hh] + [(('NT', hh), t) for t in range(NCH)], w=[('ps', si)])
            P.add('act', lambda e, ps=ps: e.activation(out=E4, in_=ps[:, :].rearrange('p (h n) -> p h n', h=4), func=AF.Exp,
                                                       scale=0.125), r=[('ps', si)], w=['E4'])
            m0s = self.c['c_m0'][:, 120 - 8 * qb:248 - 8 * qb].unsqueeze(1).broadcast_to([128, 4, 128])
            P.add('dve', lambda e, m0s=m0s: e.tensor_tensor(out=E4, in0=E4, in1=m0s, op=ALU.mult), r=['E4', 'c_m0'], w=['E4'])
            P.add('dve', lambda e: e.tensor_reduce(out=den4, in_=E4, axis=AX.X, op=ALU.add), r=['E4'], w=['den4'])
            P.add('dve', lambda e: e.tensor_scalar(out=den4, in0=den4, scalar1=1e-30, scalar2=None, op0=ALU.max),
                  r=['den4'], w=['den4'])
            P.add('dve', lambda e: e.reciprocal(out=den4, in_=den4), r=['den4'], w=['den4'])
            P.add('dve', lambda e: e.tensor_scalar(out=pcs, in0=E4[:, 0, :], scalar1=den4[:, 0:1], scalar2=None, op0=ALU.mult),
                  r=['E4', 'den4'], w=['pcs'])
            for hh in range(1, 4):
                P.add('dve', lambda e, hh=hh: e.scalar_tensor_tensor(out=pcs, in0=E4[:, hh, :], scalar=den4[:, hh:hh + 1],
                                                                     in1=pcs, op0=ALU.mult, op1=ALU.add),
                      r=['E4', 'den4', 'pcs'], w=['pcs'])
            P.add('dve', lambda e: e.tensor_reduce(out=imp, in_=pcs[:, :].rearrange('p (j f) -> p j f', f=4), axis=AX.X,
                                                   op=ALU.add), r=['pcs'], w=['imp'])
            P.add('dve', lambda e: e.tensor_tensor(out=imp[:, 1:32], in0=imp[:, 1:32], in1=pcs[:, ssl(3, 31, 4)], op=ALU.add),
                  r=['pcs', 'imp'], w=['imp'])
            P.add('dve', lambda e, qb=qb: e.tensor_tensor(out=impm, in0=imp, in1=self.c['c_keep'][:, 30 - 2 * qb:62 - 2 * qb],
                                                          op=ALU.mult), r=['imp', 'c_keep'], w=['impm'])
            P.add('dve', lambda e, qb=qb: e.tensor_tensor(out=impm, in0=impm, in1=self.c['c_addv'][:, 30 - 2 * qb:62 - 2 * qb],
                                                          op=ALU.add), r=['impm', 'c_addv'], w=['impm'])
            P.add('dve', lambda e: e.memset(impm[:, 0:1], 1e6), r=['impm'], w=['impm'])
            P.add('dve', lambda e: e.max(out=top8, in_=impm), r=['impm'], w=['top8'])
            P.add('dve', lambda e: e.tensor_scalar(out=sel[:, 64:96], in0=impm, scalar1=top8[:, 7:8], scalar2=None, op0=ALU.is_ge),
                  r=['impm', 'top8'], w=['sel'])
            P.add('pe', lambda e: e.transpose(out=self.ps[5][0:96, 0:128], in_=sel[:, 0:96], identity=ident[:]),
                  r=['sel', 'c_ident'], w=[('ps', 5)])
            for hh in range(4):
                P.add('dve', lambda e, qb=qb, hh=hh: e.tensor_scalar(
                    out=Qz[hh][64:96, qb * 128:(qb + 1) * 128], in0=self.ps[5][64:96, 0:128],
                    scalar1=30000.0, scalar2=-30000.0, op0=ALU.mult, op1=ALU.add),
                    r=[('ps', 5)], w=[(('NT', hh), qb // 4)])
        self.proj_qk_rot(w_in[:, 1536 + 64 * g:1536 + 64 * g + 64], w_sw[:, 1280 + 64 * g:1280 + 64 * g + 64], KA, 'KA', ncols=64)
        imp_stage(0)
        self.proj_qk_rot(w_in[:, 2048 + 64 * g:2048 + 64 * g + 64], w_sw[:, 1536 + 64 * g:1536 + 64 * g + 64], KB, 'KB', ncols=64)
        imp_stage(1)
        self.proj_v(w_in[:, 1792 + 64 * g:1792 + 64 * g + 64], 64, lambda gg: Vs[:, 4 * gg:4 * gg + 4, :, 0:64], 'Vs')
        imp_stage(2)
        self.proj_v(w_in[:, 2304 + 64 * g:2304 + 64 * g + 64], 64, lambda gg: Vw[:, 4 * gg:4 * gg + 4, :, 0:64], 'Vw')
        imp_stage(3)
        if NSA_STOP in (3, 5):
            continue
        for jp in range(2):
            ot_keys = []
            for h2 in range(2):
                hh = 2 * jp + h2
                head = 4 * g + hh
                for i in range(NCH):
                    if hh == 0 and i + 1 < NCH:
                        for qb in range(4 * (i + 1), 4 * (i + 2)):
                            imp_stage(qb)
                    csl = slice(i * 512, (i + 1) * 512)
                    q_ap = Qz[hh][:, csl]
                    qk = [(('QZ', hh), i), (('NT', hh), i)]
                    ai = (3, 4, 6)[self.rot('ACC', 3)]
                    self.unit(kcmpT[:, :], q_ap, ['kcmpT'] + qk, self.c['c_cmpT'][:, csl], ['c_cmpT'],
                              vcmp[:, 0:65], ['vcmp', 'vcmp1'], self.ps[ai], ai, True, True)
                    self.finalize(self.ps[ai], ('ps', ai), None, None, gate=(3 * head + 0, csl), first=True, last=False)
                    ai = (3, 4, 6)[self.rot('ACC', 3)]
                    nkb = 4 * i + 4
                    for kb in range(nkb):
                        mask = wm[:, 384 - 128 * (kb - 4 * i):384 - 128 * (kb - 4 * i) + 512] if kb >= 4 * i else None
                        self.unit(KA[:, kb * 128:(kb + 1) * 128], q_ap, [('KA', kb // 4), 'KAz', 'KAe'] + qk, mask, ['c_wm'],
                                  Vs[:, kb, 0, :], [('Vs', kb // 4), 'Vs1'], self.ps[ai], ai, kb == 0, kb == nkb - 1)
                    self.finalize(self.ps[ai], ('ps', ai), None, None, gate=(3 * head + 1, csl), first=False, last=False)
                    ai = (3, 4, 6)[self.rot('ACC', 3)]
                    kb0 = max(0, 4 * i - 4)
                    for kb in range(kb0, nkb):
                        r_ = kb - 4 * i
                        mask = wm[:, 384 - 128 * r_:384 - 128 * r_ + 512]
                        self.unit(KB[:, kb * 128:(kb + 1) * 128], q_ap, [('KB', kb // 4), 'KBz'] + qk, mask, ['c_wm'],
                                  Vw[:, kb, 0, :], [('Vw', kb // 4), 'Vw1'], self.ps[ai], ai, kb == kb0, kb == nkb - 1)
                    self.finalize(self.ps[ai], ('ps', ai), self.OT[h2][0:64, csl], ('OT', h2, i),
                                  gate=(3 * head + 2, csl), first=False, last=True)
                ot_keys.append([('OT', h2, i) for i in range(NCH)])
            self.outproj_pair(self.nsa_w_out, 2 * g + jp, ot_keys)


Builder.unit = _unit
Builder.nsa_mixer = _nsa_mixer


def _moe(self):
    P, R = self.P, self.R
    self.phase()
    self.norm(self.ffn_norm[1:2, :])
    self.phase()
    comb = R.alloc([8, NB], F32)
    lg = R.alloc([NB, 8], F32)
    rb = R.alloc([1, 8], F32)
    t8 = R.alloc([NB, 8], F32)
    w1 = R.alloc([NB], F32)
    w2 = R.alloc([NB], F32)
    ta = R.alloc([NB], F32)
    tb = R.alloc([NB], F32)
    wr = R.alloc([8, 8], BF16)
    self.load_w(wr, 'wr', self.ds_misc[0], self.moe_w_router)
    P.add('sp', lambda e: e.dma_start(out=rb[:, 0, :], in_=self.moe_b_router[0, :].partition_broadcast(128)),
          w=['rb'], dsem=self.ds_misc[1])
    ps = self.ps[7]
    for b in range(NB):
        for kc in range(8):
            P.add('pe', lambda e, b=b, kc=kc: e.matmul(ps[:, b * 8:(b + 1) * 8], lhsT=self.hnT[:, kc, b * 128:(b + 1) * 128],
                                                       rhs=wr[:, kc, :], start=(kc == 0), stop=(kc == 7)),
                  r=['wr', ('hnT', b)], w=[('ps', 7)])
    P.add('dve', lambda e: e.tensor_tensor(out=lg, in0=ps[:, 0:128].rearrange('p (b h) -> p b h', h=8),
                                           in1=rb[:, 0:1, :].broadcast_to([128, NB, 8]), op=ALU.add),
          r=[('ps', 7), 'rb'], w=['lg'])
    for b in range(NB):
        P.add('dve', lambda e, b=b: e.max(out=t8[:, b, :], in_=lg[:, b, :]), r=['lg'], w=[('t8', b)])
    t8k = [('t8', b) for b in range(NB)]
    P.add('dve', lambda e: e.tensor_tensor(out=w1, in0=t8[:, :, 1], in1=t8[:, :, 0], op=ALU.subtract), r=t8k, w=['w1'])
    P.add('act', lambda e: e.activation(out=w1, in_=w1, func=AF.Exp), r=['w1'], w=['w1'])
    P.add('dve', lambda e: e.tensor_scalar(out=w1, in0=w1, scalar1=1.0, scalar2=None, op0=ALU.add), r=['w1'], w=['w1'])
    P.add('dve', lambda e: e.reciprocal(out=w1, in_=w1), r=['w1'], w=['w1'])
    P.add('dve', lambda e: e.tensor_scalar(out=w2, in0=w1, scalar1=-1.0, scalar2=1.0, op0=ALU.mult, op1=ALU.add),
          r=['w1'], w=['w2'])
    for ex in range(8):
        P.add('dve', lambda e, ex=ex: e.tensor_tensor(out=ta, in0=lg[:, :, ex], in1=t8[:, :, 0], op=ALU.is_equal),
              r=['lg'] + t8k, w=['ta'])
        P.add('dve', lambda e: e.tensor_tensor(out=ta, in0=ta, in1=w1, op=ALU.mult), r=['ta', 'w1'], w=['ta'])
        P.add('dve', lambda e, ex=ex: e.tensor_tensor(out=tb, in0=lg[:, :, ex], in1=t8[:, :, 1], op=ALU.is_equal),
              r=['lg'] + t8k, w=['tb'])
        P.add('dve', lambda e: e.tensor_tensor(out=tb, in0=tb, in1=w2, op=ALU.mult), r=['tb', 'w2'], w=['tb'])
        P.add('dve', lambda e, ex=ex: e.tensor_tensor(out=comb[:, ex, :], in0=ta, in1=tb, op=ALU.add),
              r=['ta', 'tb'], w=['comb'])
    self.ffn_setup()
    for ex in range(8):
        self.ffn(self.moe_w_gate[ex], self.moe_w_up[ex], self.moe_w_down[ex], comb=comb[:, ex, :])


Builder.moe = _moe
```

```python
import numpy as np
import ml_dtypes
from contextlib import ExitStack
import concourse.bass as bass
import concourse.mybir as mybir
from concourse.bass_utils import run_bass_kernel_spmd

dt = mybir.dt
F32, BF16, I32 = dt.float32, dt.bfloat16, dt.int32
AF = mybir.ActivationFunctionType
ALU = mybir.AluOpType
AX = mybir.AxisListType

S = 2048
D = 1024
NB = 16
NCH = 4
DFF = 3584
PI = float(np.pi)
TWO_PI = float(2 * np.pi)

NSA_STOP = 0
PD = 4
NPT = PD + 2
ENG_NAMES = ['pe', 'act', 'dve', 'pool', 'sp']


def ssl(start, n, step):
    return slice(start, start + (n - 1) * step + 1, step)

SAME_ENG_SYNC = True


class DSem:
    def __init__(self, sem):
        self.sem = sem
        self.total = 0
        self.open = None


class Op:
    __slots__ = ('eng', 'fn', 'deps', 'dsem', 'sig', 'sem', 'val')


class Prog:
    def __init__(self, nc, stack):
        self.nc = nc
        self.stack = stack
        self.ops = []
        self.by_eng = {e: [] for e in ENG_NAMES}
        self.lastw = {}
        self.readers = {}
        self.nsem = 0
        self.bar_deps = {}
        self.bar_done = set()
        self.since_bar_dma = {}
        self.final = []
        self.uid = 0

    def new_sem(self, name):
        self.nsem += 1
        return self.stack.enter_context(self.nc.semaphore(f"{name}_{self.nsem}"))

    def dsem(self, name):
        return DSem(self.new_sem(name))

    def group(self, ds):
        ds.open = []

    def endgroup(self, ds):
        for o in ds.open:
            o.val = ds.total * 16
        ds.open = None

    def barrier(self):
        deps = dict(self.bar_deps)
        for e in ENG_NAMES:
            for o in reversed(self.by_eng[e]):
                if o.dsem is None:
                    deps[e] = o
                    o.sig = True
                    break
        for k, o in self.since_bar_dma.items():
            deps[k] = o
        self.bar_deps = deps
        self.bar_done = set()
        self.since_bar_dma = {}
        self.lastw = {}
        self.readers = {}

    def add(self, eng, fn, r=(), w=(), dsem=None):
        o = Op()
        o.eng = eng
        o.fn = fn
        o.dsem = dsem
        o.sig = False
        o.sem = None
        o.val = 0
        deps = {}

        def need(p):
            if p is None:
                return
            if p.dsem is None and dsem is None and p.eng == eng:
                if eng == 'pe' or not SAME_ENG_SYNC:
                    return
            deps[id(p)] = p

        for k in r:
            need(self.lastw.get(k))
        for k in w:
            need(self.lastw.get(k))
            rd = self.readers.get(k)
            if rd:
                for q in rd.values():
                    need(q)
        if eng not in self.bar_done:
            self.bar_done.add(eng)
            for p in self.bar_deps.values():
                need(p)
        for k in w:
            self.lastw[k] = o
            self.readers[k] = {}
        for k in r:
            d = self.readers.setdefault(k, {})
            if dsem is not None:
                self.uid += 1
                d[('dma', self.uid)] = o
            else:
                d[eng] = o
        o.deps = list(deps.values())
        for p in o.deps:
            p.sig = True
        if dsem is not None:
            dsem.total += 1
            o.sem = dsem.sem
            o.val = dsem.total * 16
            if dsem.open is not None:
                dsem.open.append(o)
            self.since_bar_dma[id(dsem)] = o
        self.ops.append(o)
        self.by_eng[eng].append(o)
        return o

    def emit(self):
        nc = self.nc
        LIMIT = 30000
        for e in ENG_NAMES:
            cur = None
            cnt = 0
            for o in self.by_eng[e]:
                if o.dsem is None and o.sig:
                    if cur is None or cnt >= LIMIT:
                        cur = self.new_sem(f"e_{e}")
                        cnt = 0
                    cnt += 1
                    o.sem = cur
                    o.val = cnt
        final = self.final

        def run(e, eng):
            known = {}
            for o in self.by_eng[e]:
                need = {}
                for p in o.deps:
                    s = p.sem
                    if s is None:
                        continue
                    cur = need.get(id(s))
                    if cur is None or cur[1] < p.val:
                        need[id(s)] = (s, p.val)
                for sid, (s, v) in need.items():
                    if known.get(sid, 0) < v:
                        eng.wait_ge(s, v)
                        known[sid] = v
                inst = o.fn(eng)
                if o.dsem is not None:
                    inst.then_inc(o.sem, 16)
                elif o.sig:
                    inst.then_inc(o.sem, 1)
            if e == 'sp':
                for o in final:
                    eng.wait_ge(o.sem, o.val)

        with nc.Block() as block:
            @block.tensor
            def _(eng):
                run('pe', eng)

            @block.scalar
            def _(eng):
                run('act', eng)

            @block.vector
            def _(eng):
                run('dve', eng)

            @block.gpsimd
            def _(eng):
                run('pool', eng)

            @block.sync
            def _(eng):
                run('sp', eng)


class Region:
    def __init__(self, ap, nelem):
        self.ap = ap
        self.n = nelem
        self.off = 0
        self.peak = 0

    def reset(self):
        self.off = 0

    def alloc(self, free_shape, dtype, parts=128):
        n = int(np.prod(free_shape))
        mult = 2 if dtype == F32 or dtype == I32 else 1
        nb = n * mult
        nb = (nb + 15) // 16 * 16
        assert self.off + nb <= self.n, f"region overflow {self.off}+{nb}>{self.n}"
        v = self.ap[0:parts, self.off:self.off + n * mult]
        self.off += nb
        self.peak = max(self.peak, self.off)
        if mult == 2:
            v = v.bitcast(dtype)
        if len(free_shape) == 2:
            v = v.rearrange('p (a b) -> p a b', b=free_shape[1])
        elif len(free_shape) == 3:
            v = v.rearrange('p (a b c) -> p a b c', b=free_shape[1], c=free_shape[2])
        return v


def _bf(a):
    return np.ascontiguousarray(a.astype(np.float32)).astype(ml_dtypes.bfloat16)


def make_consts():
    c = {}
    k = np.arange(128)[:, None]
    c['c_ident'] = np.eye(128, dtype=np.float32)
    c['c_utri'] = (k <= np.arange(128)[None, :]).astype(np.float32)
    c['c_ones'] = np.ones((128, 128), np.float32)
    os_ = np.zeros((128, 64), np.float32)
    os_[64, :] = 1.0
    c['c_os'] = _bf(os_)
    y = np.arange(-384, 1024)[None, :]
    c['c_wm'] = _bf(((y - k) >= 0) & ((y - k) < 512))
    q = np.arange(128)[None, :]
    cur = (k <= q)
    prev = (k >= q)
    c['c_dm'] = _bf(np.concatenate([cur, prev, cur, prev], axis=1))
    n = np.arange(128)[:, None]
    qq = np.arange(2048)[None, :]
    c['c_cmpT'] = _bf((16 * n + 31 <= qq) & (n <= 126))
    xq = np.arange(128)[:, None]
    xx = np.arange(-120, 128)[None, :]
    c['c_m0'] = (16 * xx + 31 <= xq).astype(np.float32)
    xs = np.arange(-30, 32)[None, :]
    curp = (xq // 64)
    fut = xs > curp
    forced = (xs == curp) | (xs == curp - 1)
    keep = (~fut) & (~forced)
    addv = np.where(fut, -1.0, np.where(forced, 1e6, 0.0))
    c['c_keep'] = keep.astype(np.float32)
    c['c_addv'] = addv.astype(np.float32)
    j = np.arange(32)[:, None, None]
    kb = np.arange(16)[None, :, None]
    kk = np.arange(128)[None, None, :]
    E = (j == 2 * kb + kk // 64)
    Ef = np.zeros((128, 16 * 128), np.float32)
    Ef[:32] = E.reshape(32, 16 * 128)
    c['c_E'] = _bf(Ef)
    Z = np.zeros((128, 48 * 64), np.float32)
    for p_ in range(48):
        Z[p_, 64 * p_:64 * p_ + 64] = 1.0
    c['c_Z'] = _bf(Z)
    half = 32
    inv_freq = (10000.0 ** (-np.arange(half, dtype=np.float32) / half)).astype(np.float32)
    pp = np.arange(128)
    rot = np.zeros((128, 4), np.float32)
    rot[:, 0] = inv_freq[(pp % 64) % 32]
    rot[:, 1] = PI / 2
    rot[:, 2] = np.where((pp % 64) < 32, PI, 0.0)
    c['c_rot'] = rot
    return c


CONST_SHAPES = None


class Slots:
    def __init__(self, P, name, aps, dsems):
        self.items = [(ap, f"{name}{i}", dsems[i]) for i, ap in enumerate(aps)]
        self.i = 0

    def get(self):
        it = self.items[self.i % len(self.items)]
        self.i += 1
        return it


class Builder:
    def __init__(self, n_seq=2, stages=('l0', 'l1'), dbg=None):
        self.n_seq = n_seq
        self.stages = stages
        self.dbg = dbg
        nc = bass.Bass("TRN2", target_bir_lowering=False)
        self.nc = nc
        self.stack = ExitStack()
        self.P = Prog(nc, self.stack)
        self.rr = {}
        self.fifo = []
        self.fins = []
        self.fseq = 0
        self.gate_free_at = 0
        self.u_busy = [False, False]
        self.acc_pending = {}
        self.acc_gen = {}
        self.tick = 0
        self._inputs()
        self._alloc()

    def din(self, name, shape, dtype=F32):
        return self.nc.dram_tensor(name, list(shape), dtype, kind="ExternalInput").ap()

    def sb(self, name, shape, dtype):
        return self.stack.enter_context(self.nc.sbuf_tensor(name, list(shape), dtype))

    def _inputs(self):
        n = self.n_seq
        self.x = self.din('x', [n, S, D])
        self.p = self.din('p', [2, n, S, 256])
        self.pos = self.din('positions', [n, S], I32)
        self.mix_norm = self.din('mix_norm', [2, D])
        self.ffn_norm = self.din('ffn_norm', [2, D])
        self.ple_norm = self.din('ple_norm', [2, D])
        self.final_norm = self.din('final_norm', [1, D])
        self.ple_gate_w = self.din('ple_gate_w', [2, D, D])
        self.ple_proj_w = self.din('ple_proj_w', [2, 256, D])
        self.fd_w_in = self.din('fd_w_in', [D, 3080])
        self.fd_w_sw = self.din('fd_w_sw', [D, 1024])
        self.fd_forget_b = self.din('fd_forget_b', [1, 8])
        self.fd_w_out = self.din('fd_w_out', [D, D])
        self.dense_w_gate = self.din('dense_w_gate', [D, DFF])
        self.dense_w_up = self.din('dense_w_up', [D, DFF])
        self.dense_w_down = self.din('dense_w_down', [DFF, D])
        self.nsa_w_in = self.din('nsa_w_in', [D, 2608])
        self.nsa_w_sw = self.din('nsa_w_sw', [D, 1792])
        self.nsa_pos_k = self.din('nsa_pos_k', [32, 64])
        self.nsa_w1_k = self.din('nsa_w1_k', [2048, 256])
        self.nsa_b1_k = self.din('nsa_b1_k', [256, 1])
        self.nsa_w2_k = self.din('nsa_w2_k', [256, 64])
        self.nsa_pos_v = self.din('nsa_pos_v', [32, 64])
        self.nsa_w1_v = self.din('nsa_w1_v', [2048, 256])
        self.nsa_b1_v = self.din('nsa_b1_v', [256, 1])
        self.nsa_w2_v = self.din('nsa_w2_v', [256, 64])
        self.nsa_w_out = self.din('nsa_w_out', [D, D])
        self.moe_w_router = self.din('moe_w_router', [D, 8])
        self.moe_b_router = self.din('moe_b_router', [1, 8])
        self.moe_w_gate = self.din('moe_w_gate', [8, D, DFF])
        self.moe_w_up = self.din('moe_w_up', [8, D, DFF])
        self.moe_w_down = self.din('moe_w_down', [8, DFF, D])
        self.cst = {}
        for k, v in make_consts().items():
            d = BF16 if v.dtype == ml_dtypes.bfloat16 else F32
            self.cst[k] = self.din(k, v.shape, d)
        self.y = self.nc.dram_tensor('y', [n, S, D], F32, kind="ExternalOutput").ap()

    def _alloc(self):
        P = self.P
        self.h = self.sb('h', [128, NB, D], F32)
        self.hnT = self.sb('hnT', [128, 8, S], BF16)
        self.cosT = self.sb('cosT', [128, S], BF16)
        self.sinT = self.sb('sinT', [128, S], BF16)
        self.c = {}
        for k, ap in self.cst.items():
            if k == 'c_E':
                continue
            self.c[k] = self.sb('s_' + k, list(ap.shape), ap.dtype)
        self.ps = [self.stack.enter_context(self.nc.psum_tensor(f"ps{i}", [128, 512], F32)) for i in range(8)]
        RN = 43520
        self.Rt = self.sb('R', [128, RN], BF16)
        self.R = Region(self.Rt, RN)
        self.ds_c = P.dsem('consts')
        self.ds_h = P.dsem('hload')
        self.ds_g = P.dsem('gtile')
        self.ds_y = P.dsem('yout')
        self.ds_misc = [P.dsem(f'misc{i}') for i in range(4)]
        self.ds_w = [P.dsem(f'w{i}') for i in range(6)]
        self.ds_wo = [P.dsem(f'wo{i}') for i in range(2)]
        self.ds_ffn = [P.dsem(f'ffn{i}') for i in range(6)]

    def defer(self, fn, delay, fin=False):
        if fin:
            self.fseq += 1
            self.fins.append((self.tick + delay, self.fseq, fn))
            self.fins.sort(key=lambda x: (x[0], x[1]))
        else:
            self.fifo.append((self.tick + delay, fn))

    def _drain(self):
        while self.fins and self.fins[0][0] <= self.tick:
            self.fins.pop(0)[2]()
        while self.fifo and self.fifo[0][0] <= self.tick:
            self.fifo.pop(0)[1]()

    def step(self):
        self.tick += 1
        self._drain()

    def flush(self):
        while self.fifo or self.fins:
            self.tick += 1
            self._drain()

    def acc_gen_of(self, key):
        return self.acc_gen.get(key, 0)

    def acc_start(self, key, gen):
        assert not (key in self.acc_pending and gen > self.acc_pending[key]), \
            f"accumulator {key} reused before its finalize was emitted"


    def step(self):
        self.tick += 1
        self._drain()

    def flush(self):
        while self.fifo or self.fins:
            self.tick += 1
            self._drain()

    def rot(self, name, n):
        i = self.rr.get(name, 0)
        self.rr[name] = i + 1
        return i % n

    def load_consts(self):
        P = self.P
        P.group(self.ds_c)
        for k, ap in self.cst.items():
            if k == 'c_E':
                continue
            P.add('sp', lambda e, k=k, ap=ap: e.dma_start(out=self.c[k][:], in_=ap), w=[k], dsem=self.ds_c)
        P.endgroup(self.ds_c)

    def load_w(self, dst, dkey, dsem, src, cast=True):
        srcv = src.rearrange('(k p) c -> p k c', p=128)
        eng = 'pool' if cast else 'sp'
        return self.P.add(eng, lambda e: e.dma_start(out=dst, in_=srcv), w=[dkey], dsem=dsem)

    def norm(self, gain_row, final_out=None):
        P, R, h = self.P, self.R, self.h
        mark = R.off
        self.gtile = R.alloc([D], F32)
        ss = R.alloc([NB], F32)
        junk = R.alloc([D], BF16)
        hs = [R.alloc([D], F32) for _ in range(2)]
        P.add('sp', lambda e: e.dma_start(out=self.gtile, in_=gain_row[0, :].partition_broadcast(128)),
              w=['gtile'], dsem=self.ds_g)
        for b in range(NB):
            P.add('act', lambda e, b=b: e.activation(out=junk, in_=h[:, b, :], func=AF.Square,
                                                     accum_out=ss[:, b:b + 1]),
                  r=self.hk(b), w=['junk', ('ss', b)])
        allss = [('ss', b) for b in range(NB)]
        P.add('dve', lambda e: e.tensor_scalar(out=ss, in0=ss, scalar1=1.0 / D, scalar2=1e-6,
                                               op0=ALU.mult, op1=ALU.add), r=allss, w=allss)
        P.add('act', lambda e: e.activation(out=ss, in_=ss, func=AF.Sqrt), r=allss, w=allss)
        P.add('dve', lambda e: e.reciprocal(out=ss, in_=ss), r=allss, w=allss)
        ident = self.c['c_ident']
        for b in range(NB):
            i = b % 2
            P.add('dve', lambda e, b=b, i=i: e.scalar_tensor_tensor(
                out=hs[i], in0=h[:, b, :], scalar=ss[:, b:b + 1], in1=self.gtile,
                op0=ALU.mult, op1=ALU.mult), r=self.hk(b) + [('ss', b), 'gtile'], w=[('hs', i)])
            if final_out is not None:
                s = final_out
                P.add('sp', lambda e, b=b, i=i, s=s: e.dma_start(out=self.y[s, b * 128:(b + 1) * 128, :], in_=hs[i]),
                      r=[('hs', i)], w=[('y', s, b)], dsem=self.ds_y)
                continue
            for half in range(2):
                pi = 6 + half
                ps = self.ps[pi]
                for j in range(4):
                    cidx = half * 4 + j
                    P.add('pe', lambda e, ps=ps, j=j, cidx=cidx, i=i: e.transpose(
                        out=ps[:, j * 128:(j + 1) * 128], in_=hs[i][:, cidx * 128:(cidx + 1) * 128],
                        identity=ident[:]), r=[('hs', i), 'c_ident'], w=[('ps', pi)])
                P.add('act', lambda e, ps=ps, half=half, b=b: e.activation(
                    out=self.hnT[:, half * 4:half * 4 + 4, b * 128:(b + 1) * 128],
                    in_=ps[:].rearrange('p (a t) -> p a t', t=128), func=AF.Copy),
                    r=[('ps', pi)], w=[('hnT', b)])
        R.off = mark

    def hk(self, b):
        return [('h', b, 0), ('h', b, 1)]

    def hn_keys(self, tc=None):
        if tc is None:
            return [('hnT', b) for b in range(NB)]
        return [('hnT', 4 * tc + j) for j in range(4)]

    def load_seq(self, s):
        P = self.P
        P.group(self.ds_h)
        for b in range(NB):
            P.add('sp', lambda e, b=b: e.dma_start(out=self.h[:, b, :], in_=self.x[s, b * 128:(b + 1) * 128, :]),
                  w=self.hk(b), dsem=self.ds_h)
        P.endgroup(self.ds_h)

    def rotary_tables(self, s):
        P, R = self.P, self.R
        mark = R.off
        posi = R.alloc([S], I32)
        posf = R.alloc([S], F32)
        t = R.alloc([S], F32)
        kf = R.alloc([S], F32)
        ki = R.alloc([S], I32)
        rot = self.c['c_rot']
        P.add('sp', lambda e: e.dma_start(out=posi, in_=self.pos[s, :].partition_broadcast(128)),
              w=['posi'], dsem=self.ds_misc[0])
        P.add('dve', lambda e: e.tensor_copy(out=posf, in_=posi), r=['posi'], w=['posf'])
        for (dst, dkey, ph) in ((self.cosT, 'cosT', 1), (self.sinT, 'sinT', 2)):
            P.add('dve', lambda e, ph=ph: e.tensor_scalar(out=t, in0=posf, scalar1=rot[:, 0:1], scalar2=rot[:, ph:ph + 1],
                                                          op0=ALU.mult, op1=ALU.add), r=['posf', 'c_rot'], w=['rt'])
            P.add('dve', lambda e: e.tensor_scalar(out=kf, in0=t, scalar1=1.0 / TWO_PI, scalar2=None, op0=ALU.mult),
                  r=['rt'], w=['rk'])
            P.add('dve', lambda e: e.tensor_copy(out=ki, in_=kf), r=['rk'], w=['rki'])
            P.add('dve', lambda e: e.tensor_copy(out=kf, in_=ki), r=['rki'], w=['rk'])
            P.add('dve', lambda e: e.scalar_tensor_tensor(out=t, in0=kf, scalar=-TWO_PI, in1=t,
                                                          op0=ALU.mult, op1=ALU.add), r=['rk', 'rt'], w=['rt'])
            P.add('dve', lambda e: e.tensor_scalar(out=kf, in0=t, scalar1=PI, scalar2=-TWO_PI,
                                                   op0=ALU.is_gt, op1=ALU.mult), r=['rt'], w=['rk'])
            P.add('dve', lambda e: e.tensor_tensor(out=t, in0=t, in1=kf, op=ALU.add), r=['rt', 'rk'], w=['rt'])
            P.add('dve', lambda e: e.tensor_scalar(out=t, in0=t, scalar1=-PI, scalar2=PI,
                                                   op0=ALU.max, op1=ALU.min), r=['rt'], w=['rt'])
            P.add('act', lambda e, dst=dst: e.activation(out=dst[:], in_=t, func=AF.Sin), r=['rt'], w=[dkey])
        R.off = mark

    def phase(self):
        self.flush()
        self.P.barrier()
        self.R.reset()

    def attn_common(self):
        R = self.R
        self.Pt = [R.alloc([512], BF16) for _ in range(NPT)]
        self.U = [R.alloc([512], F32, parts=65) for _ in range(2)]
        self.rd16 = [R.alloc([512], BF16) for _ in range(2)]
        self.Rt = [R.alloc([512], F32, parts=64) for _ in range(2)]
        for i_ in range(2):
            self.P.add('pool', lambda e, i_=i_: e.memset(self.rd16[i_][:, :], 0.0), w=[('rd16', i_)])
        self.OT = [R.alloc([S], BF16, parts=64) for _ in range(2)]
        self.Wo = [R.alloc([D], BF16, parts=64) for _ in range(2)]
        self.wsl = Slots(self.P, 'wsl', [R.alloc([8, 128], BF16) for _ in range(4)], self.ds_w)

    def proj_fm(self, wt, wkey, ncols, evac, wcols=None):
        P = self.P
        for tc in range(NCH):
            pi = 6 + self.rot('PJ', 2)
            ps = self.ps[pi]
            for kc in range(8):
                lhsT = wt[:, kc, 0:ncols] if wcols is None else wt[:, kc, wcols[0]:wcols[1]]
                P.add('pe', lambda e, ps=ps, kc=kc, tc=tc, lhsT=lhsT: e.matmul(
                    ps[0:ncols, :], lhsT=lhsT, rhs=self.hnT[:, kc, tc * 512:(tc + 1) * 512],
                    start=(kc == 0), stop=(kc == 7)), r=[wkey] + self.hn_keys(tc), w=[('ps', pi)])
            evac(tc, ps, ('ps', pi))

    def proj_qk_plain(self, src_cols, dst, dkey, split=None):
        P = self.P
        wt, wk, wds = self.wsl.get()
        self.load_w(wt, wk, wds, src_cols)

        def evac(tc, ps, pk):
            sl = slice(tc * 512, (tc + 1) * 512)
            if split is None:
                P.add('act', lambda e: e.activation(out=dst[:, sl], in_=ps[:, :], func=AF.Copy), r=[pk], w=[(dkey, tc)])
            else:
                for hh in range(2):
                    rows = slice(64 * hh, 64 * hh + 64)
                    P.add('act', lambda e, hh=hh, rows=rows: e.activation(out=split[hh][rows, sl], in_=ps[rows, :], func=AF.Copy),
                          r=[pk], w=[(('QZ', hh), tc)])
        self.proj_fm(wt, wk, 128, evac)

    def proj_qk_rot(self, src_cols, src_sw_cols, dst, dkey, dup=False, split=None, ncols=128):
        P, R = self.P, self.R
        wt, wk, wds = self.wsl.get()
        wt2, wk2, wds2 = self.wsl.get()
        if dup:
            P.group(wds)
            self.load_w(wt[:, :, 0:64], wk, wds, src_cols)
            self.load_w(wt[:, :, 64:128], wk + 'b', wds, src_cols)
            P.endgroup(wds)
            P.group(wds2)
            self.load_w(wt2[:, :, 0:64], wk2, wds2, src_sw_cols)
            self.load_w(wt2[:, :, 64:128], wk2 + 'b', wds2, src_sw_cols)
            P.endgroup(wds2)
            wkeys, wkeys2 = [wk, wk + 'b'], [wk2, wk2 + 'b']
        else:
            self.load_w(wt[:, :, 0:ncols], wk, wds, src_cols)
            self.load_w(wt2[:, :, 0:ncols], wk2, wds2, src_sw_cols)
            wkeys, wkeys2 = [wk], [wk2]
        for tc in range(NCH):
            sl = slice(tc * 512, (tc + 1) * 512)
            pa, pb = ((6, 7), (0, 1), (2, 3))[self.rot('rotp', 3)]
            for (pi, w_, wkk) in ((pa, wt, wkeys), (pb, wt2, wkeys2)):
                ps = self.ps[pi]
                for kc in range(8):
                    P.add('pe', lambda e, ps=ps, kc=kc, w_=w_, sl=sl: e.matmul(
                        ps[0:ncols, :], lhsT=w_[:, kc, 0:ncols], rhs=self.hnT[:, kc, sl], start=(kc == 0), stop=(kc == 7)),
                        r=wkk + self.hn_keys(tc), w=[('ps', pi)])
            ti = self.rot('rt', 2)
            t1 = self.rtmp[ti]
            P.add('dve', lambda e, t1=t1, sl=sl, pa=pa: e.tensor_tensor(out=t1[0:ncols, :], in0=self.ps[pa][0:ncols, :], in1=self.cosT[0:ncols, sl],
                                                                 op=ALU.mult), r=[('ps', pa), 'cosT'], w=[('rtmp', ti)])
            if split is None:
                P.add('dve', lambda e, sl=sl, pb=pb: e.tensor_tensor(out=dst[0:ncols, sl], in0=self.ps[pb][0:ncols, :], in1=self.sinT[0:ncols, sl],
                                                              op=ALU.mult), r=[('ps', pb), 'sinT'], w=[(dkey, tc)])
                P.add('pool', lambda e, t1=t1, sl=sl: e.tensor_tensor(out=dst[0:ncols, sl], in0=dst[0:ncols, sl], in1=t1[0:ncols, :], op=ALU.add),
                      r=[('rtmp', ti), (dkey, tc)], w=[(dkey, tc)])
            else:
                t2 = self.rtmp2[ti]
                P.add('dve', lambda e, t2=t2, sl=sl, pb=pb: e.tensor_tensor(out=t2, in0=self.ps[pb][:, :], in1=self.sinT[:, sl],
                                                                     op=ALU.mult), r=[('ps', pb), 'sinT'], w=[('rtmp2', ti)])
                for hh in range(2):
                    rows = slice(64 * hh, 64 * hh + 64)
                    P.add('pool', lambda e, t1=t1, t2=t2, sl=sl, hh=hh, rows=rows: e.tensor_tensor(
                        out=split[hh][rows, sl], in0=t1[rows, :], in1=t2[rows, :], op=ALU.add),
                        r=[('rtmp', ti), ('rtmp2', ti)], w=[(('QZ', hh), tc)])

    def proj_v(self, src_cols, ncols, dst_fn, dkey, tok_fn=None, nblk=NB):
        P = self.P
        wt, wk, wds = self.wsl.get()
        self.load_w(wt[:, :, 0:ncols], wk, wds, src_cols)
        for g in range(nblk // 4):
            pi = 6 + self.rot('PJ', 2)
            ps = self.ps[pi]
            for bb in range(4):
                blk = 4 * g + bb
                tsl = slice(blk * 128, (blk + 1) * 128) if tok_fn is None else tok_fn(blk)
                for kc in range(8):
                    P.add('pe', lambda e, ps=ps, bb=bb, kc=kc, tsl=tsl: e.matmul(
                        ps[:, bb * ncols:(bb + 1) * ncols], lhsT=self.hnT[:, kc, tsl], rhs=wt[:, kc, 0:ncols],
                        start=(kc == 0), stop=(kc == 7)), r=[wk] + self.hn_keys(), w=[('ps', pi)])
            P.add('act', lambda e, ps=ps, g=g: e.activation(
                out=dst_fn(g), in_=ps[:, 0:4 * ncols].rearrange('p (b h d) -> p b h d', b=4, d=64), func=AF.Copy),
                r=[('ps', pi)], w=[(dkey, g)])

    def finalize(self, acc, acck, dst, dkey, gate=None, first=True, last=True, extra=None):
        P = self.P
        ui = self.rot('U', 2)
        U = self.U[ui]
        bc = self.ps[5]
        ones = self.c['c_ones']

        rd = self.rd16[ui]
        os16 = self.c['c_os']
        pend = [acck] if extra is None else [('ps', 3), ('ps', 4), ('ps', 6)]
        for k_ in pend:
            self.acc_pending[k_] = self.acc_gen_of(k_)
            self.acc_gen[k_] = self.acc_gen_of(k_) + 1

        Rt = self.Rt[ui]
        src_ = U[0:64, :] if extra is not None else acc[0:64, :]
        srck = ('U', ui) if extra is not None else acck

        def stage_a():
            if extra is None:
                P.add('act', lambda e: e.activation(out=rd[64:65, :], in_=acc[64:65, :], func=AF.Copy, bias=1e-18),
                      r=[acck], w=[('rd16', ui)])
            else:
                for k_ in pend:
                    self.acc_pending.pop(k_, None)
                extra(U, ('U', ui))
                P.add('dve', lambda e: e.tensor_scalar(out=rd[64:65, :], in0=U[64:65, :], scalar1=1e-18, scalar2=None,
                                                       op0=ALU.add), r=[('U', ui)], w=[('rd16', ui)])

        def stage_b():
            if extra is None:
                for k_ in pend:
                    self.acc_pending.pop(k_, None)
            P.add('pe', lambda e: e.matmul(bc[0:64, :], lhsT=os16[:, 0:64], rhs=rd[:, :], start=True, stop=True),
                  r=[('rd16', ui), 'c_os'], w=[('ps', 5)])
            P.add('act', lambda e: e.activation(out=bc[0:64, :], in_=bc[0:64, :], func=AF.Ln), r=[('ps', 5)], w=[('ps', 5)])
            P.add('act', lambda e: e.activation(out=Rt[0:64, :], in_=bc[0:64, :], func=AF.Exp, scale=-1.0),
                  r=[('ps', 5)], w=[('Rt', ui)])
            if gate is None:
                P.add('dve', lambda e: e.tensor_tensor(out=dst, in0=src_, in1=Rt[0:64, :], op=ALU.mult),
                      r=[srck, ('Rt', ui)], w=[dkey])
            else:
                P.add('dve', lambda e: e.tensor_tensor(out=U[0:64, :], in0=src_, in1=Rt[0:64, :], op=ALU.mult),
                      r=[srck, ('Rt', ui)], w=[('U', ui)])

        bg = self.ps[7]

        def stage_c():
            if first:
                P.add('dve', lambda e: e.tensor_tensor(out=self.gacc[0:64, :], in0=U[0:64, :], in1=bg[0:64, :], op=ALU.mult),
                      r=[('U', ui), ('ps', 7)], w=['gacc'])
            else:
                P.add('dve', lambda e: e.tensor_tensor(out=U[0:64, :], in0=U[0:64, :], in1=bg[0:64, :], op=ALU.mult),
                      r=[('U', ui), ('ps', 7)], w=[('U', ui)])
                if last:
                    P.add('dve', lambda e: e.tensor_tensor(out=dst, in0=U[0:64, :], in1=self.gacc[0:64, :], op=ALU.add),
                          r=[('U', ui), 'gacc'], w=[dkey])
                else:
                    P.add('dve', lambda e: e.tensor_tensor(out=self.gacc[0:64, :], in0=U[0:64, :], in1=self.gacc[0:64, :],
                                                           op=ALU.add), r=[('U', ui), 'gacc'], w=['gacc'])

        def stage_g():
            hc, sl = gate
            Z = self.c['c_Z']
            P.add('pe', lambda e: e.matmul(bg[0:64, :], lhsT=Z[:, hc * 64:hc * 64 + 64], rhs=self.sigT[:, sl],
                                           start=True, stop=True), r=['sigT', 'c_Z'], w=[('ps', 7)])
        def first_stage():
            assert not self.u_busy[ui], "finalize scratch slot reused too early"
            self.u_busy[ui] = True
            stage_a()

        def last_stage():
            (stage_c if gate is not None else stage_b)()
            self.u_busy[ui] = False
        self.defer(first_stage, PD + 1, fin=True)
        if gate is not None:
            g_delay = max(PD + 2, self.gate_free_at - self.tick)
            c_delay = max(PD + 5, g_delay + 2)
            self.gate_free_at = self.tick + c_delay
            self.defer(stage_g, g_delay, fin=True)
            self.defer(stage_b, PD + 3, fin=True)
            self.defer(last_stage, c_delay, fin=True)
        else:
            self.defer(last_stage, PD + 3, fin=True)

    def outproj_pair(self, w_out, pair, ot_keys):
        P = self.P
        self.flush()
        wkeys = []
        for hh in range(2):
            r0 = (2 * pair + hh) * 64
            src = w_out[r0:r0 + 64, :]
            P.add('pool', lambda e, hh=hh, src=src: e.dma_start(out=self.Wo[hh][:, :], in_=src), w=[('Wo', hh)],
                  dsem=self.ds_wo[hh])
            wkeys.append(('Wo', hh))
        for b in range(NB):
            for half in range(2):
                pi = 6 + self.rot('PJ', 2)
                ps = self.ps[pi]
                for hh in range(2):
                    P.add('pe', lambda e, ps=ps, hh=hh, b=b, half=half: e.matmul(
                        ps[:, :], lhsT=self.OT[hh][0:64, b * 128:(b + 1) * 128],
                        rhs=self.Wo[hh][0:64, half * 512:(half + 1) * 512], start=(hh == 0), stop=(hh == 1)),
                        r=[('Wo', hh)] + ot_keys[hh], w=[('ps', pi)])
                hsl = self.h[:, b, half * 512:(half + 1) * 512]
                P.add('dve', lambda e, ps=ps, hsl=hsl: e.tensor_tensor(out=hsl, in0=ps[:, :], in1=hsl, op=ALU.add),
                      r=[('ps', pi), ('h', b, half)], w=[('h', b, half)])

    def l0_mixer(self, s):
        P, R = self.P, self.R
        self.phase()
        self.attn_common()
        self.rtmp = [R.alloc([512], F32) for _ in range(2)]
        self.rtmp2 = [R.alloc([512], F32) for _ in range(2)]
        mark0 = R.off
        Qz = [R.alloc([S], BF16) for _ in range(2)]
        KT = R.alloc([S], BF16)
        Vt = R.alloc([NB, 2, 65], BF16)
        P.add('pool', lambda e: e.memset(Qz[0][64:128, :], 0.0), w=['qz0'])
        P.add('pool', lambda e: e.memset(Qz[1][0:64, :], 0.0), w=['qz1'])
        w_in = self.fd_w_in
        fb = R.alloc([1, 8], F32)
        lg = R.alloc([NB, 8], F32)
        cl = R.alloc([NB, 8], F32)
        offs = R.alloc([NB, 8], F32)
        tot = R.alloc([NB, 8], F32)
        Bt = R.alloc([8, NB, NB], F32)
        P.add('sp', lambda e: e.dma_start(out=fb[:, 0, :], in_=self.fd_forget_b[0, :].partition_broadcast(128)),
              w=['fb'], dsem=self.ds_misc[1])
        wf, wfk, wfds = self.wsl.get()
        self.load_w(wf[:, :, 0:8], wfk, wfds, w_in[:, 3072:3080])
        psf = self.ps[4]
        for b in range(NB):
            for kc in range(8):
                P.add('pe', lambda e, b=b, kc=kc: e.matmul(psf[:, b * 8:(b + 1) * 8],
                                                           lhsT=self.hnT[:, kc, b * 128:(b + 1) * 128], rhs=wf[:, kc, 0:8],
                                                           start=(kc == 0), stop=(kc == 7)),
                      r=[wfk] + self.hn_keys(), w=[('ps', 4)])
        P.add('dve', lambda e: e.tensor_tensor(out=lg, in0=psf[:, 0:128].rearrange('p (b h) -> p b h', h=8),
                                               in1=fb[:, 0:1, :].broadcast_to([128, NB, 8]), op=ALU.add),
              r=[('ps', 4), 'fb'], w=['lg'])
        P.add('act', lambda e: e.activation(out=lg, in_=lg, func=AF.Exp, scale=-1.0), r=['lg'], w=['lg'])
        P.add('dve', lambda e: e.tensor_scalar(out=lg, in0=lg, scalar1=1.0, scalar2=None, op0=ALU.add), r=['lg'], w=['lg'])
        P.add('act', lambda e: e.activation(out=lg, in_=lg, func=AF.Ln), r=['lg'], w=['lg'])
        lg2 = lg.rearrange('p b h -> p (b h)')
        P.add('pe', lambda e: e.matmul(self.ps[6][:, 0:128], lhsT=self.c['c_utri'][:], rhs=lg2, start=True, stop=True),
              r=['lg', 'c_utri'], w=[('ps', 6)])
        P.add('pe', lambda e: e.matmul(self.ps[7][:, 0:128], lhsT=self.c['c_ones'][:], rhs=lg2, start=True, stop=True),
              r=['lg', 'c_ones'], w=[('ps', 7)])
        P.add('act', lambda e: e.activation(out=tot, in_=self.ps[7][:, 0:128].rearrange('p (b h) -> p b h', h=8),
                                            func=AF.Copy), r=[('ps', 7)], w=['tot'])
        P.add('pool', lambda e: e.memset(offs[:, 0, :], 0.0), w=[('offs', 0)])
        for j in range(1, NB):
            P.add('dve', lambda e, j=j: e.tensor_tensor(out=offs[:, j, :], in0=offs[:, j - 1, :], in1=tot[:, j - 1, :],
                                                        op=ALU.add), r=[('offs', j - 1), 'tot'], w=[('offs', j)])
        allo = [('offs', j) for j in range(NB)]
        P.add('dve', lambda e: e.tensor_tensor(out=cl, in0=self.ps[6][:, 0:128].rearrange('p (b h) -> p b h', h=8),
                                               in1=offs, op=ALU.add), r=[('ps', 6)] + allo, w=['cl'])
        for hh in range(8):
            for qb in range(NB):
                P.add('dve', lambda e, hh=hh, qb=qb: e.tensor_scalar(
                    out=Bt[:, hh, qb, :], in0=cl[:, :, hh], scalar1=offs[:, qb, hh:hh + 1], scalar2=None,
                    op0=ALU.subtract), r=['cl'] + allo, w=[('Bt', hh)])
        wm = self.c['c_wm']
        for hp in range(4):
            c0 = hp * 128
            self.proj_qk_plain(w_in[:, c0:c0 + 128], None, None, split=Qz)
            self.proj_qk_plain(w_in[:, 512 + c0:512 + c0 + 128], KT, 'KT')
            P.add('pool', lambda e: e.memset(Vt[:, :, :, 64:65], 1.0), w=['Vones'])
            self.proj_v(w_in[:, 1024 + c0:1024 + c0 + 128], 128, lambda g: Vt[:, 4 * g:4 * g + 4, :, 0:64], 'Vt')
            qk = [('QT', t) for t in range(NCH)]
            kk = [('KT', t) for t in range(NCH)]
            ot_keys = []
            for hh in range(2):
                head = 2 * hp + hh
                rows = slice(64 * hh, 64 * hh + 64)
                for i in range(NCH):
                    ai = (3, 4, 6)[self.rot('ACC', 3)]
                    acc = self.ps[ai]
                    nkb = 4 * i + 4
                    for kb in range(nkb):
                        self.step()
                        si = self.rot('S', 3)
                        ps = self.ps[si]
                        P.add('pe', lambda e, ps=ps, kb=kb, i=i, hh=hh: e.matmul(
                            ps[:, :], lhsT=KT[:, kb * 128:(kb + 1) * 128], rhs=Qz[hh][:, i * 512:(i + 1) * 512],
                            start=True, stop=True), r=[('KT', kb // 4), (('QZ', hh), i), 'qz0', 'qz1'], w=[('ps', si)])
                        pi = self.rot('Pt', NPT)
                        pt = self.Pt[pi]
                        r0 = max(0, kb - 4 * i)
                        pkeys = []
                        for rr in range(r0, 4):
                            qb = 4 * i + rr
                            P.add('act', lambda e, ps=ps, pt=pt, rr=rr, qb=qb, kb=kb, head=head: e.activation(
                                out=pt[:, rr * 128:(rr + 1) * 128], in_=ps[:, rr * 128:(rr + 1) * 128], func=AF.Exp,
                                scale=0.125, bias=Bt[:, head, qb, kb:kb + 1]),
                                r=[('ps', si), ('Bt', head)], w=[('Pt', pi, rr)])
                            pkeys.append(('Pt', pi, rr))
                        c_lo = r0 * 128
                        if kb >= 4 * i:
                            r = kb - 4 * i
                            P.add('dve', lambda e, pt=pt, r=r, c_lo=c_lo: e.tensor_tensor(
                                out=pt[:, c_lo:512], in0=pt[:, c_lo:512], in1=wm[:, 384 - 128 * r + c_lo:384 - 128 * r + 512],
                                op=ALU.mult), r=pkeys + ['c_wm'], w=pkeys)
                        def back(acc=acc, pt=pt, kb=kb, hh=hh, c_lo=c_lo, st=(kb == 0), sp=(kb == nkb - 1), pkeys=pkeys, ai=ai,
                                 gen=self.acc_gen_of(('ps', ai))):
                            self.acc_start(('ps', ai), gen)
                            P.add('pe', lambda e: e.matmul(
                                acc[0:65, c_lo:512], lhsT=Vt[:, kb, hh, :], rhs=pt[:, c_lo:512], start=st, stop=sp),
                                r=pkeys + [('Vt', kb // 4), 'Vones'], w=[('ps', ai)])
                        self.defer(back, PD)
                    self.finalize(acc, ('ps', ai), self.OT[hh][0:64, i * 512:(i + 1) * 512], ('OT', hh, i))
                ot_keys.append([('OT', hh, i) for i in range(NCH)])
            self.outproj_pair(self.fd_w_out, hp, ot_keys)
        self.flush()
        P.barrier()
        R.off = mark0
        Qz = [R.alloc([S], BF16) for _ in range(2)]
        KT = R.alloc([S], BF16)
        P.add('pool', lambda e: e.memset(Qz[0][64:128, :], 0.0), w=['qz0'])
        P.add('pool', lambda e: e.memset(Qz[1][0:64, :], 0.0), w=['qz1'])
        Vd = [R.alloc([NB, 2, 65], BF16) for _ in range(3)]
        self.Vd = Vd
        dm3 = self.c['c_dm'][:, :].rearrange('p (a b) -> p a b', b=128)
        for dp in range(4):
            c0 = dp * 128
            self.proj_qk_rot(w_in[:, 1536 + c0:1536 + c0 + 128], self.fd_w_sw[:, c0:c0 + 128], None, None, split=Qz)
            self.proj_qk_rot(w_in[:, 2048 + c0:2048 + c0 + 128], self.fd_w_sw[:, 512 + c0:512 + c0 + 128], KT, 'KT')
            for pat in range(3):
                P.add('pool', lambda e, pat=pat: e.memset(Vd[pat][:, :, :, 64:65], 1.0), w=[('Vones', pat)])
            vsrc = w_in[:, 2560 + c0:2560 + c0 + 128]
            self.proj_v(vsrc, 128, lambda g: Vd[0][:, 4 * g:4 * g + 4, :, 0:64], ('Vd', 0))
            self.proj_v(vsrc, 128, lambda g: Vd[1][:, 4 * g:4 * g + 4, :, 0:64], ('Vd', 1),
                        tok_fn=lambda blk: ssl((blk // 4) + 512 * (blk % 4), 128, 4))
            self.proj_v(vsrc, 128, lambda g: Vd[2][:, 4 * g:4 * g + 4, :, 0:64], ('Vd', 2),
                        tok_fn=lambda blk: ssl(blk, 128, 16))
            ot_keys = []
            for hh in range(2):
                rows = slice(64 * hh, 64 * hh + 64)
                for i in range(NCH):
                    acc1, acc4, acc16 = self.ps[3], self.ps[4], self.ps[6]
                    for g in range(2):
                        slots = []
                        for qb in (4 * i + 2 * g, 4 * i + 2 * g + 1):
                            hasp = qb >= 1
                            kb = max(qb - 1, 0)
                            qsl = slice(qb * 128, qb * 128 + 128)
                            slots.append((qsl, qsl, (acc1, ('ps', 3), (qb - 4 * i) * 128, 0, qb, True, not hasp)))
                            slots.append((slice(kb * 128, kb * 128 + 128), qsl,
                                          (acc1, ('ps', 3), (qb - 4 * i) * 128, 0, kb, False, True) if hasp else None))
                        self.dil_group(KT, Qz[hh], rows, hh, slots, dm3, ['c_dm'], 128)
                    for g in range(2):
                        slots = []
                        for r_ in (2 * g, 2 * g + 1):
                            ip = max(i - 1, 0)
                            qsl = ssl(r_ + 512 * i, 128, 4)
                            ksl = ssl(r_ + 512 * ip, 128, 4)
                            slots.append((qsl, qsl, (acc4, ('ps', 4), r_ * 128, 1, r_ * 4 + i, True, i == 0)))
                            slots.append((ksl, qsl, (acc4, ('ps', 4), r_ * 128, 1, r_ * 4 + ip, False, True) if i >= 1 else None))
                        self.dil_group(KT, Qz[hh], rows, hh, slots, dm3, ['c_dm'], 128)
                    slots = []
                    for r_ in range(16):
                        slots.append((ssl(r_, 128, 16), ssl(r_ + 512 * i, 32, 16),
                                      (acc16, ('ps', 6), r_ * 32, 2, r_, True, True)))
                    m16 = self.c['c_wm'][:, 384 + 32 * i:416 + 32 * i].unsqueeze(1).broadcast_to([128, 16, 32])
                    self.dil_group(KT, Qz[hh], rows, hh, slots, m16, ['c_wm'], 32)

                    def extra(U, uk):
                        P.add('act', lambda e: e.activation(out=U[0:65, :], in_=self.ps[3][0:65, :], func=AF.Copy),
                              r=[('ps', 3)], w=[uk])
                        P.add('dve', lambda e: e.tensor_tensor(
                            out=U[0:65, :].rearrange('p (q r) -> p r q', r=4),
                            in0=self.ps[4][0:65, :].rearrange('p (r q) -> p r q', r=4),
                            in1=U[0:65, :].rearrange('p (q r) -> p r q', r=4), op=ALU.add), r=[('ps', 4), uk], w=[uk])
                        P.add('dve', lambda e: e.tensor_tensor(
                            out=U[0:65, :].rearrange('p (q r) -> p r q', r=16),
                            in0=self.ps[6][0:65, :].rearrange('p (r q) -> p r q', r=16),
                            in1=U[0:65, :].rearrange('p (q r) -> p r q', r=16), op=ALU.add), r=[('ps', 6), uk], w=[uk])
                    self.finalize(None, None, self.OT[hh][0:64, i * 512:(i + 1) * 512], ('OT', hh, i), extra=extra)
                ot_keys.append([('OT', hh, i) for i in range(NCH)])
            self.outproj_pair(self.fd_w_out, 4 + dp, ot_keys)

    def dil_group(self, KT, QT, rows, hh, slots, mask_ap, mkeys, width):
        P = self.P
        Vd = self.Vd
        allq = [(('QZ', hh), t) for t in range(NCH)] + ['qz0', 'qz1']
        allk = [('KT', t) for t in range(NCH)]
        self.step()
        si = self.rot('Sd', 3)
        ps = self.ps[si]
        for j, (ksl, qsl, pv) in enumerate(slots):
            P.add('pe', lambda e, j=j, ksl=ksl, qsl=qsl: e.matmul(
                ps[:, j * width:(j + 1) * width], lhsT=KT[:, ksl], rhs=QT[:, qsl],
                start=True, stop=True), r=allk + allq, w=[('ps', si)])
        pi = self.rot('Pt', NPT)
        pt = self.Pt[pi]
        P.add('act', lambda e: e.activation(out=pt[:, :], in_=ps[:, :], func=AF.Exp, scale=0.125),
              r=[('ps', si)], w=[('Pt', pi, 0)])
        ptv = pt[:, :].rearrange('p (a b) -> p a b', b=width)
        P.add('dve', lambda e: e.tensor_tensor(out=ptv, in0=ptv, in1=mask_ap, op=ALU.mult),
              r=[('Pt', pi, 0)] + mkeys, w=[('Pt', pi, 0)])
        gens = {k_: self.acc_gen_of(k_) for k_ in (('ps', 3), ('ps', 4), ('ps', 6))}

        def back():
            for j, (ksl, qsl, pv) in enumerate(slots):
                if pv is None:
                    continue
                acc, ak, col0, vpat, vblk, st, sp = pv
                self.acc_start(ak, gens[ak])
                vkeys = [(('Vd', vpat), g) for g in range(4)] + [('Vones', vpat)]
                P.add('pe', lambda e, j=j, acc=acc, col0=col0, vpat=vpat, vblk=vblk, st=st, sp=sp: e.matmul(
                    acc[0:65, col0:col0 + width], lhsT=Vd[vpat][:, vblk, hh, :], rhs=pt[:, j * width:(j + 1) * width],
                    start=st, stop=sp), r=[('Pt', pi, 0)] + vkeys, w=[ak])
        self.defer(back, PD)

    def dump_h(self, s):
        P = self.P
        for b in range(NB):
            o = P.add('sp', lambda e, b=b: e.dma_start(out=self.y[s, b * 128:(b + 1) * 128, :], in_=self.h[:, b, :]),
                      r=self.hk(b), w=[('y', s, b)], dsem=self.ds_y)
        self.P.final.append(o)

    def build(self):
        P = self.P
        st = self.stages
        self.load_consts()
        for s in range(self.n_seq):
            self.phase()
            self.load_seq(s)
            self.rotary_tables(s)
            if 'l0' in st:
                self.phase()
                self.norm(self.mix_norm[0:1, :])
                self.l0_mixer(s)
                if 'l0ffn' in st:
                    self.phase()
                    self.norm(self.ffn_norm[0:1, :])
                    self.phase()
                    self.ffn_setup()
                    if 'noffn' not in st:
                        self.ffn(self.dense_w_gate, self.dense_w_up, self.dense_w_down, None)
                    if 'nople' not in st:
                        self.ple(s, 0)
            if 'l1' in st:
                self.phase()
                self.norm(self.mix_norm[1:2, :])
                self.nsa_mixer(s)
                if 'l1ffn' in st:
                    self.moe()
                    self.ple(s, 1)
            if 'final' in st:
                self.phase()
                self.norm(self.final_norm[0:1, :], final_out=s)
            else:
                self.dump_h(s)
        if 'final' in st:
            last = [o for o in P.by_eng['sp'] if o.dsem is self.ds_y][-1]
            P.final.append(last)
        P.emit()
        return self.nc


SWAP64 = np.concatenate([np.arange(32, 64), np.arange(0, 32)])


def _swap_cols(w):
    d, n = w.shape
    idx = (np.arange(n) // 64) * 64 + SWAP64[np.arange(n) % 64]
    return np.ascontiguousarray(w[:, idx])


def host_inputs(inputs, n_cores, n_seq, consts):
    f = lambda a: np.ascontiguousarray(np.asarray(a, dtype=np.float32))
    shared = {}
    for k in ('mix_norm', 'ffn_norm', 'ple_norm', 'ple_gate_w', 'ple_proj_w'):
        shared[k] = f(inputs[k])
    shared['final_norm'] = f(inputs['final_norm']).reshape(1, D)
    for k in ('fd_w_in', 'fd_forget_b', 'fd_w_out', 'dense_w_gate', 'dense_w_up', 'dense_w_down', 'nsa_w_in',
              'nsa_pos_k', 'nsa_w1_k', 'nsa_w2_k', 'nsa_pos_v', 'nsa_w1_v', 'nsa_w2_v', 'nsa_w_out',
              'moe_w_router', 'moe_b_router', 'moe_w_gate', 'moe_w_up', 'moe_w_down'):
        shared[k] = f(inputs[k])[0]
    shared['nsa_b1_k'] = f(inputs['nsa_b1_k'])[0].reshape(256, 1)
    shared['nsa_b1_v'] = f(inputs['nsa_b1_v'])[0].reshape(256, 1)
    shared['fd_w_sw'] = _swap_cols(shared['fd_w_in'][:, 1536:2560])
    nw = shared['nsa_w_in']
    shared['nsa_w_sw'] = _swap_cols(np.concatenate([nw[:, 0:1280], nw[:, 1536:1792], nw[:, 2048:2304]], axis=1))
    shared.update(consts)
    x = f(inputs['x'])
    p = f(inputs['p'])
    pos = np.ascontiguousarray(np.asarray(inputs['positions'], dtype=np.int32))
    maps = []
    for c in range(n_cores):
        m = dict(shared)
        m['x'] = np.ascontiguousarray(x[c * n_seq:(c + 1) * n_seq])
        m['p'] = np.ascontiguousarray(p[:, c * n_seq:(c + 1) * n_seq])
        m['positions'] = np.ascontiguousarray(pos[c * n_seq:(c + 1) * n_seq])
        maps.append(m)
    return maps


_CACHE = {}


def kernel(**inputs):
    n_cores, n_seq = 8, 2
    if 'nc' not in _CACHE:
        b = Builder(n_seq=n_seq, stages=('l0', 'l0ffn', 'l1', 'l1ffn', 'final'))
        _CACHE['nc'] = b.build()
        _CACHE['b'] = b
    nc = _CACHE['nc']
    maps = host_inputs(inputs, n_cores, n_seq, make_consts())
    res = run_bass_kernel_spmd(nc, maps, core_ids=list(range(n_cores)))
    out = np.concatenate([np.asarray(r['y'], dtype=np.float32) for r in res.results], axis=0)
    return out


def _ffn_setup(self):
    R = self.R
    self.Wg = [R.alloc([8, 512], BF16) for _ in range(2)]
    self.Wu = [R.alloc([8, 512], BF16) for _ in range(2)]
    self.Wd = [R.alloc([4, D], BF16) for _ in range(2)]
    self.At = [R.alloc([4, 512], BF16) for _ in range(2)]
    self.Sg = [R.alloc([512], F32) for _ in range(2)]


def _ffn(self, wg, wu, wd, comb=None):
    P, R = self.P, self.R
    for fg in range(7):
        wi = self.rot('ffnw', 2)
        Wg, Wu, Wd = self.Wg[wi], self.Wu[wi], self.Wd[wi]
        fsl = slice(fg * 512, (fg + 1) * 512)
        self.load_w(Wg, ('Wg', wi), self.ds_ffn[wi], wg[:, fsl])
        self.load_w(Wu, ('Wu', wi), self.ds_ffn[2 + wi], wu[:, fsl])
        self.load_w(Wd, ('Wd', wi), self.ds_ffn[4 + wi], wd[fsl, :])
        for tc in range(NCH):
            ai = self.rot('At', 2)
            At = self.At[ai]
            for fc in range(4):
                gi = self.rot('Gp', 2)
                ui = 2 + self.rot('Up', 2)
                for (pi, W_, wk) in ((gi, Wg, ('Wg', wi)), (ui, Wu, ('Wu', wi))):
                    ps = self.ps[pi]
                    for kc in range(8):
                        P.add('pe', lambda e, ps=ps, W_=W_, kc=kc, fc=fc, tc=tc: e.matmul(
                            ps[:, :], lhsT=W_[:, kc, fc * 128:(fc + 1) * 128], rhs=self.hnT[:, kc, tc * 512:(tc + 1) * 512],
                            start=(kc == 0), stop=(kc == 7)), r=[wk] + self.hn_keys(tc), w=[('ps', pi)])
                si = self.rot('Sg', 2)
                Sg = self.Sg[si]
                P.add('act', lambda e, Sg=Sg, gi=gi: e.activation(out=Sg, in_=self.ps[gi][:, :], func=AF.Silu),
                      r=[('ps', gi)], w=[('Sg', si)])
                P.add('dve', lambda e, Sg=Sg, ui=ui, At=At, fc=fc: e.tensor_tensor(
                    out=At[:, fc, :], in0=self.ps[ui][:, :], in1=Sg, op=ALU.mult),
                    r=[('ps', ui), ('Sg', si)], w=[('At', ai, fc)])
            akeys = [('At', ai, fc) for fc in range(4)]
            for bb in range(4):
                b = 4 * tc + bb
                for half in range(2):
                    yi = 4 + self.rot('Yp', 3)
                    ps = self.ps[yi]
                    for fc in range(4):
                        P.add('pe', lambda e, ps=ps, At=At, Wd=Wd, fc=fc, bb=bb, half=half: e.matmul(
                            ps[:, :], lhsT=At[:, fc, bb * 128:(bb + 1) * 128], rhs=Wd[:, fc, half * 512:(half + 1) * 512],
                            start=(fc == 0), stop=(fc == 3)), r=akeys + [('Wd', wi)], w=[('ps', yi)])
                    hsl = self.h[:, b, half * 512:(half + 1) * 512]
                    if comb is None:
                        P.add('dve', lambda e, ps=ps, hsl=hsl: e.tensor_tensor(out=hsl, in0=ps[:, :], in1=hsl, op=ALU.add),
                              r=[('ps', yi), ('h', b, half)], w=[('h', b, half)])
                    else:
                        P.add('dve', lambda e, ps=ps, hsl=hsl, b=b: e.scalar_tensor_tensor(
                            out=hsl, in0=ps[:, :], scalar=comb[:, b:b + 1], in1=hsl, op0=ALU.mult, op1=ALU.add),
                            r=[('ps', yi), ('h', b, half), 'comb'], w=[('h', b, half)])


def _ple(self, s, layer):
    P, R = self.P, self.R
    self.phase()
    self.norm(self.ple_norm[layer:layer + 1, :])
    self.phase()
    Wpg = R.alloc([8, D], BF16)
    Wpp = R.alloc([2, D], BF16)
    p32 = R.alloc([NB, 256], F32)
    pT = R.alloc([2, S], BF16)
    sg = [R.alloc([512], F32) for _ in range(2)]
    self.load_w(Wpg, 'Wpg', self.ds_ffn[0], self.ple_gate_w[layer])
    self.load_w(Wpp, 'Wpp', self.ds_ffn[1], self.ple_proj_w[layer])
    P.add('sp', lambda e: e.dma_start(out=p32, in_=self.p[layer, s].rearrange('(b p) c -> p b c', p=128)),
          w=['p32'], dsem=self.ds_misc[2])
    ident = self.c['c_ident']
    for g in range(NB // 2):
        pi = 6 + self.rot('PJ', 2)
        ps = self.ps[pi]
        for bb in range(2):
            b = 2 * g + bb
            for c2 in range(2):
                j = bb * 2 + c2
                P.add('pe', lambda e, ps=ps, j=j, b=b, c2=c2: e.transpose(
                    out=ps[:, j * 128:(j + 1) * 128], in_=p32[:, b, c2 * 128:(c2 + 1) * 128], identity=ident[:]),
                    r=['p32', 'c_ident'], w=[('ps', pi)])
        P.add('act', lambda e, ps=ps, g=g: e.activation(
            out=pT[:, :, g * 256:(g + 1) * 256].rearrange('p c (b t) -> p b c t', b=2),
            in_=ps[:, :].rearrange('p (b c t) -> p b c t', b=2, c=2), func=AF.Copy), r=[('ps', pi)], w=[('pT', g)])
    for b in range(NB):
        for half in range(2):
            gi = self.rot('Gp', 2)
            ui = 2 + self.rot('Up', 2)
            csl = slice(half * 512, (half + 1) * 512)
            for kc in range(8):
                P.add('pe', lambda e, gi=gi, kc=kc, b=b, csl=csl: e.matmul(
                    self.ps[gi][:, :], lhsT=self.hnT[:, kc, b * 128:(b + 1) * 128], rhs=Wpg[:, kc, csl],
                    start=(kc == 0), stop=(kc == 7)), r=['Wpg', ('hnT', b)], w=[('ps', gi)])
            for c2 in range(2):
                P.add('pe', lambda e, ui=ui, c2=c2, b=b, csl=csl: e.matmul(
                    self.ps[ui][:, :], lhsT=pT[:, c2, b * 128:(b + 1) * 128], rhs=Wpp[:, c2, csl],
                    start=(c2 == 0), stop=(c2 == 1)), r=['Wpp', ('pT', b // 2)], w=[('ps', ui)])
            si = self.rot('Sg', 2)
            P.add('act', lambda e, si=si, gi=gi: e.activation(out=sg[si], in_=self.ps[gi][:, :], func=AF.Sigmoid),
                  r=[('ps', gi)], w=[('sg', si)])
            P.add('dve', lambda e, si=si, ui=ui: e.tensor_tensor(out=sg[si], in0=self.ps[ui][:, :], in1=sg[si], op=ALU.mult),
                  r=[('ps', ui), ('sg', si)], w=[('sg', si)])
            hsl = self.h[:, b, csl]
            P.add('pool', lambda e, si=si, hsl=hsl: e.tensor_tensor(out=hsl, in0=hsl, in1=sg[si], op=ALU.add),
                  r=[('sg', si), ('h', b, half)], w=[('h', b, half)])


Builder.ffn = _ffn
Builder.ffn_setup = _ffn_setup
Builder.ple = _ple


def _unit(self, lhsT, rhs, rkeys, mask_ap, mkeys, v_ap, vkeys, acc, ai, st, sp, aug=None):
    P = self.P
    self.step()
    si = self.rot('S', 3)
    ps = self.ps[si]
    P.add('pe', lambda e: e.matmul(ps[:, :], lhsT=lhsT, rhs=rhs, start=True, stop=(aug is None)), r=rkeys, w=[('ps', si)])
    if aug is not None:
        l2, r2, k2 = aug
        P.add('pe', lambda e: e.matmul(ps[:, :], lhsT=l2, rhs=r2, start=False, stop=True), r=k2, w=[('ps', si)])
    pi = self.rot('Pt', NPT)
    pt = self.Pt[pi]
    P.add('act', lambda e: e.activation(out=pt[:, :], in_=ps[:, :], func=AF.Exp, scale=0.125), r=[('ps', si)], w=[('Pt', pi, 0)])
    if mask_ap is not None:
        meng = 'pool' if self.rot('meng', 3) == 2 else 'dve'
        P.add(meng, lambda e: e.tensor_tensor(out=pt[:, :], in0=pt[:, :], in1=mask_ap, op=ALU.mult),
              r=[('Pt', pi, 0)] + mkeys, w=[('Pt', pi, 0)])
    gen = self.acc_gen_of(('ps', ai))

    def back():
        self.acc_start(('ps', ai), gen)
        P.add('pe', lambda e: e.matmul(acc[0:65, :], lhsT=v_ap, rhs=pt[:, :], start=st, stop=sp),
              r=[('Pt', pi, 0)] + vkeys, w=[('ps', ai)])
    self.defer(back, PD)


def _nsa_mixer(self, s):
    P, R = self.P, self.R
    self.phase()
    self.attn_common()
    self.rtmp = [R.alloc([512], F32) for _ in range(2)]
    self.sigT = R.alloc([S], BF16)
    P.add('pool', lambda e: e.memset(self.sigT[:, :], 0.0), w=['sigT'])
    self.gacc = R.alloc([512], F32, parts=64)
    Qz = [R.alloc([S], BF16) for _ in range(4)]
    KA = R.alloc([S], BF16)
    KB = R.alloc([S], BF16)
    Vs = R.alloc([NB, 1, 65], BF16)
    Vw = R.alloc([NB, 1, 65], BF16)
    W1c = R.alloc([8, 256], BF16, parts=64)
    W2k = R.alloc([2, 64], BF16)
    W2v = R.alloc([2, 64], BF16)
    gel = [R.alloc([128], BF16) for _ in range(2)]
    xs = R.alloc([128], F32)
    x2 = R.alloc([128], F32)
    hb = R.alloc([2], F32)
    b1t = R.alloc([2], F32)
    pos32 = R.alloc([64], F32, parts=32)
    posT = R.alloc([32], BF16, parts=64)
    kcmpT = R.alloc([128], BF16)
    vcmp = R.alloc([65], BF16)
    E4 = R.alloc([4, 128], F32)
    pcs = R.alloc([128], F32)
    den4 = R.alloc([4], F32)
    imp = R.alloc([32], F32)
    impm = R.alloc([32], F32)
    top8 = R.alloc([8], F32)
    sel = R.alloc([96], F32)
    w_in, w_sw = self.nsa_w_in, self.nsa_w_sw
    ident = self.c['c_ident']
    wm = self.c['c_wm']
    wt, wk, wds = self.wsl.get()
    self.load_w(wt[:, :, 0:48], wk, wds, w_in[:, 2560:2608])

    def evac_g(tc, ps, pk):
        P.add('act', lambda e: e.activation(out=self.sigT[0:48, tc * 512:(tc + 1) * 512], in_=ps[0:48, :], func=AF.Sigmoid),
              r=[pk], w=['sigT'])
    self.proj_fm(wt, wk, 48, evac_g)
    if NSA_STOP == 1:
        return
    P.add('pool', lambda e: e.memset(kcmpT[:, :], 0.0), w=['kcmpT'])
    P.add('pool', lambda e: e.memset(sel[:, :], 0.0), w=['sel'])
    for hh in range(4):
        P.add('pool', lambda e, hh=hh: e.memset(Qz[hh][64:128, :], 0.0), w=[(('NT', hh), t) for t in range(NCH)])
    P.add('pool', lambda e: e.memset(KA[64:128, :], 0.0), w=['KAz'])
    P.add('pool', lambda e: e.memset(KB[64:128, :], 0.0), w=['KBz'])
    P.add('sp', lambda e: e.dma_start(out=KA[64:96, :], in_=self.cst['c_E'][0:32, :]), r=['KAz'], w=['KAe'], dsem=self.ds_misc[0])
    P.add('pool', lambda e: e.memset(vcmp[:, :], 0.0), w=['vcmp'])
    P.add('pool', lambda e: e.memset(Vs[:, :, :, 64:65], 1.0), w=['Vs1'])
    P.add('pool', lambda e: e.memset(Vw[:, :, :, 64:65], 1.0), w=['Vw1'])
    P.add('pool', lambda e: e.memset(gel[0][:, :], 0.0), w=[('gel', 0)])
    P.add('pool', lambda e: e.memset(gel[1][:, :], 0.0), w=[('gel', 1)])
    allq = [[(('QZ', hh), t) for t in range(NCH)] for hh in range(4)]
    KAk = [('KA', t) for t in range(NCH)]
    KBk = [('KB', t) for t in range(NCH)]
    for g in range(4):
        for hh in range(4):
            c0 = (4 * g + hh) * 64
            self.proj_qk_rot(w_in[:, c0:c0 + 64], w_sw[:, c0:c0 + 64], Qz[hh], ('QZ', hh), ncols=64)
        self.proj_qk_rot(w_in[:, 1024 + 64 * g:1024 + 64 * g + 64], w_sw[:, 1024 + 64 * g:1024 + 64 * g + 64], KA, 'KA', ncols=64)
        wt, wk, wds = self.wsl.get()
        self.load_w(wt[:, :, 0:64], wk, wds, w_in[:, 1280 + 64 * g:1280 + 64 * g + 64])

        def evac_vc(tc, ps, pk):
            P.add('act', lambda e: e.activation(out=KB[0:64, tc * 512:(tc + 1) * 512], in_=ps[0:64, :], func=AF.Copy),
                  r=[pk], w=[('KB', tc)])
        self.proj_fm(wt, wk, 64, evac_vc)
        for which in range(2):
            src = KA if which == 0 else KB
            skeys = KAk if which == 0 else KBk
            w1 = self.nsa_w1_k if which == 0 else self.nsa_w1_v
            w2 = self.nsa_w2_k if which == 0 else self.nsa_w2_v
            b1 = self.nsa_b1_k if which == 0 else self.nsa_b1_v
            posd = self.nsa_pos_k if which == 0 else self.nsa_pos_v
            P.add('sp', lambda e, posd=posd: e.dma_start(out=pos32[0:32, :], in_=posd), w=['pos32'], dsem=self.ds_misc[0])
            P.add('sp', lambda e, b1=b1: e.dma_start(out=b1t[:, 0:1], in_=b1[0:128, :]), w=[('b1t', 0)], dsem=self.ds_misc[1])
            P.add('sp', lambda e, b1=b1: e.dma_start(out=b1t[:, 1:2], in_=b1[128:256, :]), w=[('b1t', 1)], dsem=self.ds_misc[2])
            if which == 0:
                self.load_w(W2k[:, :, :], 'W2a', self.ds_misc[3], w2)
                w2keys = ['W2a']
            else:
                self.load_w(W2v[:, :, :], 'W2v', self.ds_misc[3], w2)
                w2keys = ['W2v']
            P.add('pe', lambda e: e.transpose(out=self.ps[5][0:64, 0:32], in_=pos32[0:32, 0:64], identity=ident[0:32, 0:32]),
                  r=['pos32', 'c_ident'], w=[('ps', 5)])
            P.add('act', lambda e: e.activation(out=posT[0:64, :], in_=self.ps[5][0:64, 0:32], func=AF.Copy),
                  r=[('ps', 5)], w=['posT'])
            first = True
            for ic in range(4):
                srcw = w1[ic * 512:(ic + 1) * 512, :].rearrange('(i d) c -> d i c', d=64)
                P.add('pool', lambda e, srcw=srcw: e.dma_start(out=W1c[0:64, :, :], in_=srcw), w=['W1c'], dsem=self.ds_w[4])
                for ii in range(8):
                    i = ic * 8 + ii
                    for hc in range(2):
                        P.add('pe', lambda e, ii=ii, i=i, hc=hc, src=src: e.matmul(
                            self.ps[6 + hc][:, 0:127], lhsT=W1c[0:64, ii, hc * 128:(hc + 1) * 128],
                            rhs=src[0:64, ssl(i, 127, 16)], start=(i == 0), stop=(i == 31)),
                            r=['W1c'] + skeys, w=[('ps', 6 + hc)])
                        P.add('pe', lambda e, ii=ii, i=i, hc=hc, st=first: e.matmul(
                            self.ps[5][:, hc:hc + 1], lhsT=W1c[0:64, ii, hc * 128:(hc + 1) * 128],
                            rhs=posT[0:64, i:i + 1], start=st, stop=(i == 31), skip_group_check=True),
                            r=['W1c', 'posT'], w=[('ps', 5)])
                        first = False
            P.add('dve', lambda e: e.tensor_tensor(out=hb[:, 0:2], in0=self.ps[5][:, 0:2], in1=b1t[:, 0:2], op=ALU.add),
                  r=[('ps', 5), ('b1t', 0), ('b1t', 1)], w=['hb'])
            for hc in range(2):
                pk = ('ps', 6 + hc)
                psh = self.ps[6 + hc]
                P.add('act', lambda e, psh=psh, hc=hc: e.activation(out=xs[:, 0:127], in_=psh[:, 0:127], func=AF.Identity,
                                                                    bias=hb[:, hc:hc + 1]), r=[pk, 'hb'], w=['xs'])
                P.add('dve', lambda e: e.tensor_tensor(out=x2[:, 0:127], in0=xs[:, 0:127], in1=xs[:, 0:127], op=ALU.mult),
                      r=['xs'], w=['x2'])
                P.add('dve', lambda e: e.tensor_scalar(out=x2[:, 0:127], in0=x2[:, 0:127], scalar1=0.044715, scalar2=1.0,
                                                       op0=ALU.mult, op1=ALU.add), r=['x2'], w=['x2'])
                P.add('dve', lambda e: e.tensor_tensor(out=x2[:, 0:127], in0=x2[:, 0:127], in1=xs[:, 0:127], op=ALU.mult),
                      r=['x2', 'xs'], w=['x2'])
                P.add('act', lambda e: e.activation(out=x2[:, 0:127], in_=x2[:, 0:127], func=AF.Sigmoid,
                                                    scale=float(2.0 * np.sqrt(2.0 / np.pi))), r=['x2'], w=['x2'])
                P.add('dve', lambda e, hc=hc: e.tensor_tensor(out=gel[hc][:, 0:127], in0=xs[:, 0:127], in1=x2[:, 0:127],
                                                              op=ALU.mult), r=['x2', 'xs'], w=[('gel', hc)])
            if which == 0:
                for hc in range(2):
                    P.add('pe', lambda e, hc=hc: e.matmul(self.ps[6][0:64, 0:127], lhsT=W2k[:, hc, :], rhs=gel[hc][:, 0:127],
                                                          start=(hc == 0), stop=(hc == 1)),
                          r=[('gel', hc)] + w2keys, w=[('ps', 6)])
                P.add('act', lambda e: e.activation(out=kcmpT[0:64, 0:127], in_=self.ps[6][0:64, 0:127], func=AF.Copy),
                      r=[('ps', 6)], w=['kcmpT'])
            else:
                for hc in range(2):
                    P.add('pe', lambda e, hc=hc: e.matmul(self.ps[6][0:127, 0:64], lhsT=gel[hc][:, 0:127], rhs=W2v[:, hc, :],
                                                          start=(hc == 0), stop=(hc == 1)),
                          r=[('gel', hc)] + w2keys, w=[('ps', 6)])
                P.add('act', lambda e: e.activation(out=vcmp[0:127, 0:64], in_=self.ps[6][0:127, 0:64], func=AF.Copy),
                      r=[('ps', 6)], w=['vcmp'])
                P.add('pool', lambda e: e.memset(vcmp[:, 64:65], 1.0), r=[], w=['vcmp1'])
        if NSA_STOP == 2:
            continue
        def imp_stage(qb):
            si = self.rot('S', 3)
            ps = self.ps[si]
            for hh in range(4):
                P.add('pe', lambda e, ps=ps, hh=hh, qb=qb: e.matmul(
                    ps[:, hh * 128:(hh + 1) * 128], lhsT=Qz[hh][:, qb * 128:(qb + 1) * 128], rhs=kcmpT[:, :],
                    start=True, stop=True), r=['kcmpT'] + allq[hh] + [(('NT', hh), t) for t in range(NCH)], w=[('ps', si)])
            P.add('act', lambda e, ps=ps: e.activation(out=E4, in_=ps[:, :].rearrange('p (h n) -> p h n', h=4), func=AF.Exp,
                                                       scale=0.125), r=[('ps', si)], w=['E4'])
            m0s = self.c['c_m0'][:, 120 - 8 * qb:248 - 8 * qb].unsqueeze(1).broadcast_to([128, 4, 128])
            P.add('dve', lambda e, m0s=m0s: e.tensor_tensor(out=E4, in0=E4, in1=m0s, op=ALU.mult), r=['E4', 'c_m0'], w=['E4'])
            P.add('dve', lambda e: e.tensor_reduce(out=den4, in_=E4, axis=AX.X, op=ALU.add), r=['E4'], w=['den4'])
            P.add('dve', lambda e: e.tensor_scalar(out=den4, in0=den4, scalar1=1e-30, scalar2=None, op0=ALU.max),
                  r=['den4'], w=['den4'])
            P.add('dve', lambda e: e.reciprocal(out=den4, in_=den4), r=['den4'], w=['den4'])
            P.add('dve', lambda e: e.tensor_scalar(out=pcs, in0=E4[:, 0, :], scalar1=den4[:, 0:1], scalar2=None, op0=ALU.mult),
                  r=['E4', 'den4'], w=['pcs'])
            for hh in range(1, 4):
                P.add('dve', lambda e, hh=hh: e.scalar_tensor_tensor(out=pcs, in0=E4[:, hh, :], scalar=den4[:, hh:hh + 1],
                                                                     in1=pcs, op0=ALU.mult, op1=ALU.add),
                      r=['E4', 'den4', 'pcs'], w=['pcs'])
            P.add('dve', lambda e: e.tensor_reduce(out=imp, in_=pcs[:, :].rearrange('p (j f) -> p j f', f=4), axis=AX.X,
                                                   op=ALU.add), r=['pcs'], w=['imp'])
            P.add('dve', lambda e: e.tensor_tensor(out=imp[:, 1:32], in0=imp[:, 1:32], in1=pcs[:, ssl(3, 31, 4)], op=ALU.add),
                  r=['pcs', 'imp'], w=['imp'])
            P.add('dve', lambda e, qb=qb: e.tensor_tensor(out=impm, in0=imp, in1=self.c['c_keep'][:, 30 - 2 * qb:62 - 2 * qb],
                                                          op=ALU.mult), r=['imp', 'c_keep'], w=['impm'])
            P.add('dve', lambda e, qb=qb: e.tensor_tensor(out=impm, in0=impm, in1=self.c['c_addv'][:, 30 - 2 * qb:62 - 2 * qb],
                                                          op=ALU.add), r=['impm', 'c_addv'], w=['impm'])
            P.add('dve', lambda e: e.memset(impm[:, 0:1], 1e6), r=['impm'], w=['impm'])
            P.add('dve', lambda e: e.max(out=top8, in_=impm), r=['impm'], w=['top8'])
            P.add('dve', lambda e: e.tensor_scalar(out=sel[:, 64:96], in0=impm, scalar1=top8[:, 7:8], scalar2=None, op0=ALU.is_ge),
                  r=['impm', 'top8'], w=['sel'])
            P.add('pe', lambda e: e.transpose(out=self.ps[5][0:96, 0:128], in_=sel[:, 0:96], identity=ident[:]),
                  r=['sel', 'c_ident'], w=[('ps', 5)])
            for hh in range(4):
                P.add('dve', lambda e, qb=qb, hh=hh: e.tensor_scalar(
                    out=Qz[hh][64:96, qb * 128:(qb + 1) * 128], in0=self.ps[5][64:96, 0:128],
                    scalar1=30000.0, scalar2=-30000.0, op0=ALU.mult, op1=ALU.add),
                    r=[('ps', 5)], w=[(('NT', hh), qb // 4)])
        self.proj_qk_rot(w_in[:, 1536 + 64 * g:1536 + 64 * g + 64], w_sw[:, 1280 + 64 * g:1280 + 64 * g + 64], KA, 'KA', ncols=64)
        imp_stage(0)
        self.proj_qk_rot(w_in[:, 2048 + 64 * g:2048 + 64 * g + 64], w_sw[:, 1536 + 64 * g:1536 + 64 * g + 64], KB, 'KB', ncols=64)
        imp_stage(1)
        self.proj_v(w_in[:, 1792 + 64 * g:1792 + 64 * g + 64], 64, lambda gg: Vs[:, 4 * gg:4 * gg + 4, :, 0:64], 'Vs')
        imp_stage(2)
        self.proj_v(w_in[:, 2304 + 64 * g:2304 + 64 * g + 64], 64, lambda gg: Vw[:, 4 * gg:4 * gg + 4, :, 0:64], 'Vw')
        imp_stage(3)
        if NSA_STOP in (3, 5):
            continue
        for jp in range(2):
            ot_keys = []
            for h2 in range(2):
                hh = 2 * jp + h2
                head = 4 * g + hh
                for i in range(NCH):
                    if hh == 0 and i + 1 < NCH:
                        for qb in range(4 * (i + 1), 4 * (i + 2)):
                            imp_stage(qb)
                    csl = slice(i * 512, (i + 1) * 512)
                    q_ap = Qz[hh][:, csl]
                    qk = [(('QZ', hh), i), (('NT', hh), i)]
                    ai = (3, 4, 6)[self.rot('ACC', 3)]
                    self.unit(kcmpT[:, :], q_ap, ['kcmpT'] + qk, self.c['c_cmpT'][:, csl], ['c_cmpT'],
                              vcmp[:, 0:65], ['vcmp', 'vcmp1'], self.ps[ai], ai, True, True)
                    self.finalize(self.ps[ai], ('ps', ai), None, None, gate=(3 * head + 0, csl), first=True, last=False)
                    ai = (3, 4, 6)[self.rot('ACC', 3)]
                    nkb = 4 * i + 4
                    for kb in range(nkb):
                        mask = wm[:, 384 - 128 * (kb - 4 * i):384 - 128 * (kb - 4 * i) + 512] if kb >= 4 * i else None
                        self.unit(KA[:, kb * 128:(kb + 1) * 128], q_ap, [('KA', kb // 4), 'KAz', 'KAe'] + qk, mask, ['c_wm'],
                                  Vs[:, kb, 0, :], [('Vs', kb // 4), 'Vs1'], self.ps[ai], ai, kb == 0, kb == nkb - 1)
                    self.finalize(self.ps[ai], ('ps', ai), None, None, gate=(3 * head + 1, csl), first=False, last=False)
                    ai = (3, 4, 6)[self.rot('ACC', 3)]
                    kb0 = max(0, 4 * i - 4)
                    for kb in range(kb0, nkb):
                        r_ = kb - 4 * i
                        mask = wm[:, 384 - 128 * r_:384 - 128 * r_ + 512]
                        self.unit(KB[:, kb * 128:(kb + 1) * 128], q_ap, [('KB', kb // 4), 'KBz'] + qk, mask, ['c_wm'],
                                  Vw[:, kb, 0, :], [('Vw', kb // 4), 'Vw1'], self.ps[ai], ai, kb == kb0, kb == nkb - 1)
                    self.finalize(self.ps[ai], ('ps', ai), self.OT[h2][0:64, csl], ('OT', h2, i),
                                  gate=(3 * head + 2, csl), first=False, last=True)
                ot_keys.append([('OT', h2, i) for i in range(NCH)])
            self.outproj_pair(self.nsa_w_out, 2 * g + jp, ot_keys)


Builder.unit = _unit
Builder.nsa_mixer = _nsa_mixer


def _moe(self):
    P, R = self.P, self.R
    self.phase()
    self.norm(self.ffn_norm[1:2, :])
    self.phase()
    comb = R.alloc([8, NB], F32)
    lg = R.alloc([NB, 8], F32)
    rb = R.alloc([1, 8], F32)
    t8 = R.alloc([NB, 8], F32)
    w1 = R.alloc([NB], F32)
    w2 = R.alloc([NB], F32)
    ta = R.alloc([NB], F32)
    tb = R.alloc([NB], F32)
    wr = R.alloc([8, 8], BF16)
    self.load_w(wr, 'wr', self.ds_misc[0], self.moe_w_router)
    P.add('sp', lambda e: e.dma_start(out=rb[:, 0, :], in_=self.moe_b_router[0, :].partition_broadcast(128)),
          w=['rb'], dsem=self.ds_misc[1])
    ps = self.ps[7]
    for b in range(NB):
        for kc in range(8):
            P.add('pe', lambda e, b=b, kc=kc: e.matmul(ps[:, b * 8:(b + 1) * 8], lhsT=self.hnT[:, kc, b * 128:(b + 1) * 128],
                                                       rhs=wr[:, kc, :], start=(kc == 0), stop=(kc == 7)),
                  r=['wr', ('hnT', b)], w=[('ps', 7)])
    P.add('dve', lambda e: e.tensor_tensor(out=lg, in0=ps[:, 0:128].rearrange('p (b h) -> p b h', h=8),
                                           in1=rb[:, 0:1, :].broadcast_to([128, NB, 8]), op=ALU.add),
          r=[('ps', 7), 'rb'], w=['lg'])
    for b in range(NB):
        P.add('dve', lambda e, b=b: e.max(out=t8[:, b, :], in_=lg[:, b, :]), r=['lg'], w=[('t8', b)])
    t8k = [('t8', b) for b in range(NB)]
    P.add('dve', lambda e: e.tensor_tensor(out=w1, in0=t8[:, :, 1], in1=t8[:, :, 0], op=ALU.subtract), r=t8k, w=['w1'])
    P.add('act', lambda e: e.activation(out=w1, in_=w1, func=AF.Exp), r=['w1'], w=['w1'])
    P.add('dve', lambda e: e.tensor_scalar(out=w1, in0=w1, scalar1=1.0, scalar2=None, op0=ALU.add), r=['w1'], w=['w1'])
    P.add('dve', lambda e: e.reciprocal(out=w1, in_=w1), r=['w1'], w=['w1'])
    P.add('dve', lambda e: e.tensor_scalar(out=w2, in0=w1, scalar1=-1.0, scalar2=1.0, op0=ALU.mult, op1=ALU.add),
          r=['w1'], w=['w2'])
    for ex in range(8):
        P.add('dve', lambda e, ex=ex: e.tensor_tensor(out=ta, in0=lg[:, :, ex], in1=t8[:, :, 0], op=ALU.is_equal),
              r=['lg'] + t8k, w=['ta'])
        P.add('dve', lambda e: e.tensor_tensor(out=ta, in0=ta, in1=w1, op=ALU.mult), r=['ta', 'w1'], w=['ta'])
        P.add('dve', lambda e, ex=ex: e.tensor_tensor(out=tb, in0=lg[:, :, ex], in1=t8[:, :, 1], op=ALU.is_equal),
              r=['lg'] + t8k, w=['tb'])
        P.add('dve', lambda e: e.tensor_tensor(out=tb, in0=tb, in1=w2, op=ALU.mult), r=['tb', 'w2'], w=['tb'])
        P.add('dve', lambda e, ex=ex: e.tensor_tensor(out=comb[:, ex, :], in0=ta, in1=tb, op=ALU.add),
              r=['ta', 'tb'], w=['comb'])
    self.ffn_setup()
    for ex in range(8):
        self.ffn(self.moe_w_gate[ex], self.moe_w_up[ex], self.moe_w_down[ex], comb=comb[:, ex, :])


Builder.moe = _moe
```

```python
import numpy as np
import ml_dtypes
from contextlib import ExitStack
import concourse.bass as bass
import concourse.mybir as mybir
from concourse.bass_utils import run_bass_kernel_spmd

dt = mybir.dt
F32, BF16, I32 = dt.float32, dt.bfloat16, dt.int32
AF = mybir.ActivationFunctionType
ALU = mybir.AluOpType
AX = mybir.AxisListType

S = 2048
D = 1024
NB = 16
NCH = 4
DFF = 3584
PI = float(np.pi)
TWO_PI = float(2 * np.pi)

NSA_STOP = 0
PD = 4
NPT = PD + 2
ENG_NAMES = ['pe', 'act', 'dve', 'pool', 'sp']


def ssl(start, n, step):
    return slice(start, start + (n - 1) * step + 1, step)

SAME_ENG_SYNC = True


class DSem:
    def __init__(self, sem):
        self.sem = sem
        self.total = 0
        self.open = None


class Op:
    __slots__ = ('eng', 'fn', 'deps', 'dsem', 'sig', 'sem', 'val')


class Prog:
    def __init__(self, nc, stack):
        self.nc = nc
        self.stack = stack
        self.ops = []
        self.by_eng = {e: [] for e in ENG_NAMES}
        self.lastw = {}
        self.readers = {}
        self.nsem = 0
        self.bar_deps = {}
        self.bar_done = set()
        self.since_bar_dma = {}
        self.final = []
        self.uid = 0

    def new_sem(self, name):
        self.nsem += 1
        return self.stack.enter_context(self.nc.semaphore(f"{name}_{self.nsem}"))

    def dsem(self, name):
        return DSem(self.new_sem(name))

    def group(self, ds):
        ds.open = []

    def endgroup(self, ds):
        for o in ds.open:
            o.val = ds.total * 16
        ds.open = None

    def barrier(self):
        deps = dict(self.bar_deps)
        for e in ENG_NAMES:
            for o in reversed(self.by_eng[e]):
                if o.dsem is None:
                    deps[e] = o
                    o.sig = True
                    break
        for k, o in self.since_bar_dma.items():
            deps[k] = o
        self.bar_deps = deps
        self.bar_done = set()
        self.since_bar_dma = {}
        self.lastw = {}
        self.readers = {}

    def add(self, eng, fn, r=(), w=(), dsem=None):
        o = Op()
        o.eng = eng
        o.fn = fn
        o.dsem = dsem
        o.sig = False
        o.sem = None
        o.val = 0
        deps = {}

        def need(p):
            if p is None:
                return
            if p.dsem is None and dsem is None and p.eng == eng:
                if eng == 'pe' or not SAME_ENG_SYNC:
                    return
            deps[id(p)] = p

        for k in r:
            need(self.lastw.get(k))
        for k in w:
            need(self.lastw.get(k))
            rd = self.readers.get(k)
            if rd:
                for q in rd.values():
                    need(q)
        if eng not in self.bar_done:
            self.bar_done.add(eng)
            for p in self.bar_deps.values():
                need(p)
        for k in w:
            self.lastw[k] = o
            self.readers[k] = {}
        for k in r:
            d = self.readers.setdefault(k, {})
            if dsem is not None:
                self.uid += 1
                d[('dma', self.uid)] = o
            else:
                d[eng] = o
        o.deps = list(deps.values())
        for p in o.deps:
            p.sig = True
        if dsem is not None:
            dsem.total += 1
            o.sem = dsem.sem
            o.val = dsem.total * 16
            if dsem.open is not None:
                dsem.open.append(o)
            self.since_bar_dma[id(dsem)] = o
        self.ops.append(o)
        self.by_eng[eng].append(o)
        return o

    def emit(self):
        nc = self.nc
        LIMIT = 30000
        for e in ENG_NAMES:
            cur = None
            cnt = 0
            for o in self.by_eng[e]:
                if o.dsem is None and o.sig:
                    if cur is None or cnt >= LIMIT:
                        cur = self.new_sem(f"e_{e}")
                        cnt = 0
                    cnt += 1
                    o.sem = cur
                    o.val = cnt
        final = self.final

        def run(e, eng):
            known = {}
            for o in self.by_eng[e]:
                need = {}
                for p in o.deps:
                    s = p.sem
                    if s is None:
                        continue
                    cur = need.get(id(s))
                    if cur is None or cur[1] < p.val:
                        need[id(s)] = (s, p.val)
                for sid, (s, v) in need.items():
                    if known.get(sid, 0) < v:
                        eng.wait_ge(s, v)
                        known[sid] = v
                inst = o.fn(eng)
                if o.dsem is not None:
                    inst.then_inc(o.sem, 16)
                elif o.sig:
                    inst.then_inc(o.sem, 1)
            if e == 'sp':
                for o in final:
                    eng.wait_ge(o.sem, o.val)

        with nc.Block() as block:
            @block.tensor
            def _(eng):
                run('pe', eng)

            @block.scalar
            def _(eng):
                run('act', eng)

            @block.vector
            def _(eng):
                run('dve', eng)

            @block.gpsimd
            def _(eng):
                run('pool', eng)

            @block.sync
            def _(eng):
                run('sp', eng)


class Region:
    def __init__(self, ap, nelem):
        self.ap = ap
        self.n = nelem
        self.off = 0
        self.peak = 0

    def reset(self):
        self.off = 0

    def alloc(self, free_shape, dtype, parts=128):
        n = int(np.prod(free_shape))
        mult = 2 if dtype == F32 or dtype == I32 else 1
        nb = n * mult
        nb = (nb + 15) // 16 * 16
        assert self.off + nb <= self.n, f"region overflow {self.off}+{nb}>{self.n}"
        v = self.ap[0:parts, self.off:self.off + n * mult]
        self.off += nb
        self.peak = max(self.peak, self.off)
        if mult == 2:
            v = v.bitcast(dtype)
        if len(free_shape) == 2:
            v = v.rearrange('p (a b) -> p a b', b=free_shape[1])
        elif len(free_shape) == 3:
            v = v.rearrange('p (a b c) -> p a b c', b=free_shape[1], c=free_shape[2])
        return v


def _bf(a):
    return np.ascontiguousarray(a.astype(np.float32)).astype(ml_dtypes.bfloat16)


def make_consts():
    c = {}
    k = np.arange(128)[:, None]
    c['c_ident'] = np.eye(128, dtype=np.float32)
    c['c_utri'] = (k <= np.arange(128)[None, :]).astype(np.float32)
    c['c_ones'] = np.ones((128, 128), np.float32)
    os_ = np.zeros((128, 64), np.float32)
    os_[64, :] = 1.0
    c['c_os'] = _bf(os_)
    y = np.arange(-384, 1024)[None, :]
    c['c_wm'] = _bf(((y - k) >= 0) & ((y - k) < 512))
    q = np.arange(128)[None, :]
    cur = (k <= q)
    prev = (k >= q)
    c['c_dm'] = _bf(np.concatenate([cur, prev, cur, prev], axis=1))
    n = np.arange(128)[:, None]
    qq = np.arange(2048)[None, :]
    c['c_cmpT'] = _bf((16 * n + 31 <= qq) & (n <= 126))
    xq = np.arange(128)[:, None]
    xx = np.arange(-120, 128)[None, :]
    c['c_m0'] = (16 * xx + 31 <= xq).astype(np.float32)
    xs = np.arange(-30, 32)[None, :]
    curp = (xq // 64)
    fut = xs > curp
    forced = (xs == curp) | (xs == curp - 1)
    keep = (~fut) & (~forced)
    addv = np.where(fut, -1.0, np.where(forced, 1e6, 0.0))
    c['c_keep'] = keep.astype(np.float32)
    c['c_addv'] = addv.astype(np.float32)
    j = np.arange(32)[:, None, None]
    kb = np.arange(16)[None, :, None]
    kk = np.arange(128)[None, None, :]
    E = (j == 2 * kb + kk // 64)
    Ef = np.zeros((128, 16 * 128), np.float32)
    Ef[:32] = E.reshape(32, 16 * 128)
    c['c_E'] = _bf(Ef)
    Z = np.zeros((128, 48 * 64), np.float32)
    for p_ in range(48):
        Z[p_, 64 * p_:64 * p_ + 64] = 1.0
    c['c_Z'] = _bf(Z)
    half = 32
    inv_freq = (10000.0 ** (-np.arange(half, dtype=np.float32) / half)).astype(np.float32)
    pp = np.arange(128)
    rot = np.zeros((128, 4), np.float32)
    rot[:, 0] = inv_freq[(pp % 64) % 32]
    rot[:, 1] = PI / 2
    rot[:, 2] = np.where((pp % 64) < 32, PI, 0.0)
    c['c_rot'] = rot
    return c


CONST_SHAPES = None


class Slots:
    def __init__(self, P, name, aps, dsems):
        self.items = [(ap, f"{name}{i}", dsems[i]) for i, ap in enumerate(aps)]
        self.i = 0

    def get(self):
        it = self.items[self.i % len(self.items)]
        self.i += 1
        return it


class Builder:
    def __init__(self, n_seq=2, stages=('l0', 'l1'), dbg=None):
        self.n_seq = n_seq
        self.stages = stages
        self.dbg = dbg
        nc = bass.Bass("TRN2", target_bir_lowering=False)
        self.nc = nc
        self.stack = ExitStack()
        self.P = Prog(nc, self.stack)
        self.rr = {}
        self.fifo = []
        self.fins = []
        self.fseq = 0
        self.gate_free_at = 0
        self.u_busy = [False, False]
        self.acc_pending = {}
        self.acc_gen = {}
        self.tick = 0
        self._inputs()
        self._alloc()

    def din(self, name, shape, dtype=F32):
        return self.nc.dram_tensor(name, list(shape), dtype, kind="ExternalInput").ap()

    def sb(self, name, shape, dtype):
        return self.stack.enter_context(self.nc.sbuf_tensor(name, list(shape), dtype))

    def _inputs(self):
        n = self.n_seq
        self.x = self.din('x', [n, S, D])
        self.p = self.din('p', [2, n, S, 256])
        self.pos = self.din('positions', [n, S], I32)
        self.mix_norm = self.din('mix_norm', [2, D])
        self.ffn_norm = self.din('ffn_norm', [2, D])
        self.ple_norm = self.din('ple_norm', [2, D])
        self.final_norm = self.din('final_norm', [1, D])
        self.ple_gate_w = self.din('ple_gate_w', [2, D, D])
        self.ple_proj_w = self.din('ple_proj_w', [2, 256, D])
        self.fd_w_in = self.din('fd_w_in', [D, 3080])
        self.fd_w_sw = self.din('fd_w_sw', [D, 1024])
        self.fd_forget_b = self.din('fd_forget_b', [1, 8])
        self.fd_w_out = self.din('fd_w_out', [D, D])
        self.dense_w_gate = self.din('dense_w_gate', [D, DFF])
        self.dense_w_up = self.din('dense_w_up', [D, DFF])
        self.dense_w_down = self.din('dense_w_down', [DFF, D])
        self.nsa_w_in = self.din('nsa_w_in', [D, 2608])
        self.nsa_w_sw = self.din('nsa_w_sw', [D, 1792])
        self.nsa_pos_k = self.din('nsa_pos_k', [32, 64])
        self.nsa_w1_k = self.din('nsa_w1_k', [2048, 256])
        self.nsa_b1_k = self.din('nsa_b1_k', [256, 1])
        self.nsa_w2_k = self.din('nsa_w2_k', [256, 64])
        self.nsa_pos_v = self.din('nsa_pos_v', [32, 64])
        self.nsa_w1_v = self.din('nsa_w1_v', [2048, 256])
        self.nsa_b1_v = self.din('nsa_b1_v', [256, 1])
        self.nsa_w2_v = self.din('nsa_w2_v', [256, 64])
        self.nsa_w_out = self.din('nsa_w_out', [D, D])
        self.moe_w_router = self.din('moe_w_router', [D, 8])
        self.moe_b_router = self.din('moe_b_router', [1, 8])
        self.moe_w_gate = self.din('moe_w_gate', [8, D, DFF])
        self.moe_w_up = self.din('moe_w_up', [8, D, DFF])
        self.moe_w_down = self.din('moe_w_down', [8, DFF, D])
        self.cst = {}
        for k, v in make_consts().items():
            d = BF16 if v.dtype == ml_dtypes.bfloat16 else F32
            self.cst[k] = self.din(k, v.shape, d)
        self.y = self.nc.dram_tensor('y', [n, S, D], F32, kind="ExternalOutput").ap()

    def _alloc(self):
        P = self.P
        self.h = self.sb('h', [128, NB, D], F32)
        self.hnT = self.sb('hnT', [128, 8, S], BF16)
        self.cosT = self.sb('cosT', [128, S], BF16)
        self.sinT = self.sb('sinT', [128, S], BF16)
        self.c = {}
        for k, ap in self.cst.items():
            if k == 'c_E':
                continue
            self.c[k] = self.sb('s_' + k, list(ap.shape), ap.dtype)
        self.ps = [self.stack.enter_context(self.nc.psum_tensor(f"ps{i}", [128, 512], F32)) for i in range(8)]
        RN = 43520
        self.Rt = self.sb('R', [128, RN], BF16)
        self.R = Region(self.Rt, RN)
        self.ds_c = P.dsem('consts')
        self.ds_h = P.dsem('hload')
        self.ds_g = P.dsem('gtile')
        self.ds_y = P.dsem('yout')
        self.ds_misc = [P.dsem(f'misc{i}') for i in range(4)]
        self.ds_w = [P.dsem(f'w{i}') for i in range(6)]
        self.ds_wo = [P.dsem(f'wo{i}') for i in range(2)]
        self.ds_ffn = [P.dsem(f'ffn{i}') for i in range(6)]

    def defer(self, fn, delay, fin=False):
        if fin:
            self.fseq += 1
            self.fins.append((self.tick + delay, self.fseq, fn))
            self.fins.sort(key=lambda x: (x[0], x[1]))
        else:
            self.fifo.append((self.tick + delay, fn))

    def _drain(self):
        while self.fins and self.fins[0][0] <= self.tick:
            self.fins.pop(0)[2]()
        while self.fifo and self.fifo[0][0] <= self.tick:
            self.fifo.pop(0)[1]()

    def step(self):
        self.tick += 1
        self._drain()

    def flush(self):
        while self.fifo or self.fins:
            self.tick += 1
            self._drain()

    def acc_gen_of(self, key):
        return self.acc_gen.get(key, 0)

    def acc_start(self, key, gen):
        assert not (key in self.acc_pending and gen > self.acc_pending[key]), \
            f"accumulator {key} reused before its finalize was emitted"


    def step(self):
        self.tick += 1
        self._drain()

    def flush(self):
        while self.fifo or self.fins:
            self.tick += 1
            self._drain()

    def rot(self, name, n):
        i = self.rr.get(name, 0)
        self.rr[name] = i + 1
        return i % n

    def load_consts(self):
        P = self.P
        P.group(self.ds_c)
        for k, ap in self.cst.items():
            if k == 'c_E':
                continue
            P.add('sp', lambda e, k=k, ap=ap: e.dma_start(out=self.c[k][:], in_=ap), w=[k], dsem=self.ds_c)
        P.endgroup(self.ds_c)

    def load_w(self, dst, dkey, dsem, src, cast=True):
        srcv = src.rearrange('(k p) c -> p k c', p=128)
        eng = 'pool' if cast else 'sp'
        return self.P.add(eng, lambda e: e.dma_start(out=dst, in_=srcv), w=[dkey], dsem=dsem)

    def norm(self, gain_row, final_out=None):
        P, R, h = self.P, self.R, self.h
        mark = R.off
        self.gtile = R.alloc([D], F32)
        ss = R.alloc([NB], F32)
        junk = R.alloc([D], BF16)
        hs = [R.alloc([D], F32) for _ in range(2)]
        P.add('sp', lambda e: e.dma_start(out=self.gtile, in_=gain_row[0, :].partition_broadcast(128)),
              w=['gtile'], dsem=self.ds_g)
        for b in range(NB):
            P.add('act', lambda e, b=b: e.activation(out=junk, in_=h[:, b, :], func=AF.Square,
                                                     accum_out=ss[:, b:b + 1]),
                  r=self.hk(b), w=['junk', ('ss', b)])
        allss = [('ss', b) for b in range(NB)]
        P.add('dve', lambda e: e.tensor_scalar(out=ss, in0=ss, scalar1=1.0 / D, scalar2=1e-6,
                                               op0=ALU.mult, op1=ALU.add), r=allss, w=allss)
        P.add('act', lambda e: e.activation(out=ss, in_=ss, func=AF.Sqrt), r=allss, w=allss)
        P.add('dve', lambda e: e.reciprocal(out=ss, in_=ss), r=allss, w=allss)
        ident = self.c['c_ident']
        for b in range(NB):
            i = b % 2
            P.add('dve', lambda e, b=b, i=i: e.scalar_tensor_tensor(
                out=hs[i], in0=h[:, b, :], scalar=ss[:, b:b + 1], in1=self.gtile,
                op0=ALU.mult, op1=ALU.mult), r=self.hk(b) + [('ss', b), 'gtile'], w=[('hs', i)])
            if final_out is not None:
                s = final_out
                P.add('sp', lambda e, b=b, i=i, s=s: e.dma_start(out=self.y[s, b * 128:(b + 1) * 128, :], in_=hs[i]),
                      r=[('hs', i)], w=[('y', s, b)], dsem=self.ds_y)
                continue
            for half in range(2):
                pi = 6 + half
                ps = self.ps[pi]
                for j in range(4):
                    cidx = half * 4 + j
                    P.add('pe', lambda e, ps=ps, j=j, cidx=cidx, i=i: e.transpose(
                        out=ps[:, j * 128:(j + 1) * 128], in_=hs[i][:, cidx * 128:(cidx + 1) * 128],
                        identity=ident[:]), r=[('hs', i), 'c_ident'], w=[('ps', pi)])
                P.add('act', lambda e, ps=ps, half=half, b=b: e.activation(
                    out=self.hnT[:, half * 4:half * 4 + 4, b * 128:(b + 1) * 128],
                    in_=ps[:].rearrange('p (a t) -> p a t', t=128), func=AF.Copy),
                    r=[('ps', pi)], w=[('hnT', b)])
        R.off = mark

    def hk(self, b):
        return [('h', b, 0), ('h', b, 1)]

    def hn_keys(self, tc=None):
        if tc is None:
            return [('hnT', b) for b in range(NB)]
        return [('hnT', 4 * tc + j) for j in range(4)]

    def load_seq(self, s):
        P = self.P
        P.group(self.ds_h)
        for b in range(NB):
            P.add('sp', lambda e, b=b: e.dma_start(out=self.h[:, b, :], in_=self.x[s, b * 128:(b + 1) * 128, :]),
                  w=self.hk(b), dsem=self.ds_h)
        P.endgroup(self.ds_h)

    def rotary_tables(self, s):
        P, R = self.P, self.R
        mark = R.off
        posi = R.alloc([S], I32)
        posf = R.alloc([S], F32)
        t = R.alloc([S], F32)
        kf = R.alloc([S], F32)
        ki = R.alloc([S], I32)
        rot = self.c['c_rot']
        P.add('sp', lambda e: e.dma_start(out=posi, in_=self.pos[s, :].partition_broadcast(128)),
              w=['posi'], dsem=self.ds_misc[0])
        P.add('dve', lambda e: e.tensor_copy(out=posf, in_=posi), r=['posi'], w=['posf'])
        for (dst, dkey, ph) in ((self.cosT, 'cosT', 1), (self.sinT, 'sinT', 2)):
            P.add('dve', lambda e, ph=ph: e.tensor_scalar(out=t, in0=posf, scalar1=rot[:, 0:1], scalar2=rot[:, ph:ph + 1],
                                                          op0=ALU.mult, op1=ALU.add), r=['posf', 'c_rot'], w=['rt'])
            P.add('dve', lambda e: e.tensor_scalar(out=kf, in0=t, scalar1=1.0 / TWO_PI, scalar2=None, op0=ALU.mult),
                  r=['rt'], w=['rk'])
            P.add('dve', lambda e: e.tensor_copy(out=ki, in_=kf), r=['rk'], w=['rki'])
            P.add('dve', lambda e: e.tensor_copy(out=kf, in_=ki), r=['rki'], w=['rk'])
            P.add('dve', lambda e: e.scalar_tensor_tensor(out=t, in0=kf, scalar=-TWO_PI, in1=t,
                                                          op0=ALU.mult, op1=ALU.add), r=['rk', 'rt'], w=['rt'])
            P.add('dve', lambda e: e.tensor_scalar(out=kf, in0=t, scalar1=PI, scalar2=-TWO_PI,
                                                   op0=ALU.is_gt, op1=ALU.mult), r=['rt'], w=['rk'])
            P.add('dve', lambda e: e.tensor_tensor(out=t, in0=t, in1=kf, op=ALU.add), r=['rt', 'rk'], w=['rt'])
            P.add('dve', lambda e: e.tensor_scalar(out=t, in0=t, scalar1=-PI, scalar2=PI,
                                                   op0=ALU.max, op1=ALU.min), r=['rt'], w=['rt'])
            P.add('act', lambda e, dst=dst: e.activation(out=dst[:], in_=t, func=AF.Sin), r=['rt'], w=[dkey])
        R.off = mark

    def phase(self):
        self.flush()
        self.P.barrier()
        self.R.reset()

    def attn_common(self):
        R = self.R
        self.Pt = [R.alloc([512], BF16) for _ in range(NPT)]
        self.U = [R.alloc([512], F32, parts=65) for _ in range(2)]
        self.rd16 = [R.alloc([512], BF16) for _ in range(2)]
        self.Rt = [R.alloc([512], F32, parts=64) for _ in range(2)]
        for i_ in range(2):
            self.P.add('pool', lambda e, i_=i_: e.memset(self.rd16[i_][:, :], 0.0), w=[('rd16', i_)])
        self.OT = [R.alloc([S], BF16) for _ in range(2)]
        self.Wo = [R.alloc([D], BF16) for _ in range(2)]
        for i_ in range(2):
            self.P.add('pool', lambda e, i_=i_: e.memset(self.OT[i_][64:128, :], 0.0), w=[('OTz', i_)])
            self.P.add('pool', lambda e, i_=i_: e.memset(self.Wo[i_][64:128, :], 0.0), w=[('Woz', i_)])
        self.wsl = Slots(self.P, 'wsl', [R.alloc([8, 128], BF16) for _ in range(4)], self.ds_w)

    def proj_fm(self, wt, wkey, ncols, evac, wcols=None):
        P = self.P
        for tc in range(NCH):
            pi = 6 + self.rot('PJ', 2)
            ps = self.ps[pi]
            for kc in range(8):
                lhsT = wt[:, kc, 0:ncols] if wcols is None else wt[:, kc, wcols[0]:wcols[1]]
                P.add('pe', lambda e, ps=ps, kc=kc, tc=tc, lhsT=lhsT: e.matmul(
                    ps[0:ncols, :], lhsT=lhsT, rhs=self.hnT[:, kc, tc * 512:(tc + 1) * 512],
                    start=(kc == 0), stop=(kc == 7)), r=[wkey] + self.hn_keys(tc), w=[('ps', pi)])
            evac(tc, ps, ('ps', pi))

    def proj_qk_plain(self, src_cols, dst, dkey, split=None, eng='act'):
        P = self.P
        wt, wk, wds = self.wsl.get()
        self.load_w(wt, wk, wds, src_cols)

        def evac(tc, ps, pk):
            sl = slice(tc * 512, (tc + 1) * 512)
            def cp(e, o, i):
                return e.activation(out=o, in_=i, func=AF.Copy) if eng == 'act' else e.tensor_copy(out=o, in_=i)
            if split is None:
                P.add(eng, lambda e: cp(e, dst[:, sl], ps[:, :]), r=[pk], w=[(dkey, tc)])
            else:
                for hh in range(2):
                    rows = slice(64 * hh, 64 * hh + 64)
                    P.add(eng, lambda e, hh=hh, rows=rows: cp(e, split[hh][rows, sl], ps[rows, :]),
                          r=[pk], w=[(('QZ', hh), tc)])
        self.proj_fm(wt, wk, 128, evac)

    def proj_qk_rot(self, src_cols, src_sw_cols, dst, dkey, dup=False, split=None, ncols=128):
        P, R = self.P, self.R
        wt, wk, wds = self.wsl.get()
        wt2, wk2, wds2 = self.wsl.get()
        if dup:
            P.group(wds)
            self.load_w(wt[:, :, 0:64], wk, wds, src_cols)
            self.load_w(wt[:, :, 64:128], wk + 'b', wds, src_cols)
            P.endgroup(wds)
            P.group(wds2)
            self.load_w(wt2[:, :, 0:64], wk2, wds2, src_sw_cols)
            self.load_w(wt2[:, :, 64:128], wk2 + 'b', wds2, src_sw_cols)
            P.endgroup(wds2)
            wkeys, wkeys2 = [wk, wk + 'b'], [wk2, wk2 + 'b']
        else:
            self.load_w(wt[:, :, 0:ncols], wk, wds, src_cols)
            self.load_w(wt2[:, :, 0:ncols], wk2, wds2, src_sw_cols)
            wkeys, wkeys2 = [wk], [wk2]
        for tc in range(NCH):
            sl = slice(tc * 512, (tc + 1) * 512)
            pa, pb = ((6, 7), (0, 1), (2, 3))[self.rot('rotp', 3)]
            for (pi, w_, wkk) in ((pa, wt, wkeys), (pb, wt2, wkeys2)):
                ps = self.ps[pi]
                for kc in range(8):
                    P.add('pe', lambda e, ps=ps, kc=kc, w_=w_, sl=sl: e.matmul(
                        ps[0:ncols, :], lhsT=w_[:, kc, 0:ncols], rhs=self.hnT[:, kc, sl], start=(kc == 0), stop=(kc == 7)),
                        r=wkk + self.hn_keys(tc), w=[('ps', pi)])
            ti = self.rot('rt', 2)
            t1 = self.rtmp[ti]
            P.add('dve', lambda e, t1=t1, sl=sl, pa=pa: e.tensor_tensor(out=t1[0:ncols, :], in0=self.ps[pa][0:ncols, :], in1=self.cosT[0:ncols, sl],
                                                                 op=ALU.mult), r=[('ps', pa), 'cosT'], w=[('rtmp', ti)])
            if split is None:
                P.add('dve', lambda e, sl=sl, pb=pb: e.tensor_tensor(out=dst[0:ncols, sl], in0=self.ps[pb][0:ncols, :], in1=self.sinT[0:ncols, sl],
                                                              op=ALU.mult), r=[('ps', pb), 'sinT'], w=[(dkey, tc)])
                P.add('pool', lambda e, t1=t1, sl=sl: e.tensor_tensor(out=dst[0:ncols, sl], in0=dst[0:ncols, sl], in1=t1[0:ncols, :], op=ALU.add),
                      r=[('rtmp', ti), (dkey, tc)], w=[(dkey, tc)])
            else:
                t2 = self.rtmp2[ti]
                P.add('dve', lambda e, t2=t2, sl=sl, pb=pb: e.tensor_tensor(out=t2, in0=self.ps[pb][:, :], in1=self.sinT[:, sl],
                                                                     op=ALU.mult), r=[('ps', pb), 'sinT'], w=[('rtmp2', ti)])
                for hh in range(2):
                    rows = slice(64 * hh, 64 * hh + 64)
                    P.add('pool', lambda e, t1=t1, t2=t2, sl=sl, hh=hh, rows=rows: e.tensor_tensor(
                        out=split[hh][rows, sl], in0=t1[rows, :], in1=t2[rows, :], op=ALU.add),
                        r=[('rtmp', ti), ('rtmp2', ti)], w=[(('QZ', hh), tc)])

    def proj_v(self, src_cols, ncols, dst_fn, dkey, tok_fn=None, nblk=NB, eng='act'):
        P = self.P
        wt, wk, wds = self.wsl.get()
        self.load_w(wt[:, :, 0:ncols], wk, wds, src_cols)
        for g in range(nblk // 4):
            pi = 6 + self.rot('PJ', 2)
            ps = self.ps[pi]
            for bb in range(4):
                blk = 4 * g + bb
                tsl = slice(blk * 128, (blk + 1) * 128) if tok_fn is None else tok_fn(blk)
                for kc in range(8):
                    P.add('pe', lambda e, ps=ps, bb=bb, kc=kc, tsl=tsl: e.matmul(
                        ps[:, bb * ncols:(bb + 1) * ncols], lhsT=self.hnT[:, kc, tsl], rhs=wt[:, kc, 0:ncols],
                        start=(kc == 0), stop=(kc == 7)), r=[wk] + self.hn_keys(), w=[('ps', pi)])
            if eng == 'act':
                P.add('act', lambda e, ps=ps, g=g: e.activation(
                    out=dst_fn(g), in_=ps[:, 0:4 * ncols].rearrange('p (b h d) -> p b h d', b=4, d=64), func=AF.Copy),
                    r=[('ps', pi)], w=[(dkey, g)])
            else:
                P.add(eng, lambda e, ps=ps, g=g: e.tensor_copy(
                    out=dst_fn(g), in_=ps[:, 0:4 * ncols].rearrange('p (b h d) -> p b h d', b=4, d=64)),
                    r=[('ps', pi)], w=[(dkey, g)])

    def finalize(self, acc, acck, dst, dkey, gate=None, first=True, last=True, extra=None):
        P = self.P
        ui = self.rot('U', 2)
        U = self.U[ui]
        bc = self.ps[5]
        ones = self.c['c_ones']

        rd = self.rd16[ui]
        os16 = self.c['c_os']
        pend = [acck] if extra is None else [('ps', 3), ('ps', 4), ('ps', 6)]
        for k_ in pend:
            self.acc_pending[k_] = self.acc_gen_of(k_)
            self.acc_gen[k_] = self.acc_gen_of(k_) + 1

        Rt = self.Rt[ui]
        src_ = U[0:64, :] if extra is not None else acc[0:64, :]
        srck = ('U', ui) if extra is not None else acck

        def stage_a():
            if extra is None:
                P.add('act', lambda e: e.activation(out=rd[64:65, :], in_=acc[64:65, :], func=AF.Copy, bias=1e-18),
                      r=[acck], w=[('rd16', ui)])
            else:
                for k_ in pend:
                    self.acc_pending.pop(k_, None)
                extra(U, ('U', ui))
                P.add('dve', lambda e: e.tensor_scalar(out=rd[64:65, :], in0=U[64:65, :], scalar1=1e-18, scalar2=None,
                                                       op0=ALU.add), r=[('U', ui)], w=[('rd16', ui)])

        def stage_b():
            if extra is None:
                for k_ in pend:
                    self.acc_pending.pop(k_, None)
            P.add('pe', lambda e: e.matmul(bc[0:64, :], lhsT=os16[:, 0:64], rhs=rd[:, :], start=True, stop=True),
                  r=[('rd16', ui), 'c_os'], w=[('ps', 5)])
            P.add('act', lambda e: e.activation(out=bc[0:64, :], in_=bc[0:64, :], func=AF.Ln), r=[('ps', 5)], w=[('ps', 5)])
            P.add('act', lambda e: e.activation(out=Rt[0:64, :], in_=bc[0:64, :], func=AF.Exp, scale=-1.0),
                  r=[('ps', 5)], w=[('Rt', ui)])
            if gate is None:
                P.add('dve', lambda e: e.tensor_tensor(out=dst, in0=src_, in1=Rt[0:64, :], op=ALU.mult),
                      r=[srck, ('Rt', ui)], w=[dkey])
            else:
                P.add('dve', lambda e: e.tensor_tensor(out=U[0:64, :], in0=src_, in1=Rt[0:64, :], op=ALU.mult),
                      r=[srck, ('Rt', ui)], w=[('U', ui)])

        bg = self.ps[7]

        def stage_c():
            if first:
                P.add('dve', lambda e: e.tensor_tensor(out=self.gacc[0:64, :], in0=U[0:64, :], in1=bg[0:64, :], op=ALU.mult),
                      r=[('U', ui), ('ps', 7)], w=['gacc'])
            else:
                P.add('dve', lambda e: e.tensor_tensor(out=U[0:64, :], in0=U[0:64, :], in1=bg[0:64, :], op=ALU.mult),
                      r=[('U', ui), ('ps', 7)], w=[('U', ui)])
                if last:
                    P.add('dve', lambda e: e.tensor_tensor(out=dst, in0=U[0:64, :], in1=self.gacc[0:64, :], op=ALU.add),
                          r=[('U', ui), 'gacc'], w=[dkey])
                else:
                    P.add('dve', lambda e: e.tensor_tensor(out=self.gacc[0:64, :], in0=U[0:64, :], in1=self.gacc[0:64, :],
                                                           op=ALU.add), r=[('U', ui), 'gacc'], w=['gacc'])

        def stage_g():
            hc, sl = gate
            Z = self.c['c_Z']
            P.add('pe', lambda e: e.matmul(bg[0:64, :], lhsT=Z[:, hc * 64:hc * 64 + 64], rhs=self.sigT[:, sl],
                                           start=True, stop=True), r=['sigT', 'c_Z'], w=[('ps', 7)])
        def first_stage():
            assert not self.u_busy[ui], "finalize scratch slot reused too early"
            self.u_busy[ui] = True
            stage_a()

        def last_stage():
            (stage_c if gate is not None else stage_b)()
            self.u_busy[ui] = False
        self.defer(first_stage, PD + 1, fin=True)
        if gate is not None:
            g_delay = max(PD + 2, self.gate_free_at - self.tick)
            c_delay = max(PD + 5, g_delay + 2)
            self.gate_free_at = self.tick + c_delay
            self.defer(stage_g, g_delay, fin=True)
            self.defer(stage_b, PD + 3, fin=True)
            self.defer(last_stage, c_delay, fin=True)
        else:
            self.defer(last_stage, PD + 3, fin=True)

    def outproj_pair(self, w_out, pair, ot_keys):
        P = self.P
        self.flush()
        wkeys = []
        for hh in range(2):
            r0 = (2 * pair + hh) * 64
            src = w_out[r0:r0 + 64, :]
            P.add('pool', lambda e, hh=hh, src=src: e.dma_start(out=self.Wo[hh][0:64, :], in_=src), w=[('Wo', hh)],
                  dsem=self.ds_wo[hh])
            wkeys.append(('Wo', hh))
        for b in range(NB):
            for half in range(2):
                pi = 6 + self.rot('PJ', 2)
                ps = self.ps[pi]
                for hh in range(2):
                    P.add('pe', lambda e, ps=ps, hh=hh, b=b, half=half: e.matmul(
                        ps[:, :], lhsT=self.OT[hh][:, b * 128:(b + 1) * 128],
                        rhs=self.Wo[hh][:, half * 512:(half + 1) * 512], start=(hh == 0), stop=(hh == 1)),
                        r=[('Wo', hh), ('OTz', hh), ('Woz', hh)] + ot_keys[hh], w=[('ps', pi)])
                hsl = self.h[:, b, half * 512:(half + 1) * 512]
                P.add('dve', lambda e, ps=ps, hsl=hsl: e.tensor_tensor(out=hsl, in0=ps[:, :], in1=hsl, op=ALU.add),
                      r=[('ps', pi), ('h', b, half)], w=[('h', b, half)])

    def l0_mixer(self, s):
        P, R = self.P, self.R
        self.phase()
        self.attn_common()
        self.rtmp = [R.alloc([512], F32) for _ in range(2)]
        self.rtmp2 = [R.alloc([512], F32) for _ in range(2)]
        mark0 = R.off
        Qz = [R.alloc([S], BF16) for _ in range(2)]
        KT = R.alloc([S], BF16)
        Vt = R.alloc([NB, 2, 65], BF16)
        P.add('pool', lambda e: e.memset(Qz[0][64:128, :], 0.0), w=['qz0'])
        P.add('pool', lambda e: e.memset(Qz[1][0:64, :], 0.0), w=['qz1'])
        w_in = self.fd_w_in
        fb = R.alloc([1, 8], F32)
        lg = R.alloc([NB, 8], F32)
        cl = R.alloc([NB, 8], F32)
        offs = R.alloc([NB, 8], F32)
        tot = R.alloc([NB, 8], F32)
        Bt = R.alloc([8, NB, NB], F32)
        P.add('sp', lambda e: e.dma_start(out=fb[:, 0, :], in_=self.fd_forget_b[0, :].partition_broadcast(128)),
              w=['fb'], dsem=self.ds_misc[1])
        wf, wfk, wfds = self.wsl.get()
        self.load_w(wf[:, :, 0:8], wfk, wfds, w_in[:, 3072:3080])
        psf = self.ps[4]
        for b in range(NB):
            for kc in range(8):
                P.add('pe', lambda e, b=b, kc=kc: e.matmul(psf[:, b * 8:(b + 1) * 8],
                                                           lhsT=self.hnT[:, kc, b * 128:(b + 1) * 128], rhs=wf[:, kc, 0:8],
                                                           start=(kc == 0), stop=(kc == 7)),
                      r=[wfk] + self.hn_keys(), w=[('ps', 4)])
        P.add('dve', lambda e: e.tensor_tensor(out=lg, in0=psf[:, 0:128].rearrange('p (b h) -> p b h', h=8),
                                               in1=fb[:, 0:1, :].broadcast_to([128, NB, 8]), op=ALU.add),
              r=[('ps', 4), 'fb'], w=['lg'])
        P.add('act', lambda e: e.activation(out=lg, in_=lg, func=AF.Exp, scale=-1.0), r=['lg'], w=['lg'])
        P.add('dve', lambda e: e.tensor_scalar(out=lg, in0=lg, scalar1=1.0, scalar2=None, op0=ALU.add), r=['lg'], w=['lg'])
        P.add('act', lambda e: e.activation(out=lg, in_=lg, func=AF.Ln), r=['lg'], w=['lg'])
        lg2 = lg.rearrange('p b h -> p (b h)')
        P.add('pe', lambda e: e.matmul(self.ps[6][:, 0:128], lhsT=self.c['c_utri'][:], rhs=lg2, start=True, stop=True),
              r=['lg', 'c_utri'], w=[('ps', 6)])
        P.add('pe', lambda e: e.matmul(self.ps[7][:, 0:128], lhsT=self.c['c_ones'][:], rhs=lg2, start=True, stop=True),
              r=['lg', 'c_ones'], w=[('ps', 7)])
        P.add('act', lambda e: e.activation(out=tot, in_=self.ps[7][:, 0:128].rearrange('p (b h) -> p b h', h=8),
                                            func=AF.Copy), r=[('ps', 7)], w=['tot'])
        P.add('pool', lambda e: e.memset(offs[:, 0, :], 0.0), w=[('offs', 0)])
        for j in range(1, NB):
            P.add('dve', lambda e, j=j: e.tensor_tensor(out=offs[:, j, :], in0=offs[:, j - 1, :], in1=tot[:, j - 1, :],
                                                        op=ALU.add), r=[('offs', j - 1), 'tot'], w=[('offs', j)])
        allo = [('offs', j) for j in range(NB)]
        P.add('dve', lambda e: e.tensor_tensor(out=cl, in0=self.ps[6][:, 0:128].rearrange('p (b h) -> p b h', h=8),
                                               in1=offs, op=ALU.add), r=[('ps', 6)] + allo, w=['cl'])
        for hh in range(8):
            for qb in range(NB):
                P.add('dve', lambda e, hh=hh, qb=qb: e.tensor_scalar(
                    out=Bt[:, hh, qb, :], in0=cl[:, :, hh], scalar1=offs[:, qb, hh:hh + 1], scalar2=None,
                    op0=ALU.subtract), r=['cl'] + allo, w=[('Bt', hh)])
        wm = self.c['c_wm']
        for hp in range(4):
            c0 = hp * 128
            self.proj_qk_plain(w_in[:, c0:c0 + 128], None, None, split=Qz, eng='dve')
            self.proj_qk_plain(w_in[:, 512 + c0:512 + c0 + 128], KT, 'KT', eng='dve')
            P.add('pool', lambda e: e.memset(Vt[:, :, :, 64:65], 1.0), w=['Vones'])
            self.proj_v(w_in[:, 1024 + c0:1024 + c0 + 128], 128, lambda g: Vt[:, 4 * g:4 * g + 4, :, 0:64], 'Vt', eng='dve')
            qk = [('QT', t) for t in range(NCH)]
            kk = [('KT', t) for t in range(NCH)]
            ot_keys = []
            for hh in range(2):
                head = 2 * hp + hh
                rows = slice(64 * hh, 64 * hh + 64)
                for i in range(NCH):
                    ai = (3, 4, 6)[self.rot('ACC', 3)]
                    acc = self.ps[ai]
                    nkb = 4 * i + 4
                    for kb in range(nkb):
                        self.step()
                        si = self.rot('S', 3)
                        ps = self.ps[si]
                        P.add('pe', lambda e, ps=ps, kb=kb, i=i, hh=hh: e.matmul(
                            ps[:, :], lhsT=KT[:, kb * 128:(kb + 1) * 128], rhs=Qz[hh][:, i * 512:(i + 1) * 512],
                            start=True, stop=True), r=[('KT', kb // 4), (('QZ', hh), i), 'qz0', 'qz1'], w=[('ps', si)])
                        pi = self.rot('Pt', NPT)
                        pt = self.Pt[pi]
                        r0 = max(0, kb - 4 * i)
                        pkeys = []
                        for rr in range(r0, 4):
                            qb = 4 * i + rr
                            P.add('act', lambda e, ps=ps, pt=pt, rr=rr, qb=qb, kb=kb, head=head: e.activation(
                                out=pt[:, rr * 128:(rr + 1) * 128], in_=ps[:, rr * 128:(rr + 1) * 128], func=AF.Exp,
                                scale=0.125, bias=Bt[:, head, qb, kb:kb + 1]),
                                r=[('ps', si), ('Bt', head)], w=[('Pt', pi, rr)])
                            pkeys.append(('Pt', pi, rr))
                        c_lo = r0 * 128
                        if kb >= 4 * i:
                            r = kb - 4 * i
                            P.add('dve', lambda e, pt=pt, r=r, c_lo=c_lo: e.tensor_tensor(
                                out=pt[:, c_lo:512], in0=pt[:, c_lo:512], in1=wm[:, 384 - 128 * r + c_lo:384 - 128 * r + 512],
                                op=ALU.mult), r=pkeys + ['c_wm'], w=pkeys)
                        def back(acc=acc, pt=pt, kb=kb, hh=hh, c_lo=c_lo, st=(kb == 0), sp=(kb == nkb - 1), pkeys=pkeys, ai=ai,
                                 gen=self.acc_gen_of(('ps', ai))):
                            self.acc_start(('ps', ai), gen)
                            P.add('pe', lambda e: e.matmul(
                                acc[0:65, c_lo:512], lhsT=Vt[:, kb, hh, :], rhs=pt[:, c_lo:512], start=st, stop=sp),
                                r=pkeys + [('Vt', kb // 4), 'Vones'], w=[('ps', ai)])
                        self.defer(back, PD)
                    self.finalize(acc, ('ps', ai), self.OT[hh][0:64, i * 512:(i + 1) * 512], ('OT', hh, i))
                ot_keys.append([('OT', hh, i) for i in range(NCH)])
            self.outproj_pair(self.fd_w_out, hp, ot_keys)
        self.flush()
        P.barrier()
        R.off = mark0
        Qz = [R.alloc([S], BF16) for _ in range(2)]
        KT = R.alloc([S], BF16)
        P.add('pool', lambda e: e.memset(Qz[0][64:128, :], 0.0), w=['qz0'])
        P.add('pool', lambda e: e.memset(Qz[1][0:64, :], 0.0), w=['qz1'])
        Vd = [R.alloc([NB, 2, 65], BF16) for _ in range(3)]
        self.Vd = Vd
        dm3 = self.c['c_dm'][:, :].rearrange('p (a b) -> p a b', b=128)
        for dp in range(4):
            c0 = dp * 128
            self.proj_qk_rot(w_in[:, 1536 + c0:1536 + c0 + 128], self.fd_w_sw[:, c0:c0 + 128], None, None, split=Qz)
            self.proj_qk_rot(w_in[:, 2048 + c0:2048 + c0 + 128], self.fd_w_sw[:, 512 + c0:512 + c0 + 128], KT, 'KT')
            for pat in range(3):
                P.add('pool', lambda e, pat=pat: e.memset(Vd[pat][:, :, :, 64:65], 1.0), w=[('Vones', pat)])
            vsrc = w_in[:, 2560 + c0:2560 + c0 + 128]
            self.proj_v(vsrc, 128, lambda g: Vd[0][:, 4 * g:4 * g + 4, :, 0:64], ('Vd', 0))
            self.proj_v(vsrc, 128, lambda g: Vd[1][:, 4 * g:4 * g + 4, :, 0:64], ('Vd', 1),
                        tok_fn=lambda blk: ssl((blk // 4) + 512 * (blk % 4), 128, 4))
            self.proj_v(vsrc, 128, lambda g: Vd[2][:, 4 * g:4 * g + 4, :, 0:64], ('Vd', 2),
                        tok_fn=lambda blk: ssl(blk, 128, 16))
            ot_keys = []
            for hh in range(2):
                rows = slice(64 * hh, 64 * hh + 64)
                for i in range(NCH):
                    acc1, acc4, acc16 = self.ps[3], self.ps[4], self.ps[6]
                    for g in range(2):
                        slots = []
                        for qb in (4 * i + 2 * g, 4 * i + 2 * g + 1):
                            hasp = qb >= 1
                            kb = max(qb - 1, 0)
                            qsl = slice(qb * 128, qb * 128 + 128)
                            slots.append((qsl, qsl, (acc1, ('ps', 3), (qb - 4 * i) * 128, 0, qb, True, not hasp)))
                            slots.append((slice(kb * 128, kb * 128 + 128), qsl,
                                          (acc1, ('ps', 3), (qb - 4 * i) * 128, 0, kb, False, True) if hasp else None))
                        self.dil_group(KT, Qz[hh], rows, hh, slots, dm3, ['c_dm'], 128)
                    for g in range(2):
                        slots = []
                        for r_ in (2 * g, 2 * g + 1):
                            ip = max(i - 1, 0)
                            qsl = ssl(r_ + 512 * i, 128, 4)
                            ksl = ssl(r_ + 512 * ip, 128, 4)
                            slots.append((qsl, qsl, (acc4, ('ps', 4), r_ * 128, 1, r_ * 4 + i, True, i == 0)))
                            slots.append((ksl, qsl, (acc4, ('ps', 4), r_ * 128, 1, r_ * 4 + ip, False, True) if i >= 1 else None))
                        self.dil_group(KT, Qz[hh], rows, hh, slots, dm3, ['c_dm'], 128)
                    slots = []
                    for r_ in range(16):
                        slots.append((ssl(r_, 128, 16), ssl(r_ + 512 * i, 32, 16),
                                      (acc16, ('ps', 6), r_ * 32, 2, r_, True, True)))
                    m16 = self.c['c_wm'][:, 384 + 32 * i:416 + 32 * i].unsqueeze(1).broadcast_to([128, 16, 32])
                    self.dil_group(KT, Qz[hh], rows, hh, slots, m16, ['c_wm'], 32)

                    def extra(U, uk):
                        P.add('act', lambda e: e.activation(out=U[0:65, :], in_=self.ps[3][0:65, :], func=AF.Copy),
                              r=[('ps', 3)], w=[uk])
                        P.add('dve', lambda e: e.tensor_tensor(
                            out=U[0:65, :].rearrange('p (q r) -> p r q', r=4),
                            in0=self.ps[4][0:65, :].rearrange('p (r q) -> p r q', r=4),
                            in1=U[0:65, :].rearrange('p (q r) -> p r q', r=4), op=ALU.add), r=[('ps', 4), uk], w=[uk])
                        P.add('dve', lambda e: e.tensor_tensor(
                            out=U[0:65, :].rearrange('p (q r) -> p r q', r=16),
                            in0=self.ps[6][0:65, :].rearrange('p (r q) -> p r q', r=16),
                            in1=U[0:65, :].rearrange('p (q r) -> p r q', r=16), op=ALU.add), r=[('ps', 6), uk], w=[uk])
                    self.finalize(None, None, self.OT[hh][0:64, i * 512:(i + 1) * 512], ('OT', hh, i), extra=extra)
                ot_keys.append([('OT', hh, i) for i in range(NCH)])
            self.outproj_pair(self.fd_w_out, 4 + dp, ot_keys)

    def dil_group(self, KT, QT, rows, hh, slots, mask_ap, mkeys, width):
        P = self.P
        Vd = self.Vd
        allq = [(('QZ', hh), t) for t in range(NCH)] + ['qz0', 'qz1']
        allk = [('KT', t) for t in range(NCH)]
        self.step()
        si = self.rot('Sd', 3)
        ps = self.ps[si]
        for j, (ksl, qsl, pv) in enumerate(slots):
            P.add('pe', lambda e, j=j, ksl=ksl, qsl=qsl: e.matmul(
                ps[:, j * width:(j + 1) * width], lhsT=KT[:, ksl], rhs=QT[:, qsl],
                start=True, stop=True), r=allk + allq, w=[('ps', si)])
        pi = self.rot('Pt', NPT)
        pt = self.Pt[pi]
        P.add('act', lambda e: e.activation(out=pt[:, :], in_=ps[:, :], func=AF.Exp, scale=0.125),
              r=[('ps', si)], w=[('Pt', pi, 0)])
        ptv = pt[:, :].rearrange('p (a b) -> p a b', b=width)
        P.add('dve', lambda e: e.tensor_tensor(out=ptv, in0=ptv, in1=mask_ap, op=ALU.mult),
              r=[('Pt', pi, 0)] + mkeys, w=[('Pt', pi, 0)])
        gens = {k_: self.acc_gen_of(k_) for k_ in (('ps', 3), ('ps', 4), ('ps', 6))}

        def back():
            for j, (ksl, qsl, pv) in enumerate(slots):
                if pv is None:
                    continue
                acc, ak, col0, vpat, vblk, st, sp = pv
                self.acc_start(ak, gens[ak])
                vkeys = [(('Vd', vpat), g) for g in range(4)] + [('Vones', vpat)]
                P.add('pe', lambda e, j=j, acc=acc, col0=col0, vpat=vpat, vblk=vblk, st=st, sp=sp: e.matmul(
                    acc[0:65, col0:col0 + width], lhsT=Vd[vpat][:, vblk, hh, :], rhs=pt[:, j * width:(j + 1) * width],
                    start=st, stop=sp), r=[('Pt', pi, 0)] + vkeys, w=[ak])
        self.defer(back, PD)

    def dump_h(self, s):
        P = self.P
        for b in range(NB):
            o = P.add('sp', lambda e, b=b: e.dma_start(out=self.y[s, b * 128:(b + 1) * 128, :], in_=self.h[:, b, :]),
                      r=self.hk(b), w=[('y', s, b)], dsem=self.ds_y)
        self.P.final.append(o)

    def build(self):
        P = self.P
        st = self.stages
        self.load_consts()
        for s in range(self.n_seq):
            self.phase()
            self.load_seq(s)
            self.rotary_tables(s)
            if 'l0' in st:
                self.phase()
                self.norm(self.mix_norm[0:1, :])
                self.l0_mixer(s)
                if 'l0ffn' in st:
                    self.phase()
                    self.norm(self.ffn_norm[0:1, :])
                    self.phase()
                    self.ffn_setup()
                    if 'noffn' not in st:
                        self.ffn(self.dense_w_gate, self.dense_w_up, self.dense_w_down, None)
                    if 'nople' not in st:
                        self.ple(s, 0)
            if 'l1' in st:
                self.phase()
                self.norm(self.mix_norm[1:2, :])
                self.nsa_mixer(s)
                if 'l1ffn' in st:
                    self.moe()
                    self.ple(s, 1)
            if 'final' in st:
                self.phase()
                self.norm(self.final_norm[0:1, :], final_out=s)
            else:
                self.dump_h(s)
        if 'final' in st:
            last = [o for o in P.by_eng['sp'] if o.dsem is self.ds_y][-1]
            P.final.append(last)
        P.emit()
        return self.nc


SWAP64 = np.concatenate([np.arange(32, 64), np.arange(0, 32)])


def _swap_cols(w):
    d, n = w.shape
    idx = (np.arange(n) // 64) * 64 + SWAP64[np.arange(n) % 64]
    return np.ascontiguousarray(w[:, idx])


def host_inputs(inputs, n_cores, n_seq, consts):
    f = lambda a: np.ascontiguousarray(np.asarray(a, dtype=np.float32))
    shared = {}
    for k in ('mix_norm', 'ffn_norm', 'ple_norm', 'ple_gate_w', 'ple_proj_w'):
        shared[k] = f(inputs[k])
    shared['final_norm'] = f(inputs['final_norm']).reshape(1, D)
    for k in ('fd_w_in', 'fd_forget_b', 'fd_w_out', 'dense_w_gate', 'dense_w_up', 'dense_w_down', 'nsa_w_in',
              'nsa_pos_k', 'nsa_w1_k', 'nsa_w2_k', 'nsa_pos_v', 'nsa_w1_v', 'nsa_w2_v', 'nsa_w_out',
              'moe_w_router', 'moe_b_router', 'moe_w_gate', 'moe_w_up', 'moe_w_down'):
        shared[k] = f(inputs[k])[0]
    shared['nsa_b1_k'] = f(inputs['nsa_b1_k'])[0].reshape(256, 1)
    shared['nsa_b1_v'] = f(inputs['nsa_b1_v'])[0].reshape(256, 1)
    shared['fd_w_sw'] = _swap_cols(shared['fd_w_in'][:, 1536:2560])
    nw = shared['nsa_w_in']
    shared['nsa_w_sw'] = _swap_cols(np.concatenate([nw[:, 0:1280], nw[:, 1536:1792], nw[:, 2048:2304]], axis=1))
    shared.update(consts)
    x = f(inputs['x'])
    p = f(inputs['p'])
    pos = np.ascontiguousarray(np.asarray(inputs['positions'], dtype=np.int32))
    maps = []
    for c in range(n_cores):
        m = dict(shared)
        m['x'] = np.ascontiguousarray(x[c * n_seq:(c + 1) * n_seq])
        m['p'] = np.ascontiguousarray(p[:, c * n_seq:(c + 1) * n_seq])
        m['positions'] = np.ascontiguousarray(pos[c * n_seq:(c + 1) * n_seq])
        maps.append(m)
    return maps


_CACHE = {}


def kernel(**inputs):
    n_cores, n_seq = 8, 2
    if 'nc' not in _CACHE:
        b = Builder(n_seq=n_seq, stages=('l0', 'l0ffn', 'l1', 'l1ffn', 'final'))
        _CACHE['nc'] = b.build()
        _CACHE['b'] = b
    nc = _CACHE['nc']
    maps = host_inputs(inputs, n_cores, n_seq, make_consts())
    res = run_bass_kernel_spmd(nc, maps, core_ids=list(range(n_cores)))
    out = np.concatenate([np.asarray(r['y'], dtype=np.float32) for r in res.results], axis=0)
    return out


def _ffn_setup(self):
    R = self.R
    self.Wg = [R.alloc([8, 512], BF16) for _ in range(2)]
    self.Wu = [R.alloc([8, 512], BF16) for _ in range(2)]
    self.Wd = [R.alloc([4, D], BF16) for _ in range(2)]
    self.At = [R.alloc([4, 512], BF16) for _ in range(2)]
    self.Sg = [R.alloc([512], F32) for _ in range(2)]


def _ffn(self, wg, wu, wd, comb=None):
    P, R = self.P, self.R
    for fg in range(7):
        wi = self.rot('ffnw', 2)
        Wg, Wu, Wd = self.Wg[wi], self.Wu[wi], self.Wd[wi]
        fsl = slice(fg * 512, (fg + 1) * 512)
        self.load_w(Wg, ('Wg', wi), self.ds_ffn[wi], wg[:, fsl])
        self.load_w(Wu, ('Wu', wi), self.ds_ffn[2 + wi], wu[:, fsl])
        self.load_w(Wd, ('Wd', wi), self.ds_ffn[4 + wi], wd[fsl, :])
        for tc in range(NCH):
            ai = self.rot('At', 2)
            At = self.At[ai]
            for fc in range(4):
                gi = self.rot('Gp', 2)
                ui = 2 + self.rot('Up', 2)
                for (pi, W_, wk) in ((gi, Wg, ('Wg', wi)), (ui, Wu, ('Wu', wi))):
                    ps = self.ps[pi]
                    for kc in range(8):
                        P.add('pe', lambda e, ps=ps, W_=W_, kc=kc, fc=fc, tc=tc: e.matmul(
                            ps[:, :], lhsT=W_[:, kc, fc * 128:(fc + 1) * 128], rhs=self.hnT[:, kc, tc * 512:(tc + 1) * 512],
                            start=(kc == 0), stop=(kc == 7)), r=[wk] + self.hn_keys(tc), w=[('ps', pi)])
                si = self.rot('Sg', 2)
                Sg = self.Sg[si]
                P.add('act', lambda e, Sg=Sg, gi=gi: e.activation(out=Sg, in_=self.ps[gi][:, :], func=AF.Silu),
                      r=[('ps', gi)], w=[('Sg', si)])
                P.add('dve', lambda e, Sg=Sg, ui=ui, At=At, fc=fc: e.tensor_tensor(
                    out=At[:, fc, :], in0=self.ps[ui][:, :], in1=Sg, op=ALU.mult),
                    r=[('ps', ui), ('Sg', si)], w=[('At', ai, fc)])
            akeys = [('At', ai, fc) for fc in range(4)]
            for bb in range(4):
                b = 4 * tc + bb
                for half in range(2):
                    yi = 4 + self.rot('Yp', 3)
                    ps = self.ps[yi]
                    for fc in range(4):
                        P.add('pe', lambda e, ps=ps, At=At, Wd=Wd, fc=fc, bb=bb, half=half: e.matmul(
                            ps[:, :], lhsT=At[:, fc, bb * 128:(bb + 1) * 128], rhs=Wd[:, fc, half * 512:(half + 1) * 512],
                            start=(fc == 0), stop=(fc == 3)), r=akeys + [('Wd', wi)], w=[('ps', yi)])
                    hsl = self.h[:, b, half * 512:(half + 1) * 512]
                    if comb is None:
                        P.add('dve', lambda e, ps=ps, hsl=hsl: e.tensor_tensor(out=hsl, in0=ps[:, :], in1=hsl, op=ALU.add),
                              r=[('ps', yi), ('h', b, half)], w=[('h', b, half)])
                    else:
                        P.add('dve', lambda e, ps=ps, hsl=hsl, b=b: e.scalar_tensor_tensor(
                            out=hsl, in0=ps[:, :], scalar=comb[:, b:b + 1], in1=hsl, op0=ALU.mult, op1=ALU.add),
                            r=[('ps', yi), ('h', b, half), 'comb'], w=[('h', b, half)])


def _ple(self, s, layer):
    P, R = self.P, self.R
    self.phase()
    self.norm(self.ple_norm[layer:layer + 1, :])
    self.phase()
    Wpg = R.alloc([8, D], BF16)
    Wpp = R.alloc([2, D], BF16)
    p32 = R.alloc([NB, 256], F32)
    pT = R.alloc([2, S], BF16)
    sg = [R.alloc([512], F32) for _ in range(2)]
    self.load_w(Wpg, 'Wpg', self.ds_ffn[0], self.ple_gate_w[layer])
    self.load_w(Wpp, 'Wpp', self.ds_ffn[1], self.ple_proj_w[layer])
    P.add('sp', lambda e: e.dma_start(out=p32, in_=self.p[layer, s].rearrange('(b p) c -> p b c', p=128)),
          w=['p32'], dsem=self.ds_misc[2])
    ident = self.c['c_ident']
    for g in range(NB // 2):
        pi = 6 + self.rot('PJ', 2)
        ps = self.ps[pi]
        for bb in range(2):
            b = 2 * g + bb
            for c2 in range(2):
                j = bb * 2 + c2
                P.add('pe', lambda e, ps=ps, j=j, b=b, c2=c2: e.transpose(
                    out=ps[:, j * 128:(j + 1) * 128], in_=p32[:, b, c2 * 128:(c2 + 1) * 128], identity=ident[:]),
                    r=['p32', 'c_ident'], w=[('ps', pi)])
        P.add('act', lambda e, ps=ps, g=g: e.activation(
            out=pT[:, :, g * 256:(g + 1) * 256].rearrange('p c (b t) -> p b c t', b=2),
            in_=ps[:, :].rearrange('p (b c t) -> p b c t', b=2, c=2), func=AF.Copy), r=[('ps', pi)], w=[('pT', g)])
    for b in range(NB):
        for half in range(2):
            gi = self.rot('Gp', 2)
            ui = 2 + self.rot('Up', 2)
            csl = slice(half * 512, (half + 1) * 512)
            for kc in range(8):
                P.add('pe', lambda e, gi=gi, kc=kc, b=b, csl=csl: e.matmul(
                    self.ps[gi][:, :], lhsT=self.hnT[:, kc, b * 128:(b + 1) * 128], rhs=Wpg[:, kc, csl],
                    start=(kc == 0), stop=(kc == 7)), r=['Wpg', ('hnT', b)], w=[('ps', gi)])
            for c2 in range(2):
                P.add('pe', lambda e, ui=ui, c2=c2, b=b, csl=csl: e.matmul(
                    self.ps[ui][:, :], lhsT=pT[:, c2, b * 128:(b + 1) * 128], rhs=Wpp[:, c2, csl],
                    start=(c2 == 0), stop=(c2 == 1)), r=['Wpp', ('pT', b // 2)], w=[('ps', ui)])
            si = self.rot('Sg', 2)
            P.add('act', lambda e, si=si, gi=gi: e.activation(out=sg[si], in_=self.ps[gi][:, :], func=AF.Sigmoid),
                  r=[('ps', gi)], w=[('sg', si)])
            P.add('dve', lambda e, si=si, ui=ui: e.tensor_tensor(out=sg[si], in0=self.ps[ui][:, :], in1=sg[si], op=ALU.mult),
                  r=[('ps', ui), ('sg', si)], w=[('sg', si)])
            hsl = self.h[:, b, csl]
            P.add('pool', lambda e, si=si, hsl=hsl: e.tensor_tensor(out=hsl, in0=hsl, in1=sg[si], op=ALU.add),
                  r=[('sg', si), ('h', b, half)], w=[('h', b, half)])


Builder.ffn = _ffn
Builder.ffn_setup = _ffn_setup
Builder.ple = _ple


def _unit(self, lhsT, rhs, rkeys, mask_ap, mkeys, v_ap, vkeys, acc, ai, st, sp, aug=None):
    P = self.P
    self.step()
    si = self.rot('S', 3)
    ps = self.ps[si]
    P.add('pe', lambda e: e.matmul(ps[:, :], lhsT=lhsT, rhs=rhs, start=True, stop=(aug is None)), r=rkeys, w=[('ps', si)])
    if aug is not None:
        l2, r2, k2 = aug
        P.add('pe', lambda e: e.matmul(ps[:, :], lhsT=l2, rhs=r2, start=False, stop=True), r=k2, w=[('ps', si)])
    pi = self.rot('Pt', NPT)
    pt = self.Pt[pi]
    P.add('act', lambda e: e.activation(out=pt[:, :], in_=ps[:, :], func=AF.Exp, scale=0.125), r=[('ps', si)], w=[('Pt', pi, 0)])
    if mask_ap is not None:
        meng = 'pool' if self.rot('meng', 3) == 2 else 'dve'
        P.add(meng, lambda e: e.tensor_tensor(out=pt[:, :], in0=pt[:, :], in1=mask_ap, op=ALU.mult),
              r=[('Pt', pi, 0)] + mkeys, w=[('Pt', pi, 0)])
    gen = self.acc_gen_of(('ps', ai))

    def back():
        self.acc_start(('ps', ai), gen)
        P.add('pe', lambda e: e.matmul(acc[0:65, :], lhsT=v_ap, rhs=pt[:, :], start=st, stop=sp),
              r=[('Pt', pi, 0)] + vkeys, w=[('ps', ai)])
    self.defer(back, PD)


def _nsa_mixer(self, s):
    P, R = self.P, self.R
    self.phase()
    self.attn_common()
    self.rtmp = [R.alloc([512], F32) for _ in range(2)]
    self.sigT = R.alloc([S], BF16)
    P.add('pool', lambda e: e.memset(self.sigT[:, :], 0.0), w=['sigT'])
    self.gacc = R.alloc([512], F32, parts=64)
    Qz = [R.alloc([S], BF16) for _ in range(4)]
    KA = R.alloc([S], BF16)
    KB = R.alloc([S], BF16)
    Vs = R.alloc([NB, 1, 65], BF16)
    Vw = R.alloc([NB, 1, 65], BF16)
    W1c = R.alloc([8, 256], BF16, parts=64)
    W2k = R.alloc([2, 64], BF16)
    W2v = R.alloc([2, 64], BF16)
    gel = [R.alloc([128], BF16) for _ in range(2)]
    xs = R.alloc([128], F32)
    x2 = R.alloc([128], F32)
    hb = R.alloc([2], F32)
    b1t = R.alloc([2], F32)
    pos32 = R.alloc([64], F32, parts=32)
    posT = R.alloc([32], BF16, parts=64)
    kcmpT = R.alloc([128], BF16)
    vcmp = R.alloc([65], BF16)
    E4 = R.alloc([4, 128], F32)
    pcs = R.alloc([128], F32)
    den4 = R.alloc([4], F32)
    imp = R.alloc([32], F32)
    impm = R.alloc([32], F32)
    top8 = R.alloc([8], F32)
    sel = R.alloc([96], F32)
    w_in, w_sw = self.nsa_w_in, self.nsa_w_sw
    ident = self.c['c_ident']
    wm = self.c['c_wm']
    wt, wk, wds = self.wsl.get()
    self.load_w(wt[:, :, 0:48], wk, wds, w_in[:, 2560:2608])

    def evac_g(tc, ps, pk):
        P.add('act', lambda e: e.activation(out=self.sigT[0:48, tc * 512:(tc + 1) * 512], in_=ps[0:48, :], func=AF.Sigmoid),
              r=[pk], w=['sigT'])
    self.proj_fm(wt, wk, 48, evac_g)
    if NSA_STOP == 1:
        return
    P.add('pool', lambda e: e.memset(kcmpT[:, :], 0.0), w=['kcmpT'])
    P.add('pool', lambda e: e.memset(sel[:, :], 0.0), w=['sel'])
    for hh in range(4):
        P.add('pool', lambda e, hh=hh: e.memset(Qz[hh][64:128, :], 0.0), w=[(('NT', hh), t) for t in range(NCH)])
    P.add('pool', lambda e: e.memset(KA[64:128, :], 0.0), w=['KAz'])
    P.add('pool', lambda e: e.memset(KB[64:128, :], 0.0), w=['KBz'])
    P.add('sp', lambda e: e.dma_start(out=KA[64:96, :], in_=self.cst['c_E'][0:32, :]), r=['KAz'], w=['KAe'], dsem=self.ds_misc[0])
    P.add('pool', lambda e: e.memset(vcmp[:, :], 0.0), w=['vcmp'])
    P.add('pool', lambda e: e.memset(Vs[:, :, :, 64:65], 1.0), w=['Vs1'])
    P.add('pool', lambda e: e.memset(Vw[:, :, :, 64:65], 1.0), w=['Vw1'])
    P.add('pool', lambda e: e.memset(gel[0][:, :], 0.0), w=[('gel', 0)])
    P.add('pool', lambda e: e.memset(gel[1][:, :], 0.0), w=[('gel', 1)])
    allq = [[(('QZ', hh), t) for t in range(NCH)] for hh in range(4)]
    KAk = [('KA', t) for t in range(NCH)]
    KBk = [('KB', t) for t in range(NCH)]
    for g in range(4):
        for hh in range(4):
            c0 = (4 * g + hh) * 64
            self.proj_qk_rot(w_in[:, c0:c0 + 64], w_sw[:, c0:c0 + 64], Qz[hh], ('QZ', hh), ncols=64)
        self.proj_qk_rot(w_in[:, 1024 + 64 * g:1024 + 64 * g + 64], w_sw[:, 1024 + 64 * g:1024 + 64 * g + 64], KA, 'KA', ncols=64)
        wt, wk, wds = self.wsl.get()
        self.load_w(wt[:, :, 0:64], wk, wds, w_in[:, 1280 + 64 * g:1280 + 64 * g + 64])

        def evac_vc(tc, ps, pk):
            P.add('act', lambda e: e.activation(out=KB[0:64, tc * 512:(tc + 1) * 512], in_=ps[0:64, :], func=AF.Copy),
                  r=[pk], w=[('KB', tc)])
        self.proj_fm(wt, wk, 64, evac_vc)
        for which in range(2):
            src = KA if which == 0 else KB
            skeys = KAk if which == 0 else KBk
            w1 = self.nsa_w1_k if which == 0 else self.nsa_w1_v
            w2 = self.nsa_w2_k if which == 0 else self.nsa_w2_v
            b1 = self.nsa_b1_k if which == 0 else self.nsa_b1_v
            posd = self.nsa_pos_k if which == 0 else self.nsa_pos_v
            P.add('sp', lambda e, posd=posd: e.dma_start(out=pos32[0:32, :], in_=posd), w=['pos32'], dsem=self.ds_misc[0])
            P.add('sp', lambda e, b1=b1: e.dma_start(out=b1t[:, 0:1], in_=b1[0:128, :]), w=[('b1t', 0)], dsem=self.ds_misc[1])
            P.add('sp', lambda e, b1=b1: e.dma_start(out=b1t[:, 1:2], in_=b1[128:256, :]), w=[('b1t', 1)], dsem=self.ds_misc[2])
            if which == 0:
                self.load_w(W2k[:, :, :], 'W2a', self.ds_misc[3], w2)
                w2keys = ['W2a']
            else:
                self.load_w(W2v[:, :, :], 'W2v', self.ds_misc[3], w2)
                w2keys = ['W2v']
            P.add('pe', lambda e: e.transpose(out=self.ps[5][0:64, 0:32], in_=pos32[0:32, 0:64], identity=ident[0:32, 0:32]),
                  r=['pos32', 'c_ident'], w=[('ps', 5)])
            P.add('act', lambda e: e.activation(out=posT[0:64, :], in_=self.ps[5][0:64, 0:32], func=AF.Copy),
                  r=[('ps', 5)], w=['posT'])
            first = True
            for ic in range(4):
                srcw = w1[ic * 512:(ic + 1) * 512, :].rearrange('(i d) c -> d i c', d=64)
                P.add('pool', lambda e, srcw=srcw: e.dma_start(out=W1c[0:64, :, :], in_=srcw), w=['W1c'], dsem=self.ds_w[4])
                for ii in range(8):
                    i = ic * 8 + ii
                    for hc in range(2):
                        P.add('pe', lambda e, ii=ii, i=i, hc=hc, src=src: e.matmul(
                            self.ps[6 + hc][:, 0:127], lhsT=W1c[0:64, ii, hc * 128:(hc + 1) * 128],
                            rhs=src[0:64, ssl(i, 127, 16)], start=(i == 0), stop=(i == 31)),
                            r=['W1c'] + skeys, w=[('ps', 6 + hc)])
                        P.add('pe', lambda e, ii=ii, i=i, hc=hc, st=first: e.matmul(
                            self.ps[5][:, hc:hc + 1], lhsT=W1c[0:64, ii, hc * 128:(hc + 1) * 128],
                            rhs=posT[0:64, i:i + 1], start=st, stop=(i == 31), skip_group_check=True),
                            r=['W1c', 'posT'], w=[('ps', 5)])
                        first = False
            P.add('dve', lambda e: e.tensor_tensor(out=hb[:, 0:2], in0=self.ps[5][:, 0:2], in1=b1t[:, 0:2], op=ALU.add),
                  r=[('ps', 5), ('b1t', 0), ('b1t', 1)], w=['hb'])
            for hc in range(2):
                pk = ('ps', 6 + hc)
                psh = self.ps[6 + hc]
                P.add('act', lambda e, psh=psh, hc=hc: e.activation(out=xs[:, 0:127], in_=psh[:, 0:127], func=AF.Identity,
                                                                    bias=hb[:, hc:hc + 1]), r=[pk, 'hb'], w=['xs'])
                P.add('dve', lambda e: e.tensor_tensor(out=x2[:, 0:127], in0=xs[:, 0:127], in1=xs[:, 0:127], op=ALU.mult),
                      r=['xs'], w=['x2'])
                P.add('dve', lambda e: e.tensor_scalar(out=x2[:, 0:127], in0=x2[:, 0:127], scalar1=0.044715, scalar2=1.0,
                                                       op0=ALU.mult, op1=ALU.add), r=['x2'], w=['x2'])
                P.add('dve', lambda e: e.tensor_tensor(out=x2[:, 0:127], in0=x2[:, 0:127], in1=xs[:, 0:127], op=ALU.mult),
                      r=['x2', 'xs'], w=['x2'])
                P.add('act', lambda e: e.activation(out=x2[:, 0:127], in_=x2[:, 0:127], func=AF.Sigmoid,
                                                    scale=float(2.0 * np.sqrt(2.0 / np.pi))), r=['x2'], w=['x2'])
                P.add('dve', lambda e, hc=hc: e.tensor_tensor(out=gel[hc][:, 0:127], in0=xs[:, 0:127], in1=x2[:, 0:127],
                                                              op=ALU.mult), r=['x2', 'xs'], w=[('gel', hc)])
            if which == 0:
                for hc in range(2):
                    P.add('pe', lambda e, hc=hc: e.matmul(self.ps[6][0:64, 0:127], lhsT=W2k[:, hc, :], rhs=gel[hc][:, 0:127],
                                                          start=(hc == 0), stop=(hc == 1)),
                          r=[('gel', hc)] + w2keys, w=[('ps', 6)])
                P.add('act', lambda e: e.activation(out=kcmpT[0:64, 0:127], in_=self.ps[6][0:64, 0:127], func=AF.Copy),
                      r=[('ps', 6)], w=['kcmpT'])
            else:
                for hc in range(2):
                    P.add('pe', lambda e, hc=hc: e.matmul(self.ps[6][0:127, 0:64], lhsT=gel[hc][:, 0:127], rhs=W2v[:, hc, :],
                                                          start=(hc == 0), stop=(hc == 1)),
                          r=[('gel', hc)] + w2keys, w=[('ps', 6)])
                P.add('act', lambda e: e.activation(out=vcmp[0:127, 0:64], in_=self.ps[6][0:127, 0:64], func=AF.Copy),
                      r=[('ps', 6)], w=['vcmp'])
                P.add('pool', lambda e: e.memset(vcmp[:, 64:65], 1.0), r=[], w=['vcmp1'])
        if NSA_STOP == 2:
            continue
        def imp_stage(qb):
            si = self.rot('S', 3)
            ps = self.ps[si]
            for hh in range(4):
                P.add('pe', lambda e, ps=ps, hh=hh, qb=qb: e.matmul(
                    ps[:, hh * 128:(hh + 1) * 128], lhsT=Qz[hh][:, qb * 128:(qb + 1) * 128], rhs=kcmpT[:, :],
                    start=True, stop=True), r=['kcmpT'] + allq[hh] + [(('NT', hh), t) for t in range(NCH)], w=[('ps', si)])
            P.add('act', lambda e, ps=ps: e.activation(out=E4, in_=ps[:, :].rearrange('p (h n) -> p h n', h=4), func=AF.Exp,
                                                       scale=0.125), r=[('ps', si)], w=['E4'])
            m0s = self.c['c_m0'][:, 120 - 8 * qb:248 - 8 * qb].unsqueeze(1).broadcast_to([128, 4, 128])
            P.add('dve', lambda e, m0s=m0s: e.tensor_tensor(out=E4, in0=E4, in1=m0s, op=ALU.mult), r=['E4', 'c_m0'], w=['E4'])
            P.add('dve', lambda e: e.tensor_reduce(out=den4, in_=E4, axis=AX.X, op=ALU.add), r=['E4'], w=['den4'])
            P.add('dve', lambda e: e.tensor_scalar(out=den4, in0=den4, scalar1=1e-30, scalar2=None, op0=ALU.max),
                  r=['den4'], w=['den4'])
            P.add('dve', lambda e: e.reciprocal(out=den4, in_=den4), r=['den4'], w=['den4'])
            P.add('dve', lambda e: e.tensor_scalar(out=pcs, in0=E4[:, 0, :], scalar1=den4[:, 0:1], scalar2=None, op0=ALU.mult),
                  r=['E4', 'den4'], w=['pcs'])
            for hh in range(1, 4):
                P.add('dve', lambda e, hh=hh: e.scalar_tensor_tensor(out=pcs, in0=E4[:, hh, :], scalar=den4[:, hh:hh + 1],
                                                                     in1=pcs, op0=ALU.mult, op1=ALU.add),
                      r=['E4', 'den4', 'pcs'], w=['pcs'])
            P.add('dve', lambda e: e.tensor_reduce(out=imp, in_=pcs[:, :].rearrange('p (j f) -> p j f', f=4), axis=AX.X,
                                                   op=ALU.add), r=['pcs'], w=['imp'])
            P.add('dve', lambda e: e.tensor_tensor(out=imp[:, 1:32], in0=imp[:, 1:32], in1=pcs[:, ssl(3, 31, 4)], op=ALU.add),
                  r=['pcs', 'imp'], w=['imp'])
            P.add('dve', lambda e, qb=qb: e.tensor_tensor(out=impm, in0=imp, in1=self.c['c_keep'][:, 30 - 2 * qb:62 - 2 * qb],
                                                          op=ALU.mult), r=['imp', 'c_keep'], w=['impm'])
            P.add('dve', lambda e, qb=qb: e.tensor_tensor(out=impm, in0=impm, in1=self.c['c_addv'][:, 30 - 2 * qb:62 - 2 * qb],
                                                          op=ALU.add), r=['impm', 'c_addv'], w=['impm'])
            P.add('dve', lambda e: e.memset(impm[:, 0:1], 1e6), r=['impm'], w=['impm'])
            P.add('dve', lambda e: e.max(out=top8, in_=impm), r=['impm'], w=['top8'])
            P.add('dve', lambda e: e.tensor_scalar(out=sel[:, 64:96], in0=impm, scalar1=top8[:, 7:8], scalar2=None, op0=ALU.is_ge),
                  r=['impm', 'top8'], w=['sel'])
            P.add('pe', lambda e: e.transpose(out=self.ps[5][0:96, 0:128], in_=sel[:, 0:96], identity=ident[:]),
                  r=['sel', 'c_ident'], w=[('ps', 5)])
            for hh in range(4):
                P.add('dve', lambda e, qb=qb, hh=hh: e.tensor_scalar(
                    out=Qz[hh][64:96, qb * 128:(qb + 1) * 128], in0=self.ps[5][64:96, 0:128],
                    scalar1=30000.0, scalar2=-30000.0, op0=ALU.mult, op1=ALU.add),
                    r=[('ps', 5)], w=[(('NT', hh), qb // 4)])
        self.proj_qk_rot(w_in[:, 1536 + 64 * g:1536 + 64 * g + 64], w_sw[:, 1280 + 64 * g:1280 + 64 * g + 64], KA, 'KA', ncols=64)
        imp_stage(0)
        self.proj_qk_rot(w_in[:, 2048 + 64 * g:2048 + 64 * g + 64], w_sw[:, 1536 + 64 * g:1536 + 64 * g + 64], KB, 'KB', ncols=64)
        imp_stage(1)
        self.proj_v(w_in[:, 1792 + 64 * g:1792 + 64 * g + 64], 64, lambda gg: Vs[:, 4 * gg:4 * gg + 4, :, 0:64], 'Vs')
        imp_stage(2)
        self.proj_v(w_in[:, 2304 + 64 * g:2304 + 64 * g + 64], 64, lambda gg: Vw[:, 4 * gg:4 * gg + 4, :, 0:64], 'Vw')
        imp_stage(3)
        if NSA_STOP in (3, 5):
            continue
        for jp in range(2):
            ot_keys = []
            for h2 in range(2):
                hh = 2 * jp + h2
                head = 4 * g + hh
                for i in range(NCH):
                    if hh == 0 and i + 1 < NCH:
                        for qb in range(4 * (i + 1), 4 * (i + 2)):
                            imp_stage(qb)
                    csl = slice(i * 512, (i + 1) * 512)
                    q_ap = Qz[hh][:, csl]
                    qk = [(('QZ', hh), i), (('NT', hh), i)]
                    ai = (3, 4, 6)[self.rot('ACC', 3)]
                    self.unit(kcmpT[:, :], q_ap, ['kcmpT'] + qk, self.c['c_cmpT'][:, csl], ['c_cmpT'],
                              vcmp[:, 0:65], ['vcmp', 'vcmp1'], self.ps[ai], ai, True, True)
                    self.finalize(self.ps[ai], ('ps', ai), None, None, gate=(3 * head + 0, csl), first=True, last=False)
                    ai = (3, 4, 6)[self.rot('ACC', 3)]
                    nkb = 4 * i + 4
                    for kb in range(nkb):
                        mask = wm[:, 384 - 128 * (kb - 4 * i):384 - 128 * (kb - 4 * i) + 512] if kb >= 4 * i else None
                        self.unit(KA[:, kb * 128:(kb + 1) * 128], q_ap, [('KA', kb // 4), 'KAz', 'KAe'] + qk, mask, ['c_wm'],
                                  Vs[:, kb, 0, :], [('Vs', kb // 4), 'Vs1'], self.ps[ai], ai, kb == 0, kb == nkb - 1)
                    self.finalize(self.ps[ai], ('ps', ai), None, None, gate=(3 * head + 1, csl), first=False, last=False)
                    ai = (3, 4, 6)[self.rot('ACC', 3)]
                    kb0 = max(0, 4 * i - 4)
                    for kb in range(kb0, nkb):
                        r_ = kb - 4 * i
                        mask = wm[:, 384 - 128 * r_:384 - 128 * r_ + 512]
                        self.unit(KB[:, kb * 128:(kb + 1) * 128], q_ap, [('KB', kb // 4), 'KBz'] + qk, mask, ['c_wm'],
                                  Vw[:, kb, 0, :], [('Vw', kb // 4), 'Vw1'], self.ps[ai], ai, kb == kb0, kb == nkb - 1)
                    self.finalize(self.ps[ai], ('ps', ai), self.OT[h2][0:64, csl], ('OT', h2, i),
                                  gate=(3 * head + 2, csl), first=False, last=True)
                ot_keys.append([('OT', h2, i) for i in range(NCH)])
            self.outproj_pair(self.nsa_w_out, 2 * g + jp, ot_keys)


Builder.unit = _unit
Builder.nsa_mixer = _nsa_mixer


def _moe(self):
    P, R = self.P, self.R
    self.phase()
    self.norm(self.ffn_norm[1:2, :])
    self.phase()
    comb = R.alloc([8, NB], F32)
    lg = R.alloc([NB, 8], F32)
    rb = R.alloc([1, 8], F32)
    t8 = R.alloc([NB, 8], F32)
    w1 = R.alloc([NB], F32)
    w2 = R.alloc([NB], F32)
    ta = R.alloc([NB], F32)
    tb = R.alloc([NB], F32)
    wr = R.alloc([8, 8], BF16)
    self.load_w(wr, 'wr', self.ds_misc[0], self.moe_w_router)
    P.add('sp', lambda e: e.dma_start(out=rb[:, 0, :], in_=self.moe_b_router[0, :].partition_broadcast(128)),
          w=['rb'], dsem=self.ds_misc[1])
    ps = self.ps[7]
    for b in range(NB):
        for kc in range(8):
            P.add('pe', lambda e, b=b, kc=kc: e.matmul(ps[:, b * 8:(b + 1) * 8], lhsT=self.hnT[:, kc, b * 128:(b + 1) * 128],
                                                       rhs=wr[:, kc, :], start=(kc == 0), stop=(kc == 7)),
                  r=['wr', ('hnT', b)], w=[('ps', 7)])
    P.add('dve', lambda e: e.tensor_tensor(out=lg, in0=ps[:, 0:128].rearrange('p (b h) -> p b h', h=8),
                                           in1=rb[:, 0:1, :].broadcast_to([128, NB, 8]), op=ALU.add),
          r=[('ps', 7), 'rb'], w=['lg'])
    for b in range(NB):
        P.add('dve', lambda e, b=b: e.max(out=t8[:, b, :], in_=lg[:, b, :]), r=['lg'], w=[('t8', b)])
    t8k = [('t8', b) for b in range(NB)]
    P.add('dve', lambda e: e.tensor_tensor(out=w1, in0=t8[:, :, 1], in1=t8[:, :, 0], op=ALU.subtract), r=t8k, w=['w1'])
    P.add('act', lambda e: e.activation(out=w1, in_=w1, func=AF.Exp), r=['w1'], w=['w1'])
    P.add('dve', lambda e: e.tensor_scalar(out=w1, in0=w1, scalar1=1.0, scalar2=None, op0=ALU.add), r=['w1'], w=['w1'])
    P.add('dve', lambda e: e.reciprocal(out=w1, in_=w1), r=['w1'], w=['w1'])
    P.add('dve', lambda e: e.tensor_scalar(out=w2, in0=w1, scalar1=-1.0, scalar2=1.0, op0=ALU.mult, op1=ALU.add),
          r=['w1'], w=['w2'])
    for ex in range(8):
        P.add('dve', lambda e, ex=ex: e.tensor_tensor(out=ta, in0=lg[:, :, ex], in1=t8[:, :, 0], op=ALU.is_equal),
              r=['lg'] + t8k, w=['ta'])
        P.add('dve', lambda e: e.tensor_tensor(out=ta, in0=ta, in1=w1, op=ALU.mult), r=['ta', 'w1'], w=['ta'])
        P.add('dve', lambda e, ex=ex: e.tensor_tensor(out=tb, in0=lg[:, :, ex], in1=t8[:, :, 1], op=ALU.is_equal),
              r=['lg'] + t8k, w=['tb'])
        P.add('dve', lambda e: e.tensor_tensor(out=tb, in0=tb, in1=w2, op=ALU.mult), r=['tb', 'w2'], w=['tb'])
        P.add('dve', lambda e, ex=ex: e.tensor_tensor(out=comb[:, ex, :], in0=ta, in1=tb, op=ALU.add),
              r=['ta', 'tb'], w=['comb'])
    self.ffn_setup()
    for ex in range(8):
        self.ffn(self.moe_w_gate[ex], self.moe_w_up[ex], self.moe_w_down[ex], comb=comb[:, ex, :])


Builder.moe = _moe
```
